# Optimizing a Trainium2 kernel written in Bass

```python
import jax, jax.numpy as jnp
from jax import lax
import numpy as np

D_MODEL = 1024
BATCH = 8
SEQ = 4096
DEPTH = 1

GRID_W = 64
ATTN_HEADS = 8
ATTN_HEAD_DIM = 64
ATTN_WIDTH = ATTN_HEADS * ATTN_HEAD_DIM
NA_KH = 8
NA_KW = 16
MLSTM_HEADS = 4
MLSTM_HEAD_DIM = 128
MLSTM_WIDTH = MLSTM_HEADS * MLSTM_HEAD_DIM
MLSTM_CHUNK = 64
MLSTM_CONV_W = 5
N_GATES = 4 * MLSTM_HEADS
MIX_WIDTH = ATTN_WIDTH + MLSTM_WIDTH
IN_WIDTH = 3 * ATTN_WIDTH + 4 * MLSTM_WIDTH + N_GATES
PEER_HEADS = 8
PEER_NKEYS = 128
PEER_EXPERTS = PEER_NKEYS * PEER_NKEYS
PEER_TOPK = 16
PEER_DKEY = 256
PEER_DHALF = PEER_DKEY // 2
PEER_BLOCK = 128
EPS = 1e-6

kernel_name = 'hymba_natten_mlstm_peer_encoder'


def rms_norm(x, w):
    xf = x.astype(jnp.float32)
    y = xf * lax.rsqrt(jnp.mean(xf * xf, axis=-1, keepdims=True) + EPS)
    return (y * w.astype(jnp.float32)).astype(x.dtype)


def neighborhood_attention(q, k, v, rpb):
    B, S, H, Dh = q.shape
    rows = S // GRID_W
    kh = min(NA_KH, rows)
    qg = q.reshape(B, rows, GRID_W, H, Dh)
    kg = k.reshape(B, rows, GRID_W, H, Dh)
    vg = v.reshape(B, rows, GRID_W, H, Dh)
    cols = jnp.arange(GRID_W)
    col_start = jnp.clip(cols - NA_KW // 2, 0, GRID_W - NA_KW)
    col_idx = col_start[:, None] + jnp.arange(NA_KW)[None, :]
    dcol = col_idx - cols[:, None]
    rpb_c = rpb[:, :, dcol + NA_KW - 1].astype(jnp.float32)
    scale = ATTN_HEAD_DIM ** -0.5

    def row_block(r):
        rs = jnp.clip(r - kh // 2, 0, rows - kh)
        kr = lax.dynamic_slice_in_dim(kg, rs, kh, axis=1)
        vr = lax.dynamic_slice_in_dim(vg, rs, kh, axis=1)
        kn = kr[:, :, col_idx]
        vn = vr[:, :, col_idx]
        qr = lax.dynamic_index_in_dim(qg, r, axis=1, keepdims=False)
        s = jnp.einsum('bchd,bicjhd->bhcij', qr, kn).astype(jnp.float32) * scale
        drow = rs + jnp.arange(kh) - r
        bias = jnp.transpose(rpb_c[:, drow + NA_KH - 1], (0, 2, 1, 3))
        s = s + bias[None]
        p = jax.nn.softmax(s.reshape(B, H, GRID_W, kh * NA_KW), axis=-1)
        p = p.reshape(B, H, GRID_W, kh, NA_KW).astype(v.dtype)
        return jnp.einsum('bhcij,bicjhd->bchd', p, vn)

    out = lax.map(row_block, jnp.arange(rows))
    return jnp.transpose(out, (1, 0, 2, 3, 4)).reshape(B, S, H, Dh)


def centred_depthwise_conv(x, w, b):
    pad = (w.shape[0] - 1) // 2
    y = lax.conv_general_dilated(x, w[:, None, :], window_strides=(1,), padding=[(pad, pad)],
                                 dimension_numbers=('NWC', 'WIO', 'NWC'),
                                 feature_group_count=x.shape[-1])
    return y + b


def mlstm_chunkwise(q, k, v, log_i, log_f):
    B, H, S, D = q.shape
    L = MLSTM_CHUNK
    nc = S // L
    to_chunks = lambda t: jnp.moveaxis(t.reshape(B, H, nc, L, *t.shape[3:]), 2, 0)
    tril = jnp.tril(jnp.ones((L, L), dtype=bool))

    def step(carry, inp):
        C, n, m = carry
        qc, kc, vc, ic, fc = inp
        b = jnp.cumsum(fc, axis=-1)
        dmat = jnp.where(tril, b[..., :, None] - b[..., None, :] + ic[..., None, :], -jnp.inf)
        m_inter = b + m[..., None]
        m_t = jnp.maximum(jnp.max(dmat, axis=-1), m_inter)
        w = jnp.exp(dmat - m_t[..., None])
        s = jnp.einsum('bhld,bhsd->bhls', qc, kc) * w
        inter = jnp.exp(m_inter - m_t)
        num = jnp.einsum('bhls,bhsd->bhld', s, vc) + inter[..., None] * jnp.einsum('bhld,bhde->bhle', qc, C)
        den = jnp.sum(s, axis=-1) + inter * jnp.einsum('bhld,bhd->bhl', qc, n)
        h = num / jnp.maximum(jnp.abs(den), jnp.exp(-m_t))[..., None]
        m_new = m_t[..., -1]
        wk = jnp.exp(b[..., -1:] - b + ic - m_new[..., None])
        carry_decay = jnp.exp(b[..., -1] + m - m_new)
        C_new = carry_decay[..., None, None] * C + jnp.einsum('bhs,bhsd,bhse->bhde', wk, kc, vc)
        n_new = carry_decay[..., None] * n + jnp.einsum('bhs,bhsd->bhd', wk, kc)
        return (C_new, n_new, m_new), h

    init = (jnp.zeros((B, H, D, D), jnp.float32), jnp.zeros((B, H, D), jnp.float32),
            jnp.zeros((B, H), jnp.float32))
    _, hs = lax.scan(step, init, (to_chunks(q), to_chunks(k), to_chunks(v),
                                  to_chunks(log_i), to_chunks(log_f)))
    return jnp.moveaxis(hs, 0, 2).reshape(B, H, S, D)


def peer(xn, w_q, sub_keys, u_tab, v_tab):
    B, S, D = xn.shape
    T = B * S
    xt = xn.reshape(T, D)
    q = (xt @ w_q).reshape(T, PEER_HEADS, 2, PEER_DHALF)
    s = jnp.einsum('thpd,hpkd->thpk', q, sub_keys).astype(jnp.float32)
    s1, i1 = lax.top_k(s[:, :, 0], PEER_TOPK)
    s2, i2 = lax.top_k(s[:, :, 1], PEER_TOPK)
    cand = (s1[..., :, None] + s2[..., None, :]).reshape(T, PEER_HEADS, PEER_TOPK * PEER_TOPK)
    cidx = (i1[..., :, None] * PEER_NKEYS + i2[..., None, :]).reshape(T, PEER_HEADS, PEER_TOPK * PEER_TOPK)
    top_s, top_j = lax.top_k(cand, PEER_TOPK)
    eidx = jnp.take_along_axis(cidx, top_j, axis=-1)
    g = jax.nn.softmax(top_s, axis=-1).astype(xn.dtype)
    nb = T // PEER_BLOCK

    def block(args):
        xb, eb, gb = args
        u = u_tab[eb]
        a = jax.nn.gelu(jnp.einsum('td,thkd->thk', xb, u), approximate=False)
        return jnp.einsum('thk,thkd->td', gb * a, v_tab[eb])

    y = lax.map(block, (xt.reshape(nb, PEER_BLOCK, D),
                        eidx.reshape(nb, PEER_BLOCK, PEER_HEADS, PEER_TOPK),
                        g.reshape(nb, PEER_BLOCK, PEER_HEADS, PEER_TOPK)))
    return y.reshape(B, S, D)


def hybrid_layer(x, norm1_w, w_in, q_norm_w, k_norm_w, attn_rpb, attn_out_norm_w,
                 mlstm_conv_w, mlstm_conv_b, mlstm_gate_b, mlstm_norm_w, w_out,
                 norm2_w, peer_w_q, peer_sub_keys, peer_u, peer_v):
    B, S, _ = x.shape
    h = rms_norm(x, norm1_w) @ w_in
    o0 = 0
    aq = h[..., o0:o0 + ATTN_WIDTH]; o0 += ATTN_WIDTH
    ak = h[..., o0:o0 + ATTN_WIDTH]; o0 += ATTN_WIDTH
    av = h[..., o0:o0 + ATTN_WIDTH]; o0 += ATTN_WIDTH
    mqk = h[..., o0:o0 + 2 * MLSTM_WIDTH]; o0 += 2 * MLSTM_WIDTH
    mv = h[..., o0:o0 + MLSTM_WIDTH]; o0 += MLSTM_WIDTH
    mo = h[..., o0:o0 + MLSTM_WIDTH]; o0 += MLSTM_WIDTH
    mg = h[..., o0:o0 + N_GATES]

    ahs = (B, S, ATTN_HEADS, ATTN_HEAD_DIM)
    aq = rms_norm(aq.reshape(ahs), q_norm_w)
    ak = rms_norm(ak.reshape(ahs), k_norm_w)
    a_out = neighborhood_attention(aq, ak, av.reshape(ahs), attn_rpb).reshape(B, S, ATTN_WIDTH)
    a_out = rms_norm(a_out, attn_out_norm_w)

    mqk = jax.nn.silu(centred_depthwise_conv(mqk, mlstm_conv_w, mlstm_conv_b))
    to_heads = lambda t: jnp.transpose(t.reshape(B, S, MLSTM_HEADS, MLSTM_HEAD_DIM), (0, 2, 1, 3)).astype(jnp.float32)
    mq = to_heads(mqk[..., :MLSTM_WIDTH])
    mk = to_heads(mqk[..., MLSTM_WIDTH:]) * (MLSTM_HEAD_DIM ** -0.5)
    mvh = to_heads(mv)
    gates = mg.astype(jnp.float32).reshape(B, S, 4, MLSTM_HEADS) + mlstm_gate_b.astype(jnp.float32)
    gates = jnp.transpose(gates, (2, 0, 3, 1))
    li_f, lf_f = gates[0], jax.nn.log_sigmoid(gates[1])
    li_b, lf_b = gates[2], jax.nn.log_sigmoid(gates[3])
    h_fwd = mlstm_chunkwise(mq, mk, mvh, li_f, lf_f)
    flip = lambda t: jnp.flip(t, axis=2)
    h_bwd = flip(mlstm_chunkwise(flip(mq), flip(mk), flip(mvh),
                                 jnp.flip(li_b, axis=-1), jnp.flip(lf_b, axis=-1)))
    hm = jnp.transpose(h_fwd + h_bwd, (0, 2, 1, 3))
    hm = rms_norm(hm, mlstm_norm_w.reshape(MLSTM_HEADS, MLSTM_HEAD_DIM))
    hm = hm.reshape(B, S, MLSTM_WIDTH).astype(x.dtype) * jax.nn.sigmoid(mo)

    x = x + jnp.concatenate([a_out, hm], axis=-1) @ w_out

    x = x + peer(rms_norm(x, norm2_w), peer_w_q, peer_sub_keys, peer_u, peer_v)
    return x


def setup_inputs(seed: int = 0) -> dict:
    key = jax.random.key(seed)
    ks = jax.random.split(key, 20)
    nrm = lambda k, shape, scale: jax.random.normal(k, shape, jnp.float32) * scale
    gain = lambda k, shape: 1.0 + 0.05 * jax.random.normal(k, shape, jnp.float32)
    gate_base = jnp.array([0.0, 3.0, 0.0, 3.0], jnp.float32)[:, None]
    return {
        'x': nrm(ks[0], (BATCH, SEQ, D_MODEL), 1.0),
        'norm1_w': gain(ks[1], (DEPTH, D_MODEL)),
        'w_in': nrm(ks[2], (DEPTH, D_MODEL, IN_WIDTH), D_MODEL ** -0.5),
        'q_norm_w': gain(ks[3], (DEPTH, ATTN_HEAD_DIM)),
        'k_norm_w': gain(ks[4], (DEPTH, ATTN_HEAD_DIM)),
        'attn_rpb': nrm(ks[5], (DEPTH, ATTN_HEADS, 2 * NA_KH - 1, 2 * NA_KW - 1), 0.1),
        'attn_out_norm_w': gain(ks[6], (DEPTH, ATTN_WIDTH)),
        'mlstm_conv_w': nrm(ks[7], (DEPTH, MLSTM_CONV_W, 2 * MLSTM_WIDTH), MLSTM_CONV_W ** -0.5),
        'mlstm_conv_b': nrm(ks[8], (DEPTH, 2 * MLSTM_WIDTH), 0.02),
        'mlstm_gate_b': gate_base[None] + nrm(ks[9], (DEPTH, 4, MLSTM_HEADS), 0.5),
        'mlstm_norm_w': gain(ks[10], (DEPTH, MLSTM_WIDTH)),
        'w_out': nrm(ks[11], (DEPTH, MIX_WIDTH, D_MODEL), MIX_WIDTH ** -0.5),
        'norm2_w': gain(ks[12], (DEPTH, D_MODEL)),
        'peer_w_q': nrm(ks[13], (DEPTH, D_MODEL, PEER_HEADS * PEER_DKEY), D_MODEL ** -0.5),
        'peer_sub_keys': nrm(ks[14], (DEPTH, PEER_HEADS, 2, PEER_NKEYS, PEER_DHALF), PEER_DHALF ** -0.5),
        'peer_u': nrm(ks[15], (DEPTH, PEER_EXPERTS, D_MODEL), D_MODEL ** -0.5),
        'peer_v': nrm(ks[16], (DEPTH, PEER_EXPERTS, D_MODEL), PEER_HEADS ** -0.5),
    }


def reference(x, norm1_w, w_in, q_norm_w, k_norm_w, attn_rpb, attn_out_norm_w,
              mlstm_conv_w, mlstm_conv_b, mlstm_gate_b, mlstm_norm_w, w_out,
              norm2_w, peer_w_q, peer_sub_keys, peer_u, peer_v):
    for l in range(DEPTH):
        x = hybrid_layer(x, norm1_w[l], w_in[l], q_norm_w[l], k_norm_w[l], attn_rpb[l],
                         attn_out_norm_w[l], mlstm_conv_w[l], mlstm_conv_b[l], mlstm_gate_b[l],
                         mlstm_norm_w[l], w_out[l], norm2_w[l], peer_w_q[l], peer_sub_keys[l],
                         peer_u[l], peer_v[l])
    return x
```

```python
import numpy as np
import ml_dtypes
import concourse.bass as bass
import concourse.mybir as mybir
from concourse.bass_utils import run_bass_kernel_spmd
from contextlib import ExitStack

F32 = mybir.dt.float32
BF16 = mybir.dt.bfloat16
I32 = mybir.dt.int32
U32 = mybir.dt.uint32
ALU = mybir.AluOpType
AF = mybir.ActivationFunctionType
AX = mybir.AxisListType

S = 4096
D = 1024
NT = 32
INW = 3600
EPS = 1e-6
NEG = -30000.0


class Buf:
    __slots__ = ("name", "w", "r")

    def __init__(self, name=""):
        self.name = name
        self.w = {}
        self.r = {}


class Prog:
    ENG = ("pe", "act", "dve", "pool", "sp")
    NRINGS = {"sp": 12, "act": 6, "pool": 48}

    def __init__(self, nc, stack):
        self.nc = nc
        self.sem = {e: stack.enter_context(nc.semaphore("s_" + e)) for e in self.ENG}
        self.cnt = {e: 0 for e in self.ENG}
        self.ops = {e: [] for e in self.ENG}
        self.seen = {e: {} for e in self.ENG}
        self.ring = {}
        self.ring_i = {}
        self.ring_tok = {}
        for q in ("sp", "act", "pool"):
            self.ring[q] = [stack.enter_context(nc.semaphore("r_%s%d" % (q, i)))
                            for i in range(self.NRINGS[q])]
            self.ring_i[q] = 0
            self.ring_tok[q] = [None] * self.NRINGS[q]
        self.final_tokens = []

    def _need(self, eng, tok, waits):
        sem, val, teng = tok
        if teng == eng and eng == "pe":
            return
        k = id(sem)
        if self.seen[eng].get(k, 0) >= val:
            return
        self.seen[eng][k] = val
        waits.append((sem, val))

    def _deps(self, eng, reads, writes):
        waits = []
        for b in reads:
            for t in b.w.values():
                self._need(eng, t, waits)
        for b in writes:
            for t in b.w.values():
                self._need(eng, t, waits)
            for t in b.r.values():
                self._need(eng, t, waits)
        best = {}
        for sem, val in waits:
            k = id(sem)
            if k not in best or best[k][1] < val:
                best[k] = (sem, val)
        return list(best.values())

    def _commit(self, tok, reads, writes):
        k = id(tok[0])
        for b in reads:
            b.r[k] = tok
        for b in writes:
            b.w = {k: tok}
            b.r = {}

    def op(self, eng, fn, reads=(), writes=()):
        waits = self._deps(eng, reads, writes)
        self.cnt[eng] += 1
        tok = (self.sem[eng], self.cnt[eng], eng)
        self.ops[eng].append((waits, fn, (self.sem[eng], 1)))
        self._commit(tok, reads, writes)
        return tok

    def dma(self, q, fn, reads=(), writes=(), final=False):
        waits = self._deps(q, reads, writes)
        i = self.ring_i[q]
        nr = self.NRINGS[q]
        slot = i % nr
        prev = self.ring_tok[q][slot]
        if prev is not None:
            w2 = []
            self._need(q, prev, w2)
            waits = waits + w2
        sem = self.ring[q][slot]
        val = 16 * (i // nr + 1)
        self.ring_i[q] = i + 1
        tok = (sem, val, "dma_" + q)
        self.ring_tok[q][slot] = tok
        self.ops[q].append((waits, fn, (sem, 16)))
        self._commit(tok, reads, writes)
        if final:
            self.final_tokens.append(tok)
        return tok

    def _all_tokens(self):
        toks = []
        for e in ("pe", "act", "dve", "pool"):
            if self.cnt[e] > 0:
                toks.append((self.sem[e], self.cnt[e], e + "_all"))
        for q in ("act", "pool", "sp"):
            for t in self.ring_tok[q]:
                if t is not None:
                    toks.append(t)
        return toks

    def barrier(self):
        toks = self._all_tokens()
        for e in self.ENG:
            waits = []
            for t in toks:
                sem, val, teng = t
                k = id(sem)
                if self.seen[e].get(k, 0) >= val:
                    continue
                if teng == e + "_all":
                    continue
                self.seen[e][k] = val
                waits.append((sem, val))
            if waits:
                self.ops[e].append((waits, None, None))

    def emit(self):
        nc = self.nc
        fw = []
        for t in self._all_tokens():
            self._need("sp", t, fw)
        final_waits = fw

        def run(e, handle, extra=None):
            for waits, fn, inc in self.ops[e]:
                for sem, val in waits:
                    handle.wait_ge(sem, val)
                if fn is not None:
                    ins = fn(handle)
                    ins.then_inc(inc[0], inc[1])
            if extra:
                for sem, val in extra:
                    handle.wait_ge(sem, val)

        with nc.Block() as block:
            @block.tensor
            def _(h):
                run("pe", h)

            @block.scalar
            def _(h):
                run("act", h)

            @block.vector
            def _(h):
                run("dve", h)

            @block.gpsimd
            def _(h):
                run("pool", h)

            @block.sync
            def _(h):
                run("sp", h, final_waits)


class Ctx:
    pass


def bufs(n, name=""):
    return [Buf("%s%d" % (name, i)) for i in range(n)]


def phase_a(c):
    nc, P = c.nc, c.P
    with ExitStack() as st:
        sb = lambda n, s, d: st.enter_context(nc.sbuf_tensor(n, s, d))
        ps = lambda n, s, d: st.enter_context(nc.psum_tensor(n, s, d))
        Wbf = sb("a_Wbf", [128, 8, INW], BF16)
        stage = [sb("a_stage%d" % i, [128, INW], F32) for i in range(2)]
        w1 = sb("a_w1", [128, 8], F32)
        identb = sb("a_identb", [128, 128], BF16)
        gq = sb("a_gq", [128, 64], F32)
        gk = sb("a_gk", [128, 64], F32)
        xt = [sb("a_xt%d" % i, [128, D], F32) for i in range(2)]
        junk = sb("a_junk", [128, D], F32)
        ss = sb("a_ss", [128, 4], F32)
        xn = sb("a_xn", [128, D], BF16)
        xnT = sb("a_xnT", [128, 8, 128], BF16)
        sq = sb("a_sq", [128, 512], F32)
        tmp = sb("a_tmp", [128, 512], F32)
        s8 = sb("a_s8", [128, 24], F32)
        qn = sb("a_qn", [128, 512], BF16)
        qTs = sb("a_qTs", [128, 4, 128], BF16)
        ob = [sb("a_ob%d" % i, [128, 512], BF16) for i in range(2)]
        fm = [sb("a_fm%d" % i, [128, 4, 128], F32) for i in range(2)]
        gsb = sb("a_gsb", [4, 4, 128], F32)
        ps_t = ps("a_ps_t", [128, D], BF16)
        ps_g = [ps("a_ps_g%d" % i, [128, 512], F32) for i in range(2)]
        ps_q = ps("a_ps_q", [128, 4, 128], BF16)
        ps_f = [ps("a_ps_f%d" % i, [128, 4, 128], F32) for i in range(2)]
        ps_gt = ps("a_ps_gt", [4, 4, 128], F32)

        bW = bufs(8, "W")
        bst = bufs(2, "st")
        bc = Buf("consts")
        bxt = bufs(2, "xt")
        bjunk, bss, bxn, bxnT, bsq, btmp, bs8, bqn, bqTs = [Buf(n) for n in
            ("junk", "ss", "xn", "xnT", "sq", "tmp", "s8", "qn", "qTs")]
        bob = bufs(2, "ob")
        bfm = bufs(2, "fm")
        bgsb = Buf("gsb")
        bps_t, bps_q, bps_gt = Buf("ps_t"), Buf("ps_q"), Buf("ps_gt")
        bps_g = bufs(2, "ps_g")
        bps_f = bufs(2, "ps_f")

        P.dma("sp", lambda h: h.dma_start(out=w1[:], in_=c.norm1_w), writes=[bc])
        P.dma("sp", lambda h: h.dma_start(out=identb[:], in_=c.identb), writes=[bc])
        P.dma("sp", lambda h: h.dma_start(out=gq[:], in_=c.gq), writes=[bc])
        P.dma("sp", lambda h: h.dma_start(out=gk[:], in_=c.gk), writes=[bc])
        for kc in range(8):
            s_ = stage[kc % 2]
            P.dma("sp", lambda h, kc=kc, s_=s_: h.dma_start(out=s_[:], in_=c.w_in[kc * 128:(kc + 1) * 128, :]),
                  writes=[bst[kc % 2]])
            P.op("dve" if kc % 2 == 0 else "pool",
                 lambda h, kc=kc, s_=s_: h.tensor_scalar(out=Wbf[:, kc, :], in0=s_[:], scalar1=w1[:, kc:kc + 1],
                                                         scalar2=None, op0=ALU.mult),
                 reads=[bst[kc % 2], bc], writes=[bW[kc]])

        gi = [0]

        def mm_group_tok(cols, sub=None):
            k = gi[0] % 2
            gi[0] += 1
            c0, c1 = cols
            for kc in range(8):
                P.op("pe", lambda h, kc=kc, k=k: h.matmul(ps_g[k][:, 0:c1 - c0], lhsT=xnT[:, kc, :],
                                                       rhs=Wbf[:, kc, c0:c1], start=(kc == 0), stop=(kc == 7)),
                     reads=[bxnT, bW[kc]], writes=[bps_g[k]])
            return k

        oi = [0]
        fi = [0]
        for i in range(NT):
            x_ = xt[i % 2]
            bx_ = bxt[i % 2]
            P.dma("sp", lambda h, i=i, x_=x_: h.dma_start(out=x_[:], in_=c.x[i * 128:(i + 1) * 128, :]), writes=[bx_])
            P.op("act", lambda h, x_=x_: h.activation(out=junk[:], in_=x_[:], func=AF.Square, accum_out=ss[:, 0:1]),
                 reads=[bx_], writes=[bjunk, bss])
            P.op("act", lambda h: h.activation(out=ss[:, 1:2], in_=ss[:, 0:1], func=AF.Sqrt, scale=1.0 / D, bias=EPS),
                 reads=[bss], writes=[bss])
            P.op("dve", lambda h: h.reciprocal(out=ss[:, 2:3], in_=ss[:, 1:2]), reads=[bss], writes=[bss])
            P.op("dve", lambda h, x_=x_: h.tensor_scalar(out=xn[:], in0=x_[:], scalar1=ss[:, 2:3], scalar2=None,
                                                          op0=ALU.mult), reads=[bx_, bss], writes=[bxn])
            for kc in range(8):
                P.op("pe", lambda h, kc=kc: h.transpose(out=ps_t[:, kc * 128:(kc + 1) * 128],
                                                        in_=xn[:, kc * 128:(kc + 1) * 128], identity=identb[:]),
                     reads=[bxn, bc], writes=[bps_t])
            P.op("act", lambda h: h.copy(out=xnT[:].rearrange("p k t -> p (k t)"), in_=ps_t[:]),
                 reads=[bps_t], writes=[bxnT])

            for which, (c0, gain, dst) in enumerate(((0, gq, c.QT), (512, gk, c.KT))):
                k = mm_group_tok((c0, c0 + 512))
                P.op("act", lambda h, k=k: h.activation(out=sq[:], in_=ps_g[k][:], func=AF.Square),
                     reads=[bps_g[k]], writes=[bsq])
                P.op("dve", lambda h: h.tensor_reduce(out=s8[:, 0:8], in_=sq[:].rearrange("p (a b) -> p a b", b=64),
                                                      axis=AX.X, op=ALU.add), reads=[bsq], writes=[bs8])
                P.op("act", lambda h: h.activation(out=s8[:, 8:16], in_=s8[:, 0:8], func=AF.Sqrt, scale=1.0 / 64,
                                                   bias=EPS), reads=[bs8], writes=[bs8])
                P.op("dve", lambda h: h.reciprocal(out=s8[:, 16:24], in_=s8[:, 8:16]), reads=[bs8], writes=[bs8])
                P.op("dve", lambda h, k=k: h.tensor_tensor(
                    out=tmp[:].rearrange("p (a b) -> p a b", b=64),
                    in0=ps_g[k][:].rearrange("p (a b) -> p a b", b=64),
                    in1=s8[:, 16:24].unsqueeze(2).to_broadcast([128, 8, 64]), op=ALU.mult),
                    reads=[bps_g[k], bs8], writes=[btmp])
                P.op("pool", lambda h, gain=gain: h.tensor_tensor(
                    out=qn[:].rearrange("p (a b) -> p a b", b=64),
                    in0=tmp[:].rearrange("p (a b) -> p a b", b=64),
                    in1=gain[:].unsqueeze(1).to_broadcast([128, 8, 64]), op=ALU.mult),
                    reads=[btmp, bc], writes=[bqn])
                for hp in range(4):
                    P.op("pe", lambda h, hp=hp: h.transpose(out=ps_q[:, hp, :], in_=qn[:, hp * 128:(hp + 1) * 128],
                                                            identity=identb[:]), reads=[bqn, bc], writes=[bps_q])
                P.op("act", lambda h: h.copy(out=qTs[:], in_=ps_q[:]), reads=[bps_q], writes=[bqTs])
                P.dma("sp", lambda h, i=i, dst=dst: h.dma_start(
                    out=dst[:, :, i * 128:(i + 1) * 128].rearrange("a p t -> p a t"), in_=qTs[:]),
                    reads=[bqTs], writes=[c.bQKT[which][i]])

            for (c0, dst, bdst, fn) in ((1024, c.V, c.bV, AF.Copy), (2560, c.MV, c.bMV, AF.Copy),
                                        (3072, c.SIGO, c.bSIGO, AF.Sigmoid)):
                k = mm_group_tok((c0, c0 + 512))
                o = oi[0] % 2
                oi[0] += 1
                P.op("act", lambda h, k=k, o=o, fn=fn: h.activation(out=ob[o][:], in_=ps_g[k][:], func=fn),
                     reads=[bps_g[k]], writes=[bob[o]])
                P.dma("sp", lambda h, i=i, o=o, dst=dst: h.dma_start(out=dst[i * 128:(i + 1) * 128, :], in_=ob[o][:]),
                      reads=[bob[o]], writes=[bdst[i]])

            for half in range(2):
                f = fi[0] % 2
                fi[0] += 1
                for cc in range(4):
                    ch = half * 4 + cc
                    col = 1536 + ch * 128
                    for kc in range(8):
                        P.op("pe", lambda h, kc=kc, f=f, cc=cc, col=col: h.matmul(
                            ps_f[f][:, cc, :], lhsT=Wbf[:, kc, col:col + 128], rhs=xnT[:, kc, :],
                            start=(kc == 0), stop=(kc == 7)), reads=[bxnT, bW[kc]], writes=[bps_f[f]])
                P.op("dve", lambda h, f=f: h.tensor_copy(out=fm[f][:], in_=ps_f[f][:]), reads=[bps_f[f]], writes=[bfm[f]])
                P.dma("sp", lambda h, i=i, f=f, half=half: h.dma_start(
                    out=c.MQKT[half * 512:(half + 1) * 512, i * 128:(i + 1) * 128].rearrange("(a p) t -> p a t", p=128),
                    in_=fm[f][:]), reads=[bfm[f]], writes=[c.bMQKT[i]])
            for g in range(4):
                col = 3584 + 4 * g
                for kc in range(8):
                    P.op("pe", lambda h, kc=kc, g=g, col=col: h.matmul(
                        ps_gt[:, g, :], lhsT=Wbf[:, kc, col:col + 4], rhs=xnT[:, kc, :],
                        start=(kc == 0), stop=(kc == 7)), reads=[bxnT, bW[kc]], writes=[bps_gt])
            P.op("dve", lambda h: h.tensor_copy(out=gsb[:], in_=ps_gt[:]), reads=[bps_gt], writes=[bgsb])
            P.dma("sp", lambda h, i=i: h.dma_start(out=c.GT[:, :, i * 128:(i + 1) * 128].rearrange("g a t -> a g t"),
                                                   in_=gsb[:]), reads=[bgsb], writes=[c.bGT[i]])
    P.barrier()


def phase_b(c):
    nc, P = c.nc, c.P
    with ExitStack() as st:
        sb = lambda n, s, d: st.enter_context(nc.sbuf_tensor(n, s, d))
        ps = lambda n, s, d: st.enter_context(nc.psum_tensor(n, s, d))
        QT = sb("b_QT", [128, 4, S], BF16)
        KT = sb("b_KT", [128, 4, S], BF16)
        V = sb("b_V", [128, NT, 8, 65], BF16)
        TBI = sb("b_TBI", [128, 8, 896], F32)
        TBA = sb("b_TBA", [128, 8, 896], F32)
        MI = sb("b_MI", [128, 896], F32)
        MA = sb("b_MA", [128, 896], F32)
        gao = sb("b_gao", [128, 512], F32)
        sT = [sb("b_sT%d" % i, [128, 640], F32) for i in range(2)]
        pT = [sb("b_pT%d" % i, [128, 640], BF16) for i in range(2)]
        ao = sb("b_ao", [128, 512], F32)
        junk = sb("b_junk", [128, 512], F32)
        rc = [sb("b_rc%d" % i, [128, 1], F32) for i in range(2)]
        ss = sb("b_ss", [128, 4], F32)
        aob = [sb("b_aob%d" % i, [128, 512], BF16) for i in range(2)]
        ps_s = [ps("b_ps_s%d" % i, [128, 1024], F32) for i in range(2)]
        ps_o = [ps("b_ps_o%d" % i, [128, 128], F32) for i in range(2)]

        bQT, bKT = bufs(4, "bQT"), bufs(4, "bKT")
        bVt = bufs(NT, "bV")
        bones, btb, bm, bgao = Buf("ones"), Buf("tb"), Buf("m"), Buf("gao")
        bsT, bpT, brc, baob = bufs(2, "sT"), bufs(2, "pT"), bufs(2, "rc"), bufs(2, "aob")
        bao, bjunk, bss = Buf("ao"), Buf("junk"), Buf("ss")
        bps_s, bps_o = bufs(2, "ps_s"), bufs(2, "ps_o")

        P.dma("sp", lambda h: h.dma_start(out=TBI[:], in_=c.rpbg), writes=[btb])
        P.dma("sp", lambda h: h.dma_start(out=MI[:], in_=c.mask_i), writes=[bm])
        P.dma("sp", lambda h: h.dma_start(out=MA[:], in_=c.mask_a), writes=[bm])
        P.dma("sp", lambda h: h.dma_start(out=gao[:], in_=c.gao), writes=[bgao])
        for hp in range(4):
            P.dma("sp", lambda h, hp=hp: h.dma_start(out=QT[:, hp, :], in_=c.QT[hp]),
                  reads=c.bQKT[0], writes=[bQT[hp]])
            P.dma("act", lambda h, hp=hp: h.dma_start(out=KT[:, hp, :], in_=c.KT[hp]),
                  reads=c.bQKT[1], writes=[bKT[hp]])
        P.op("pool", lambda h: h.memset(V[:, :, :, 64:65], 1.0), writes=[bones])
        for i in range(NT):
            P.dma("sp" if i % 2 == 0 else "act", lambda h, i=i: h.dma_start(
                out=V[:, i, :, 0:64], in_=c.V[i * 128:(i + 1) * 128, :].rearrange("p (a b) -> p a b", b=64)),
                reads=[c.bV[i]], writes=[bVt[i]])
        for hd in range(8):
            P.op("dve", lambda h, hd=hd: h.tensor_tensor(out=TBA[:, hd, :], in0=TBI[:, hd, :], in1=MA[:], op=ALU.add),
                 reads=[btb, bm], writes=[btb])
        for hd in range(8):
            P.op("dve", lambda h, hd=hd: h.tensor_tensor(out=TBI[:, hd, :], in0=TBI[:, hd, :], in1=MI[:], op=ALU.add),
                 reads=[btb, bm], writes=[btb])

        it = 0
        for j in range(NT):
            if 2 <= j <= 29:
                kts = list(range(j - 2, j + 3)); tb = TBI; s0 = 1
            elif j == 0:
                kts = [0, 1, 2, 3]; tb = TBA; s0 = 3
            elif j == 1:
                kts = [0, 1, 2, 3]; tb = TBA; s0 = 2
            elif j == 30:
                kts = [28, 29, 30, 31]; tb = TBA; s0 = 1
            else:
                kts = [28, 29, 30, 31]; tb = TBA; s0 = 0
            n = len(kts)
            for hd in range(8):
                hp, hh = hd // 2, hd % 2
                k = it % 2
                it += 1
                p0, p1 = hh * 64, hh * 64 + 64
                for idx, kt in enumerate(kts):
                    P.op("pe", lambda h, k=k, idx=idx, kt=kt, hp=hp, p0=p0, p1=p1, j=j: h.matmul(
                        ps_s[k][:, idx * 128:(idx + 1) * 128], lhsT=KT[p0:p1, hp, kt * 128:(kt + 1) * 128],
                        rhs=QT[p0:p1, hp, j * 128:(j + 1) * 128], start=True, stop=True),
                        reads=[bKT[hp], bQT[hp]], writes=[bps_s[k]])
                P.op("dve", lambda h, k=k, n=n, tb=tb, s0=s0, hd=hd: h.scalar_tensor_tensor(
                    out=sT[k][:, 0:n * 128], in0=ps_s[k][:, 0:n * 128], scalar=0.125,
                    in1=tb[:, hd, s0 * 128:(s0 + n) * 128], op0=ALU.mult, op1=ALU.add),
                    reads=[bps_s[k], btb], writes=[bsT[k]])
                P.op("act", lambda h, k=k, n=n: h.activation(out=pT[k][:, 0:n * 128], in_=sT[k][:, 0:n * 128],
                                                             func=AF.Exp), reads=[bsT[k]], writes=[bpT[k]])
                for idx, kt in enumerate(kts):
                    P.op("pe", lambda h, k=k, idx=idx, kt=kt, hd=hd, n=n: h.matmul(
                        ps_o[k][:, 0:65], lhsT=pT[k][:, idx * 128:(idx + 1) * 128], rhs=V[:, kt, hd, :],
                        start=(idx == 0), stop=(idx == n - 1)),
                        reads=[bpT[k], bVt[kt], bones], writes=[bps_o[k]])
                P.op("dve", lambda h, k=k: h.reciprocal(out=rc[k][:], in_=ps_o[k][:, 64:65]),
                     reads=[bps_o[k]], writes=[brc[k]])
                P.op("dve", lambda h, k=k, hd=hd: h.tensor_scalar(
                    out=ao[:, hd * 64:(hd + 1) * 64], in0=ps_o[k][:, 0:64], scalar1=rc[k][:], scalar2=None,
                    op0=ALU.mult), reads=[bps_o[k], brc[k]], writes=[bao])
            o = j % 2
            P.op("act", lambda h: h.activation(out=junk[:], in_=ao[:], func=AF.Square, accum_out=ss[:, 0:1]),
                 reads=[bao], writes=[bjunk, bss])
            P.op("act", lambda h: h.activation(out=ss[:, 1:2], in_=ss[:, 0:1], func=AF.Sqrt, scale=1.0 / 512, bias=EPS),
                 reads=[bss], writes=[bss])
            P.op("dve", lambda h: h.reciprocal(out=ss[:, 2:3], in_=ss[:, 1:2]), reads=[bss], writes=[bss])
            P.op("dve", lambda h, o=o: h.scalar_tensor_tensor(out=aob[o][:], in0=ao[:], scalar=ss[:, 2:3], in1=gao[:],
                                                          op0=ALU.mult, op1=ALU.mult),
                 reads=[bao, bss, bgao], writes=[baob[o]])
            P.dma("sp", lambda h, j=j, o=o: h.dma_start(out=c.AOUT[j * 128:(j + 1) * 128, :], in_=aob[o][:]),
                  reads=[baob[o]], writes=[c.bAOUT[j]])
    P.barrier()


def phase_c(c):
    nc, P = c.nc, c.P
    with ExitStack() as st0:
        sb0 = lambda n, s, d: st0.enter_context(nc.sbuf_tensor(n, s, d))
        COLS = sb0("c_COLS", [128, NT, 24], F32)
        bCOLS = Buf("COLS")
        with ExitStack() as st:
            sb = lambda n, s, d: st.enter_context(nc.sbuf_tensor(n, s, d))
            ps = lambda n, s, d: st.enter_context(nc.psum_tensor(n, s, d))
            G1, G2, CL, Aa, AA, ZER, T1 = [sb("c1_" + n, [4, S], F32) for n in ("G1", "G2", "CL", "Aa", "AA", "ZER", "T1")]
            bG1, bG2, bCL, bAa, bAA, bZER, bT1 = [Buf(n) for n in ("G1", "G2", "CL", "Aa", "AA", "ZER", "T1")]
            BCR = sb("c1_BCR", [4, NT, 257], F32)
            ROWS = sb("c1_ROWS", [24, S], F32)
            gb = sb("c1_gb", [4, 4], F32)
            ngb = sb("c1_ngb", [4, 4], F32)
            AE = sb("c1_AE", [4, NT], F32)
            APv = sb("c1_AP", [4, NT], F32)
            dd = sb("c1_dd", [4, NT], F32)
            identf = sb("c1_identf", [128, 128], F32)
            ps_c = ps("c1_ps_c", [128, NT, 32], F32)
            bBCR, bgb, bAE, bAPv, bdd, bid, bps_c = [Buf(n) for n in ("BCR", "gb", "AE", "AP", "dd", "id", "ps_c")]
            bROWS = bufs(6, "ROWS")
            P.dma("sp", lambda h: h.dma_start(out=gb[:], in_=c.gate_b), writes=[bgb])
            P.dma("sp", lambda h: h.dma_start(out=identf[:], in_=c.identf), writes=[bid])
            P.op("dve", lambda h: h.tensor_scalar(out=ngb[:], in0=gb[:], scalar1=-1.0, scalar2=None, op0=ALU.mult),
                 reads=[bgb], writes=[bgb])
            P.op("pool", lambda h: h.memset(ZER[:], 0.0), writes=[bZER])
            for d in range(2):
                rv = (lambda t: t[:, :]) if d == 0 else (lambda t: t[:, ::-1])
                gi_, gf_ = 2 * d, 2 * d + 1
                P.dma("sp", lambda h, gi_=gi_: h.dma_start(out=G1[:], in_=c.GT[gi_]), reads=c.bGT, writes=[bG1])
                P.dma("sp", lambda h, gf_=gf_: h.dma_start(out=G2[:], in_=c.GT[gf_]), reads=c.bGT, writes=[bG2])
                P.op("act", lambda h, gf_=gf_: h.activation(out=G2[:], in_=G2[:], func=AF.Exp, scale=-1.0,
                                                            bias=ngb[:, gf_:gf_ + 1]), reads=[bG2, bgb], writes=[bG2])
                P.op("act", lambda h: h.activation(out=G2[:], in_=G2[:], func=AF.Ln, bias=1.0), reads=[bG2], writes=[bG2])
                P.op("dve", lambda h, rv=rv: h.tensor_tensor_scan(out=rv(CL), data0=rv(G2), data1=ZER[:], initial=0.0,
                                                                  op0=ALU.add, op1=ALU.add),
                     reads=[bG2, bZER], writes=[bCL])
                P.op("dve", lambda h, gi_=gi_: h.scalar_tensor_tensor(out=Aa[:], in0=G1[:], scalar=gb[:, gi_:gi_ + 1],
                                                                      in1=CL[:], op0=ALU.add, op1=ALU.add),
                     reads=[bG1, bgb, bCL], writes=[bAa])
                P.op("dve", lambda h, rv=rv: h.tensor_tensor_scan(out=rv(AA), data0=rv(Aa), data1=ZER[:], initial=0.0,
                                                                  op0=ALU.max, op1=ALU.add),
                     reads=[bAa, bZER], writes=[bAA])
                P.op("dve", lambda h: h.tensor_tensor(out=T1[:], in0=CL[:], in1=AA[:], op=ALU.subtract),
                     reads=[bCL, bAA], writes=[bT1])
                P.op("act", lambda h: h.activation(out=T1[:], in_=T1[:], func=AF.Exp), reads=[bT1], writes=[bT1])
                P.dma("sp", lambda h, d=d: h.dma_start(out=ROWS[12 * d + 8:12 * d + 12, :], in_=T1[:]),
                      reads=[bT1], writes=[bROWS[3 * d + 2]])
                P.dma("sp", lambda h, d=d: h.dma_start(out=ROWS[12 * d:12 * d + 4, :], in_=Aa[:]),
                      reads=[bAa], writes=[bROWS[3 * d]])
                AAv = AA[:].rearrange("p (c t) -> p c t", t=128)
                epos = 127 if d == 0 else 0
                P.op("dve", lambda h, AAv=AAv, epos=epos: h.tensor_copy(out=AE[:], in_=AAv[:, :, epos]),
                     reads=[bAA], writes=[bAE])
                P.op("dve", lambda h: h.memset(APv[:], 0.0), writes=[bAPv])
                if d == 0:
                    P.op("dve", lambda h: h.tensor_copy(out=APv[:, 1:NT], in_=AE[:, 0:NT - 1]), reads=[bAE], writes=[bAPv])
                else:
                    P.op("dve", lambda h: h.tensor_copy(out=APv[:, 0:NT - 1], in_=AE[:, 1:NT]), reads=[bAE], writes=[bAPv])
                G1v = G1[:].rearrange("p (c t) -> p c t", t=128)
                G2v = G2[:].rearrange("p (c t) -> p c t", t=128)
                Aav = Aa[:].rearrange("p (c t) -> p c t", t=128)
                P.op("dve", lambda h, G1v=G1v, Aav=Aav: h.tensor_tensor(
                    out=G1v, in0=Aav, in1=AE[:].unsqueeze(2).to_broadcast([4, NT, 128]), op=ALU.subtract),
                    reads=[bAa, bAE], writes=[bG1])
                P.op("act", lambda h: h.activation(out=G1[:], in_=G1[:], func=AF.Exp), reads=[bG1], writes=[bG1])
                P.dma("sp", lambda h, d=d: h.dma_start(out=ROWS[12 * d + 4:12 * d + 8, :], in_=G1[:]),
                      reads=[bG1], writes=[bROWS[3 * d + 1]])
                P.op("dve", lambda h, AAv=AAv: h.tensor_scalar(out=BCR[:, :, 0:128], in0=AAv, scalar1=-1.0, scalar2=None,
                                                               op0=ALU.mult), reads=[bAA], writes=[bBCR])
                P.op("dve", lambda h, G2v=G2v, AAv=AAv: h.tensor_tensor(
                    out=G2v, in0=APv[:].unsqueeze(2).to_broadcast([4, NT, 128]), in1=AAv, op=ALU.subtract),
                    reads=[bAA, bAPv], writes=[bG2])
                P.op("act", lambda h, G2v=G2v: h.activation(out=BCR[:, :, 128:256], in_=G2v, func=AF.Exp),
                     reads=[bG2], writes=[bBCR])
                P.op("dve", lambda h: h.tensor_tensor(out=dd[:], in0=APv[:], in1=AE[:], op=ALU.subtract),
                     reads=[bAPv, bAE], writes=[bdd])
                P.op("act", lambda h: h.activation(out=BCR[:, :, 256], in_=dd[:], func=AF.Exp), reads=[bdd], writes=[bBCR])
                P.dma("sp", lambda h, d=d: h.dma_start(out=c.BCRD[d], in_=BCR[:]), reads=[bBCR], writes=[c.bBCRD[d]])
            for ch in range(NT):
                P.op("pe", lambda h, ch=ch: h.transpose(out=ps_c[:, ch, 0:24], in_=ROWS[0:24, ch * 128:(ch + 1) * 128],
                                                        identity=identf[0:24, 0:24]), reads=bROWS + [bid], writes=[bps_c])
            P.op("dve", lambda h: h.tensor_copy(out=COLS[:], in_=ps_c[:, :, 0:24]), reads=[bps_c], writes=[bCOLS])
        P.barrier()

        with ExitStack() as st:
            sb = lambda n, s, d: st.enter_context(nc.sbuf_tensor(n, s, d))
            ps = lambda n, s, d: st.enter_context(nc.psum_tensor(n, s, d))
            QKT = sb("c_QKT", [128, 8, S], BF16)
            bQKT = bufs(8, "cQKT")
            cw = sb("c_cw", [128, 8, 5], F32)
            cb = sb("c_cb", [128, 8], F32)
            bcw = Buf("cw")
            P.dma("sp", lambda h: h.dma_start(out=cw[:], in_=c.conv_w), writes=[bcw])
            P.dma("sp", lambda h: h.dma_start(out=cb[:], in_=c.conv_b), writes=[bcw])
            with ExitStack() as st2:
                sb2 = lambda n, s, d: st2.enter_context(nc.sbuf_tensor(n, s, d))
                xpad = [sb2("c2_xpad%d" % i, [128, S + 4], F32) for i in range(2)]
                acc = [sb2("c2_acc%d" % i, [128, S], F32) for i in range(2)]
                bxp, bacc = bufs(2, "xpad"), bufs(2, "acc")
                for i in range(2):
                    P.op("pool", lambda h, i=i: h.memset(xpad[i][:, 0:2], 0.0), writes=[bxp[i]])
                    P.op("pool", lambda h, i=i: h.memset(xpad[i][:, S + 2:S + 4], 0.0), writes=[bxp[i]])
                for cc in range(8):
                    k = cc % 2
                    P.dma("sp", lambda h, cc=cc, k=k: h.dma_start(out=xpad[k][:, 2:S + 2], in_=c.MQKT[cc * 128:(cc + 1) * 128, :]),
                          reads=c.bMQKT, writes=[bxp[k]])
                    P.op("dve", lambda h, cc=cc, k=k: h.tensor_scalar(out=acc[k][:], in0=xpad[k][:, 0:S], scalar1=cw[:, cc, 0:1],
                                                                      scalar2=cb[:, cc:cc + 1], op0=ALU.mult, op1=ALU.add),
                         reads=[bxp[k], bcw], writes=[bacc[k]])
                    for j in range(1, 5):
                        P.op("dve", lambda h, cc=cc, k=k, j=j: h.scalar_tensor_tensor(
                            out=acc[k][:], in0=xpad[k][:, j:j + S], scalar=cw[:, cc, j:j + 1], in1=acc[k][:],
                            op0=ALU.mult, op1=ALU.add), reads=[bxp[k], bcw, bacc[k]], writes=[bacc[k]])
                    if cc < 4:
                        P.op("act", lambda h, cc=cc, k=k: h.activation(out=QKT[:, cc, :], in_=acc[k][:], func=AF.Silu),
                             reads=[bacc[k]], writes=[bQKT[cc]])
                    else:
                        P.op("act", lambda h, cc=cc, k=k: h.activation(out=acc[k][:], in_=acc[k][:], func=AF.Silu),
                             reads=[bacc[k]], writes=[bacc[k]])
                        P.op("pool", lambda h, cc=cc, k=k: h.tensor_scalar(out=QKT[:, cc, :], in0=acc[k][:], scalar1=128.0 ** -0.5,
                                                                           scalar2=None, op0=ALU.mult),
                             reads=[bacc[k]], writes=[bQKT[cc]])
            P.barrier()

            MVs = sb("c_MVs", [128, NT, 4, 129], BF16)
            bMVs = bufs(NT, "cMV")
            bones = Buf("ones")
            SEL = sb("c_SEL", [4, 4, 128], F32)
            MSK = sb("c_MSK", [128, 2, 128], F32)
            identb = sb("c_identb", [128, 128], BF16)
            gmn = sb("c_gmn", [128, 512], F32)
            bconst = Buf("const")
            P.dma("sp", lambda h: h.dma_start(out=SEL[:], in_=c.sel), writes=[bconst])
            P.dma("sp", lambda h: h.dma_start(out=MSK[:], in_=c.msk), writes=[bconst])
            P.dma("sp", lambda h: h.dma_start(out=identb[:], in_=c.identb), writes=[bconst])
            P.dma("sp", lambda h: h.dma_start(out=gmn[:], in_=c.gmn), writes=[bconst])
            P.op("pool", lambda h: h.memset(MVs[:, :, :, 128:129], 1.0), writes=[bones])
            for i in range(NT):
                P.dma("sp" if i % 2 == 0 else "act", lambda h, i=i: h.dma_start(
                    out=MVs[:, i, :, 0:128], in_=c.MV[i * 128:(i + 1) * 128, :].rearrange("p (a b) -> p a b", b=128)),
                    reads=[c.bMV[i]], writes=[bMVs[i]])
            Cst = [sb("c_C%d" % i, [128, 129], F32) for i in range(4)]
            Cbf = [sb("c_Cbf%d" % i, [128, 129], BF16) for i in range(4)]
            bC, bCbf = bufs(4, "C"), bufs(4, "Cbf")
            bcr = [sb("c_bcr%d" % i, [4, 257], F32) for i in range(2)]
            bbcr = bufs(2, "bcr")
            Gt = [sb("c_G%d" % i, [128, 128], F32) for i in range(2)]
            Wt = [sb("c_W%d" % i, [128, 128], F32) for i in range(2)]
            PT = [sb("c_PT%d" % i, [128, 128], BF16) for i in range(2)]
            qs = [sb("c_qs%d" % i, [128, 128], BF16) for i in range(2)]
            kw = [sb("c_kw%d" % i, [128, 128], BF16) for i in range(2)]
            dec = [sb("c_dec%d" % i, [128, 1], F32) for i in range(2)]
            dn = [sb("c_dn%d" % i, [128, 2], F32) for i in range(2)]
            bG, bW, bPT, bqs, bkw, bdec, bdn = [bufs(2, n) for n in ("G", "W", "PT", "qs", "kw", "dec", "dn")]
            hbuf = [sb("c_hbuf%d" % i, [128, 512], F32) for i in range(2)]
            bhbuf = bufs(2, "hbuf")
            hf = sb("c_hf", [128, 512], F32)
            sg = sb("c_sg", [128, 512], BF16)
            sq = sb("c_sq", [128, 512], F32)
            s4 = sb("c_s4", [128, 12], F32)
            hmo = [sb("c_hmo%d" % i, [128, 512], BF16) for i in range(2)]
            bhf, bsg, bsq, bs4 = Buf("hf"), Buf("sg"), Buf("sq"), Buf("s4")
            bhmo = bufs(2, "hmo")
            ps_bc = [ps("c_ps_bc%d" % i, [128, 512], F32) for i in range(2)]
            ps_st = [ps("c_ps_st%d" % i, [128, 128], F32) for i in range(2)]
            ps_n = [ps("c_ps_n%d" % i, [128, 512], F32) for i in range(2)]
            ps_kt = ps("c_ps_kt", [128, 128], BF16)
            ps_dc = ps("c_ps_dc", [128, 512], F32)
            bps_bc, bps_st, bps_n = bufs(2, "ps_bc"), bufs(2, "ps_st"), bufs(2, "ps_n")
            bps_kt, bps_dc = Buf("ps_kt"), Buf("ps_dc")

            it = 0
            for d in range(2):
                for hd in range(4):
                    P.op("pool", lambda h, hd=hd: h.memset(Cst[hd][:], 0.0), writes=[bC[hd]])
                    P.op("pool", lambda h, hd=hd: h.memset(Cbf[hd][:], 0.0), writes=[bCbf[hd]])
                order = list(range(NT)) if d == 0 else list(range(NT - 1, -1, -1))
                for ci, ch in enumerate(order):
                    kb = ci % 2
                    P.dma("sp", lambda h, d=d, ch=ch, kb=kb: h.dma_start(out=bcr[kb][:], in_=c.BCRD[d, :, ch, :]),
                          reads=[c.bBCRD[d]], writes=[bbcr[kb]])
                    hb = hbuf[ci % 2]
                    bhb = bhbuf[ci % 2]
                    tsl = slice(ch * 128, (ch + 1) * 128)
                    for hd in range(4):
                        k = it % 2
                        it += 1
                        a_col = COLS[:, ch, 12 * d + hd:12 * d + hd + 1]
                        wk_col = COLS[:, ch, 12 * d + 4 + hd:12 * d + 5 + hd]
                        emt_col = COLS[:, ch, 12 * d + 8 + hd:12 * d + 9 + hd]
                        P.op("pe", lambda h, k=k, kb=kb, hd=hd: h.matmul(ps_bc[k][:, 0:257], lhsT=SEL[:, hd, :], rhs=bcr[kb][:],
                                                                         start=True, stop=True),
                             reads=[bconst, bbcr[kb]], writes=[bps_bc[k]])
                        P.op("pe", lambda h, k=k, hd=hd, tsl=tsl: h.matmul(ps_st[k][:], lhsT=QKT[:, 4 + hd, tsl], rhs=QKT[:, hd, tsl],
                                                                          start=True, stop=True),
                             reads=[bQKT[4 + hd], bQKT[hd]], writes=[bps_st[k]])
                        P.op("dve", lambda h, k=k, d=d: h.tensor_tensor(out=Gt[k][:], in0=ps_bc[k][:, 0:128], in1=MSK[:, d, :],
                                                                       op=ALU.add), reads=[bps_bc[k], bconst], writes=[bG[k]])
                        P.op("act", lambda h, k=k, a_col=a_col: h.activation(out=Wt[k][:], in_=Gt[k][:], func=AF.Exp, bias=a_col),
                             reads=[bG[k], bCOLS], writes=[bW[k]])
                        P.op("dve", lambda h, k=k: h.tensor_tensor(out=PT[k][:], in0=ps_st[k][:], in1=Wt[k][:], op=ALU.mult),
                             reads=[bps_st[k], bW[k]], writes=[bPT[k]])
                        P.op("dve", lambda h, k=k, hd=hd, tsl=tsl: h.tensor_tensor(out=qs[k][:], in0=ps_bc[k][:, 128:256],
                                                                                  in1=QKT[:, hd, tsl], op=ALU.mult),
                             reads=[bps_bc[k], bQKT[hd]], writes=[bqs[k]])
                        P.op("act", lambda h, k=k: h.copy(out=dec[k][:], in_=ps_bc[k][:, 256:257]),
                             reads=[bps_bc[k]], writes=[bdec[k]])
                        P.op("pe", lambda h, k=k, ch=ch, hd=hd: h.matmul(ps_n[k][:, 0:129], lhsT=PT[k][:], rhs=MVs[:, ch, hd, :],
                                                                         start=True, stop=False),
                             reads=[bPT[k], bMVs[ch], bones], writes=[bps_n[k]])
                        P.op("pe", lambda h, k=k, hd=hd: h.matmul(ps_n[k][:, 0:129], lhsT=qs[k][:], rhs=Cbf[hd][:],
                                                                  start=False, stop=True),
                             reads=[bqs[k], bCbf[hd]], writes=[bps_n[k]])
                        P.op("pe", lambda h, hd=hd, tsl=tsl: h.transpose(out=ps_kt[:], in_=QKT[:, 4 + hd, tsl], identity=identb[:]),
                             reads=[bQKT[4 + hd], bconst], writes=[bps_kt])
                        P.op("act", lambda h, k=k, wk_col=wk_col: h.activation(out=kw[k][:], in_=ps_kt[:], func=AF.Copy, scale=wk_col),
                             reads=[bps_kt, bCOLS], writes=[bkw[k]])
                        P.op("pe", lambda h, k=k, ch=ch, hd=hd: h.matmul(ps_dc[:, 0:129], lhsT=kw[k][:], rhs=MVs[:, ch, hd, :],
                                                                         start=True, stop=True),
                             reads=[bkw[k], bMVs[ch], bones], writes=[bps_dc])
                        P.op("dve", lambda h, k=k, hd=hd: h.scalar_tensor_tensor(out=Cst[hd][:], in0=Cst[hd][:], scalar=dec[k][:],
                                                                                 in1=ps_dc[:, 0:129], op0=ALU.mult, op1=ALU.add),
                             reads=[bC[hd], bdec[k], bps_dc], writes=[bC[hd]])
                        P.op("act", lambda h, hd=hd: h.copy(out=Cbf[hd][:], in_=Cst[hd][:]), reads=[bC[hd]], writes=[bCbf[hd]])
                        P.op("act", lambda h, k=k: h.activation(out=dn[k][:, 1:2], in_=ps_n[k][:, 128:129], func=AF.Abs),
                             reads=[bps_n[k]], writes=[bdn[k]])
                        P.op("dve", lambda h, k=k, emt_col=emt_col: h.tensor_scalar(out=dn[k][:, 0:1], in0=dn[k][:, 1:2],
                                                                                    scalar1=emt_col, scalar2=None, op0=ALU.max),
                             reads=[bdn[k], bCOLS], writes=[bdn[k]])
                        P.op("dve", lambda h, k=k: h.reciprocal(out=dn[k][:, 1:2], in_=dn[k][:, 0:1]), reads=[bdn[k]], writes=[bdn[k]])
                        P.op("dve", lambda h, k=k, hd=hd, hb=hb: h.tensor_scalar(out=hb[:, hd * 128:(hd + 1) * 128], in0=ps_n[k][:, 0:128],
                                                                                 scalar1=dn[k][:, 1:2], scalar2=None, op0=ALU.mult),
                             reads=[bps_n[k], bdn[k]], writes=[bhb])
                    if d == 0:
                        P.dma("sp", lambda h, ch=ch, hb=hb: h.dma_start(out=c.HF[ch * 128:(ch + 1) * 128, :], in_=hb[:]),
                              reads=[bhb], writes=[c.bHF[ch]])
                    else:
                        o = ci % 2
                        P.dma("sp", lambda h, ch=ch: h.dma_start(out=hf[:], in_=c.HF[ch * 128:(ch + 1) * 128, :]),
                              reads=[c.bHF[ch]], writes=[bhf])
                        P.dma("sp", lambda h, ch=ch: h.dma_start(out=sg[:], in_=c.SIGO[ch * 128:(ch + 1) * 128, :]),
                              reads=[c.bSIGO[ch]], writes=[bsg])
                        P.op("pool", lambda h, hb=hb: h.tensor_tensor(out=hf[:], in0=hf[:], in1=hb[:], op=ALU.add),
                             reads=[bhb, bhf], writes=[bhf])
                        P.op("act", lambda h: h.activation(out=sq[:], in_=hf[:], func=AF.Square), reads=[bhf], writes=[bsq])
                        P.op("dve", lambda h: h.tensor_reduce(out=s4[:, 0:4], in_=sq[:].rearrange("p (a b) -> p a b", b=128),
                                                              axis=AX.X, op=ALU.add), reads=[bsq], writes=[bs4])
                        P.op("act", lambda h: h.activation(out=s4[:, 4:8], in_=s4[:, 0:4], func=AF.Sqrt, scale=1.0 / 128, bias=EPS),
                             reads=[bs4], writes=[bs4])
                        P.op("dve", lambda h: h.reciprocal(out=s4[:, 8:12], in_=s4[:, 4:8]), reads=[bs4], writes=[bs4])
                        P.op("dve", lambda h: h.tensor_tensor(out=sq[:].rearrange("p (a b) -> p a b", b=128),
                                                              in0=hf[:].rearrange("p (a b) -> p a b", b=128),
                                                              in1=s4[:, 8:12].unsqueeze(2).to_broadcast([128, 4, 128]), op=ALU.mult),
                             reads=[bhf, bs4, bsq], writes=[bsq])
                        P.op("pool", lambda h: h.tensor_tensor(out=sq[:], in0=sq[:], in1=gmn[:], op=ALU.mult),
                             reads=[bsq, bconst], writes=[bsq])
                        P.op("pool", lambda h, o=o: h.tensor_tensor(out=hmo[o][:], in0=sq[:], in1=sg[:], op=ALU.mult),
                             reads=[bsq, bsg], writes=[bhmo[o]])
                        P.dma("sp", lambda h, ch=ch, o=o: h.dma_start(out=c.HM[ch * 128:(ch + 1) * 128, :], in_=hmo[o][:]),
                              reads=[bhmo[o]], writes=[c.bHM[ch]])
    P.barrier()


def phase_d(c):
    nc, P = c.nc, c.P
    with ExitStack() as st:
        sb = lambda n, s, d: st.enter_context(nc.sbuf_tensor(n, s, d))
        ps = lambda n, s, d: st.enter_context(nc.psum_tensor(n, s, d))
        Wo = sb("d_Wo", [128, 8, D], BF16)
        Wq = sb("d_Wq", [128, 8, 2048], BF16)
        SKT = sb("d_SKT", [128, 16, 128], BF16)
        stage = [sb("d_stage%d" % i, [128, 2048], F32) for i in range(2)]
        gn2 = sb("d_gn2", [128, D], F32)
        identb = sb("d_identb", [128, 128], BF16)
        THR = sb("d_THR", [128, 16], F32)
        IOT = sb("d_IOT", [128, 16], F32)
        bconst = Buf("dconst")
        bst = bufs(2, "dst")
        bWo, bWq = bufs(8, "Wo"), bufs(8, "Wq")
        bSKT = Buf("SKT")
        for (t, src) in ((gn2, c.gn2), (identb, c.identb), (THR, c.thr), (IOT, c.iot)):
            P.dma("sp", lambda h, t=t, src=src: h.dma_start(out=t[:], in_=src), writes=[bconst])
        n = 0
        for kc in range(8):
            k = n % 2; n += 1
            P.dma("sp", lambda h, kc=kc, k=k: h.dma_start(out=stage[k][:, 0:D], in_=c.w_out[kc * 128:(kc + 1) * 128, :]), writes=[bst[k]])
            P.op("dve", lambda h, kc=kc, k=k: h.tensor_copy(out=Wo[:, kc, :], in_=stage[k][:, 0:D]), reads=[bst[k]], writes=[bWo[kc]])
        for kc in range(8):
            k = n % 2; n += 1
            P.dma("sp", lambda h, kc=kc, k=k: h.dma_start(out=stage[k][:], in_=c.w_q[kc * 128:(kc + 1) * 128, :]), writes=[bst[k]])
            P.op("dve", lambda h, kc=kc, k=k: h.tensor_copy(out=Wq[:, kc, :], in_=stage[k][:]), reads=[bst[k]], writes=[bWq[kc]])
        k = n % 2; n += 1
        P.dma("sp", lambda h, k=k: h.dma_start(out=stage[k][:].rearrange("p (a b) -> p a b", b=128), in_=c.skt), writes=[bst[k]])
        P.op("dve", lambda h, k=k: h.tensor_copy(out=SKT[:].rearrange("p a b -> p (a b)"), in_=stage[k][:]), reads=[bst[k]], writes=[bSKT])

        cat = sb("d_cat", [128, D], BF16)
        catT = sb("d_catT", [128, 8, 128], BF16)
        xt = sb("d_xt", [128, D], F32)
        x1 = sb("d_x1", [128, D], F32)
        junk = sb("d_junk", [128, D], F32)
        ss = sb("d_ss", [128, 4], F32)
        xn2 = sb("d_xn2", [128, D], F32)
        xn2b = sb("d_xn2b", [128, D], BF16)
        xn2T = sb("d_xn2T", [128, 8, 128], BF16)
        qT = sb("d_qT", [128, 16, 128], BF16)
        sc = sb("d_sc", [128, 16, 128], F32)
        sc2 = sb("d_sc2", [128, 16, 128], F32)
        m8 = sb("d_m8", [128, 16, 16], F32)
        i8 = sb("d_i8", [128, 16, 16], U32)
        i8f = sb("d_i8f", [128, 16, 16], F32)
        cand = sb("d_cand", [128, 8, 256], F32)
        cand2 = sb("d_cand2", [128, 8, 256], F32)
        t8 = sb("d_t8", [128, 8, 16], F32)
        j8 = sb("d_j8", [128, 8, 16], U32)
        jf = sb("d_jf", [128, 128], F32)
        T4 = sb("d_T4", [128, 128, 16], F32)
        af = sb("d_af", [128, 128], F32)
        bf_ = sb("d_bf", [128, 128], F32)
        E1 = sb("d_E1", [128, 128], F32)
        E2 = sb("d_E2", [128, 128], F32)
        eidx = sb("d_eidx", [128, 128], I32)
        gts = sb("d_gts", [128, 8, 16], F32)
        g8 = sb("d_g8", [128, 16], F32)
        adot = sb("d_adot", [128, 128], F32)
        ga = sb("d_ga", [128, 128], F32)
        NG = 4
        ug = [sb("d_ug%d" % i, [128, D], F32) for i in range(NG)]
        vg = [sb("d_vg%d" % i, [128, D], F32) for i in range(NG)]
        yacc = sb("d_y", [128, D], F32)
        ps_t = ps("d_ps_t", [128, D], BF16)
        ps_o = [ps("d_ps_o%d" % i, [128, 512], F32) for i in range(2)]
        ps_q = [ps("d_ps_q%d" % i, [128, 4, 128], F32) for i in range(2)]
        ps_s = [ps("d_ps_s%d" % i, [128, 4, 128], F32) for i in range(2)]
        (bcat, bcatT, bxt, bx1, bjunk, bss, bxn2, bxn2b, bxn2T, bqT, bsc, bsc2, bm8, bi8, bi8f, bcand, bcand2, bt8, bj8,
         bjf, bT4, baf, bbf, bE1, bE2, beidx, bgts, bg8, badot, bga, by, bps_t) = [Buf(n_) for n_ in (
            "cat", "catT", "xt", "x1", "junk", "ss", "xn2", "xn2b", "xn2T", "qT", "sc", "sc2", "m8", "i8", "i8f", "cand", "cand2",
            "t8", "j8", "jf", "T4", "af", "bf", "E1", "E2", "eidx", "gts", "g8", "adot", "ga", "y", "ps_t")]
        bug, bvg = bufs(NG, "ug"), bufs(NG, "vg")
        bps_o, bps_q, bps_s = bufs(2, "ps_o"), bufs(2, "ps_q"), bufs(2, "ps_s")

        for i in range(NT):
            rows = slice(i * 128, (i + 1) * 128)
            P.dma("sp", lambda h, rows=rows: h.dma_start(out=cat[:, 0:512], in_=c.AOUT[rows, :]), reads=[c.bAOUT[i]], writes=[bcat])
            P.dma("sp", lambda h, rows=rows: h.dma_start(out=cat[:, 512:1024], in_=c.HM[rows, :]), reads=[c.bHM[i]], writes=[bcat])
            P.dma("sp", lambda h, rows=rows: h.dma_start(out=xt[:], in_=c.x[rows, :]), writes=[bxt])
            for kc in range(8):
                P.op("pe", lambda h, kc=kc: h.transpose(out=ps_t[:, kc * 128:(kc + 1) * 128], in_=cat[:, kc * 128:(kc + 1) * 128],
                                                        identity=identb[:]), reads=[bcat, bconst], writes=[bps_t])
            P.op("act", lambda h: h.copy(out=catT[:].rearrange("p k t -> p (k t)"), in_=ps_t[:]), reads=[bps_t], writes=[bcatT])
            for g in range(2):
                for kc in range(8):
                    P.op("pe", lambda h, g=g, kc=kc: h.matmul(ps_o[g][:], lhsT=catT[:, kc, :], rhs=Wo[:, kc, g * 512:(g + 1) * 512],
                                                             start=(kc == 0), stop=(kc == 7)), reads=[bcatT, bWo[kc]], writes=[bps_o[g]])
                P.op("dve", lambda h, g=g: h.tensor_tensor(out=x1[:, g * 512:(g + 1) * 512], in0=ps_o[g][:], in1=xt[:, g * 512:(g + 1) * 512],
                                                          op=ALU.add), reads=[bps_o[g], bxt], writes=[bx1])
            P.op("act", lambda h: h.activation(out=junk[:], in_=x1[:], func=AF.Square, accum_out=ss[:, 0:1]), reads=[bx1], writes=[bjunk, bss])
            P.op("act", lambda h: h.activation(out=ss[:, 1:2], in_=ss[:, 0:1], func=AF.Sqrt, scale=1.0 / D, bias=EPS), reads=[bss], writes=[bss])
            P.op("dve", lambda h: h.reciprocal(out=ss[:, 2:3], in_=ss[:, 1:2]), reads=[bss], writes=[bss])
            P.op("dve", lambda h: h.scalar_tensor_tensor(out=xn2[:], in0=x1[:], scalar=ss[:, 2:3], in1=gn2[:], op0=ALU.mult, op1=ALU.mult),
                 reads=[bx1, bss, bconst], writes=[bxn2])
            P.op("act", lambda h: h.copy(out=xn2b[:], in_=xn2[:]), reads=[bxn2], writes=[bxn2b])
            for kc in range(8):
                P.op("pe", lambda h, kc=kc: h.transpose(out=ps_t[:, kc * 128:(kc + 1) * 128], in_=xn2b[:, kc * 128:(kc + 1) * 128],
                                                        identity=identb[:]), reads=[bxn2b, bconst], writes=[bps_t])
            P.op("act", lambda h: h.copy(out=xn2T[:].rearrange("p k t -> p (k t)"), in_=ps_t[:]), reads=[bps_t], writes=[bxn2T])
            for qg in range(4):
                k = qg % 2
                for cc in range(4):
                    hp = qg * 4 + cc
                    for kc in range(8):
                        P.op("pe", lambda h, k=k, cc=cc, hp=hp, kc=kc: h.matmul(ps_q[k][:, cc, :], lhsT=Wq[:, kc, hp * 128:(hp + 1) * 128],
                                                                              rhs=xn2T[:, kc, :], start=(kc == 0), stop=(kc == 7)),
                             reads=[bWq[kc], bxn2T], writes=[bps_q[k]])
                P.op("act", lambda h, k=k, qg=qg: h.copy(out=qT[:, qg * 4:(qg + 1) * 4, :], in_=ps_q[k][:]), reads=[bps_q[k]], writes=[bqT])
            for qg in range(4):
                k = qg % 2
                for cc in range(4):
                    hp = qg * 4 + cc
                    P.op("pe", lambda h, k=k, cc=cc, hp=hp: h.matmul(ps_s[k][:, cc, :], lhsT=qT[:, hp, :], rhs=SKT[:, hp, :],
                                                                   start=True, stop=True), reads=[bqT, bSKT], writes=[bps_s[k]])
                P.op("act", lambda h, k=k, qg=qg: h.copy(out=sc[:, qg * 4:(qg + 1) * 4, :], in_=ps_s[k][:]), reads=[bps_s[k]], writes=[bsc])
            for g in range(16):
                P.op("dve", lambda h, g=g: h.max(out=m8[:, g, 0:8], in_=sc[:, g, :]), reads=[bsc], writes=[bm8])
                P.op("dve", lambda h, g=g: h.max_index(out=i8[:, g, 0:8], in_max=m8[:, g, 0:8], in_values=sc[:, g, :]),
                     reads=[bsc, bm8], writes=[bi8])
                P.op("dve", lambda h, g=g: h.match_replace(out=sc2[:, g, :], in_to_replace=m8[:, g, 0:8], in_values=sc[:, g, :],
                                                           imm_value=-1e30), reads=[bsc, bm8], writes=[bsc2])
                P.op("dve", lambda h, g=g: h.max(out=m8[:, g, 8:16], in_=sc2[:, g, :]), reads=[bsc2], writes=[bm8])
                P.op("dve", lambda h, g=g: h.max_index(out=i8[:, g, 8:16], in_max=m8[:, g, 8:16], in_values=sc2[:, g, :]),
                     reads=[bsc2, bm8], writes=[bi8])
            m8v = m8[:].rearrange("p (a b) k -> p a b k", b=2)
            P.op("dve", lambda h, m8v=m8v: h.tensor_tensor(
                out=cand[:].rearrange("p a (x y) -> p a x y", y=16),
                in0=m8v[:, :, 0, :].unsqueeze(3).to_broadcast([128, 8, 16, 16]),
                in1=m8v[:, :, 1, :].unsqueeze(2).to_broadcast([128, 8, 16, 16]), op=ALU.add), reads=[bm8], writes=[bcand])
            for hd in range(8):
                P.op("dve", lambda h, hd=hd: h.max(out=t8[:, hd, 0:8], in_=cand[:, hd, :]), reads=[bcand], writes=[bt8])
                P.op("dve", lambda h, hd=hd: h.max_index(out=j8[:, hd, 0:8], in_max=t8[:, hd, 0:8], in_values=cand[:, hd, :]),
                     reads=[bcand, bt8], writes=[bj8])
                P.op("dve", lambda h, hd=hd: h.match_replace(out=cand2[:, hd, :], in_to_replace=t8[:, hd, 0:8], in_values=cand[:, hd, :],
                                                             imm_value=-1e30), reads=[bcand, bt8], writes=[bcand2])
                P.op("dve", lambda h, hd=hd: h.max(out=t8[:, hd, 8:16], in_=cand2[:, hd, :]), reads=[bcand2], writes=[bt8])
                P.op("dve", lambda h, hd=hd: h.max_index(out=j8[:, hd, 8:16], in_max=t8[:, hd, 8:16], in_values=cand2[:, hd, :]),
                     reads=[bcand2, bt8], writes=[bj8])
            P.op("dve", lambda h: h.tensor_copy(out=i8f[:], in_=i8[:]), reads=[bi8], writes=[bi8f])
            P.op("dve", lambda h: h.tensor_copy(out=jf[:], in_=j8[:].rearrange("p a k -> p (a k)")), reads=[bj8], writes=[bjf])
            P.op("dve", lambda h: h.tensor_tensor(out=T4[:], in0=jf[:].unsqueeze(2).to_broadcast([128, 128, 16]),
                                                  in1=THR[:].unsqueeze(1).to_broadcast([128, 128, 16]), op=ALU.is_ge),
                 reads=[bjf, bconst], writes=[bT4])
            P.op("dve", lambda h: h.tensor_reduce(out=af[:], in_=T4[:], axis=AX.X, op=ALU.add), reads=[bT4], writes=[baf])
            P.op("dve", lambda h: h.scalar_tensor_tensor(out=bf_[:], in0=af[:], scalar=-16.0, in1=jf[:], op0=ALU.mult, op1=ALU.add),
                 reads=[baf, bjf], writes=[bbf])
            i8v = i8f[:].rearrange("p (a b) k -> p a b k", b=2)
            for side, (idxt, Et, bE) in enumerate(((af, E1, bE1), (bf_, E2, bE2))):
                P.op("dve", lambda h, idxt=idxt: h.tensor_tensor(out=T4[:], in0=idxt[:].unsqueeze(2).to_broadcast([128, 128, 16]),
                                                                in1=IOT[:].unsqueeze(1).to_broadcast([128, 128, 16]), op=ALU.is_equal),
                     reads=[baf, bbf, bconst, bT4], writes=[bT4])
                P.op("dve", lambda h, side=side, i8v=i8v: h.tensor_tensor(
                    out=T4[:].rearrange("p (a k) x -> p a k x", k=16), in0=T4[:].rearrange("p (a k) x -> p a k x", k=16),
                    in1=i8v[:, :, side, :].unsqueeze(2).to_broadcast([128, 8, 16, 16]), op=ALU.mult),
                    reads=[bT4, bi8f], writes=[bT4])
                P.op("dve", lambda h, Et=Et: h.tensor_reduce(out=Et[:], in_=T4[:], axis=AX.X, op=ALU.add), reads=[bT4], writes=[bE])
            P.op("dve", lambda h: h.scalar_tensor_tensor(out=E1[:], in0=E1[:], scalar=128.0, in1=E2[:], op0=ALU.mult, op1=ALU.add),
                 reads=[bE1, bE2], writes=[bE1])
            P.op("dve", lambda h: h.tensor_copy(out=eidx[:], in_=E1[:]), reads=[bE1], writes=[beidx])
            P.op("dve", lambda h: h.tensor_tensor(out=gts[:], in0=t8[:], in1=t8[:, :, 0:1].to_broadcast([128, 8, 16]), op=ALU.subtract),
                 reads=[bt8], writes=[bgts])
            P.op("act", lambda h: h.activation(out=gts[:], in_=gts[:], func=AF.Exp), reads=[bgts], writes=[bgts])
            P.op("dve", lambda h: h.tensor_reduce(out=g8[:, 0:8], in_=gts[:], axis=AX.X, op=ALU.add), reads=[bgts], writes=[bg8])
            P.op("dve", lambda h: h.reciprocal(out=g8[:, 8:16], in_=g8[:, 0:8]), reads=[bg8], writes=[bg8])
            P.op("dve", lambda h: h.tensor_tensor(out=gts[:], in0=gts[:], in1=g8[:, 8:16].unsqueeze(2).to_broadcast([128, 8, 16]),
                                                  op=ALU.mult), reads=[bgts, bg8], writes=[bgts])
            if "EIDX" in c.dbg:
                P.dma("sp", lambda h, rows=rows: h.dma_start(out=c.EIDX[rows, :], in_=eidx[:]), reads=[beidx], writes=[c.bOUT[i]])
                P.dma("sp", lambda h, rows=rows: h.dma_start(out=c.GTS[rows, :], in_=gts[:].rearrange("p a k -> p (a k)")), reads=[bgts], writes=[c.bOUT[i]])
                P.dma("sp", lambda h, rows=rows: h.dma_start(out=c.X1[rows, :], in_=x1[:]), reads=[bx1], writes=[c.bOUT[i]])
            if "noexp" in c.dbg or i >= c.nexp:
                P.dma("sp", lambda h, rows=rows: h.dma_start(out=c.out[rows, :], in_=x1[:]), reads=[bx1], writes=[c.bOUT[i]])
                continue
            for sl in range(128):
                k = sl % NG
                P.dma("pool", lambda h, sl=sl, k=k: h.indirect_dma_start(
                    out=ug[k][:], out_offset=None, in_=c.peer_u,
                    in_offset=bass.IndirectOffsetOnAxis(ap=eidx[:, sl:sl + 1], axis=0)),
                    reads=[beidx], writes=[bug[k]])
                P.op("dve", lambda h, sl=sl, k=k: h.scalar_tensor_tensor(out=junk[:], in0=ug[k][:], scalar=1.0, in1=xn2[:],
                                                                        op0=ALU.mult, op1=ALU.mult, accum_out=adot[:, sl:sl + 1]),
                     reads=[bug[k], bxn2], writes=[bjunk, badot])
            P.op("act", lambda h: h.activation(out=ga[:], in_=adot[:], func=AF.Gelu), reads=[badot], writes=[bga])
            P.op("dve", lambda h: h.tensor_tensor(out=ga[:], in0=ga[:], in1=gts[:].rearrange("p a k -> p (a k)"), op=ALU.mult),
                 reads=[bga, bgts], writes=[bga])
            for sl in range(128):
                k = sl % NG
                P.dma("pool", lambda h, sl=sl, k=k: h.indirect_dma_start(
                    out=vg[k][:], out_offset=None, in_=c.peer_v,
                    in_offset=bass.IndirectOffsetOnAxis(ap=eidx[:, sl:sl + 1], axis=0)),
                    reads=[beidx], writes=[bvg[k]])
                if sl == 0:
                    P.op("dve", lambda h, sl=sl, k=k: h.scalar_tensor_tensor(out=yacc[:], in0=vg[k][:], scalar=ga[:, sl:sl + 1], in1=x1[:],
                                                                            op0=ALU.mult, op1=ALU.add),
                         reads=[bvg[k], bga, bx1], writes=[by])
                else:
                    P.op("dve", lambda h, sl=sl, k=k: h.scalar_tensor_tensor(out=yacc[:], in0=vg[k][:], scalar=ga[:, sl:sl + 1], in1=yacc[:],
                                                                            op0=ALU.mult, op1=ALU.add),
                         reads=[bvg[k], bga, by], writes=[by])
            P.dma("sp", lambda h, rows=rows: h.dma_start(out=c.out[rows, :], in_=yacc[:]), reads=[by], writes=[c.bOUT[i]])
    P.barrier()


def build(dbg=(), phases="abcd"):
    nc = bass.Bass("TRN2", target_bir_lowering=False)
    c = Ctx()
    c.nc = nc
    ext_in = lambda n, s, d: nc.dram_tensor(n, s, d, kind="ExternalInput").ap()

    def scratch(n, s, d):
        kind = "ExternalOutput" if n in dbg else "Internal"
        return nc.dram_tensor(n, s, d, kind=kind).ap()

    c.x = ext_in("x", [S, D], F32)
    c.w_in = ext_in("w_in", [D, INW], F32)
    c.norm1_w = ext_in("norm1_w", [128, 8], F32)
    c.identb = ext_in("identb", [128, 128], BF16)
    c.gq = ext_in("gq", [128, 64], F32)
    c.gk = ext_in("gk", [128, 64], F32)
    c.rpbg = ext_in("rpbg", [128, 8, 896], F32)
    c.mask_i = ext_in("mask_i", [128, 896], F32)
    c.mask_a = ext_in("mask_a", [128, 896], F32)
    c.gao = ext_in("gao", [128, 512], F32)
    c.gate_b = ext_in("gate_b", [4, 4], F32)
    c.identf = ext_in("identf", [128, 128], F32)
    c.conv_w = ext_in("conv_w", [128, 8, 5], F32)
    c.conv_b = ext_in("conv_b", [128, 8], F32)
    c.sel = ext_in("sel", [4, 4, 128], F32)
    c.msk = ext_in("msk", [128, 2, 128], F32)
    c.gmn = ext_in("gmn", [128, 512], F32)
    c.w_out = ext_in("w_out", [D, D], F32)
    c.w_q = ext_in("w_q", [D, 2048], F32)
    c.skt = ext_in("skt", [128, 16, 128], F32)
    c.gn2 = ext_in("gn2", [128, D], F32)
    c.thr = ext_in("thr", [128, 16], F32)
    c.iot = ext_in("iot", [128, 16], F32)
    c.peer_u = ext_in("peer_u", [16384, D], F32)
    c.peer_v = ext_in("peer_v", [16384, D], F32)
    c.out = nc.dram_tensor("out", [S, D], F32, kind="ExternalOutput").ap()
    c.bOUT = bufs(NT, "OUT")
    c.dbg = dbg
    c.nexp = NT
    for d_ in dbg:
        if d_.startswith("exp"):
            c.nexp = int(d_[3:])
    if "EIDX" in dbg:
        c.EIDX = scratch("EIDX", [S, 128], I32)
        c.GTS = scratch("GTS", [S, 128], F32)
        c.X1 = scratch("X1", [S, D], F32)

    c.QT = scratch("QT", [4, 128, S], BF16)
    c.KT = scratch("KT", [4, 128, S], BF16)
    c.V = scratch("V", [S, 512], BF16)
    c.MV = scratch("MV", [S, 512], BF16)
    c.SIGO = scratch("SIGO", [S, 512], BF16)
    c.MQKT = scratch("MQKT", [1024, S], F32)
    c.GT = scratch("GT", [4, 4, S], F32)
    c.AOUT = scratch("AOUT", [S, 512], BF16)
    c.BCRD = scratch("BCRD", [2, 4, NT, 257], F32)
    c.bBCRD = bufs(2, "BCRD")
    c.HF = scratch("HF", [S, 512], F32)
    c.bHF = bufs(NT, "HF")
    c.HM = scratch("HM", [S, 512], BF16)
    c.bHM = bufs(NT, "HM")
    c.bAOUT = bufs(NT, "AOUT")
    c.bQKT = [bufs(NT, "QT"), bufs(NT, "KT")]
    c.bV, c.bMV, c.bSIGO, c.bMQKT, c.bGT = (bufs(NT, n) for n in ("V", "MV", "SIGO", "MQKT", "GT"))

    with ExitStack() as st:
        c.P = Prog(nc, st)
        if "a" in phases:
            phase_a(c)
        if "b" in phases:
            phase_b(c)
        if "c" in phases:
            phase_c(c)
        if "d" in phases:
            phase_d(c)
        c.P.emit()
    return nc


def host_inputs(inputs, b):
    f32 = np.float32
    m = {}
    m["x"] = np.ascontiguousarray(inputs["x"][b], dtype=f32)
    m["w_in"] = np.ascontiguousarray(inputs["w_in"][0], dtype=f32)
    m["norm1_w"] = np.ascontiguousarray(inputs["norm1_w"][0].reshape(8, 128).T, dtype=f32)
    m["identb"] = np.eye(128, dtype=f32).astype(ml_dtypes.bfloat16)
    m["gq"] = np.ascontiguousarray(np.broadcast_to(inputs["q_norm_w"][0][None, :], (128, 64)), dtype=f32)
    m["gk"] = np.ascontiguousarray(np.broadcast_to(inputs["k_norm_w"][0][None, :], (128, 64)), dtype=f32)
    p = np.arange(128); kr = p // 64; kc = p % 64
    col = np.arange(128); rq = col // 64; cc = col % 64
    dt = np.arange(-3, 4)
    drow = 2 * dt[None, :, None] + kr[:, None, None] - rq[None, None, :]
    dcol = kc[:, None, None] - cc[None, None, :] + 0 * dt[None, :, None]
    rpb = inputs["attn_rpb"][0]
    g = rpb[:, np.clip(drow + 7, 0, 14), np.clip(dcol + 15, 0, 30)]
    m["rpbg"] = np.ascontiguousarray(g.transpose(1, 0, 2, 3).reshape(128, 8, 896), dtype=f32)
    cs = np.clip(cc - 8, 0, 48)
    colvalid = (kc[:, None, None] >= cs[None, None, :]) & (kc[:, None, None] < cs[None, None, :] + 16)
    colvalid = colvalid & (dt[None, :, None] > -100)
    m["mask_a"] = np.where(colvalid & (np.abs(drow) <= 7), 0.0, NEG).astype(f32).reshape(128, 896)
    m["mask_i"] = np.where(colvalid & (drow >= -4) & (drow <= 3), 0.0, NEG).astype(f32).reshape(128, 896)
    m["gate_b"] = np.ascontiguousarray(inputs["mlstm_gate_b"][0].T, dtype=f32)
    m["identf"] = np.eye(128, dtype=f32)
    m["conv_w"] = np.ascontiguousarray(inputs["mlstm_conv_w"][0].reshape(5, 8, 128).transpose(2, 1, 0), dtype=f32)
    m["conv_b"] = np.ascontiguousarray(inputs["mlstm_conv_b"][0].reshape(8, 128).T, dtype=f32)
    sel = np.zeros((4, 4, 128), f32)
    for hh in range(4):
        sel[hh, hh, :] = 1.0
    m["sel"] = sel
    ii = np.arange(128)
    msk = np.zeros((128, 2, 128), f32)
    msk[:, 0, :] = np.where(ii[:, None] <= ii[None, :], 0.0, NEG)
    msk[:, 1, :] = np.where(ii[:, None] >= ii[None, :], 0.0, NEG)
    m["msk"] = msk
    m["gmn"] = np.ascontiguousarray(np.broadcast_to(inputs["mlstm_norm_w"][0][None, :], (128, 512)), dtype=f32)
    m["w_out"] = np.ascontiguousarray(inputs["w_out"][0], dtype=f32)
    m["w_q"] = np.ascontiguousarray(inputs["peer_w_q"][0], dtype=f32)
    m["skt"] = np.ascontiguousarray(inputs["peer_sub_keys"][0].reshape(16, 128, 128).transpose(2, 0, 1), dtype=f32)
    m["gn2"] = np.ascontiguousarray(np.broadcast_to(inputs["norm2_w"][0][None, :], (128, D)), dtype=f32)
    thr = (np.arange(16, dtype=f32) + 1.0) * 16.0
    thr[15] = 1e9
    m["thr"] = np.ascontiguousarray(np.broadcast_to(thr[None, :], (128, 16)), dtype=f32)
    m["iot"] = np.ascontiguousarray(np.broadcast_to(np.arange(16, dtype=f32)[None, :], (128, 16)), dtype=f32)
    m["peer_u"] = np.ascontiguousarray(inputs["peer_u"][0], dtype=f32)
    m["peer_v"] = np.ascontiguousarray(inputs["peer_v"][0], dtype=f32)
    m["gao"] = np.ascontiguousarray(np.broadcast_to(inputs["attn_out_norm_w"][0][None, :], (128, 512)), dtype=f32)
    return m


def kernel(**inputs):
    nc = build()
    in_maps = [host_inputs(inputs, b) for b in range(8)]
    res = run_bass_kernel_spmd(nc, in_maps, core_ids=list(range(8)))
    return np.stack([r["out"] for r in res.results], axis=0).astype(np.float32)
```

```python
import numpy as np
import ml_dtypes
import concourse.bass as bass
import concourse.mybir as mybir
from concourse.bass_utils import run_bass_kernel_spmd
from contextlib import ExitStack

F32 = mybir.dt.float32
BF16 = mybir.dt.bfloat16
I32 = mybir.dt.int32
U32 = mybir.dt.uint32
ALU = mybir.AluOpType
AF = mybir.ActivationFunctionType
AX = mybir.AxisListType

S = 4096
D = 1024
NT = 32
INW = 3600
EPS = 1e-6
NEG = -30000.0


class Buf:
    __slots__ = ("name", "w", "r")

    def __init__(self, name=""):
        self.name = name
        self.w = {}
        self.r = {}


class Prog:
    ENG = ("pe", "act", "dve", "pool", "sp")
    NRINGS = {"sp": 12, "act": 6, "pool": 48}

    def __init__(self, nc, stack):
        self.nc = nc
        self.sem = {e: stack.enter_context(nc.semaphore("s_" + e)) for e in self.ENG}
        self.cnt = {e: 0 for e in self.ENG}
        self.ops = {e: [] for e in self.ENG}
        self.seen = {e: {} for e in self.ENG}
        self.ring = {}
        self.ring_i = {}
        self.ring_tok = {}
        for q in ("sp", "act", "pool"):
            self.ring[q] = [stack.enter_context(nc.semaphore("r_%s%d" % (q, i)))
                            for i in range(self.NRINGS[q])]
            self.ring_i[q] = 0
            self.ring_tok[q] = [None] * self.NRINGS[q]
        self.final_tokens = []

    def _need(self, eng, tok, waits):
        sem, val, teng = tok
        if teng == eng and eng == "pe":
            return
        k = id(sem)
        if self.seen[eng].get(k, 0) >= val:
            return
        self.seen[eng][k] = val
        waits.append((sem, val))

    def _deps(self, eng, reads, writes):
        waits = []
        for b in reads:
            for t in b.w.values():
                self._need(eng, t, waits)
        for b in writes:
            for t in b.w.values():
                self._need(eng, t, waits)
            for t in b.r.values():
                self._need(eng, t, waits)
        best = {}
        for sem, val in waits:
            k = id(sem)
            if k not in best or best[k][1] < val:
                best[k] = (sem, val)
        return list(best.values())

    def _commit(self, tok, reads, writes):
        k = id(tok[0])
        for b in reads:
            b.r[k] = tok
        for b in writes:
            b.w = {k: tok}
            b.r = {}

    def op(self, eng, fn, reads=(), writes=()):
        waits = self._deps(eng, reads, writes)
        self.cnt[eng] += 1
        tok = (self.sem[eng], self.cnt[eng], eng)
        self.ops[eng].append((waits, fn, (self.sem[eng], 1)))
        self._commit(tok, reads, writes)
        return tok

    def dma(self, q, fn, reads=(), writes=(), final=False):
        waits = self._deps(q, reads, writes)
        i = self.ring_i[q]
        nr = self.NRINGS[q]
        slot = i % nr
        prev = self.ring_tok[q][slot]
        if prev is not None:
            w2 = []
            self._need(q, prev, w2)
            waits = waits + w2
        sem = self.ring[q][slot]
        val = 16 * (i // nr + 1)
        self.ring_i[q] = i + 1
        tok = (sem, val, "dma_" + q)
        self.ring_tok[q][slot] = tok
        self.ops[q].append((waits, fn, (sem, 16)))
        self._commit(tok, reads, writes)
        if final:
            self.final_tokens.append(tok)
        return tok

    def _all_tokens(self):
        toks = []
        for e in ("pe", "act", "dve", "pool"):
            if self.cnt[e] > 0:
                toks.append((self.sem[e], self.cnt[e], e + "_all"))
        for q in ("act", "pool", "sp"):
            for t in self.ring_tok[q]:
                if t is not None:
                    toks.append(t)
        return toks

    def barrier(self):
        toks = self._all_tokens()
        for e in self.ENG:
            waits = []
            for t in toks:
                sem, val, teng = t
                k = id(sem)
                if self.seen[e].get(k, 0) >= val:
                    continue
                if teng == e + "_all":
                    continue
                self.seen[e][k] = val
                waits.append((sem, val))
            if waits:
                self.ops[e].append((waits, None, None))

    def emit(self):
        nc = self.nc
        fw = []
        for t in self._all_tokens():
            self._need("sp", t, fw)
        final_waits = fw

        def run(e, handle, extra=None):
            for waits, fn, inc in self.ops[e]:
                for sem, val in waits:
                    handle.wait_ge(sem, val)
                if fn is not None:
                    ins = fn(handle)
                    ins.then_inc(inc[0], inc[1])
            if extra:
                for sem, val in extra:
                    handle.wait_ge(sem, val)

        with nc.Block() as block:
            @block.tensor
            def _(h):
                run("pe", h)

            @block.scalar
            def _(h):
                run("act", h)

            @block.vector
            def _(h):
                run("dve", h)

            @block.gpsimd
            def _(h):
                run("pool", h)

            @block.sync
            def _(h):
                run("sp", h, final_waits)


class Ctx:
    pass


def bufs(n, name=""):
    return [Buf("%s%d" % (name, i)) for i in range(n)]


def phase_a(c):
    nc, P = c.nc, c.P
    with ExitStack() as st:
        sb = lambda n, s, d: st.enter_context(nc.sbuf_tensor(n, s, d))
        ps = lambda n, s, d: st.enter_context(nc.psum_tensor(n, s, d))
        Wbf = sb("a_Wbf", [128, 8, INW], BF16)
        stage = [sb("a_stage%d" % i, [128, INW], F32) for i in range(2)]
        w1 = sb("a_w1", [128, 8], F32)
        identb = sb("a_identb", [128, 128], BF16)
        gq = sb("a_gq", [128, 64], F32)
        gk = sb("a_gk", [128, 64], F32)
        xt = [sb("a_xt%d" % i, [128, D], F32) for i in range(2)]
        junk = sb("a_junk", [128, D], F32)
        ss = sb("a_ss", [128, 4], F32)
        xn = sb("a_xn", [128, D], BF16)
        xnT = sb("a_xnT", [128, 8, 128], BF16)
        sq = sb("a_sq", [128, 512], F32)
        tmp = sb("a_tmp", [128, 512], F32)
        s8 = sb("a_s8", [128, 24], F32)
        qn = sb("a_qn", [128, 512], BF16)
        qTs = sb("a_qTs", [128, 4, 128], BF16)
        ob = [sb("a_ob%d" % i, [128, 512], BF16) for i in range(2)]
        fm = [sb("a_fm%d" % i, [128, 4, 128], F32) for i in range(2)]
        gsb = sb("a_gsb", [4, 4, 128], F32)
        ps_t = ps("a_ps_t", [128, D], BF16)
        ps_g = [ps("a_ps_g%d" % i, [128, 512], F32) for i in range(2)]
        ps_q = ps("a_ps_q", [128, 4, 128], BF16)
        ps_f = [ps("a_ps_f%d" % i, [128, 4, 128], F32) for i in range(2)]
        ps_gt = ps("a_ps_gt", [4, 4, 128], F32)

        bW = bufs(8, "W")
        bst = bufs(2, "st")
        bc = Buf("consts")
        bxt = bufs(2, "xt")
        bjunk, bss, bxn, bxnT, bsq, btmp, bs8, bqn, bqTs = [Buf(n) for n in
            ("junk", "ss", "xn", "xnT", "sq", "tmp", "s8", "qn", "qTs")]
        bob = bufs(2, "ob")
        bfm = bufs(2, "fm")
        bgsb = Buf("gsb")
        bps_t, bps_q, bps_gt = Buf("ps_t"), Buf("ps_q"), Buf("ps_gt")
        bps_g = bufs(2, "ps_g")
        bps_f = bufs(2, "ps_f")

        P.dma("sp", lambda h: h.dma_start(out=w1[:], in_=c.norm1_w), writes=[bc])
        P.dma("sp", lambda h: h.dma_start(out=identb[:], in_=c.identb), writes=[bc])
        P.dma("sp", lambda h: h.dma_start(out=gq[:], in_=c.gq), writes=[bc])
        P.dma("sp", lambda h: h.dma_start(out=gk[:], in_=c.gk), writes=[bc])
        for kc in range(8):
            s_ = stage[kc % 2]
            P.dma("sp", lambda h, kc=kc, s_=s_: h.dma_start(out=s_[:], in_=c.w_in[kc * 128:(kc + 1) * 128, :]),
                  writes=[bst[kc % 2]])
            P.op("dve" if kc % 2 == 0 else "pool",
                 lambda h, kc=kc, s_=s_: h.tensor_scalar(out=Wbf[:, kc, :], in0=s_[:], scalar1=w1[:, kc:kc + 1],
                                                         scalar2=None, op0=ALU.mult),
                 reads=[bst[kc % 2], bc], writes=[bW[kc]])

        gi = [0]

        def mm_group_tok(cols, sub=None):
            k = gi[0] % 2
            gi[0] += 1
            c0, c1 = cols
            for kc in range(8):
                P.op("pe", lambda h, kc=kc, k=k: h.matmul(ps_g[k][:, 0:c1 - c0], lhsT=xnT[:, kc, :],
                                                       rhs=Wbf[:, kc, c0:c1], start=(kc == 0), stop=(kc == 7)),
                     reads=[bxnT, bW[kc]], writes=[bps_g[k]])
            return k

        oi = [0]
        fi = [0]
        for i in range(NT):
            x_ = xt[i % 2]
            bx_ = bxt[i % 2]
            P.dma("sp", lambda h, i=i, x_=x_: h.dma_start(out=x_[:], in_=c.x[i * 128:(i + 1) * 128, :]), writes=[bx_])
            P.op("act", lambda h, x_=x_: h.activation(out=junk[:], in_=x_[:], func=AF.Square, accum_out=ss[:, 0:1]),
                 reads=[bx_], writes=[bjunk, bss])
            P.op("act", lambda h: h.activation(out=ss[:, 1:2], in_=ss[:, 0:1], func=AF.Sqrt, scale=1.0 / D, bias=EPS),
                 reads=[bss], writes=[bss])
            P.op("dve", lambda h: h.reciprocal(out=ss[:, 2:3], in_=ss[:, 1:2]), reads=[bss], writes=[bss])
            P.op("dve", lambda h, x_=x_: h.tensor_scalar(out=xn[:], in0=x_[:], scalar1=ss[:, 2:3], scalar2=None,
                                                          op0=ALU.mult), reads=[bx_, bss], writes=[bxn])
            for kc in range(8):
                P.op("pe", lambda h, kc=kc: h.transpose(out=ps_t[:, kc * 128:(kc + 1) * 128],
                                                        in_=xn[:, kc * 128:(kc + 1) * 128], identity=identb[:]),
                     reads=[bxn, bc], writes=[bps_t])
            P.op("act", lambda h: h.copy(out=xnT[:].rearrange("p k t -> p (k t)"), in_=ps_t[:]),
                 reads=[bps_t], writes=[bxnT])

            for which, (c0, gain, dst) in enumerate(((0, gq, c.QT), (512, gk, c.KT))):
                k = mm_group_tok((c0, c0 + 512))
                P.op("act", lambda h, k=k: h.activation(out=sq[:], in_=ps_g[k][:], func=AF.Square),
                     reads=[bps_g[k]], writes=[bsq])
                P.op("dve", lambda h: h.tensor_reduce(out=s8[:, 0:8], in_=sq[:].rearrange("p (a b) -> p a b", b=64),
                                                      axis=AX.X, op=ALU.add), reads=[bsq], writes=[bs8])
                P.op("act", lambda h: h.activation(out=s8[:, 8:16], in_=s8[:, 0:8], func=AF.Sqrt, scale=1.0 / 64,
                                                   bias=EPS), reads=[bs8], writes=[bs8])
                P.op("dve", lambda h: h.reciprocal(out=s8[:, 16:24], in_=s8[:, 8:16]), reads=[bs8], writes=[bs8])
                P.op("dve", lambda h, k=k: h.tensor_tensor(
                    out=tmp[:].rearrange("p (a b) -> p a b", b=64),
                    in0=ps_g[k][:].rearrange("p (a b) -> p a b", b=64),
                    in1=s8[:, 16:24].unsqueeze(2).to_broadcast([128, 8, 64]), op=ALU.mult),
                    reads=[bps_g[k], bs8], writes=[btmp])
                P.op("pool", lambda h, gain=gain: h.tensor_tensor(
                    out=qn[:].rearrange("p (a b) -> p a b", b=64),
                    in0=tmp[:].rearrange("p (a b) -> p a b", b=64),
                    in1=gain[:].unsqueeze(1).to_broadcast([128, 8, 64]), op=ALU.mult),
                    reads=[btmp, bc], writes=[bqn])
                for hp in range(4):
                    P.op("pe", lambda h, hp=hp: h.transpose(out=ps_q[:, hp, :], in_=qn[:, hp * 128:(hp + 1) * 128],
                                                            identity=identb[:]), reads=[bqn, bc], writes=[bps_q])
                P.op("act", lambda h: h.copy(out=qTs[:], in_=ps_q[:]), reads=[bps_q], writes=[bqTs])
                P.dma("sp", lambda h, i=i, dst=dst: h.dma_start(
                    out=dst[:, :, i * 128:(i + 1) * 128].rearrange("a p t -> p a t"), in_=qTs[:]),
                    reads=[bqTs], writes=[c.bQKT[which][i]])

            for (c0, dst, bdst, fn) in ((1024, c.V, c.bV, AF.Copy), (2560, c.MV, c.bMV, AF.Copy),
                                        (3072, c.SIGO, c.bSIGO, AF.Sigmoid)):
                k = mm_group_tok((c0, c0 + 512))
                o = oi[0] % 2
                oi[0] += 1
                P.op("act", lambda h, k=k, o=o, fn=fn: h.activation(out=ob[o][:], in_=ps_g[k][:], func=fn),
                     reads=[bps_g[k]], writes=[bob[o]])
                P.dma("sp", lambda h, i=i, o=o, dst=dst: h.dma_start(out=dst[i * 128:(i + 1) * 128, :], in_=ob[o][:]),
                      reads=[bob[o]], writes=[bdst[i]])

            for half in range(2):
                f = fi[0] % 2
                fi[0] += 1
                for cc in range(4):
                    ch = half * 4 + cc
                    col = 1536 + ch * 128
                    for kc in range(8):
                        P.op("pe", lambda h, kc=kc, f=f, cc=cc, col=col: h.matmul(
                            ps_f[f][:, cc, :], lhsT=Wbf[:, kc, col:col + 128], rhs=xnT[:, kc, :],
                            start=(kc == 0), stop=(kc == 7)), reads=[bxnT, bW[kc]], writes=[bps_f[f]])
                P.op("dve", lambda h, f=f: h.tensor_copy(out=fm[f][:], in_=ps_f[f][:]), reads=[bps_f[f]], writes=[bfm[f]])
                P.dma("sp", lambda h, i=i, f=f, half=half: h.dma_start(
                    out=c.MQKT[half * 512:(half + 1) * 512, i * 128:(i + 1) * 128].rearrange("(a p) t -> p a t", p=128),
                    in_=fm[f][:]), reads=[bfm[f]], writes=[c.bMQKT[i]])
            for g in range(4):
                col = 3584 + 4 * g
                for kc in range(8):
                    P.op("pe", lambda h, kc=kc, g=g, col=col: h.matmul(
                        ps_gt[:, g, :], lhsT=Wbf[:, kc, col:col + 4], rhs=xnT[:, kc, :],
                        start=(kc == 0), stop=(kc == 7)), reads=[bxnT, bW[kc]], writes=[bps_gt])
            P.op("dve", lambda h: h.tensor_copy(out=gsb[:], in_=ps_gt[:]), reads=[bps_gt], writes=[bgsb])
            P.dma("sp", lambda h, i=i: h.dma_start(out=c.GT[:, :, i * 128:(i + 1) * 128].rearrange("g a t -> a g t"),
                                                   in_=gsb[:]), reads=[bgsb], writes=[c.bGT[i]])
    P.barrier()


def phase_b(c):
    nc, P = c.nc, c.P
    with ExitStack() as st:
        sb = lambda n, s, d: st.enter_context(nc.sbuf_tensor(n, s, d))
        ps = lambda n, s, d: st.enter_context(nc.psum_tensor(n, s, d))
        QT = sb("b_QT", [128, 4, S], BF16)
        KT = sb("b_KT", [128, 4, S], BF16)
        V = sb("b_V", [128, NT, 8, 65], BF16)
        TBI = sb("b_TBI", [128, 8, 896], F32)
        TBA = sb("b_TBA", [128, 8, 896], F32)
        MI = sb("b_MI", [128, 896], F32)
        MA = sb("b_MA", [128, 896], F32)
        gao = sb("b_gao", [128, 512], F32)
        sT = [sb("b_sT%d" % i, [128, 640], F32) for i in range(2)]
        pT = [sb("b_pT%d" % i, [128, 640], BF16) for i in range(2)]
        ao = sb("b_ao", [128, 512], F32)
        junk = sb("b_junk", [128, 512], F32)
        rc = [sb("b_rc%d" % i, [128, 1], F32) for i in range(2)]
        ss = sb("b_ss", [128, 4], F32)
        aob = [sb("b_aob%d" % i, [128, 512], BF16) for i in range(2)]
        ps_s = [ps("b_ps_s%d" % i, [128, 1024], F32) for i in range(2)]
        ps_o = [ps("b_ps_o%d" % i, [128, 128], F32) for i in range(2)]

        bQT, bKT = bufs(4, "bQT"), bufs(4, "bKT")
        bVt = bufs(NT, "bV")
        bones, btb, bm, bgao = Buf("ones"), Buf("tb"), Buf("m"), Buf("gao")
        bsT, bpT, brc, baob = bufs(2, "sT"), bufs(2, "pT"), bufs(2, "rc"), bufs(2, "aob")
        bao, bjunk, bss = Buf("ao"), Buf("junk"), Buf("ss")
        bps_s, bps_o = bufs(2, "ps_s"), bufs(2, "ps_o")

        P.dma("sp", lambda h: h.dma_start(out=TBI[:], in_=c.rpbg), writes=[btb])
        P.dma("sp", lambda h: h.dma_start(out=MI[:], in_=c.mask_i), writes=[bm])
        P.dma("sp", lambda h: h.dma_start(out=MA[:], in_=c.mask_a), writes=[bm])
        P.dma("sp", lambda h: h.dma_start(out=gao[:], in_=c.gao), writes=[bgao])
        for hp in range(4):
            P.dma("sp", lambda h, hp=hp: h.dma_start(out=QT[:, hp, :], in_=c.QT[hp]),
                  reads=c.bQKT[0], writes=[bQT[hp]])
            P.dma("act", lambda h, hp=hp: h.dma_start(out=KT[:, hp, :], in_=c.KT[hp]),
                  reads=c.bQKT[1], writes=[bKT[hp]])
        P.op("pool", lambda h: h.memset(V[:, :, :, 64:65], 1.0), writes=[bones])
        for i in range(NT):
            P.dma("sp" if i % 2 == 0 else "act", lambda h, i=i: h.dma_start(
                out=V[:, i, :, 0:64], in_=c.V[i * 128:(i + 1) * 128, :].rearrange("p (a b) -> p a b", b=64)),
                reads=[c.bV[i]], writes=[bVt[i]])
        for hd in range(8):
            P.op("dve", lambda h, hd=hd: h.tensor_tensor(out=TBA[:, hd, :], in0=TBI[:, hd, :], in1=MA[:], op=ALU.add),
                 reads=[btb, bm], writes=[btb])
        for hd in range(8):
            P.op("dve", lambda h, hd=hd: h.tensor_tensor(out=TBI[:, hd, :], in0=TBI[:, hd, :], in1=MI[:], op=ALU.add),
                 reads=[btb, bm], writes=[btb])

        it = 0
        for j in range(NT):
            if 2 <= j <= 29:
                kts = list(range(j - 2, j + 3)); tb = TBI; s0 = 1
            elif j == 0:
                kts = [0, 1, 2, 3]; tb = TBA; s0 = 3
            elif j == 1:
                kts = [0, 1, 2, 3]; tb = TBA; s0 = 2
            elif j == 30:
                kts = [28, 29, 30, 31]; tb = TBA; s0 = 1
            else:
                kts = [28, 29, 30, 31]; tb = TBA; s0 = 0
            n = len(kts)
            for hd in range(8):
                hp, hh = hd // 2, hd % 2
                k = it % 2
                it += 1
                p0, p1 = hh * 64, hh * 64 + 64
                for idx, kt in enumerate(kts):
                    P.op("pe", lambda h, k=k, idx=idx, kt=kt, hp=hp, p0=p0, p1=p1, j=j: h.matmul(
                        ps_s[k][:, idx * 128:(idx + 1) * 128], lhsT=KT[p0:p1, hp, kt * 128:(kt + 1) * 128],
                        rhs=QT[p0:p1, hp, j * 128:(j + 1) * 128], start=True, stop=True),
                        reads=[bKT[hp], bQT[hp]], writes=[bps_s[k]])
                P.op("dve", lambda h, k=k, n=n, tb=tb, s0=s0, hd=hd: h.scalar_tensor_tensor(
                    out=sT[k][:, 0:n * 128], in0=ps_s[k][:, 0:n * 128], scalar=0.125,
                    in1=tb[:, hd, s0 * 128:(s0 + n) * 128], op0=ALU.mult, op1=ALU.add),
                    reads=[bps_s[k], btb], writes=[bsT[k]])
                P.op("act", lambda h, k=k, n=n: h.activation(out=pT[k][:, 0:n * 128], in_=sT[k][:, 0:n * 128],
                                                             func=AF.Exp), reads=[bsT[k]], writes=[bpT[k]])
                for idx, kt in enumerate(kts):
                    P.op("pe", lambda h, k=k, idx=idx, kt=kt, hd=hd, n=n: h.matmul(
                        ps_o[k][:, 0:65], lhsT=pT[k][:, idx * 128:(idx + 1) * 128], rhs=V[:, kt, hd, :],
                        start=(idx == 0), stop=(idx == n - 1)),
                        reads=[bpT[k], bVt[kt], bones], writes=[bps_o[k]])
                P.op("dve", lambda h, k=k: h.reciprocal(out=rc[k][:], in_=ps_o[k][:, 64:65]),
                     reads=[bps_o[k]], writes=[brc[k]])
                P.op("dve", lambda h, k=k, hd=hd: h.tensor_scalar(
                    out=ao[:, hd * 64:(hd + 1) * 64], in0=ps_o[k][:, 0:64], scalar1=rc[k][:], scalar2=None,
                    op0=ALU.mult), reads=[bps_o[k], brc[k]], writes=[bao])
            o = j % 2
            P.op("act", lambda h: h.activation(out=junk[:], in_=ao[:], func=AF.Square, accum_out=ss[:, 0:1]),
                 reads=[bao], writes=[bjunk, bss])
            P.op("act", lambda h: h.activation(out=ss[:, 1:2], in_=ss[:, 0:1], func=AF.Sqrt, scale=1.0 / 512, bias=EPS),
                 reads=[bss], writes=[bss])
            P.op("dve", lambda h: h.reciprocal(out=ss[:, 2:3], in_=ss[:, 1:2]), reads=[bss], writes=[bss])
            P.op("dve", lambda h, o=o: h.scalar_tensor_tensor(out=aob[o][:], in0=ao[:], scalar=ss[:, 2:3], in1=gao[:],
                                                          op0=ALU.mult, op1=ALU.mult),
                 reads=[bao, bss, bgao], writes=[baob[o]])
            P.dma("sp", lambda h, j=j, o=o: h.dma_start(out=c.AOUT[j * 128:(j + 1) * 128, :], in_=aob[o][:]),
                  reads=[baob[o]], writes=[c.bAOUT[j]])
    P.barrier()


def phase_c(c):
    nc, P = c.nc, c.P
    with ExitStack() as st0:
        sb0 = lambda n, s, d: st0.enter_context(nc.sbuf_tensor(n, s, d))
        COLS = sb0("c_COLS", [128, NT, 24], F32)
        bCOLS = Buf("COLS")
        with ExitStack() as st:
            sb = lambda n, s, d: st.enter_context(nc.sbuf_tensor(n, s, d))
            ps = lambda n, s, d: st.enter_context(nc.psum_tensor(n, s, d))
            G1, G2, CL, Aa, AA, ZER, T1 = [sb("c1_" + n, [4, S], F32) for n in ("G1", "G2", "CL", "Aa", "AA", "ZER", "T1")]
            bG1, bG2, bCL, bAa, bAA, bZER, bT1 = [Buf(n) for n in ("G1", "G2", "CL", "Aa", "AA", "ZER", "T1")]
            BCR = sb("c1_BCR", [4, NT, 257], F32)
            ROWS = sb("c1_ROWS", [24, S], F32)
            gb = sb("c1_gb", [4, 4], F32)
            ngb = sb("c1_ngb", [4, 4], F32)
            AE = sb("c1_AE", [4, NT], F32)
            APv = sb("c1_AP", [4, NT], F32)
            dd = sb("c1_dd", [4, NT], F32)
            identf = sb("c1_identf", [128, 128], F32)
            ps_c = ps("c1_ps_c", [128, NT, 32], F32)
            bBCR, bgb, bAE, bAPv, bdd, bid, bps_c = [Buf(n) for n in ("BCR", "gb", "AE", "AP", "dd", "id", "ps_c")]
            bROWS = bufs(6, "ROWS")
            P.dma("sp", lambda h: h.dma_start(out=gb[:], in_=c.gate_b), writes=[bgb])
            P.dma("sp", lambda h: h.dma_start(out=identf[:], in_=c.identf), writes=[bid])
            P.op("dve", lambda h: h.tensor_scalar(out=ngb[:], in0=gb[:], scalar1=-1.0, scalar2=None, op0=ALU.mult),
                 reads=[bgb], writes=[bgb])
            P.op("pool", lambda h: h.memset(ZER[:], 0.0), writes=[bZER])
            for d in range(2):
                rv = (lambda t: t[:, :]) if d == 0 else (lambda t: t[:, ::-1])
                gi_, gf_ = 2 * d, 2 * d + 1
                P.dma("sp", lambda h, gi_=gi_: h.dma_start(out=G1[:], in_=c.GT[gi_]), reads=c.bGT, writes=[bG1])
                P.dma("sp", lambda h, gf_=gf_: h.dma_start(out=G2[:], in_=c.GT[gf_]), reads=c.bGT, writes=[bG2])
                P.op("act", lambda h, gf_=gf_: h.activation(out=G2[:], in_=G2[:], func=AF.Exp, scale=-1.0,
                                                            bias=ngb[:, gf_:gf_ + 1]), reads=[bG2, bgb], writes=[bG2])
                P.op("act", lambda h: h.activation(out=G2[:], in_=G2[:], func=AF.Ln, bias=1.0), reads=[bG2], writes=[bG2])
                P.op("dve", lambda h, rv=rv: h.tensor_tensor_scan(out=rv(CL), data0=rv(G2), data1=ZER[:], initial=0.0,
                                                                  op0=ALU.add, op1=ALU.add),
                     reads=[bG2, bZER], writes=[bCL])
                P.op("dve", lambda h, gi_=gi_: h.scalar_tensor_tensor(out=Aa[:], in0=G1[:], scalar=gb[:, gi_:gi_ + 1],
                                                                      in1=CL[:], op0=ALU.add, op1=ALU.add),
                     reads=[bG1, bgb, bCL], writes=[bAa])
                P.op("dve", lambda h, rv=rv: h.tensor_tensor_scan(out=rv(AA), data0=rv(Aa), data1=ZER[:], initial=0.0,
                                                                  op0=ALU.max, op1=ALU.add),
                     reads=[bAa, bZER], writes=[bAA])
                P.op("dve", lambda h: h.tensor_tensor(out=T1[:], in0=CL[:], in1=AA[:], op=ALU.subtract),
                     reads=[bCL, bAA], writes=[bT1])
                P.op("act", lambda h: h.activation(out=T1[:], in_=T1[:], func=AF.Exp), reads=[bT1], writes=[bT1])
                P.dma("sp", lambda h, d=d: h.dma_start(out=ROWS[12 * d + 8:12 * d + 12, :], in_=T1[:]),
                      reads=[bT1], writes=[bROWS[3 * d + 2]])
                P.dma("sp", lambda h, d=d: h.dma_start(out=ROWS[12 * d:12 * d + 4, :], in_=Aa[:]),
                      reads=[bAa], writes=[bROWS[3 * d]])
                AAv = AA[:].rearrange("p (c t) -> p c t", t=128)
                epos = 127 if d == 0 else 0
                P.op("dve", lambda h, AAv=AAv, epos=epos: h.tensor_copy(out=AE[:], in_=AAv[:, :, epos]),
                     reads=[bAA], writes=[bAE])
                P.op("dve", lambda h: h.memset(APv[:], 0.0), writes=[bAPv])
                if d == 0:
                    P.op("dve", lambda h: h.tensor_copy(out=APv[:, 1:NT], in_=AE[:, 0:NT - 1]), reads=[bAE], writes=[bAPv])
                else:
                    P.op("dve", lambda h: h.tensor_copy(out=APv[:, 0:NT - 1], in_=AE[:, 1:NT]), reads=[bAE], writes=[bAPv])
                G1v = G1[:].rearrange("p (c t) -> p c t", t=128)
                G2v = G2[:].rearrange("p (c t) -> p c t", t=128)
                Aav = Aa[:].rearrange("p (c t) -> p c t", t=128)
                P.op("dve", lambda h, G1v=G1v, Aav=Aav: h.tensor_tensor(
                    out=G1v, in0=Aav, in1=AE[:].unsqueeze(2).to_broadcast([4, NT, 128]), op=ALU.subtract),
                    reads=[bAa, bAE], writes=[bG1])
                P.op("act", lambda h: h.activation(out=G1[:], in_=G1[:], func=AF.Exp), reads=[bG1], writes=[bG1])
                P.dma("sp", lambda h, d=d: h.dma_start(out=ROWS[12 * d + 4:12 * d + 8, :], in_=G1[:]),
                      reads=[bG1], writes=[bROWS[3 * d + 1]])
                P.op("dve", lambda h, AAv=AAv: h.tensor_scalar(out=BCR[:, :, 0:128], in0=AAv, scalar1=-1.0, scalar2=None,
                                                               op0=ALU.mult), reads=[bAA], writes=[bBCR])
                P.op("dve", lambda h, G2v=G2v, AAv=AAv: h.tensor_tensor(
                    out=G2v, in0=APv[:].unsqueeze(2).to_broadcast([4, NT, 128]), in1=AAv, op=ALU.subtract),
                    reads=[bAA, bAPv], writes=[bG2])
                P.op("act", lambda h, G2v=G2v: h.activation(out=BCR[:, :, 128:256], in_=G2v, func=AF.Exp),
                     reads=[bG2], writes=[bBCR])
                P.op("dve", lambda h: h.tensor_tensor(out=dd[:], in0=APv[:], in1=AE[:], op=ALU.subtract),
                     reads=[bAPv, bAE], writes=[bdd])
                P.op("act", lambda h: h.activation(out=BCR[:, :, 256], in_=dd[:], func=AF.Exp), reads=[bdd], writes=[bBCR])
                P.dma("sp", lambda h, d=d: h.dma_start(out=c.BCRD[d], in_=BCR[:]), reads=[bBCR], writes=[c.bBCRD[d]])
            for ch in range(NT):
                P.op("pe", lambda h, ch=ch: h.transpose(out=ps_c[:, ch, 0:24], in_=ROWS[0:24, ch * 128:(ch + 1) * 128],
                                                        identity=identf[0:24, 0:24]), reads=bROWS + [bid], writes=[bps_c])
            P.op("dve", lambda h: h.tensor_copy(out=COLS[:], in_=ps_c[:, :, 0:24]), reads=[bps_c], writes=[bCOLS])
        P.barrier()

        with ExitStack() as st:
            sb = lambda n, s, d: st.enter_context(nc.sbuf_tensor(n, s, d))
            ps = lambda n, s, d: st.enter_context(nc.psum_tensor(n, s, d))
            QKT = sb("c_QKT", [128, 8, S], BF16)
            bQKT = bufs(8, "cQKT")
            cw = sb("c_cw", [128, 8, 5], F32)
            cb = sb("c_cb", [128, 8], F32)
            bcw = Buf("cw")
            P.dma("sp", lambda h: h.dma_start(out=cw[:], in_=c.conv_w), writes=[bcw])
            P.dma("sp", lambda h: h.dma_start(out=cb[:], in_=c.conv_b), writes=[bcw])
            with ExitStack() as st2:
                sb2 = lambda n, s, d: st2.enter_context(nc.sbuf_tensor(n, s, d))
                xpad = [sb2("c2_xpad%d" % i, [128, S + 4], F32) for i in range(2)]
                acc = [sb2("c2_acc%d" % i, [128, S], F32) for i in range(2)]
                bxp, bacc = bufs(2, "xpad"), bufs(2, "acc")
                for i in range(2):
                    P.op("pool", lambda h, i=i: h.memset(xpad[i][:, 0:2], 0.0), writes=[bxp[i]])
                    P.op("pool", lambda h, i=i: h.memset(xpad[i][:, S + 2:S + 4], 0.0), writes=[bxp[i]])
                for cc in range(8):
                    k = cc % 2
                    P.dma("sp", lambda h, cc=cc, k=k: h.dma_start(out=xpad[k][:, 2:S + 2], in_=c.MQKT[cc * 128:(cc + 1) * 128, :]),
                          reads=c.bMQKT, writes=[bxp[k]])
                    P.op("dve", lambda h, cc=cc, k=k: h.tensor_scalar(out=acc[k][:], in0=xpad[k][:, 0:S], scalar1=cw[:, cc, 0:1],
                                                                      scalar2=cb[:, cc:cc + 1], op0=ALU.mult, op1=ALU.add),
                         reads=[bxp[k], bcw], writes=[bacc[k]])
                    for j in range(1, 5):
                        P.op("dve", lambda h, cc=cc, k=k, j=j: h.scalar_tensor_tensor(
                            out=acc[k][:], in0=xpad[k][:, j:j + S], scalar=cw[:, cc, j:j + 1], in1=acc[k][:],
                            op0=ALU.mult, op1=ALU.add), reads=[bxp[k], bcw, bacc[k]], writes=[bacc[k]])
                    if cc < 4:
                        P.op("act", lambda h, cc=cc, k=k: h.activation(out=QKT[:, cc, :], in_=acc[k][:], func=AF.Silu),
                             reads=[bacc[k]], writes=[bQKT[cc]])
                    else:
                        P.op("act", lambda h, cc=cc, k=k: h.activation(out=acc[k][:], in_=acc[k][:], func=AF.Silu),
                             reads=[bacc[k]], writes=[bacc[k]])
                        P.op("pool", lambda h, cc=cc, k=k: h.tensor_scalar(out=QKT[:, cc, :], in0=acc[k][:], scalar1=128.0 ** -0.5,
                                                                           scalar2=None, op0=ALU.mult),
                             reads=[bacc[k]], writes=[bQKT[cc]])
            P.barrier()

            MVs = sb("c_MVs", [128, NT, 4, 129], BF16)
            bMVs = bufs(NT, "cMV")
            bones = Buf("ones")
            SEL = sb("c_SEL", [4, 4, 128], F32)
            MSK = sb("c_MSK", [128, 2, 128], F32)
            identb = sb("c_identb", [128, 128], BF16)
            gmn = sb("c_gmn", [128, 512], F32)
            bconst = Buf("const")
            P.dma("sp", lambda h: h.dma_start(out=SEL[:], in_=c.sel), writes=[bconst])
            P.dma("sp", lambda h: h.dma_start(out=MSK[:], in_=c.msk), writes=[bconst])
            P.dma("sp", lambda h: h.dma_start(out=identb[:], in_=c.identb), writes=[bconst])
            P.dma("sp", lambda h: h.dma_start(out=gmn[:], in_=c.gmn), writes=[bconst])
            P.op("pool", lambda h: h.memset(MVs[:, :, :, 128:129], 1.0), writes=[bones])
            for i in range(NT):
                P.dma("sp" if i % 2 == 0 else "act", lambda h, i=i: h.dma_start(
                    out=MVs[:, i, :, 0:128], in_=c.MV[i * 128:(i + 1) * 128, :].rearrange("p (a b) -> p a b", b=128)),
                    reads=[c.bMV[i]], writes=[bMVs[i]])
            Cst = [sb("c_C%d" % i, [128, 129], F32) for i in range(4)]
            Cbf = [sb("c_Cbf%d" % i, [128, 129], BF16) for i in range(4)]
            bC, bCbf = bufs(4, "C"), bufs(4, "Cbf")
            bcr = [sb("c_bcr%d" % i, [4, 257], F32) for i in range(2)]
            bbcr = bufs(2, "bcr")
            Gt = [sb("c_G%d" % i, [128, 128], F32) for i in range(2)]
            Wt = [sb("c_W%d" % i, [128, 128], F32) for i in range(2)]
            PT = [sb("c_PT%d" % i, [128, 128], BF16) for i in range(2)]
            qs = [sb("c_qs%d" % i, [128, 128], BF16) for i in range(2)]
            kw = [sb("c_kw%d" % i, [128, 128], BF16) for i in range(2)]
            dec = [sb("c_dec%d" % i, [128, 1], F32) for i in range(2)]
            dn = [sb("c_dn%d" % i, [128, 2], F32) for i in range(2)]
            bG, bW, bPT, bqs, bkw, bdec, bdn = [bufs(2, n) for n in ("G", "W", "PT", "qs", "kw", "dec", "dn")]
            hbuf = [sb("c_hbuf%d" % i, [128, 512], F32) for i in range(2)]
            bhbuf = bufs(2, "hbuf")
            hf = sb("c_hf", [128, 512], F32)
            sg = sb("c_sg", [128, 512], BF16)
            sq = sb("c_sq", [128, 512], F32)
            s4 = sb("c_s4", [128, 12], F32)
            hmo = [sb("c_hmo%d" % i, [128, 512], BF16) for i in range(2)]
            bhf, bsg, bsq, bs4 = Buf("hf"), Buf("sg"), Buf("sq"), Buf("s4")
            bhmo = bufs(2, "hmo")
            ps_bc = [ps("c_ps_bc%d" % i, [128, 512], F32) for i in range(2)]
            ps_st = [ps("c_ps_st%d" % i, [128, 128], F32) for i in range(2)]
            ps_n = [ps("c_ps_n%d" % i, [128, 512], F32) for i in range(2)]
            ps_kt = ps("c_ps_kt", [128, 128], BF16)
            ps_dc = ps("c_ps_dc", [128, 512], F32)
            bps_bc, bps_st, bps_n = bufs(2, "ps_bc"), bufs(2, "ps_st"), bufs(2, "ps_n")
            bps_kt, bps_dc = Buf("ps_kt"), Buf("ps_dc")

            it = 0
            for d in range(2):
                for hd in range(4):
                    P.op("pool", lambda h, hd=hd: h.memset(Cst[hd][:], 0.0), writes=[bC[hd]])
                    P.op("pool", lambda h, hd=hd: h.memset(Cbf[hd][:], 0.0), writes=[bCbf[hd]])
                order = list(range(NT)) if d == 0 else list(range(NT - 1, -1, -1))
                for ci, ch in enumerate(order):
                    kb = ci % 2
                    P.dma("sp", lambda h, d=d, ch=ch, kb=kb: h.dma_start(out=bcr[kb][:], in_=c.BCRD[d, :, ch, :]),
                          reads=[c.bBCRD[d]], writes=[bbcr[kb]])
                    hb = hbuf[ci % 2]
                    bhb = bhbuf[ci % 2]
                    tsl = slice(ch * 128, (ch + 1) * 128)
                    for hd in range(4):
                        k = it % 2
                        it += 1
                        a_col = COLS[:, ch, 12 * d + hd:12 * d + hd + 1]
                        wk_col = COLS[:, ch, 12 * d + 4 + hd:12 * d + 5 + hd]
                        emt_col = COLS[:, ch, 12 * d + 8 + hd:12 * d + 9 + hd]
                        P.op("pe", lambda h, k=k, kb=kb, hd=hd: h.matmul(ps_bc[k][:, 0:257], lhsT=SEL[:, hd, :], rhs=bcr[kb][:],
                                                                         start=True, stop=True),
                             reads=[bconst, bbcr[kb]], writes=[bps_bc[k]])
                        P.op("pe", lambda h, k=k, hd=hd, tsl=tsl: h.matmul(ps_st[k][:], lhsT=QKT[:, 4 + hd, tsl], rhs=QKT[:, hd, tsl],
                                                                          start=True, stop=True),
                             reads=[bQKT[4 + hd], bQKT[hd]], writes=[bps_st[k]])
                        P.op("dve", lambda h, k=k, d=d: h.tensor_tensor(out=Gt[k][:], in0=ps_bc[k][:, 0:128], in1=MSK[:, d, :],
                                                                       op=ALU.add), reads=[bps_bc[k], bconst], writes=[bG[k]])
                        P.op("act", lambda h, k=k, a_col=a_col: h.activation(out=Wt[k][:], in_=Gt[k][:], func=AF.Exp, bias=a_col),
                             reads=[bG[k], bCOLS], writes=[bW[k]])
                        P.op("dve", lambda h, k=k: h.tensor_tensor(out=PT[k][:], in0=ps_st[k][:], in1=Wt[k][:], op=ALU.mult),
                             reads=[bps_st[k], bW[k]], writes=[bPT[k]])
                        P.op("dve", lambda h, k=k, hd=hd, tsl=tsl: h.tensor_tensor(out=qs[k][:], in0=ps_bc[k][:, 128:256],
                                                                                  in1=QKT[:, hd, tsl], op=ALU.mult),
                             reads=[bps_bc[k], bQKT[hd]], writes=[bqs[k]])
                        P.op("act", lambda h, k=k: h.copy(out=dec[k][:], in_=ps_bc[k][:, 256:257]),
                             reads=[bps_bc[k]], writes=[bdec[k]])
                        P.op("pe", lambda h, k=k, ch=ch, hd=hd: h.matmul(ps_n[k][:, 0:129], lhsT=PT[k][:], rhs=MVs[:, ch, hd, :],
                                                                         start=True, stop=False),
                             reads=[bPT[k], bMVs[ch], bones], writes=[bps_n[k]])
                        P.op("pe", lambda h, k=k, hd=hd: h.matmul(ps_n[k][:, 0:129], lhsT=qs[k][:], rhs=Cbf[hd][:],
                                                                  start=False, stop=True),
                             reads=[bqs[k], bCbf[hd]], writes=[bps_n[k]])
                        P.op("pe", lambda h, hd=hd, tsl=tsl: h.transpose(out=ps_kt[:], in_=QKT[:, 4 + hd, tsl], identity=identb[:]),
                             reads=[bQKT[4 + hd], bconst], writes=[bps_kt])
                        P.op("act", lambda h, k=k, wk_col=wk_col: h.activation(out=kw[k][:], in_=ps_kt[:], func=AF.Copy, scale=wk_col),
                             reads=[bps_kt, bCOLS], writes=[bkw[k]])
                        P.op("pe", lambda h, k=k, ch=ch, hd=hd: h.matmul(ps_dc[:, 0:129], lhsT=kw[k][:], rhs=MVs[:, ch, hd, :],
                                                                         start=True, stop=True),
                             reads=[bkw[k], bMVs[ch], bones], writes=[bps_dc])
                        P.op("dve", lambda h, k=k, hd=hd: h.scalar_tensor_tensor(out=Cst[hd][:], in0=Cst[hd][:], scalar=dec[k][:],
                                                                                 in1=ps_dc[:, 0:129], op0=ALU.mult, op1=ALU.add),
                             reads=[bC[hd], bdec[k], bps_dc], writes=[bC[hd]])
                        P.op("act", lambda h, hd=hd: h.copy(out=Cbf[hd][:], in_=Cst[hd][:]), reads=[bC[hd]], writes=[bCbf[hd]])
                        P.op("act", lambda h, k=k: h.activation(out=dn[k][:, 1:2], in_=ps_n[k][:, 128:129], func=AF.Abs),
                             reads=[bps_n[k]], writes=[bdn[k]])
                        P.op("dve", lambda h, k=k, emt_col=emt_col: h.tensor_scalar(out=dn[k][:, 0:1], in0=dn[k][:, 1:2],
                                                                                    scalar1=emt_col, scalar2=None, op0=ALU.max),
                             reads=[bdn[k], bCOLS], writes=[bdn[k]])
                        P.op("dve", lambda h, k=k: h.reciprocal(out=dn[k][:, 1:2], in_=dn[k][:, 0:1]), reads=[bdn[k]], writes=[bdn[k]])
                        P.op("dve", lambda h, k=k, hd=hd, hb=hb: h.tensor_scalar(out=hb[:, hd * 128:(hd + 1) * 128], in0=ps_n[k][:, 0:128],
                                                                                 scalar1=dn[k][:, 1:2], scalar2=None, op0=ALU.mult),
                             reads=[bps_n[k], bdn[k]], writes=[bhb])
                    if d == 0:
                        P.dma("sp", lambda h, ch=ch, hb=hb: h.dma_start(out=c.HF[ch * 128:(ch + 1) * 128, :], in_=hb[:]),
                              reads=[bhb], writes=[c.bHF[ch]])
                    else:
                        o = ci % 2
                        P.dma("sp", lambda h, ch=ch: h.dma_start(out=hf[:], in_=c.HF[ch * 128:(ch + 1) * 128, :]),
                              reads=[c.bHF[ch]], writes=[bhf])
                        P.dma("sp", lambda h, ch=ch: h.dma_start(out=sg[:], in_=c.SIGO[ch * 128:(ch + 1) * 128, :]),
                              reads=[c.bSIGO[ch]], writes=[bsg])
                        P.op("pool", lambda h, hb=hb: h.tensor_tensor(out=hf[:], in0=hf[:], in1=hb[:], op=ALU.add),
                             reads=[bhb, bhf], writes=[bhf])
                        P.op("act", lambda h: h.activation(out=sq[:], in_=hf[:], func=AF.Square), reads=[bhf], writes=[bsq])
                        P.op("dve", lambda h: h.tensor_reduce(out=s4[:, 0:4], in_=sq[:].rearrange("p (a b) -> p a b", b=128),
                                                              axis=AX.X, op=ALU.add), reads=[bsq], writes=[bs4])
                        P.op("act", lambda h: h.activation(out=s4[:, 4:8], in_=s4[:, 0:4], func=AF.Sqrt, scale=1.0 / 128, bias=EPS),
                             reads=[bs4], writes=[bs4])
                        P.op("dve", lambda h: h.reciprocal(out=s4[:, 8:12], in_=s4[:, 4:8]), reads=[bs4], writes=[bs4])
                        P.op("dve", lambda h: h.tensor_tensor(out=sq[:].rearrange("p (a b) -> p a b", b=128),
                                                              in0=hf[:].rearrange("p (a b) -> p a b", b=128),
                                                              in1=s4[:, 8:12].unsqueeze(2).to_broadcast([128, 4, 128]), op=ALU.mult),
                             reads=[bhf, bs4, bsq], writes=[bsq])
                        P.op("pool", lambda h: h.tensor_tensor(out=sq[:], in0=sq[:], in1=gmn[:], op=ALU.mult),
                             reads=[bsq, bconst], writes=[bsq])
                        P.op("pool", lambda h, o=o: h.tensor_tensor(out=hmo[o][:], in0=sq[:], in1=sg[:], op=ALU.mult),
                             reads=[bsq, bsg], writes=[bhmo[o]])
                        P.dma("sp", lambda h, ch=ch, o=o: h.dma_start(out=c.HM[ch * 128:(ch + 1) * 128, :], in_=hmo[o][:]),
                              reads=[bhmo[o]], writes=[c.bHM[ch]])
    P.barrier()


def phase_d(c):
    nc, P = c.nc, c.P
    with ExitStack() as st:
        sb = lambda n, s, d: st.enter_context(nc.sbuf_tensor(n, s, d))
        ps = lambda n, s, d: st.enter_context(nc.psum_tensor(n, s, d))
        Wo = sb("d_Wo", [128, 8, D], BF16)
        Wq = sb("d_Wq", [128, 8, 2048], BF16)
        SKT = sb("d_SKT", [128, 16, 128], BF16)
        gn2 = sb("d_gn2", [128, D], F32)
        identb = sb("d_identb", [128, 128], BF16)
        THR = sb("d_THR", [128, 16], F32)
        IOT = sb("d_IOT", [128, 16], F32)
        st_stage = ExitStack()
        stage = [st_stage.enter_context(nc.sbuf_tensor("d_stage%d" % i, [128, 2048], F32)) for i in range(2)]
        bconst = Buf("dconst")
        bst = bufs(2, "dst")
        bWo, bWq = bufs(8, "Wo"), bufs(8, "Wq")
        bSKT = Buf("SKT")
        for (t, src) in ((gn2, c.gn2), (identb, c.identb), (THR, c.thr), (IOT, c.iot)):
            P.dma("sp", lambda h, t=t, src=src: h.dma_start(out=t[:], in_=src), writes=[bconst])
        n = 0
        for kc in range(8):
            k = n % 2; n += 1
            P.dma("sp", lambda h, kc=kc, k=k: h.dma_start(out=stage[k][:, 0:D], in_=c.w_out[kc * 128:(kc + 1) * 128, :]), writes=[bst[k]])
            P.op("dve", lambda h, kc=kc, k=k: h.tensor_copy(out=Wo[:, kc, :], in_=stage[k][:, 0:D]), reads=[bst[k]], writes=[bWo[kc]])
        for kc in range(8):
            k = n % 2; n += 1
            P.dma("sp", lambda h, kc=kc, k=k: h.dma_start(out=stage[k][:], in_=c.w_q[kc * 128:(kc + 1) * 128, :]), writes=[bst[k]])
            P.op("dve", lambda h, kc=kc, k=k: h.tensor_copy(out=Wq[:, kc, :], in_=stage[k][:]), reads=[bst[k]], writes=[bWq[kc]])
        k = n % 2; n += 1
        P.dma("sp", lambda h, k=k: h.dma_start(out=stage[k][:].rearrange("p (a b) -> p a b", b=128), in_=c.skt), writes=[bst[k]])
        P.op("dve", lambda h, k=k: h.tensor_copy(out=SKT[:].rearrange("p a b -> p (a b)"), in_=stage[k][:]), reads=[bst[k]], writes=[bSKT])
        P.barrier()
        st_stage.close()

        cat = sb("d_cat", [128, D], BF16)
        catT = sb("d_catT", [128, 8, 128], BF16)
        xt = sb("d_xt", [128, D], F32)
        x1 = sb("d_x1", [128, D], F32)
        junk = sb("d_junk", [128, D], F32)
        ss = sb("d_ss", [128, 4], F32)
        xn2 = sb("d_xn2", [128, D], F32)
        xn2b = sb("d_xn2b", [128, D], BF16)
        xn2T = sb("d_xn2T", [128, 8, 128], BF16)
        qT = sb("d_qT", [128, 16, 128], BF16)
        sc = sb("d_sc", [128, 16, 128], F32)
        sc2 = sc
        m8 = sb("d_m8", [128, 16, 16], F32)
        i8 = sb("d_i8", [128, 16, 16], U32)
        i8f = sb("d_i8f", [128, 16, 16], F32)
        cand = sb("d_cand", [128, 8, 256], F32)
        cand2 = cand
        t8 = sb("d_t8", [128, 8, 16], F32)
        j8 = sb("d_j8", [128, 8, 16], U32)
        jf = sb("d_jf", [128, 128], F32)
        T4 = sb("d_T4", [128, 128, 16], F32)
        af = sb("d_af", [128, 128], F32)
        bf_ = sb("d_bf", [128, 128], F32)
        E1 = sb("d_E1", [128, 128], F32)
        E2 = sb("d_E2", [128, 128], F32)
        eidx = sb("d_eidx", [128, 128], I32)
        gts = sb("d_gts", [128, 8, 16], F32)
        g8 = sb("d_g8", [128, 16], F32)
        adot = sb("d_adot", [128, 128], F32)
        ga = sb("d_ga", [128, 128], F32)
        NG = 8
        GS = 4
        uv = [sb("d_uv%d" % i, [128, 2 * D], F32) for i in range(NG)]
        identf = sb("d_identf", [128, 128], F32)
        dg = [sb("d_dg%d" % i, [128, 128], F32) for i in range(4)]
        bdg = bufs(4, "dg")
        P.dma("sp", lambda h: h.dma_start(out=identf[:], in_=c.identf), writes=[bconst])
        yacc = sb("d_y", [128, D], F32)
        ps_t = ps("d_ps_t", [128, D], BF16)
        ps_o = [ps("d_ps_o%d" % i, [128, 512], F32) for i in range(2)]
        ps_q = [ps("d_ps_q%d" % i, [128, 4, 128], F32) for i in range(2)]
        ps_s = [ps("d_ps_s%d" % i, [128, 4, 128], F32) for i in range(2)]
        (bcat, bcatT, bxt, bx1, bjunk, bss, bxn2, bxn2b, bxn2T, bqT, bsc, bsc2, bm8, bi8, bi8f, bcand, bcand2, bt8, bj8,
         bjf, bT4, baf, bbf, bE1, bE2, beidx, bgts, bg8, badot, bga, by, bps_t) = [Buf(n_) for n_ in (
            "cat", "catT", "xt", "x1", "junk", "ss", "xn2", "xn2b", "xn2T", "qT", "sc", "sc2", "m8", "i8", "i8f", "cand", "cand2",
            "t8", "j8", "jf", "T4", "af", "bf", "E1", "E2", "eidx", "gts", "g8", "adot", "ga", "y", "ps_t")]
        buv = bufs(NG, "uv")
        bps_o, bps_q, bps_s = bufs(2, "ps_o"), bufs(2, "ps_q"), bufs(2, "ps_s")

        for i in range(NT):
            rows = slice(i * 128, (i + 1) * 128)
            P.dma("sp", lambda h, rows=rows: h.dma_start(out=cat[:, 0:512], in_=c.AOUT[rows, :]), reads=[c.bAOUT[i]], writes=[bcat])
            P.dma("sp", lambda h, rows=rows: h.dma_start(out=cat[:, 512:1024], in_=c.HM[rows, :]), reads=[c.bHM[i]], writes=[bcat])
            P.dma("sp", lambda h, rows=rows: h.dma_start(out=xt[:], in_=c.x[rows, :]), writes=[bxt])
            for kc in range(8):
                P.op("pe", lambda h, kc=kc: h.transpose(out=ps_t[:, kc * 128:(kc + 1) * 128], in_=cat[:, kc * 128:(kc + 1) * 128],
                                                        identity=identb[:]), reads=[bcat, bconst], writes=[bps_t])
            P.op("act", lambda h: h.copy(out=catT[:].rearrange("p k t -> p (k t)"), in_=ps_t[:]), reads=[bps_t], writes=[bcatT])
            for g in range(2):
                for kc in range(8):
                    P.op("pe", lambda h, g=g, kc=kc: h.matmul(ps_o[g][:], lhsT=catT[:, kc, :], rhs=Wo[:, kc, g * 512:(g + 1) * 512],
                                                             start=(kc == 0), stop=(kc == 7)), reads=[bcatT, bWo[kc]], writes=[bps_o[g]])
                P.op("dve", lambda h, g=g: h.tensor_tensor(out=x1[:, g * 512:(g + 1) * 512], in0=ps_o[g][:], in1=xt[:, g * 512:(g + 1) * 512],
                                                          op=ALU.add), reads=[bps_o[g], bxt], writes=[bx1])
            P.op("act", lambda h: h.activation(out=junk[:], in_=x1[:], func=AF.Square, accum_out=ss[:, 0:1]), reads=[bx1], writes=[bjunk, bss])
            P.op("act", lambda h: h.activation(out=ss[:, 1:2], in_=ss[:, 0:1], func=AF.Sqrt, scale=1.0 / D, bias=EPS), reads=[bss], writes=[bss])
            P.op("dve", lambda h: h.reciprocal(out=ss[:, 2:3], in_=ss[:, 1:2]), reads=[bss], writes=[bss])
            P.op("dve", lambda h: h.scalar_tensor_tensor(out=xn2[:], in0=x1[:], scalar=ss[:, 2:3], in1=gn2[:], op0=ALU.mult, op1=ALU.mult),
                 reads=[bx1, bss, bconst], writes=[bxn2])
            P.op("act", lambda h: h.copy(out=xn2b[:], in_=xn2[:]), reads=[bxn2], writes=[bxn2b])
            for kc in range(8):
                P.op("pe", lambda h, kc=kc: h.transpose(out=ps_t[:, kc * 128:(kc + 1) * 128], in_=xn2b[:, kc * 128:(kc + 1) * 128],
                                                        identity=identb[:]), reads=[bxn2b, bconst], writes=[bps_t])
            P.op("act", lambda h: h.copy(out=xn2T[:].rearrange("p k t -> p (k t)"), in_=ps_t[:]), reads=[bps_t], writes=[bxn2T])
            for qg in range(4):
                k = qg % 2
                for cc in range(4):
                    hp = qg * 4 + cc
                    for kc in range(8):
                        P.op("pe", lambda h, k=k, cc=cc, hp=hp, kc=kc: h.matmul(ps_q[k][:, cc, :], lhsT=Wq[:, kc, hp * 128:(hp + 1) * 128],
                                                                              rhs=xn2T[:, kc, :], start=(kc == 0), stop=(kc == 7)),
                             reads=[bWq[kc], bxn2T], writes=[bps_q[k]])
                P.op("act", lambda h, k=k, qg=qg: h.copy(out=qT[:, qg * 4:(qg + 1) * 4, :], in_=ps_q[k][:]), reads=[bps_q[k]], writes=[bqT])
            for qg in range(4):
                k = qg % 2
                for cc in range(4):
                    hp = qg * 4 + cc
                    P.op("pe", lambda h, k=k, cc=cc, hp=hp: h.matmul(ps_s[k][:, cc, :], lhsT=qT[:, hp, :], rhs=SKT[:, hp, :],
                                                                   start=True, stop=True), reads=[bqT, bSKT], writes=[bps_s[k]])
                P.op("act", lambda h, k=k, qg=qg: h.copy(out=sc[:, qg * 4:(qg + 1) * 4, :], in_=ps_s[k][:]), reads=[bps_s[k]], writes=[bsc])
            for g in range(16):
                P.op("dve", lambda h, g=g: h.max(out=m8[:, g, 0:8], in_=sc[:, g, :]), reads=[bsc], writes=[bm8])
                P.op("dve", lambda h, g=g: h.max_index(out=i8[:, g, 0:8], in_max=m8[:, g, 0:8], in_values=sc[:, g, :]),
                     reads=[bsc, bm8], writes=[bi8])
                P.op("dve", lambda h, g=g: h.match_replace(out=sc2[:, g, :], in_to_replace=m8[:, g, 0:8], in_values=sc[:, g, :],
                                                           imm_value=-1e30), reads=[bsc, bm8], writes=[bsc2])
                P.op("dve", lambda h, g=g: h.max(out=m8[:, g, 8:16], in_=sc2[:, g, :]), reads=[bsc2], writes=[bm8])
                P.op("dve", lambda h, g=g: h.max_index(out=i8[:, g, 8:16], in_max=m8[:, g, 8:16], in_values=sc2[:, g, :]),
                     reads=[bsc2, bm8], writes=[bi8])
            m8v = m8[:].rearrange("p (a b) k -> p a b k", b=2)
            P.op("dve", lambda h, m8v=m8v: h.tensor_tensor(
                out=cand[:].rearrange("p a (x y) -> p a x y", y=16),
                in0=m8v[:, :, 0, :].unsqueeze(3).to_broadcast([128, 8, 16, 16]),
                in1=m8v[:, :, 1, :].unsqueeze(2).to_broadcast([128, 8, 16, 16]), op=ALU.add), reads=[bm8], writes=[bcand])
            for hd in range(8):
                P.op("dve", lambda h, hd=hd: h.max(out=t8[:, hd, 0:8], in_=cand[:, hd, :]), reads=[bcand], writes=[bt8])
                P.op("dve", lambda h, hd=hd: h.max_index(out=j8[:, hd, 0:8], in_max=t8[:, hd, 0:8], in_values=cand[:, hd, :]),
                     reads=[bcand, bt8], writes=[bj8])
                P.op("dve", lambda h, hd=hd: h.match_replace(out=cand2[:, hd, :], in_to_replace=t8[:, hd, 0:8], in_values=cand[:, hd, :],
                                                             imm_value=-1e30), reads=[bcand, bt8], writes=[bcand2])
                P.op("dve", lambda h, hd=hd: h.max(out=t8[:, hd, 8:16], in_=cand2[:, hd, :]), reads=[bcand2], writes=[bt8])
                P.op("dve", lambda h, hd=hd: h.max_index(out=j8[:, hd, 8:16], in_max=t8[:, hd, 8:16], in_values=cand2[:, hd, :]),
                     reads=[bcand2, bt8], writes=[bj8])
            P.op("dve", lambda h: h.tensor_copy(out=i8f[:], in_=i8[:]), reads=[bi8], writes=[bi8f])
            P.op("dve", lambda h: h.tensor_copy(out=jf[:], in_=j8[:].rearrange("p a k -> p (a k)")), reads=[bj8], writes=[bjf])
            P.op("dve", lambda h: h.tensor_tensor(out=T4[:], in0=jf[:].unsqueeze(2).to_broadcast([128, 128, 16]),
                                                  in1=THR[:].unsqueeze(1).to_broadcast([128, 128, 16]), op=ALU.is_ge),
                 reads=[bjf, bconst], writes=[bT4])
            P.op("dve", lambda h: h.tensor_reduce(out=af[:], in_=T4[:], axis=AX.X, op=ALU.add), reads=[bT4], writes=[baf])
            P.op("dve", lambda h: h.scalar_tensor_tensor(out=bf_[:], in0=af[:], scalar=-16.0, in1=jf[:], op0=ALU.mult, op1=ALU.add),
                 reads=[baf, bjf], writes=[bbf])
            i8v = i8f[:].rearrange("p (a b) k -> p a b k", b=2)
            for side, (idxt, Et, bE) in enumerate(((af, E1, bE1), (bf_, E2, bE2))):
                P.op("dve", lambda h, idxt=idxt: h.tensor_tensor(out=T4[:], in0=idxt[:].unsqueeze(2).to_broadcast([128, 128, 16]),
                                                                in1=IOT[:].unsqueeze(1).to_broadcast([128, 128, 16]), op=ALU.is_equal),
                     reads=[baf, bbf, bconst, bT4], writes=[bT4])
                P.op("dve", lambda h, side=side, i8v=i8v: h.tensor_tensor(
                    out=T4[:].rearrange("p (a k) x -> p a k x", k=16), in0=T4[:].rearrange("p (a k) x -> p a k x", k=16),
                    in1=i8v[:, :, side, :].unsqueeze(2).to_broadcast([128, 8, 16, 16]), op=ALU.mult),
                    reads=[bT4, bi8f], writes=[bT4])
                P.op("dve", lambda h, Et=Et: h.tensor_reduce(out=Et[:], in_=T4[:], axis=AX.X, op=ALU.add), reads=[bT4], writes=[bE])
            P.op("dve", lambda h: h.scalar_tensor_tensor(out=E1[:], in0=E1[:], scalar=128.0, in1=E2[:], op0=ALU.mult, op1=ALU.add),
                 reads=[bE1, bE2], writes=[bE1])
            P.op("dve", lambda h: h.tensor_copy(out=eidx[:], in_=E1[:]), reads=[bE1], writes=[beidx])
            P.op("dve", lambda h: h.tensor_tensor(out=gts[:], in0=t8[:], in1=t8[:, :, 0:1].to_broadcast([128, 8, 16]), op=ALU.subtract),
                 reads=[bt8], writes=[bgts])
            P.op("act", lambda h: h.activation(out=gts[:], in_=gts[:], func=AF.Exp), reads=[bgts], writes=[bgts])
            P.op("dve", lambda h: h.tensor_reduce(out=g8[:, 0:8], in_=gts[:], axis=AX.X, op=ALU.add), reads=[bgts], writes=[bg8])
            P.op("dve", lambda h: h.reciprocal(out=g8[:, 8:16], in_=g8[:, 0:8]), reads=[bg8], writes=[bg8])
            P.op("dve", lambda h: h.tensor_tensor(out=gts[:], in0=gts[:], in1=g8[:, 8:16].unsqueeze(2).to_broadcast([128, 8, 16]),
                                                  op=ALU.mult), reads=[bgts, bg8], writes=[bgts])
            if "EIDX" in c.dbg:
                P.dma("sp", lambda h, rows=rows: h.dma_start(out=c.EIDX[rows, :], in_=eidx[:]), reads=[beidx], writes=[c.bOUT[i]])
                P.dma("sp", lambda h, rows=rows: h.dma_start(out=c.GTS[rows, :], in_=gts[:].rearrange("p a k -> p (a k)")), reads=[bgts], writes=[c.bOUT[i]])
                P.dma("sp", lambda h, rows=rows: h.dma_start(out=c.X1[rows, :], in_=x1[:]), reads=[bx1], writes=[c.bOUT[i]])
            if "noexp" in c.dbg or i >= c.nexp:
                P.dma("sp", lambda h, rows=rows: h.dma_start(out=c.out[rows, :], in_=x1[:]), reads=[bx1], writes=[c.bOUT[i]])
                continue
            gts_f = gts[:].rearrange("p a k -> p (a k)")
            for grp in range(128 // GS):
                sls = list(range(grp * GS, (grp + 1) * GS))
                for sl in sls:
                    k = sl % NG
                    P.dma("pool", lambda h, sl=sl, k=k: h.indirect_dma_start(
                        out=uv[k][:], out_offset=None, in_=c.peer_uv,
                        in_offset=bass.IndirectOffsetOnAxis(ap=eidx[:, sl:sl + 1], axis=0)), reads=[beidx], writes=[buv[k]])
                    P.op("dve", lambda h, sl=sl, k=k: h.scalar_tensor_tensor(out=junk[:], in0=uv[k][:, 0:D], scalar=1.0, in1=xn2[:],
                                                                            op0=ALU.mult, op1=ALU.mult, accum_out=adot[:, sl:sl + 1]),
                         reads=[buv[k], bxn2], writes=[bjunk, badot])
                g0, g1 = sls[0], sls[-1] + 1
                P.op("act", lambda h, g0=g0, g1=g1: h.activation(out=ga[:, g0:g1], in_=adot[:, g0:g1], func=AF.Gelu),
                     reads=[badot], writes=[bga])
                P.op("dve", lambda h, g0=g0, g1=g1: h.tensor_tensor(out=ga[:, g0:g1], in0=ga[:, g0:g1], in1=gts_f[:, g0:g1], op=ALU.mult),
                     reads=[bga, bgts], writes=[bga])
                for sl in sls:
                    k = sl % NG
                    kd = sl % 4
                    P.op("act", lambda h, sl=sl, kd=kd: h.activation(out=dg[kd][:], in_=identf[:], func=AF.Copy, scale=ga[:, sl:sl + 1]),
                         reads=[bga, bconst], writes=[bdg[kd]])
                    for g in range(2):
                        P.op("pe", lambda h, sl=sl, k=k, kd=kd, g=g: h.matmul(ps_o[g][:], lhsT=dg[kd][:],
                                                                             rhs=uv[k][:, D + g * 512:D + (g + 1) * 512],
                                                                             start=(sl == 0), stop=(sl == 127)),
                             reads=[bdg[kd], buv[k]], writes=[bps_o[g]])
            for g in range(2):
                P.op("dve", lambda h, g=g: h.tensor_tensor(out=yacc[:, g * 512:(g + 1) * 512], in0=ps_o[g][:], in1=x1[:, g * 512:(g + 1) * 512],
                                                          op=ALU.add), reads=[bps_o[g], bx1], writes=[by])
            P.dma("sp", lambda h, rows=rows: h.dma_start(out=c.out[rows, :], in_=yacc[:]), reads=[by], writes=[c.bOUT[i]])
    P.barrier()


def build(dbg=(), phases="abcd"):
    nc = bass.Bass("TRN2", target_bir_lowering=False)
    c = Ctx()
    c.nc = nc
    ext_in = lambda n, s, d: nc.dram_tensor(n, s, d, kind="ExternalInput").ap()

    def scratch(n, s, d):
        kind = "ExternalOutput" if n in dbg else "Internal"
        return nc.dram_tensor(n, s, d, kind=kind).ap()

    c.x = ext_in("x", [S, D], F32)
    c.w_in = ext_in("w_in", [D, INW], F32)
    c.norm1_w = ext_in("norm1_w", [128, 8], F32)
    c.identb = ext_in("identb", [128, 128], BF16)
    c.gq = ext_in("gq", [128, 64], F32)
    c.gk = ext_in("gk", [128, 64], F32)
    c.rpbg = ext_in("rpbg", [128, 8, 896], F32)
    c.mask_i = ext_in("mask_i", [128, 896], F32)
    c.mask_a = ext_in("mask_a", [128, 896], F32)
    c.gao = ext_in("gao", [128, 512], F32)
    c.gate_b = ext_in("gate_b", [4, 4], F32)
    c.identf = ext_in("identf", [128, 128], F32)
    c.conv_w = ext_in("conv_w", [128, 8, 5], F32)
    c.conv_b = ext_in("conv_b", [128, 8], F32)
    c.sel = ext_in("sel", [4, 4, 128], F32)
    c.msk = ext_in("msk", [128, 2, 128], F32)
    c.gmn = ext_in("gmn", [128, 512], F32)
    c.w_out = ext_in("w_out", [D, D], F32)
    c.w_q = ext_in("w_q", [D, 2048], F32)
    c.skt = ext_in("skt", [128, 16, 128], F32)
    c.gn2 = ext_in("gn2", [128, D], F32)
    c.thr = ext_in("thr", [128, 16], F32)
    c.iot = ext_in("iot", [128, 16], F32)
    c.peer_uv = ext_in("peer_uv", [16384, 2 * D], F32)
    c.out = nc.dram_tensor("out", [S, D], F32, kind="ExternalOutput").ap()
    c.bOUT = bufs(NT, "OUT")
    c.dbg = dbg
    c.nexp = NT
    for d_ in dbg:
        if d_.startswith("exp"):
            c.nexp = int(d_[3:])
    if "EIDX" in dbg:
        c.EIDX = scratch("EIDX", [S, 128], I32)
        c.GTS = scratch("GTS", [S, 128], F32)
        c.X1 = scratch("X1", [S, D], F32)

    c.QT = scratch("QT", [4, 128, S], BF16)
    c.KT = scratch("KT", [4, 128, S], BF16)
    c.V = scratch("V", [S, 512], BF16)
    c.MV = scratch("MV", [S, 512], BF16)
    c.SIGO = scratch("SIGO", [S, 512], BF16)
    c.MQKT = scratch("MQKT", [1024, S], F32)
    c.GT = scratch("GT", [4, 4, S], F32)
    c.AOUT = scratch("AOUT", [S, 512], BF16)
    c.BCRD = scratch("BCRD", [2, 4, NT, 257], F32)
    c.bBCRD = bufs(2, "BCRD")
    c.HF = scratch("HF", [S, 512], F32)
    c.bHF = bufs(NT, "HF")
    c.HM = scratch("HM", [S, 512], BF16)
    c.bHM = bufs(NT, "HM")
    c.bAOUT = bufs(NT, "AOUT")
    c.bQKT = [bufs(NT, "QT"), bufs(NT, "KT")]
    c.bV, c.bMV, c.bSIGO, c.bMQKT, c.bGT = (bufs(NT, n) for n in ("V", "MV", "SIGO", "MQKT", "GT"))

    with ExitStack() as st:
        c.P = Prog(nc, st)
        if "a" in phases:
            phase_a(c)
        if "b" in phases:
            phase_b(c)
        if "c" in phases:
            phase_c(c)
        if "d" in phases:
            phase_d(c)
        c.P.emit()
    return nc


def host_inputs(inputs, b):
    f32 = np.float32
    m = {}
    m["x"] = np.ascontiguousarray(inputs["x"][b], dtype=f32)
    m["w_in"] = np.ascontiguousarray(inputs["w_in"][0], dtype=f32)
    m["norm1_w"] = np.ascontiguousarray(inputs["norm1_w"][0].reshape(8, 128).T, dtype=f32)
    m["identb"] = np.eye(128, dtype=f32).astype(ml_dtypes.bfloat16)
    m["gq"] = np.ascontiguousarray(np.broadcast_to(inputs["q_norm_w"][0][None, :], (128, 64)), dtype=f32)
    m["gk"] = np.ascontiguousarray(np.broadcast_to(inputs["k_norm_w"][0][None, :], (128, 64)), dtype=f32)
    p = np.arange(128); kr = p // 64; kc = p % 64
    col = np.arange(128); rq = col // 64; cc = col % 64
    dt = np.arange(-3, 4)
    drow = 2 * dt[None, :, None] + kr[:, None, None] - rq[None, None, :]
    dcol = kc[:, None, None] - cc[None, None, :] + 0 * dt[None, :, None]
    rpb = inputs["attn_rpb"][0]
    g = rpb[:, np.clip(drow + 7, 0, 14), np.clip(dcol + 15, 0, 30)]
    m["rpbg"] = np.ascontiguousarray(g.transpose(1, 0, 2, 3).reshape(128, 8, 896), dtype=f32)
    cs = np.clip(cc - 8, 0, 48)
    colvalid = (kc[:, None, None] >= cs[None, None, :]) & (kc[:, None, None] < cs[None, None, :] + 16)
    colvalid = colvalid & (dt[None, :, None] > -100)
    m["mask_a"] = np.where(colvalid & (np.abs(drow) <= 7), 0.0, NEG).astype(f32).reshape(128, 896)
    m["mask_i"] = np.where(colvalid & (drow >= -4) & (drow <= 3), 0.0, NEG).astype(f32).reshape(128, 896)
    m["gate_b"] = np.ascontiguousarray(inputs["mlstm_gate_b"][0].T, dtype=f32)
    m["identf"] = np.eye(128, dtype=f32)
    m["conv_w"] = np.ascontiguousarray(inputs["mlstm_conv_w"][0].reshape(5, 8, 128).transpose(2, 1, 0), dtype=f32)
    m["conv_b"] = np.ascontiguousarray(inputs["mlstm_conv_b"][0].reshape(8, 128).T, dtype=f32)
    sel = np.zeros((4, 4, 128), f32)
    for hh in range(4):
        sel[hh, hh, :] = 1.0
    m["sel"] = sel
    ii = np.arange(128)
    msk = np.zeros((128, 2, 128), f32)
    msk[:, 0, :] = np.where(ii[:, None] <= ii[None, :], 0.0, NEG)
    msk[:, 1, :] = np.where(ii[:, None] >= ii[None, :], 0.0, NEG)
    m["msk"] = msk
    m["gmn"] = np.ascontiguousarray(np.broadcast_to(inputs["mlstm_norm_w"][0][None, :], (128, 512)), dtype=f32)
    m["w_out"] = np.ascontiguousarray(inputs["w_out"][0], dtype=f32)
    m["w_q"] = np.ascontiguousarray(inputs["peer_w_q"][0], dtype=f32)
    m["skt"] = np.ascontiguousarray(inputs["peer_sub_keys"][0].reshape(16, 128, 128).transpose(2, 0, 1), dtype=f32)
    m["gn2"] = np.ascontiguousarray(np.broadcast_to(inputs["norm2_w"][0][None, :], (128, D)), dtype=f32)
    thr = (np.arange(16, dtype=f32) + 1.0) * 16.0
    thr[15] = 1e9
    m["thr"] = np.ascontiguousarray(np.broadcast_to(thr[None, :], (128, 16)), dtype=f32)
    m["iot"] = np.ascontiguousarray(np.broadcast_to(np.arange(16, dtype=f32)[None, :], (128, 16)), dtype=f32)
    m["peer_uv"] = np.ascontiguousarray(np.concatenate([inputs["peer_u"][0], inputs["peer_v"][0]], axis=1), dtype=f32)
    m["gao"] = np.ascontiguousarray(np.broadcast_to(inputs["attn_out_norm_w"][0][None, :], (128, 512)), dtype=f32)
    return m


def kernel(**inputs):
    nc = build()
    in_maps = [host_inputs(inputs, b) for b in range(8)]
    res = run_bass_kernel_spmd(nc, in_maps, core_ids=list(range(8)))
    return np.stack([r["out"] for r in res.results], axis=0).astype(np.float32)
```

```python
import numpy as np
import ml_dtypes
import concourse.bass as bass
import concourse.mybir as mybir
from concourse.bass_utils import run_bass_kernel_spmd
from contextlib import ExitStack

F32 = mybir.dt.float32
BF16 = mybir.dt.bfloat16
I32 = mybir.dt.int32
U32 = mybir.dt.uint32
ALU = mybir.AluOpType
AF = mybir.ActivationFunctionType
AX = mybir.AxisListType

S = 4096
D = 1024
NT = 32
INW = 3600
EPS = 1e-6
NEG = -30000.0


class Buf:
    __slots__ = ("name", "w", "r")

    def __init__(self, name=""):
        self.name = name
        self.w = {}
        self.r = {}


class Prog:
    ENG = ("pe", "act", "dve", "pool", "sp")
    NRINGS = {"sp": 12, "act": 6, "pool": 48}

    def __init__(self, nc, stack):
        self.nc = nc
        self.sem = {e: stack.enter_context(nc.semaphore("s_" + e)) for e in self.ENG}
        self.cnt = {e: 0 for e in self.ENG}
        self.ops = {e: [] for e in self.ENG}
        self.seen = {e: {} for e in self.ENG}
        self.ring = {}
        self.ring_i = {}
        self.ring_tok = {}
        for q in ("sp", "act", "pool"):
            self.ring[q] = [stack.enter_context(nc.semaphore("r_%s%d" % (q, i)))
                            for i in range(self.NRINGS[q])]
            self.ring_i[q] = 0
            self.ring_tok[q] = [None] * self.NRINGS[q]
        self.final_tokens = []

    def _need(self, eng, tok, waits):
        sem, val, teng = tok
        if teng == eng and eng == "pe":
            return
        k = id(sem)
        if self.seen[eng].get(k, 0) >= val:
            return
        self.seen[eng][k] = val
        waits.append((sem, val))

    def _deps(self, eng, reads, writes):
        waits = []
        for b in reads:
            for t in b.w.values():
                self._need(eng, t, waits)
        for b in writes:
            for t in b.w.values():
                self._need(eng, t, waits)
            for t in b.r.values():
                self._need(eng, t, waits)
        best = {}
        for sem, val in waits:
            k = id(sem)
            if k not in best or best[k][1] < val:
                best[k] = (sem, val)
        return list(best.values())

    def _commit(self, tok, reads, writes):
        k = id(tok[0])
        for b in reads:
            b.r[k] = tok
        for b in writes:
            b.w = {k: tok}
            b.r = {}

    def op(self, eng, fn, reads=(), writes=()):
        waits = self._deps(eng, reads, writes)
        self.cnt[eng] += 1
        tok = (self.sem[eng], self.cnt[eng], eng)
        self.ops[eng].append((waits, fn, (self.sem[eng], 1)))
        self._commit(tok, reads, writes)
        return tok

    def dma(self, q, fn, reads=(), writes=(), final=False):
        waits = self._deps(q, reads, writes)
        i = self.ring_i[q]
        nr = self.NRINGS[q]
        slot = i % nr
        prev = self.ring_tok[q][slot]
        if prev is not None:
            w2 = []
            self._need(q, prev, w2)
            waits = waits + w2
        sem = self.ring[q][slot]
        val = 16 * (i // nr + 1)
        self.ring_i[q] = i + 1
        tok = (sem, val, "dma_" + q)
        self.ring_tok[q][slot] = tok
        self.ops[q].append((waits, fn, (sem, 16)))
        self._commit(tok, reads, writes)
        if final:
            self.final_tokens.append(tok)
        return tok

    def _all_tokens(self):
        toks = []
        for e in ("pe", "act", "dve", "pool"):
            if self.cnt[e] > 0:
                toks.append((self.sem[e], self.cnt[e], e + "_all"))
        for q in ("act", "pool", "sp"):
            for t in self.ring_tok[q]:
                if t is not None:
                    toks.append(t)
        return toks

    def barrier(self):
        toks = self._all_tokens()
        for e in self.ENG:
            waits = []
            for t in toks:
                sem, val, teng = t
                k = id(sem)
                if self.seen[e].get(k, 0) >= val:
                    continue
                self.seen[e][k] = val
                waits.append((sem, val))
            if waits:
                self.ops[e].append((waits, None, None))

    def emit(self):
        nc = self.nc
        fw = []
        for t in self._all_tokens():
            self._need("sp", t, fw)
        final_waits = fw

        def run(e, handle, extra=None):
            for waits, fn, inc in self.ops[e]:
                for sem, val in waits:
                    handle.wait_ge(sem, val)
                if fn is not None:
                    ins = fn(handle)
                    ins.then_inc(inc[0], inc[1])
            if extra:
                for sem, val in extra:
                    handle.wait_ge(sem, val)

        with nc.Block() as block:
            @block.tensor
            def _(h):
                run("pe", h)

            @block.scalar
            def _(h):
                run("act", h)

            @block.vector
            def _(h):
                run("dve", h)

            @block.gpsimd
            def _(h):
                run("pool", h)

            @block.sync
            def _(h):
                run("sp", h, final_waits)


class Ctx:
    pass


def bufs(n, name=""):
    return [Buf("%s%d" % (name, i)) for i in range(n)]


def phase_a(c):
    nc, P = c.nc, c.P
    with ExitStack() as st:
        sb = lambda n, s, d: st.enter_context(nc.sbuf_tensor(n, s, d))
        ps = lambda n, s, d: st.enter_context(nc.psum_tensor(n, s, d))
        Wbf = sb("a_Wbf", [128, 8, INW], BF16)
        stage = [sb("a_stage%d" % i, [128, INW], F32) for i in range(2)]
        w1 = sb("a_w1", [128, 8], F32)
        identb = sb("a_identb", [128, 128], BF16)
        gq = sb("a_gq", [128, 64], F32)
        gk = sb("a_gk", [128, 64], F32)
        xt = [sb("a_xt%d" % i, [128, D], F32) for i in range(2)]
        junk = sb("a_junk", [128, D], F32)
        ss = sb("a_ss", [128, 4], F32)
        xn = sb("a_xn", [128, D], BF16)
        xnT = sb("a_xnT", [128, 8, 128], BF16)
        sq = sb("a_sq", [128, 512], F32)
        tmp = sb("a_tmp", [128, 512], F32)
        s8 = sb("a_s8", [128, 24], F32)
        qn = sb("a_qn", [128, 512], BF16)
        qTs = sb("a_qTs", [128, 4, 128], BF16)
        ob = [sb("a_ob%d" % i, [128, 512], BF16) for i in range(2)]
        fm = [sb("a_fm%d" % i, [128, 4, 128], F32) for i in range(2)]
        gsb = sb("a_gsb", [4, 4, 128], F32)
        ps_t = ps("a_ps_t", [128, D], BF16)
        ps_g = [ps("a_ps_g%d" % i, [128, 512], F32) for i in range(2)]
        ps_q = ps("a_ps_q", [128, 4, 128], BF16)
        ps_f = [ps("a_ps_f%d" % i, [128, 4, 128], F32) for i in range(2)]
        ps_gt = ps("a_ps_gt", [4, 4, 128], F32)

        bW = bufs(8, "W")
        bst = bufs(2, "st")
        bc = Buf("consts")
        bxt = bufs(2, "xt")
        bjunk, bss, bxn, bxnT, bsq, btmp, bs8, bqn, bqTs = [Buf(n) for n in
            ("junk", "ss", "xn", "xnT", "sq", "tmp", "s8", "qn", "qTs")]
        bob = bufs(2, "ob")
        bfm = bufs(2, "fm")
        bgsb = Buf("gsb")
        bps_t, bps_q, bps_gt = Buf("ps_t"), Buf("ps_q"), Buf("ps_gt")
        bps_g = bufs(2, "ps_g")
        bps_f = bufs(2, "ps_f")

        P.dma("sp", lambda h: h.dma_start(out=w1[:], in_=c.norm1_w), writes=[bc])
        P.dma("sp", lambda h: h.dma_start(out=identb[:], in_=c.identb), writes=[bc])
        P.dma("sp", lambda h: h.dma_start(out=gq[:], in_=c.gq), writes=[bc])
        P.dma("sp", lambda h: h.dma_start(out=gk[:], in_=c.gk), writes=[bc])
        for kc in range(8):
            s_ = stage[kc % 2]
            P.dma("sp", lambda h, kc=kc, s_=s_: h.dma_start(out=s_[:], in_=c.w_in[kc * 128:(kc + 1) * 128, :]),
                  writes=[bst[kc % 2]])
            P.op("dve" if kc % 2 == 0 else "pool",
                 lambda h, kc=kc, s_=s_: h.tensor_scalar(out=Wbf[:, kc, :], in0=s_[:], scalar1=w1[:, kc:kc + 1],
                                                         scalar2=None, op0=ALU.mult),
                 reads=[bst[kc % 2], bc], writes=[bW[kc]])

        gi = [0]

        def mm_group_tok(cols, sub=None):
            k = gi[0] % 2
            gi[0] += 1
            c0, c1 = cols
            for kc in range(8):
                P.op("pe", lambda h, kc=kc, k=k: h.matmul(ps_g[k][:, 0:c1 - c0], lhsT=xnT[:, kc, :],
                                                       rhs=Wbf[:, kc, c0:c1], start=(kc == 0), stop=(kc == 7)),
                     reads=[bxnT, bW[kc]], writes=[bps_g[k]])
            return k

        oi = [0]
        fi = [0]
        for i in range(NT):
            x_ = xt[i % 2]
            bx_ = bxt[i % 2]
            P.dma("sp", lambda h, i=i, x_=x_: h.dma_start(out=x_[:], in_=c.x[i * 128:(i + 1) * 128, :]), writes=[bx_])
            P.op("act", lambda h, x_=x_: h.activation(out=junk[:], in_=x_[:], func=AF.Square, accum_out=ss[:, 0:1]),
                 reads=[bx_], writes=[bjunk, bss])
            P.op("act", lambda h: h.activation(out=ss[:, 1:2], in_=ss[:, 0:1], func=AF.Sqrt, scale=1.0 / D, bias=EPS),
                 reads=[bss], writes=[bss])
            P.op("dve", lambda h: h.reciprocal(out=ss[:, 2:3], in_=ss[:, 1:2]), reads=[bss], writes=[bss])
            P.op("dve", lambda h, x_=x_: h.tensor_scalar(out=xn[:], in0=x_[:], scalar1=ss[:, 2:3], scalar2=None,
                                                          op0=ALU.mult), reads=[bx_, bss], writes=[bxn])
            for kc in range(8):
                P.op("pe", lambda h, kc=kc: h.transpose(out=ps_t[:, kc * 128:(kc + 1) * 128],
                                                        in_=xn[:, kc * 128:(kc + 1) * 128], identity=identb[:]),
                     reads=[bxn, bc], writes=[bps_t])
            P.op("act", lambda h: h.copy(out=xnT[:].rearrange("p k t -> p (k t)"), in_=ps_t[:]),
                 reads=[bps_t], writes=[bxnT])

            for which, (c0, gain, dst) in enumerate(((0, gq, c.QT), (512, gk, c.KT))):
                k = mm_group_tok((c0, c0 + 512))
                P.op("act", lambda h, k=k: h.activation(out=sq[:], in_=ps_g[k][:], func=AF.Square),
                     reads=[bps_g[k]], writes=[bsq])
                P.op("dve", lambda h: h.tensor_reduce(out=s8[:, 0:8], in_=sq[:].rearrange("p (a b) -> p a b", b=64),
                                                      axis=AX.X, op=ALU.add), reads=[bsq], writes=[bs8])
                P.op("act", lambda h: h.activation(out=s8[:, 8:16], in_=s8[:, 0:8], func=AF.Sqrt, scale=1.0 / 64,
                                                   bias=EPS), reads=[bs8], writes=[bs8])
                P.op("dve", lambda h: h.reciprocal(out=s8[:, 16:24], in_=s8[:, 8:16]), reads=[bs8], writes=[bs8])
                P.op("dve", lambda h, k=k: h.tensor_tensor(
                    out=tmp[:].rearrange("p (a b) -> p a b", b=64),
                    in0=ps_g[k][:].rearrange("p (a b) -> p a b", b=64),
                    in1=s8[:, 16:24].unsqueeze(2).to_broadcast([128, 8, 64]), op=ALU.mult),
                    reads=[bps_g[k], bs8], writes=[btmp])
                P.op("pool", lambda h, gain=gain: h.tensor_tensor(
                    out=qn[:].rearrange("p (a b) -> p a b", b=64),
                    in0=tmp[:].rearrange("p (a b) -> p a b", b=64),
                    in1=gain[:].unsqueeze(1).to_broadcast([128, 8, 64]), op=ALU.mult),
                    reads=[btmp, bc], writes=[bqn])
                for hp in range(4):
                    P.op("pe", lambda h, hp=hp: h.transpose(out=ps_q[:, hp, :], in_=qn[:, hp * 128:(hp + 1) * 128],
                                                            identity=identb[:]), reads=[bqn, bc], writes=[bps_q])
                P.op("act", lambda h: h.copy(out=qTs[:], in_=ps_q[:]), reads=[bps_q], writes=[bqTs])
                P.dma("sp", lambda h, i=i, dst=dst: h.dma_start(
                    out=dst[:, :, i * 128:(i + 1) * 128].rearrange("a p t -> p a t"), in_=qTs[:]),
                    reads=[bqTs], writes=[c.bQKT[which][i]])

            for (c0, dst, bdst, fn) in ((1024, c.V, c.bV, AF.Copy), (2560, c.MV, c.bMV, AF.Copy),
                                        (3072, c.SIGO, c.bSIGO, AF.Sigmoid)):
                k = mm_group_tok((c0, c0 + 512))
                o = oi[0] % 2
                oi[0] += 1
                P.op("act", lambda h, k=k, o=o, fn=fn: h.activation(out=ob[o][:], in_=ps_g[k][:], func=fn),
                     reads=[bps_g[k]], writes=[bob[o]])
                P.dma("sp", lambda h, i=i, o=o, dst=dst: h.dma_start(out=dst[i * 128:(i + 1) * 128, :], in_=ob[o][:]),
                      reads=[bob[o]], writes=[bdst[i]])

            for half in range(2):
                f = fi[0] % 2
                fi[0] += 1
                for cc in range(4):
                    ch = half * 4 + cc
                    col = 1536 + ch * 128
                    for kc in range(8):
                        P.op("pe", lambda h, kc=kc, f=f, cc=cc, col=col: h.matmul(
                            ps_f[f][:, cc, :], lhsT=Wbf[:, kc, col:col + 128], rhs=xnT[:, kc, :],
                            start=(kc == 0), stop=(kc == 7)), reads=[bxnT, bW[kc]], writes=[bps_f[f]])
                P.op("dve", lambda h, f=f: h.tensor_copy(out=fm[f][:], in_=ps_f[f][:]), reads=[bps_f[f]], writes=[bfm[f]])
                P.dma("sp", lambda h, i=i, f=f, half=half: h.dma_start(
                    out=c.MQKT[half * 512:(half + 1) * 512, i * 128:(i + 1) * 128].rearrange("(a p) t -> p a t", p=128),
                    in_=fm[f][:]), reads=[bfm[f]], writes=[c.bMQKT[i]])
            for g in range(4):
                col = 3584 + 4 * g
                for kc in range(8):
                    P.op("pe", lambda h, kc=kc, g=g, col=col: h.matmul(
                        ps_gt[:, g, :], lhsT=Wbf[:, kc, col:col + 4], rhs=xnT[:, kc, :],
                        start=(kc == 0), stop=(kc == 7)), reads=[bxnT, bW[kc]], writes=[bps_gt])
            P.op("dve", lambda h: h.tensor_copy(out=gsb[:], in_=ps_gt[:]), reads=[bps_gt], writes=[bgsb])
            P.dma("sp", lambda h, i=i: h.dma_start(out=c.GT[:, :, i * 128:(i + 1) * 128].rearrange("g a t -> a g t"),
                                                   in_=gsb[:]), reads=[bgsb], writes=[c.bGT[i]])
    P.barrier()


def phase_b(c):
    nc, P = c.nc, c.P
    with ExitStack() as st:
        sb = lambda n, s, d: st.enter_context(nc.sbuf_tensor(n, s, d))
        ps = lambda n, s, d: st.enter_context(nc.psum_tensor(n, s, d))
        QT = sb("b_QT", [128, 4, S], BF16)
        KT = sb("b_KT", [128, 4, S], BF16)
        V = sb("b_V", [128, NT, 8, 65], BF16)
        TBI = sb("b_TBI", [128, 8, 896], F32)
        TBA = sb("b_TBA", [128, 8, 896], F32)
        MI = sb("b_MI", [128, 896], F32)
        MA = sb("b_MA", [128, 896], F32)
        gao = sb("b_gao", [128, 512], F32)
        sT = [sb("b_sT%d" % i, [128, 640], F32) for i in range(2)]
        pT = [sb("b_pT%d" % i, [128, 640], BF16) for i in range(2)]
        ao = sb("b_ao", [128, 512], F32)
        junk = sb("b_junk", [128, 512], F32)
        rc = [sb("b_rc%d" % i, [128, 1], F32) for i in range(2)]
        ss = sb("b_ss", [128, 4], F32)
        aob = [sb("b_aob%d" % i, [128, 512], BF16) for i in range(2)]
        ps_s = [ps("b_ps_s%d" % i, [128, 1024], F32) for i in range(2)]
        ps_o = [ps("b_ps_o%d" % i, [128, 128], F32) for i in range(2)]

        bQT, bKT = bufs(4, "bQT"), bufs(4, "bKT")
        bVt = bufs(NT, "bV")
        bones, btb, bm, bgao = Buf("ones"), Buf("tb"), Buf("m"), Buf("gao")
        bsT, bpT, brc, baob = bufs(2, "sT"), bufs(2, "pT"), bufs(2, "rc"), bufs(2, "aob")
        bao, bjunk, bss = Buf("ao"), Buf("junk"), Buf("ss")
        bps_s, bps_o = bufs(2, "ps_s"), bufs(2, "ps_o")

        P.dma("sp", lambda h: h.dma_start(out=TBI[:], in_=c.rpbg), writes=[btb])
        P.dma("sp", lambda h: h.dma_start(out=MI[:], in_=c.mask_i), writes=[bm])
        P.dma("sp", lambda h: h.dma_start(out=MA[:], in_=c.mask_a), writes=[bm])
        P.dma("sp", lambda h: h.dma_start(out=gao[:], in_=c.gao), writes=[bgao])
        for hp in range(4):
            P.dma("sp", lambda h, hp=hp: h.dma_start(out=QT[:, hp, :], in_=c.QT[hp]),
                  reads=c.bQKT[0], writes=[bQT[hp]])
            P.dma("act", lambda h, hp=hp: h.dma_start(out=KT[:, hp, :], in_=c.KT[hp]),
                  reads=c.bQKT[1], writes=[bKT[hp]])
        P.op("pool", lambda h: h.memset(V[:, :, :, 64:65], 1.0), writes=[bones])
        for i in range(NT):
            P.dma("sp" if i % 2 == 0 else "act", lambda h, i=i: h.dma_start(
                out=V[:, i, :, 0:64], in_=c.V[i * 128:(i + 1) * 128, :].rearrange("p (a b) -> p a b", b=64)),
                reads=[c.bV[i]], writes=[bVt[i]])
        for hd in range(8):
            P.op("dve", lambda h, hd=hd: h.tensor_tensor(out=TBA[:, hd, :], in0=TBI[:, hd, :], in1=MA[:], op=ALU.add),
                 reads=[btb, bm], writes=[btb])
        for hd in range(8):
            P.op("dve", lambda h, hd=hd: h.tensor_tensor(out=TBI[:, hd, :], in0=TBI[:, hd, :], in1=MI[:], op=ALU.add),
                 reads=[btb, bm], writes=[btb])

        it = 0
        for j in range(NT):
            if 2 <= j <= 29:
                kts = list(range(j - 2, j + 3)); tb = TBI; s0 = 1
            elif j == 0:
                kts = [0, 1, 2, 3]; tb = TBA; s0 = 3
            elif j == 1:
                kts = [0, 1, 2, 3]; tb = TBA; s0 = 2
            elif j == 30:
                kts = [28, 29, 30, 31]; tb = TBA; s0 = 1
            else:
                kts = [28, 29, 30, 31]; tb = TBA; s0 = 0
            n = len(kts)
            for hd in range(8):
                hp, hh = hd // 2, hd % 2
                k = it % 2
                it += 1
                p0, p1 = hh * 64, hh * 64 + 64
                for idx, kt in enumerate(kts):
                    P.op("pe", lambda h, k=k, idx=idx, kt=kt, hp=hp, p0=p0, p1=p1, j=j: h.matmul(
                        ps_s[k][:, idx * 128:(idx + 1) * 128], lhsT=KT[p0:p1, hp, kt * 128:(kt + 1) * 128],
                        rhs=QT[p0:p1, hp, j * 128:(j + 1) * 128], start=True, stop=True),
                        reads=[bKT[hp], bQT[hp]], writes=[bps_s[k]])
                P.op("dve", lambda h, k=k, n=n, tb=tb, s0=s0, hd=hd: h.scalar_tensor_tensor(
                    out=sT[k][:, 0:n * 128], in0=ps_s[k][:, 0:n * 128], scalar=0.125,
                    in1=tb[:, hd, s0 * 128:(s0 + n) * 128], op0=ALU.mult, op1=ALU.add),
                    reads=[bps_s[k], btb], writes=[bsT[k]])
                P.op("act", lambda h, k=k, n=n: h.activation(out=pT[k][:, 0:n * 128], in_=sT[k][:, 0:n * 128],
                                                             func=AF.Exp), reads=[bsT[k]], writes=[bpT[k]])
                for idx, kt in enumerate(kts):
                    P.op("pe", lambda h, k=k, idx=idx, kt=kt, hd=hd, n=n: h.matmul(
                        ps_o[k][:, 0:65], lhsT=pT[k][:, idx * 128:(idx + 1) * 128], rhs=V[:, kt, hd, :],
                        start=(idx == 0), stop=(idx == n - 1)),
                        reads=[bpT[k], bVt[kt], bones], writes=[bps_o[k]])
                P.op("dve", lambda h, k=k: h.reciprocal(out=rc[k][:], in_=ps_o[k][:, 64:65]),
                     reads=[bps_o[k]], writes=[brc[k]])
                P.op("dve", lambda h, k=k, hd=hd: h.tensor_scalar(
                    out=ao[:, hd * 64:(hd + 1) * 64], in0=ps_o[k][:, 0:64], scalar1=rc[k][:], scalar2=None,
                    op0=ALU.mult), reads=[bps_o[k], brc[k]], writes=[bao])
            o = j % 2
            P.op("act", lambda h: h.activation(out=junk[:], in_=ao[:], func=AF.Square, accum_out=ss[:, 0:1]),
                 reads=[bao], writes=[bjunk, bss])
            P.op("act", lambda h: h.activation(out=ss[:, 1:2], in_=ss[:, 0:1], func=AF.Sqrt, scale=1.0 / 512, bias=EPS),
                 reads=[bss], writes=[bss])
            P.op("dve", lambda h: h.reciprocal(out=ss[:, 2:3], in_=ss[:, 1:2]), reads=[bss], writes=[bss])
            P.op("dve", lambda h, o=o: h.scalar_tensor_tensor(out=aob[o][:], in0=ao[:], scalar=ss[:, 2:3], in1=gao[:],
                                                          op0=ALU.mult, op1=ALU.mult),
                 reads=[bao, bss, bgao], writes=[baob[o]])
            P.dma("sp", lambda h, j=j, o=o: h.dma_start(out=c.AOUT[j * 128:(j + 1) * 128, :], in_=aob[o][:]),
                  reads=[baob[o]], writes=[c.bAOUT[j]])
    P.barrier()


def phase_c(c):
    nc, P = c.nc, c.P
    with ExitStack() as st0:
        sb0 = lambda n, s, d: st0.enter_context(nc.sbuf_tensor(n, s, d))
        COLS = sb0("c_COLS", [128, NT, 24], F32)
        bCOLS = Buf("COLS")
        with ExitStack() as st:
            sb = lambda n, s, d: st.enter_context(nc.sbuf_tensor(n, s, d))
            ps = lambda n, s, d: st.enter_context(nc.psum_tensor(n, s, d))
            G1, G2, CL, Aa, AA, ZER, T1 = [sb("c1_" + n, [4, S], F32) for n in ("G1", "G2", "CL", "Aa", "AA", "ZER", "T1")]
            bG1, bG2, bCL, bAa, bAA, bZER, bT1 = [Buf(n) for n in ("G1", "G2", "CL", "Aa", "AA", "ZER", "T1")]
            BCR = sb("c1_BCR", [4, NT, 257], F32)
            ROWS = sb("c1_ROWS", [24, S], F32)
            gb = sb("c1_gb", [4, 4], F32)
            ngb = sb("c1_ngb", [4, 4], F32)
            AE = sb("c1_AE", [4, NT], F32)
            APv = sb("c1_AP", [4, NT], F32)
            dd = sb("c1_dd", [4, NT], F32)
            identf = sb("c1_identf", [128, 128], F32)
            ps_c = ps("c1_ps_c", [128, NT, 32], F32)
            bBCR, bgb, bAE, bAPv, bdd, bid, bps_c = [Buf(n) for n in ("BCR", "gb", "AE", "AP", "dd", "id", "ps_c")]
            bROWS = bufs(6, "ROWS")
            P.dma("sp", lambda h: h.dma_start(out=gb[:], in_=c.gate_b), writes=[bgb])
            P.dma("sp", lambda h: h.dma_start(out=identf[:], in_=c.identf), writes=[bid])
            P.op("dve", lambda h: h.tensor_scalar(out=ngb[:], in0=gb[:], scalar1=-1.0, scalar2=None, op0=ALU.mult),
                 reads=[bgb], writes=[bgb])
            P.op("pool", lambda h: h.memset(ZER[:], 0.0), writes=[bZER])
            for d in range(2):
                rv = (lambda t: t[:, :]) if d == 0 else (lambda t: t[:, ::-1])
                gi_, gf_ = 2 * d, 2 * d + 1
                P.dma("sp", lambda h, gi_=gi_: h.dma_start(out=G1[:], in_=c.GT[gi_]), reads=c.bGT, writes=[bG1])
                P.dma("sp", lambda h, gf_=gf_: h.dma_start(out=G2[:], in_=c.GT[gf_]), reads=c.bGT, writes=[bG2])
                P.op("act", lambda h, gf_=gf_: h.activation(out=G2[:], in_=G2[:], func=AF.Exp, scale=-1.0,
                                                            bias=ngb[:, gf_:gf_ + 1]), reads=[bG2, bgb], writes=[bG2])
                P.op("act", lambda h: h.activation(out=G2[:], in_=G2[:], func=AF.Ln, bias=1.0), reads=[bG2], writes=[bG2])
                P.op("dve", lambda h, rv=rv: h.tensor_tensor_scan(out=rv(CL), data0=rv(G2), data1=ZER[:], initial=0.0,
                                                                  op0=ALU.add, op1=ALU.add),
                     reads=[bG2, bZER], writes=[bCL])
                P.op("dve", lambda h, gi_=gi_: h.scalar_tensor_tensor(out=Aa[:], in0=G1[:], scalar=gb[:, gi_:gi_ + 1],
                                                                      in1=CL[:], op0=ALU.add, op1=ALU.add),
                     reads=[bG1, bgb, bCL], writes=[bAa])
                P.op("dve", lambda h, rv=rv: h.tensor_tensor_scan(out=rv(AA), data0=rv(Aa), data1=ZER[:], initial=0.0,
                                                                  op0=ALU.max, op1=ALU.add),
                     reads=[bAa, bZER], writes=[bAA])
                P.op("dve", lambda h: h.tensor_tensor(out=T1[:], in0=CL[:], in1=AA[:], op=ALU.subtract),
                     reads=[bCL, bAA], writes=[bT1])
                P.op("act", lambda h: h.activation(out=T1[:], in_=T1[:], func=AF.Exp), reads=[bT1], writes=[bT1])
                P.dma("sp", lambda h, d=d: h.dma_start(out=ROWS[12 * d + 8:12 * d + 12, :], in_=T1[:]),
                      reads=[bT1], writes=[bROWS[3 * d + 2]])
                P.dma("sp", lambda h, d=d: h.dma_start(out=ROWS[12 * d:12 * d + 4, :], in_=Aa[:]),
                      reads=[bAa], writes=[bROWS[3 * d]])
                AAv = AA[:].rearrange("p (c t) -> p c t", t=128)
                epos = 127 if d == 0 else 0
                P.op("dve", lambda h, AAv=AAv, epos=epos: h.tensor_copy(out=AE[:], in_=AAv[:, :, epos]),
                     reads=[bAA], writes=[bAE])
                P.op("dve", lambda h: h.memset(APv[:], 0.0), writes=[bAPv])
                if d == 0:
                    P.op("dve", lambda h: h.tensor_copy(out=APv[:, 1:NT], in_=AE[:, 0:NT - 1]), reads=[bAE], writes=[bAPv])
                else:
                    P.op("dve", lambda h: h.tensor_copy(out=APv[:, 0:NT - 1], in_=AE[:, 1:NT]), reads=[bAE], writes=[bAPv])
                G1v = G1[:].rearrange("p (c t) -> p c t", t=128)
                G2v = G2[:].rearrange("p (c t) -> p c t", t=128)
                Aav = Aa[:].rearrange("p (c t) -> p c t", t=128)
                P.op("dve", lambda h, G1v=G1v, Aav=Aav: h.tensor_tensor(
                    out=G1v, in0=Aav, in1=AE[:].unsqueeze(2).to_broadcast([4, NT, 128]), op=ALU.subtract),
                    reads=[bAa, bAE], writes=[bG1])
                P.op("act", lambda h: h.activation(out=G1[:], in_=G1[:], func=AF.Exp), reads=[bG1], writes=[bG1])
                P.dma("sp", lambda h, d=d: h.dma_start(out=ROWS[12 * d + 4:12 * d + 8, :], in_=G1[:]),
                      reads=[bG1], writes=[bROWS[3 * d + 1]])
                P.op("dve", lambda h, AAv=AAv: h.tensor_scalar(out=BCR[:, :, 0:128], in0=AAv, scalar1=-1.0, scalar2=None,
                                                               op0=ALU.mult), reads=[bAA], writes=[bBCR])
                P.op("dve", lambda h, G2v=G2v, AAv=AAv: h.tensor_tensor(
                    out=G2v, in0=APv[:].unsqueeze(2).to_broadcast([4, NT, 128]), in1=AAv, op=ALU.subtract),
                    reads=[bAA, bAPv], writes=[bG2])
                P.op("act", lambda h, G2v=G2v: h.activation(out=BCR[:, :, 128:256], in_=G2v, func=AF.Exp),
                     reads=[bG2], writes=[bBCR])
                P.op("dve", lambda h: h.tensor_tensor(out=dd[:], in0=APv[:], in1=AE[:], op=ALU.subtract),
                     reads=[bAPv, bAE], writes=[bdd])
                P.op("act", lambda h: h.activation(out=BCR[:, :, 256], in_=dd[:], func=AF.Exp), reads=[bdd], writes=[bBCR])
                P.dma("sp", lambda h, d=d: h.dma_start(out=c.BCRD[d], in_=BCR[:]), reads=[bBCR], writes=[c.bBCRD[d]])
            for ch in range(NT):
                P.op("pe", lambda h, ch=ch: h.transpose(out=ps_c[:, ch, 0:24], in_=ROWS[0:24, ch * 128:(ch + 1) * 128],
                                                        identity=identf[0:24, 0:24]), reads=bROWS + [bid], writes=[bps_c])
            P.op("dve", lambda h: h.tensor_copy(out=COLS[:], in_=ps_c[:, :, 0:24]), reads=[bps_c], writes=[bCOLS])
        P.barrier()

        with ExitStack() as st:
            sb = lambda n, s, d: st.enter_context(nc.sbuf_tensor(n, s, d))
            ps = lambda n, s, d: st.enter_context(nc.psum_tensor(n, s, d))
            QKT = sb("c_QKT", [128, 8, S], BF16)
            bQKT = bufs(8, "cQKT")
            cw = sb("c_cw", [128, 8, 5], F32)
            cb = sb("c_cb", [128, 8], F32)
            bcw = Buf("cw")
            P.dma("sp", lambda h: h.dma_start(out=cw[:], in_=c.conv_w), writes=[bcw])
            P.dma("sp", lambda h: h.dma_start(out=cb[:], in_=c.conv_b), writes=[bcw])
            with ExitStack() as st2:
                sb2 = lambda n, s, d: st2.enter_context(nc.sbuf_tensor(n, s, d))
                xpad = [sb2("c2_xpad%d" % i, [128, S + 4], F32) for i in range(2)]
                acc = [sb2("c2_acc%d" % i, [128, S], F32) for i in range(2)]
                bxp, bacc = bufs(2, "xpad"), bufs(2, "acc")
                for i in range(2):
                    P.op("pool", lambda h, i=i: h.memset(xpad[i][:, 0:2], 0.0), writes=[bxp[i]])
                    P.op("pool", lambda h, i=i: h.memset(xpad[i][:, S + 2:S + 4], 0.0), writes=[bxp[i]])
                for cc in range(8):
                    k = cc % 2
                    P.dma("sp", lambda h, cc=cc, k=k: h.dma_start(out=xpad[k][:, 2:S + 2], in_=c.MQKT[cc * 128:(cc + 1) * 128, :]),
                          reads=c.bMQKT, writes=[bxp[k]])
                    P.op("dve", lambda h, cc=cc, k=k: h.tensor_scalar(out=acc[k][:], in0=xpad[k][:, 0:S], scalar1=cw[:, cc, 0:1],
                                                                      scalar2=cb[:, cc:cc + 1], op0=ALU.mult, op1=ALU.add),
                         reads=[bxp[k], bcw], writes=[bacc[k]])
                    for j in range(1, 5):
                        P.op("dve", lambda h, cc=cc, k=k, j=j: h.scalar_tensor_tensor(
                            out=acc[k][:], in0=xpad[k][:, j:j + S], scalar=cw[:, cc, j:j + 1], in1=acc[k][:],
                            op0=ALU.mult, op1=ALU.add), reads=[bxp[k], bcw, bacc[k]], writes=[bacc[k]])
                    if cc < 4:
                        P.op("act", lambda h, cc=cc, k=k: h.activation(out=QKT[:, cc, :], in_=acc[k][:], func=AF.Silu),
                             reads=[bacc[k]], writes=[bQKT[cc]])
                    else:
                        P.op("act", lambda h, cc=cc, k=k: h.activation(out=acc[k][:], in_=acc[k][:], func=AF.Silu),
                             reads=[bacc[k]], writes=[bacc[k]])
                        P.op("pool", lambda h, cc=cc, k=k: h.tensor_scalar(out=QKT[:, cc, :], in0=acc[k][:], scalar1=128.0 ** -0.5,
                                                                           scalar2=None, op0=ALU.mult),
                             reads=[bacc[k]], writes=[bQKT[cc]])
            P.barrier()

            MVs = sb("c_MVs", [128, NT, 4, 129], BF16)
            bMVs = bufs(NT, "cMV")
            bones = Buf("ones")
            SEL = sb("c_SEL", [4, 4, 128], F32)
            MSK = sb("c_MSK", [128, 2, 128], F32)
            identb = sb("c_identb", [128, 128], BF16)
            gmn = sb("c_gmn", [128, 512], F32)
            bconst = Buf("const")
            P.dma("sp", lambda h: h.dma_start(out=SEL[:], in_=c.sel), writes=[bconst])
            P.dma("sp", lambda h: h.dma_start(out=MSK[:], in_=c.msk), writes=[bconst])
            P.dma("sp", lambda h: h.dma_start(out=identb[:], in_=c.identb), writes=[bconst])
            P.dma("sp", lambda h: h.dma_start(out=gmn[:], in_=c.gmn), writes=[bconst])
            P.op("pool", lambda h: h.memset(MVs[:, :, :, 128:129], 1.0), writes=[bones])
            for i in range(NT):
                P.dma("sp" if i % 2 == 0 else "act", lambda h, i=i: h.dma_start(
                    out=MVs[:, i, :, 0:128], in_=c.MV[i * 128:(i + 1) * 128, :].rearrange("p (a b) -> p a b", b=128)),
                    reads=[c.bMV[i]], writes=[bMVs[i]])
            Cst = [sb("c_C%d" % i, [128, 129], F32) for i in range(4)]
            Cbf = [sb("c_Cbf%d" % i, [128, 129], BF16) for i in range(4)]
            bC, bCbf = bufs(4, "C"), bufs(4, "Cbf")
            bcr = [sb("c_bcr%d" % i, [4, 257], F32) for i in range(2)]
            bbcr = bufs(2, "bcr")
            Gt = [sb("c_G%d" % i, [128, 128], F32) for i in range(2)]
            Wt = [sb("c_W%d" % i, [128, 128], F32) for i in range(2)]
            PT = [sb("c_PT%d" % i, [128, 128], BF16) for i in range(2)]
            qs = [sb("c_qs%d" % i, [128, 128], BF16) for i in range(2)]
            kw = [sb("c_kw%d" % i, [128, 128], BF16) for i in range(2)]
            dec = [sb("c_dec%d" % i, [128, 1], F32) for i in range(2)]
            dn = [sb("c_dn%d" % i, [128, 2], F32) for i in range(2)]
            bG, bW, bPT, bqs, bkw, bdec, bdn = [bufs(2, n) for n in ("G", "W", "PT", "qs", "kw", "dec", "dn")]
            hbuf = [sb("c_hbuf%d" % i, [128, 512], F32) for i in range(2)]
            bhbuf = bufs(2, "hbuf")
            hf = sb("c_hf", [128, 512], F32)
            sg = sb("c_sg", [128, 512], BF16)
            sq = sb("c_sq", [128, 512], F32)
            s4 = sb("c_s4", [128, 12], F32)
            hmo = [sb("c_hmo%d" % i, [128, 512], BF16) for i in range(2)]
            bhf, bsg, bsq, bs4 = Buf("hf"), Buf("sg"), Buf("sq"), Buf("s4")
            bhmo = bufs(2, "hmo")
            ps_bc = [ps("c_ps_bc%d" % i, [128, 512], F32) for i in range(2)]
            ps_st = [ps("c_ps_st%d" % i, [128, 128], F32) for i in range(2)]
            ps_n = [ps("c_ps_n%d" % i, [128, 512], F32) for i in range(2)]
            ps_kt = ps("c_ps_kt", [128, 128], BF16)
            ps_dc = ps("c_ps_dc", [128, 512], F32)
            bps_bc, bps_st, bps_n = bufs(2, "ps_bc"), bufs(2, "ps_st"), bufs(2, "ps_n")
            bps_kt, bps_dc = Buf("ps_kt"), Buf("ps_dc")

            it = 0
            for d in range(2):
                for hd in range(4):
                    P.op("pool", lambda h, hd=hd: h.memset(Cst[hd][:], 0.0), writes=[bC[hd]])
                    P.op("pool", lambda h, hd=hd: h.memset(Cbf[hd][:], 0.0), writes=[bCbf[hd]])
                order = list(range(NT)) if d == 0 else list(range(NT - 1, -1, -1))
                for ci, ch in enumerate(order):
                    kb = ci % 2
                    P.dma("sp", lambda h, d=d, ch=ch, kb=kb: h.dma_start(out=bcr[kb][:], in_=c.BCRD[d, :, ch, :]),
                          reads=[c.bBCRD[d]], writes=[bbcr[kb]])
                    hb = hbuf[ci % 2]
                    bhb = bhbuf[ci % 2]
                    tsl = slice(ch * 128, (ch + 1) * 128)
                    for hd in range(4):
                        k = it % 2
                        it += 1
                        a_col = COLS[:, ch, 12 * d + hd:12 * d + hd + 1]
                        wk_col = COLS[:, ch, 12 * d + 4 + hd:12 * d + 5 + hd]
                        emt_col = COLS[:, ch, 12 * d + 8 + hd:12 * d + 9 + hd]
                        P.op("pe", lambda h, k=k, kb=kb, hd=hd: h.matmul(ps_bc[k][:, 0:257], lhsT=SEL[:, hd, :], rhs=bcr[kb][:],
                                                                         start=True, stop=True),
                             reads=[bconst, bbcr[kb]], writes=[bps_bc[k]])
                        P.op("pe", lambda h, k=k, hd=hd, tsl=tsl: h.matmul(ps_st[k][:], lhsT=QKT[:, 4 + hd, tsl], rhs=QKT[:, hd, tsl],
                                                                          start=True, stop=True),
                             reads=[bQKT[4 + hd], bQKT[hd]], writes=[bps_st[k]])
                        P.op("dve", lambda h, k=k, d=d: h.tensor_tensor(out=Gt[k][:], in0=ps_bc[k][:, 0:128], in1=MSK[:, d, :],
                                                                       op=ALU.add), reads=[bps_bc[k], bconst], writes=[bG[k]])
                        P.op("act", lambda h, k=k, a_col=a_col: h.activation(out=Wt[k][:], in_=Gt[k][:], func=AF.Exp, bias=a_col),
                             reads=[bG[k], bCOLS], writes=[bW[k]])
                        P.op("dve", lambda h, k=k: h.tensor_tensor(out=PT[k][:], in0=ps_st[k][:], in1=Wt[k][:], op=ALU.mult),
                             reads=[bps_st[k], bW[k]], writes=[bPT[k]])
                        P.op("dve", lambda h, k=k, hd=hd, tsl=tsl: h.tensor_tensor(out=qs[k][:], in0=ps_bc[k][:, 128:256],
                                                                                  in1=QKT[:, hd, tsl], op=ALU.mult),
                             reads=[bps_bc[k], bQKT[hd]], writes=[bqs[k]])
                        P.op("act", lambda h, k=k: h.copy(out=dec[k][:], in_=ps_bc[k][:, 256:257]),
                             reads=[bps_bc[k]], writes=[bdec[k]])
                        P.op("pe", lambda h, k=k, ch=ch, hd=hd: h.matmul(ps_n[k][:, 0:129], lhsT=PT[k][:], rhs=MVs[:, ch, hd, :],
                                                                         start=True, stop=False),
                             reads=[bPT[k], bMVs[ch], bones], writes=[bps_n[k]])
                        P.op("pe", lambda h, k=k, hd=hd: h.matmul(ps_n[k][:, 0:129], lhsT=qs[k][:], rhs=Cbf[hd][:],
                                                                  start=False, stop=True),
                             reads=[bqs[k], bCbf[hd]], writes=[bps_n[k]])
                        P.op("pe", lambda h, hd=hd, tsl=tsl: h.transpose(out=ps_kt[:], in_=QKT[:, 4 + hd, tsl], identity=identb[:]),
                             reads=[bQKT[4 + hd], bconst], writes=[bps_kt])
                        P.op("act", lambda h, k=k, wk_col=wk_col: h.activation(out=kw[k][:], in_=ps_kt[:], func=AF.Copy, scale=wk_col),
                             reads=[bps_kt, bCOLS], writes=[bkw[k]])
                        P.op("pe", lambda h, k=k, ch=ch, hd=hd: h.matmul(ps_dc[:, 0:129], lhsT=kw[k][:], rhs=MVs[:, ch, hd, :],
                                                                         start=True, stop=True),
                             reads=[bkw[k], bMVs[ch], bones], writes=[bps_dc])
                        P.op("dve", lambda h, k=k, hd=hd: h.scalar_tensor_tensor(out=Cst[hd][:], in0=Cst[hd][:], scalar=dec[k][:],
                                                                                 in1=ps_dc[:, 0:129], op0=ALU.mult, op1=ALU.add),
                             reads=[bC[hd], bdec[k], bps_dc], writes=[bC[hd]])
                        P.op("act", lambda h, hd=hd: h.copy(out=Cbf[hd][:], in_=Cst[hd][:]), reads=[bC[hd]], writes=[bCbf[hd]])
                        P.op("act", lambda h, k=k: h.activation(out=dn[k][:, 1:2], in_=ps_n[k][:, 128:129], func=AF.Abs),
                             reads=[bps_n[k]], writes=[bdn[k]])
                        P.op("dve", lambda h, k=k, emt_col=emt_col: h.tensor_scalar(out=dn[k][:, 0:1], in0=dn[k][:, 1:2],
                                                                                    scalar1=emt_col, scalar2=None, op0=ALU.max),
                             reads=[bdn[k], bCOLS], writes=[bdn[k]])
                        P.op("dve", lambda h, k=k: h.reciprocal(out=dn[k][:, 1:2], in_=dn[k][:, 0:1]), reads=[bdn[k]], writes=[bdn[k]])
                        P.op("dve", lambda h, k=k, hd=hd, hb=hb: h.tensor_scalar(out=hb[:, hd * 128:(hd + 1) * 128], in0=ps_n[k][:, 0:128],
                                                                                 scalar1=dn[k][:, 1:2], scalar2=None, op0=ALU.mult),
                             reads=[bps_n[k], bdn[k]], writes=[bhb])
                    if d == 0:
                        P.dma("sp", lambda h, ch=ch, hb=hb: h.dma_start(out=c.HF[ch * 128:(ch + 1) * 128, :], in_=hb[:]),
                              reads=[bhb], writes=[c.bHF[ch]])
                    else:
                        o = ci % 2
                        P.dma("sp", lambda h, ch=ch: h.dma_start(out=hf[:], in_=c.HF[ch * 128:(ch + 1) * 128, :]),
                              reads=[c.bHF[ch]], writes=[bhf])
                        P.dma("sp", lambda h, ch=ch: h.dma_start(out=sg[:], in_=c.SIGO[ch * 128:(ch + 1) * 128, :]),
                              reads=[c.bSIGO[ch]], writes=[bsg])
                        P.op("pool", lambda h, hb=hb: h.tensor_tensor(out=hf[:], in0=hf[:], in1=hb[:], op=ALU.add),
                             reads=[bhb, bhf], writes=[bhf])
                        P.op("act", lambda h: h.activation(out=sq[:], in_=hf[:], func=AF.Square), reads=[bhf], writes=[bsq])
                        P.op("dve", lambda h: h.tensor_reduce(out=s4[:, 0:4], in_=sq[:].rearrange("p (a b) -> p a b", b=128),
                                                              axis=AX.X, op=ALU.add), reads=[bsq], writes=[bs4])
                        P.op("act", lambda h: h.activation(out=s4[:, 4:8], in_=s4[:, 0:4], func=AF.Sqrt, scale=1.0 / 128, bias=EPS),
                             reads=[bs4], writes=[bs4])
                        P.op("dve", lambda h: h.reciprocal(out=s4[:, 8:12], in_=s4[:, 4:8]), reads=[bs4], writes=[bs4])
                        P.op("dve", lambda h: h.tensor_tensor(out=sq[:].rearrange("p (a b) -> p a b", b=128),
                                                              in0=hf[:].rearrange("p (a b) -> p a b", b=128),
                                                              in1=s4[:, 8:12].unsqueeze(2).to_broadcast([128, 4, 128]), op=ALU.mult),
                             reads=[bhf, bs4, bsq], writes=[bsq])
                        P.op("pool", lambda h: h.tensor_tensor(out=sq[:], in0=sq[:], in1=gmn[:], op=ALU.mult),
                             reads=[bsq, bconst], writes=[bsq])
                        P.op("pool", lambda h, o=o: h.tensor_tensor(out=hmo[o][:], in0=sq[:], in1=sg[:], op=ALU.mult),
                             reads=[bsq, bsg], writes=[bhmo[o]])
                        P.dma("sp", lambda h, ch=ch, o=o: h.dma_start(out=c.HM[ch * 128:(ch + 1) * 128, :], in_=hmo[o][:]),
                              reads=[bhmo[o]], writes=[c.bHM[ch]])
    P.barrier()


def phase_t(c):
    nc, P = c.nc, c.P
    JB = 4
    with ExitStack() as st:
        sb = lambda n, s, d: st.enter_context(nc.sbuf_tensor(n, s, d))
        tin = [sb("t_in%d" % i, [128, JB * 2 * D], F32) for i in range(2)]
        tout = [sb("t_out%d" % i, [128, JB * 2 * D], BF16) for i in range(2)]
        bin_, bout = bufs(2, "tin"), bufs(2, "tout")
        src = c.peer_uv.rearrange("(p j) d -> p (j d)", p=128)
        dst = c.UVB.rearrange("(p j) d -> p (j d)", p=128)
        W = JB * 2 * D
        third = W // 4
        for stp in range(128 // JB):
            k = stp % 2
            P.dma("sp", lambda h, stp=stp, k=k: h.dma_start(out=tin[k][:], in_=src[:, stp * W:(stp + 1) * W]), writes=[bin_[k]])
            P.op("dve", lambda h, k=k: h.tensor_copy(out=tout[k][:, 0:2 * third], in_=tin[k][:, 0:2 * third]), reads=[bin_[k]], writes=[bout[k]])
            P.op("act", lambda h, k=k: h.copy(out=tout[k][:, 2 * third:3 * third], in_=tin[k][:, 2 * third:3 * third]), reads=[bin_[k]], writes=[bout[k]])
            P.op("pool", lambda h, k=k: h.tensor_copy(out=tout[k][:, 3 * third:W], in_=tin[k][:, 3 * third:W]), reads=[bin_[k]], writes=[bout[k]])
            P.dma("act", lambda h, stp=stp, k=k: h.dma_start(out=dst[:, stp * W:(stp + 1) * W], in_=tout[k][:]), reads=[bout[k]])
    P.barrier()


def phase_d(c):
    nc, P = c.nc, c.P
    with ExitStack() as st:
        sb = lambda n, s, d: st.enter_context(nc.sbuf_tensor(n, s, d))
        ps = lambda n, s, d: st.enter_context(nc.psum_tensor(n, s, d))
        Wo = sb("d_Wo", [128, 8, D], BF16)
        Wq = sb("d_Wq", [128, 8, 2048], BF16)
        SKT = sb("d_SKT", [128, 16, 128], BF16)
        gn2 = sb("d_gn2", [128, D], F32)
        identb = sb("d_identb", [128, 128], BF16)
        THR = sb("d_THR", [128, 16], F32)
        IOT = sb("d_IOT", [128, 16], F32)
        st_stage = ExitStack()
        stage = [st_stage.enter_context(nc.sbuf_tensor("d_stage%d" % i, [128, 2048], F32)) for i in range(2)]
        bconst = Buf("dconst")
        bst = bufs(2, "dst")
        bWo, bWq = bufs(8, "Wo"), bufs(8, "Wq")
        bSKT = Buf("SKT")
        for (t, src) in ((gn2, c.gn2), (identb, c.identb), (THR, c.thr), (IOT, c.iot)):
            P.dma("sp", lambda h, t=t, src=src: h.dma_start(out=t[:], in_=src), writes=[bconst])
        n = 0
        for kc in range(8):
            k = n % 2; n += 1
            P.dma("sp", lambda h, kc=kc, k=k: h.dma_start(out=stage[k][:, 0:D], in_=c.w_out[kc * 128:(kc + 1) * 128, :]), writes=[bst[k]])
            P.op("dve", lambda h, kc=kc, k=k: h.tensor_copy(out=Wo[:, kc, :], in_=stage[k][:, 0:D]), reads=[bst[k]], writes=[bWo[kc]])
        for kc in range(8):
            k = n % 2; n += 1
            P.dma("sp", lambda h, kc=kc, k=k: h.dma_start(out=stage[k][:], in_=c.w_q[kc * 128:(kc + 1) * 128, :]), writes=[bst[k]])
            P.op("dve", lambda h, kc=kc, k=k: h.tensor_copy(out=Wq[:, kc, :], in_=stage[k][:]), reads=[bst[k]], writes=[bWq[kc]])
        k = n % 2; n += 1
        P.dma("sp", lambda h, k=k: h.dma_start(out=stage[k][:].rearrange("p (a b) -> p a b", b=128), in_=c.skt), writes=[bst[k]])
        P.op("dve", lambda h, k=k: h.tensor_copy(out=SKT[:].rearrange("p a b -> p (a b)"), in_=stage[k][:]), reads=[bst[k]], writes=[bSKT])
        P.barrier()
        st_stage.close()

        cat = sb("d_cat", [128, D], BF16)
        catT = sb("d_catT", [128, 8, 128], BF16)
        xt = sb("d_xt", [128, D], F32)
        x1 = sb("d_x1", [128, D], F32)
        junk = sb("d_junk", [128, D], F32)
        ss = sb("d_ss", [128, 4], F32)
        xn2 = sb("d_xn2", [128, D], F32)
        xn2b = sb("d_xn2b", [128, D], BF16)
        xn2T = sb("d_xn2T", [128, 8, 128], BF16)
        qT = sb("d_qT", [128, 16, 128], BF16)
        sc = sb("d_sc", [128, 16, 128], F32)
        sc2 = sc
        m8 = sb("d_m8", [128, 16, 16], F32)
        i8 = sb("d_i8", [128, 16, 16], U32)
        i8f = sb("d_i8f", [128, 16, 16], F32)
        cand = sb("d_cand", [128, 8, 256], F32)
        cand2 = cand
        t8 = sb("d_t8", [128, 8, 16], F32)
        j8 = sb("d_j8", [128, 8, 16], U32)
        jf = sb("d_jf", [128, 128], F32)
        T4 = sb("d_T4", [128, 128, 16], F32)
        af = sb("d_af", [128, 128], F32)
        bf_ = sb("d_bf", [128, 128], F32)
        E1 = sb("d_E1", [128, 128], F32)
        E2 = sb("d_E2", [128, 128], F32)
        eidx = sb("d_eidx", [128, 128], I32)
        gts = sb("d_gts", [128, 8, 16], F32)
        g8 = sb("d_g8", [128, 16], F32)
        adot = sb("d_adot", [128, 128], F32)
        ga = sb("d_ga", [128, 128], F32)
        NG = 8
        GS = 4
        uv = [sb("d_uv%d" % i, [128, 2 * D], BF16) for i in range(NG)]
        identf = sb("d_identf", [128, 128], F32)
        dg = [sb("d_dg%d" % i, [128, 128], BF16) for i in range(4)]
        bdg = bufs(4, "dg")
        P.dma("sp", lambda h: h.dma_start(out=identf[:], in_=c.identf), writes=[bconst])
        yacc = sb("d_y", [128, D], F32)
        ps_t = ps("d_ps_t", [128, D], BF16)
        ps_o = [ps("d_ps_o%d" % i, [128, 512], F32) for i in range(2)]
        ps_q = [ps("d_ps_q%d" % i, [128, 4, 128], F32) for i in range(2)]
        ps_s = [ps("d_ps_s%d" % i, [128, 4, 128], F32) for i in range(2)]
        (bcat, bcatT, bxt, bx1, bjunk, bss, bxn2, bxn2b, bxn2T, bqT, bsc, bsc2, bm8, bi8, bi8f, bcand, bcand2, bt8, bj8,
         bjf, bT4, baf, bbf, bE1, bE2, beidx, bgts, bg8, badot, bga, by, bps_t) = [Buf(n_) for n_ in (
            "cat", "catT", "xt", "x1", "junk", "ss", "xn2", "xn2b", "xn2T", "qT", "sc", "sc2", "m8", "i8", "i8f", "cand", "cand2",
            "t8", "j8", "jf", "T4", "af", "bf", "E1", "E2", "eidx", "gts", "g8", "adot", "ga", "y", "ps_t")]
        buv = bufs(NG, "uv")
        bps_o, bps_q, bps_s = bufs(2, "ps_o"), bufs(2, "ps_q"), bufs(2, "ps_s")

        for i in range(NT):
            rows = slice(i * 128, (i + 1) * 128)
            P.dma("sp", lambda h, rows=rows: h.dma_start(out=cat[:, 0:512], in_=c.AOUT[rows, :]), reads=[c.bAOUT[i]], writes=[bcat])
            P.dma("sp", lambda h, rows=rows: h.dma_start(out=cat[:, 512:1024], in_=c.HM[rows, :]), reads=[c.bHM[i]], writes=[bcat])
            P.dma("sp", lambda h, rows=rows: h.dma_start(out=xt[:], in_=c.x[rows, :]), writes=[bxt])
            for kc in range(8):
                P.op("pe", lambda h, kc=kc: h.transpose(out=ps_t[:, kc * 128:(kc + 1) * 128], in_=cat[:, kc * 128:(kc + 1) * 128],
                                                        identity=identb[:]), reads=[bcat, bconst], writes=[bps_t])
            P.op("act", lambda h: h.copy(out=catT[:].rearrange("p k t -> p (k t)"), in_=ps_t[:]), reads=[bps_t], writes=[bcatT])
            for g in range(2):
                for kc in range(8):
                    P.op("pe", lambda h, g=g, kc=kc: h.matmul(ps_o[g][:], lhsT=catT[:, kc, :], rhs=Wo[:, kc, g * 512:(g + 1) * 512],
                                                             start=(kc == 0), stop=(kc == 7)), reads=[bcatT, bWo[kc]], writes=[bps_o[g]])
                P.op("dve", lambda h, g=g: h.tensor_tensor(out=x1[:, g * 512:(g + 1) * 512], in0=ps_o[g][:], in1=xt[:, g * 512:(g + 1) * 512],
                                                          op=ALU.add), reads=[bps_o[g], bxt], writes=[bx1])
            P.op("act", lambda h: h.activation(out=junk[:], in_=x1[:], func=AF.Square, accum_out=ss[:, 0:1]), reads=[bx1], writes=[bjunk, bss])
            P.op("act", lambda h: h.activation(out=ss[:, 1:2], in_=ss[:, 0:1], func=AF.Sqrt, scale=1.0 / D, bias=EPS), reads=[bss], writes=[bss])
            P.op("dve", lambda h: h.reciprocal(out=ss[:, 2:3], in_=ss[:, 1:2]), reads=[bss], writes=[bss])
            P.op("dve", lambda h: h.scalar_tensor_tensor(out=xn2[:], in0=x1[:], scalar=ss[:, 2:3], in1=gn2[:], op0=ALU.mult, op1=ALU.mult),
                 reads=[bx1, bss, bconst], writes=[bxn2])
            P.op("act", lambda h: h.copy(out=xn2b[:], in_=xn2[:]), reads=[bxn2], writes=[bxn2b])
            for kc in range(8):
                P.op("pe", lambda h, kc=kc: h.transpose(out=ps_t[:, kc * 128:(kc + 1) * 128], in_=xn2b[:, kc * 128:(kc + 1) * 128],
                                                        identity=identb[:]), reads=[bxn2b, bconst], writes=[bps_t])
            P.op("act", lambda h: h.copy(out=xn2T[:].rearrange("p k t -> p (k t)"), in_=ps_t[:]), reads=[bps_t], writes=[bxn2T])
            for qg in range(4):
                k = qg % 2
                for cc in range(4):
                    hp = qg * 4 + cc
                    for kc in range(8):
                        P.op("pe", lambda h, k=k, cc=cc, hp=hp, kc=kc: h.matmul(ps_q[k][:, cc, :], lhsT=Wq[:, kc, hp * 128:(hp + 1) * 128],
                                                                              rhs=xn2T[:, kc, :], start=(kc == 0), stop=(kc == 7)),
                             reads=[bWq[kc], bxn2T], writes=[bps_q[k]])
                P.op("act", lambda h, k=k, qg=qg: h.copy(out=qT[:, qg * 4:(qg + 1) * 4, :], in_=ps_q[k][:]), reads=[bps_q[k]], writes=[bqT])
            for qg in range(4):
                k = qg % 2
                for cc in range(4):
                    hp = qg * 4 + cc
                    P.op("pe", lambda h, k=k, cc=cc, hp=hp: h.matmul(ps_s[k][:, cc, :], lhsT=qT[:, hp, :], rhs=SKT[:, hp, :],
                                                                   start=True, stop=True), reads=[bqT, bSKT], writes=[bps_s[k]])
                P.op("act", lambda h, k=k, qg=qg: h.copy(out=sc[:, qg * 4:(qg + 1) * 4, :], in_=ps_s[k][:]), reads=[bps_s[k]], writes=[bsc])
            for g in range(16):
                P.op("dve", lambda h, g=g: h.max(out=m8[:, g, 0:8], in_=sc[:, g, :]), reads=[bsc], writes=[bm8])
                P.op("dve", lambda h, g=g: h.max_index(out=i8[:, g, 0:8], in_max=m8[:, g, 0:8], in_values=sc[:, g, :]),
                     reads=[bsc, bm8], writes=[bi8])
                P.op("dve", lambda h, g=g: h.match_replace(out=sc2[:, g, :], in_to_replace=m8[:, g, 0:8], in_values=sc[:, g, :],
                                                           imm_value=-1e30), reads=[bsc, bm8], writes=[bsc2])
                P.op("dve", lambda h, g=g: h.max(out=m8[:, g, 8:16], in_=sc2[:, g, :]), reads=[bsc2], writes=[bm8])
                P.op("dve", lambda h, g=g: h.max_index(out=i8[:, g, 8:16], in_max=m8[:, g, 8:16], in_values=sc2[:, g, :]),
                     reads=[bsc2, bm8], writes=[bi8])
            m8v = m8[:].rearrange("p (a b) k -> p a b k", b=2)
            P.op("dve", lambda h, m8v=m8v: h.tensor_tensor(
                out=cand[:].rearrange("p a (x y) -> p a x y", y=16),
                in0=m8v[:, :, 0, :].unsqueeze(3).to_broadcast([128, 8, 16, 16]),
                in1=m8v[:, :, 1, :].unsqueeze(2).to_broadcast([128, 8, 16, 16]), op=ALU.add), reads=[bm8], writes=[bcand])
            for hd in range(8):
                P.op("dve", lambda h, hd=hd: h.max(out=t8[:, hd, 0:8], in_=cand[:, hd, :]), reads=[bcand], writes=[bt8])
                P.op("dve", lambda h, hd=hd: h.max_index(out=j8[:, hd, 0:8], in_max=t8[:, hd, 0:8], in_values=cand[:, hd, :]),
                     reads=[bcand, bt8], writes=[bj8])
                P.op("dve", lambda h, hd=hd: h.match_replace(out=cand2[:, hd, :], in_to_replace=t8[:, hd, 0:8], in_values=cand[:, hd, :],
                                                             imm_value=-1e30), reads=[bcand, bt8], writes=[bcand2])
                P.op("dve", lambda h, hd=hd: h.max(out=t8[:, hd, 8:16], in_=cand2[:, hd, :]), reads=[bcand2], writes=[bt8])
                P.op("dve", lambda h, hd=hd: h.max_index(out=j8[:, hd, 8:16], in_max=t8[:, hd, 8:16], in_values=cand2[:, hd, :]),
                     reads=[bcand2, bt8], writes=[bj8])
            P.op("dve", lambda h: h.tensor_copy(out=i8f[:], in_=i8[:]), reads=[bi8], writes=[bi8f])
            P.op("dve", lambda h: h.tensor_copy(out=jf[:], in_=j8[:].rearrange("p a k -> p (a k)")), reads=[bj8], writes=[bjf])
            P.op("dve", lambda h: h.tensor_tensor(out=T4[:], in0=jf[:].unsqueeze(2).to_broadcast([128, 128, 16]),
                                                  in1=THR[:].unsqueeze(1).to_broadcast([128, 128, 16]), op=ALU.is_ge),
                 reads=[bjf, bconst], writes=[bT4])
            P.op("dve", lambda h: h.tensor_reduce(out=af[:], in_=T4[:], axis=AX.X, op=ALU.add), reads=[bT4], writes=[baf])
            P.op("dve", lambda h: h.scalar_tensor_tensor(out=bf_[:], in0=af[:], scalar=-16.0, in1=jf[:], op0=ALU.mult, op1=ALU.add),
                 reads=[baf, bjf], writes=[bbf])
            i8v = i8f[:].rearrange("p (a b) k -> p a b k", b=2)
            for side, (idxt, Et, bE) in enumerate(((af, E1, bE1), (bf_, E2, bE2))):
                P.op("dve", lambda h, idxt=idxt: h.tensor_tensor(out=T4[:], in0=idxt[:].unsqueeze(2).to_broadcast([128, 128, 16]),
                                                                in1=IOT[:].unsqueeze(1).to_broadcast([128, 128, 16]), op=ALU.is_equal),
                     reads=[baf, bbf, bconst, bT4], writes=[bT4])
                P.op("dve", lambda h, side=side, i8v=i8v: h.tensor_tensor(
                    out=T4[:].rearrange("p (a k) x -> p a k x", k=16), in0=T4[:].rearrange("p (a k) x -> p a k x", k=16),
                    in1=i8v[:, :, side, :].unsqueeze(2).to_broadcast([128, 8, 16, 16]), op=ALU.mult),
                    reads=[bT4, bi8f], writes=[bT4])
                P.op("dve", lambda h, Et=Et: h.tensor_reduce(out=Et[:], in_=T4[:], axis=AX.X, op=ALU.add), reads=[bT4], writes=[bE])
            P.op("dve", lambda h: h.scalar_tensor_tensor(out=E1[:], in0=E1[:], scalar=128.0, in1=E2[:], op0=ALU.mult, op1=ALU.add),
                 reads=[bE1, bE2], writes=[bE1])
            P.op("dve", lambda h: h.tensor_copy(out=eidx[:], in_=E1[:]), reads=[bE1], writes=[beidx])
            P.op("dve", lambda h: h.tensor_tensor(out=gts[:], in0=t8[:], in1=t8[:, :, 0:1].to_broadcast([128, 8, 16]), op=ALU.subtract),
                 reads=[bt8], writes=[bgts])
            P.op("act", lambda h: h.activation(out=gts[:], in_=gts[:], func=AF.Exp), reads=[bgts], writes=[bgts])
            P.op("dve", lambda h: h.tensor_reduce(out=g8[:, 0:8], in_=gts[:], axis=AX.X, op=ALU.add), reads=[bgts], writes=[bg8])
            P.op("dve", lambda h: h.reciprocal(out=g8[:, 8:16], in_=g8[:, 0:8]), reads=[bg8], writes=[bg8])
            P.op("dve", lambda h: h.tensor_tensor(out=gts[:], in0=gts[:], in1=g8[:, 8:16].unsqueeze(2).to_broadcast([128, 8, 16]),
                                                  op=ALU.mult), reads=[bgts, bg8], writes=[bgts])
            if "EIDX" in c.dbg:
                P.dma("sp", lambda h, rows=rows: h.dma_start(out=c.EIDX[rows, :], in_=eidx[:]), reads=[beidx], writes=[c.bOUT[i]])
                P.dma("sp", lambda h, rows=rows: h.dma_start(out=c.GTS[rows, :], in_=gts[:].rearrange("p a k -> p (a k)")), reads=[bgts], writes=[c.bOUT[i]])
                P.dma("sp", lambda h, rows=rows: h.dma_start(out=c.X1[rows, :], in_=x1[:]), reads=[bx1], writes=[c.bOUT[i]])
            if "noexp" in c.dbg or i >= c.nexp:
                P.dma("sp", lambda h, rows=rows: h.dma_start(out=c.out[rows, :], in_=x1[:]), reads=[bx1], writes=[c.bOUT[i]])
                continue
            gts_f = gts[:].rearrange("p a k -> p (a k)")
            for grp in range(128 // GS):
                sls = list(range(grp * GS, (grp + 1) * GS))
                for sl in sls:
                    k = sl % NG
                    P.dma("pool", lambda h, sl=sl, k=k: h.indirect_dma_start(
                        out=uv[k][:], out_offset=None, in_=c.UVB,
                        in_offset=bass.IndirectOffsetOnAxis(ap=eidx[:, sl:sl + 1], axis=0)), reads=[beidx], writes=[buv[k]])
                    P.op("dve", lambda h, sl=sl, k=k: h.scalar_tensor_tensor(out=junk[:], in0=uv[k][:, 0:D], scalar=1.0, in1=xn2[:],
                                                                            op0=ALU.mult, op1=ALU.mult, accum_out=adot[:, sl:sl + 1]),
                         reads=[buv[k], bxn2], writes=[bjunk, badot])
                g0, g1 = sls[0], sls[-1] + 1
                P.op("act", lambda h, g0=g0, g1=g1: h.activation(out=ga[:, g0:g1], in_=adot[:, g0:g1], func=AF.Gelu),
                     reads=[badot], writes=[bga])
                P.op("dve", lambda h, g0=g0, g1=g1: h.tensor_tensor(out=ga[:, g0:g1], in0=ga[:, g0:g1], in1=gts_f[:, g0:g1], op=ALU.mult),
                     reads=[bga, bgts], writes=[bga])
                for sl in sls:
                    k = sl % NG
                    kd = sl % 4
                    P.op("act", lambda h, sl=sl, kd=kd: h.activation(out=dg[kd][:], in_=identf[:], func=AF.Copy, scale=ga[:, sl:sl + 1]),
                         reads=[bga, bconst], writes=[bdg[kd]])
                    for g in range(2):
                        P.op("pe", lambda h, sl=sl, k=k, kd=kd, g=g: h.matmul(ps_o[g][:], lhsT=dg[kd][:],
                                                                             rhs=uv[k][:, D + g * 512:D + (g + 1) * 512],
                                                                             start=(sl == 0), stop=(sl == 127)),
                             reads=[bdg[kd], buv[k]], writes=[bps_o[g]])
            for g in range(2):
                P.op("dve", lambda h, g=g: h.tensor_tensor(out=yacc[:, g * 512:(g + 1) * 512], in0=ps_o[g][:], in1=x1[:, g * 512:(g + 1) * 512],
                                                          op=ALU.add), reads=[bps_o[g], bx1], writes=[by])
            P.dma("sp", lambda h, rows=rows: h.dma_start(out=c.out[rows, :], in_=yacc[:]), reads=[by], writes=[c.bOUT[i]])
    P.barrier()


def build(dbg=(), phases="abcd"):
    nc = bass.Bass("TRN2", target_bir_lowering=False)
    c = Ctx()
    c.nc = nc
    ext_in = lambda n, s, d: nc.dram_tensor(n, s, d, kind="ExternalInput").ap()

    def scratch(n, s, d):
        kind = "ExternalOutput" if n in dbg else "Internal"
        return nc.dram_tensor(n, s, d, kind=kind).ap()

    c.x = ext_in("x", [S, D], F32)
    c.w_in = ext_in("w_in", [D, INW], F32)
    c.norm1_w = ext_in("norm1_w", [128, 8], F32)
    c.identb = ext_in("identb", [128, 128], BF16)
    c.gq = ext_in("gq", [128, 64], F32)
    c.gk = ext_in("gk", [128, 64], F32)
    c.rpbg = ext_in("rpbg", [128, 8, 896], F32)
    c.mask_i = ext_in("mask_i", [128, 896], F32)
    c.mask_a = ext_in("mask_a", [128, 896], F32)
    c.gao = ext_in("gao", [128, 512], F32)
    c.gate_b = ext_in("gate_b", [4, 4], F32)
    c.identf = ext_in("identf", [128, 128], F32)
    c.conv_w = ext_in("conv_w", [128, 8, 5], F32)
    c.conv_b = ext_in("conv_b", [128, 8], F32)
    c.sel = ext_in("sel", [4, 4, 128], F32)
    c.msk = ext_in("msk", [128, 2, 128], F32)
    c.gmn = ext_in("gmn", [128, 512], F32)
    c.w_out = ext_in("w_out", [D, D], F32)
    c.w_q = ext_in("w_q", [D, 2048], F32)
    c.skt = ext_in("skt", [128, 16, 128], F32)
    c.gn2 = ext_in("gn2", [128, D], F32)
    c.thr = ext_in("thr", [128, 16], F32)
    c.iot = ext_in("iot", [128, 16], F32)
    c.peer_uv = ext_in("peer_uv", [16384, 2 * D], F32)
    c.UVB = scratch("UVB", [16384, 2 * D], BF16)
    c.out = nc.dram_tensor("out", [S, D], F32, kind="ExternalOutput").ap()
    c.bOUT = bufs(NT, "OUT")
    c.dbg = dbg
    c.nexp = NT
    for d_ in dbg:
        if d_.startswith("exp"):
            c.nexp = int(d_[3:])
    if "EIDX" in dbg:
        c.EIDX = scratch("EIDX", [S, 128], I32)
        c.GTS = scratch("GTS", [S, 128], F32)
        c.X1 = scratch("X1", [S, D], F32)

    c.QT = scratch("QT", [4, 128, S], BF16)
    c.KT = scratch("KT", [4, 128, S], BF16)
    c.V = scratch("V", [S, 512], BF16)
    c.MV = scratch("MV", [S, 512], BF16)
    c.SIGO = scratch("SIGO", [S, 512], BF16)
    c.MQKT = scratch("MQKT", [1024, S], F32)
    c.GT = scratch("GT", [4, 4, S], F32)
    c.AOUT = scratch("AOUT", [S, 512], BF16)
    c.BCRD = scratch("BCRD", [2, 4, NT, 257], F32)
    c.bBCRD = bufs(2, "BCRD")
    c.HF = scratch("HF", [S, 512], F32)
    c.bHF = bufs(NT, "HF")
    c.HM = scratch("HM", [S, 512], BF16)
    c.bHM = bufs(NT, "HM")
    c.bAOUT = bufs(NT, "AOUT")
    c.bQKT = [bufs(NT, "QT"), bufs(NT, "KT")]
    c.bV, c.bMV, c.bSIGO, c.bMQKT, c.bGT = (bufs(NT, n) for n in ("V", "MV", "SIGO", "MQKT", "GT"))

    with ExitStack() as st:
        c.P = Prog(nc, st)
        if "a" in phases:
            phase_a(c)
        if "b" in phases:
            phase_b(c)
        if "c" in phases:
            phase_c(c)
        if "d" in phases:
            phase_t(c)
            phase_d(c)
        c.P.emit()
    return nc


def host_inputs(inputs, b):
    f32 = np.float32
    m = {}
    m["x"] = np.ascontiguousarray(inputs["x"][b], dtype=f32)
    m["w_in"] = np.ascontiguousarray(inputs["w_in"][0], dtype=f32)
    m["norm1_w"] = np.ascontiguousarray(inputs["norm1_w"][0].reshape(8, 128).T, dtype=f32)
    m["identb"] = np.eye(128, dtype=f32).astype(ml_dtypes.bfloat16)
    m["gq"] = np.ascontiguousarray(np.broadcast_to(inputs["q_norm_w"][0][None, :], (128, 64)), dtype=f32)
    m["gk"] = np.ascontiguousarray(np.broadcast_to(inputs["k_norm_w"][0][None, :], (128, 64)), dtype=f32)
    p = np.arange(128); kr = p // 64; kc = p % 64
    col = np.arange(128); rq = col // 64; cc = col % 64
    dt = np.arange(-3, 4)
    drow = 2 * dt[None, :, None] + kr[:, None, None] - rq[None, None, :]
    dcol = kc[:, None, None] - cc[None, None, :] + 0 * dt[None, :, None]
    rpb = inputs["attn_rpb"][0]
    g = rpb[:, np.clip(drow + 7, 0, 14), np.clip(dcol + 15, 0, 30)]
    m["rpbg"] = np.ascontiguousarray(g.transpose(1, 0, 2, 3).reshape(128, 8, 896), dtype=f32)
    cs = np.clip(cc - 8, 0, 48)
    colvalid = (kc[:, None, None] >= cs[None, None, :]) & (kc[:, None, None] < cs[None, None, :] + 16)
    colvalid = colvalid & (dt[None, :, None] > -100)
    m["mask_a"] = np.where(colvalid & (np.abs(drow) <= 7), 0.0, NEG).astype(f32).reshape(128, 896)
    m["mask_i"] = np.where(colvalid & (drow >= -4) & (drow <= 3), 0.0, NEG).astype(f32).reshape(128, 896)
    m["gate_b"] = np.ascontiguousarray(inputs["mlstm_gate_b"][0].T, dtype=f32)
    m["identf"] = np.eye(128, dtype=f32)
    m["conv_w"] = np.ascontiguousarray(inputs["mlstm_conv_w"][0].reshape(5, 8, 128).transpose(2, 1, 0), dtype=f32)
    m["conv_b"] = np.ascontiguousarray(inputs["mlstm_conv_b"][0].reshape(8, 128).T, dtype=f32)
    sel = np.zeros((4, 4, 128), f32)
    for hh in range(4):
        sel[hh, hh, :] = 1.0
    m["sel"] = sel
    ii = np.arange(128)
    msk = np.zeros((128, 2, 128), f32)
    msk[:, 0, :] = np.where(ii[:, None] <= ii[None, :], 0.0, NEG)
    msk[:, 1, :] = np.where(ii[:, None] >= ii[None, :], 0.0, NEG)
    m["msk"] = msk
    m["gmn"] = np.ascontiguousarray(np.broadcast_to(inputs["mlstm_norm_w"][0][None, :], (128, 512)), dtype=f32)
    m["w_out"] = np.ascontiguousarray(inputs["w_out"][0], dtype=f32)
    m["w_q"] = np.ascontiguousarray(inputs["peer_w_q"][0], dtype=f32)
    m["skt"] = np.ascontiguousarray(inputs["peer_sub_keys"][0].reshape(16, 128, 128).transpose(2, 0, 1), dtype=f32)
    m["gn2"] = np.ascontiguousarray(np.broadcast_to(inputs["norm2_w"][0][None, :], (128, D)), dtype=f32)
    thr = (np.arange(16, dtype=f32) + 1.0) * 16.0
    thr[15] = 1e9
    m["thr"] = np.ascontiguousarray(np.broadcast_to(thr[None, :], (128, 16)), dtype=f32)
    m["iot"] = np.ascontiguousarray(np.broadcast_to(np.arange(16, dtype=f32)[None, :], (128, 16)), dtype=f32)
    m["peer_uv"] = np.ascontiguousarray(np.concatenate([inputs["peer_u"][0], inputs["peer_v"][0]], axis=1), dtype=f32)
    m["gao"] = np.ascontiguousarray(np.broadcast_to(inputs["attn_out_norm_w"][0][None, :], (128, 512)), dtype=f32)
    return m


def kernel(**inputs):
    nc = build()
    in_maps = [host_inputs(inputs, b) for b in range(8)]
    res = run_bass_kernel_spmd(nc, in_maps, core_ids=list(range(8)))
    return np.stack([r["out"] for r in res.results], axis=0).astype(np.float32)
```

```python
import numpy as np
import ml_dtypes
import concourse.bass as bass
import concourse.mybir as mybir
from concourse.bass_utils import run_bass_kernel_spmd
from contextlib import ExitStack

F32 = mybir.dt.float32
BF16 = mybir.dt.bfloat16
I32 = mybir.dt.int32
U32 = mybir.dt.uint32
ALU = mybir.AluOpType
AF = mybir.ActivationFunctionType
AX = mybir.AxisListType

S = 4096
D = 1024
NT = 32
INW = 3600
EPS = 1e-6
NEG = -30000.0


class Buf:
    __slots__ = ("name", "w", "r")

    def __init__(self, name=""):
        self.name = name
        self.w = {}
        self.r = {}


class Prog:
    ENG = ("pe", "act", "dve", "pool", "sp")
    NRINGS = {"sp": 12, "act": 6, "pool": 48}

    def __init__(self, nc, stack):
        self.nc = nc
        self.sem = {e: stack.enter_context(nc.semaphore("s_" + e)) for e in self.ENG}
        self.ops = {e: [] for e in self.ENG}
        self.seen = {e: {} for e in self.ENG}
        self.ring = {}
        self.ring_i = {}
        self.ring_tok = {}
        for q in ("sp", "act", "pool"):
            self.ring[q] = [stack.enter_context(nc.semaphore("r_%s%d" % (q, i)))
                            for i in range(self.NRINGS[q])]
            self.ring_i[q] = 0
            self.ring_tok[q] = [None] * self.NRINGS[q]

    @staticmethod
    def _key(tok):
        return ("c", tok[1]) if tok[0] == "c" else ("d", id(tok[1]))

    @staticmethod
    def _val(tok):
        return tok[2]

    def _need(self, eng, tok, waits, same_ok=False):
        if tok[0] == "c" and tok[1] == eng and (eng == "pe" or same_ok):
            return
        k = self._key(tok)
        if self.seen[eng].get(k, -1) >= tok[2]:
            return
        self.seen[eng][k] = tok[2]
        waits.append(tok)

    def _deps(self, eng, reads, writes):
        waits = []
        for b in reads:
            for t in b.w.values():
                self._need(eng, t, waits)
        for b in writes:
            for t in b.w.values():
                self._need(eng, t, waits)
            for t in b.r.values():
                self._need(eng, t, waits)
        return waits

    def _commit(self, tok, reads, writes):
        k = self._key(tok)
        for b in reads:
            b.r[k] = tok
        for b in writes:
            b.w = {k: tok}
            b.r = {}

    def op(self, eng, fn, reads=(), writes=()):
        waits = self._deps(eng, reads, writes)
        tok = ("c", eng, len(self.ops[eng]))
        self.ops[eng].append([waits, fn, None])
        self._commit(tok, reads, writes)
        return tok

    def dma(self, q, fn, reads=(), writes=(), final=False):
        waits = self._deps(q, reads, writes)
        i = self.ring_i[q]
        nr = self.NRINGS[q]
        slot = i % nr
        prev = self.ring_tok[q][slot]
        if prev is not None:
            self._need(q, prev, waits)
        sem = self.ring[q][slot]
        val = 16 * (i // nr + 1)
        self.ring_i[q] = i + 1
        tok = ("d", sem, val, q)
        self.ring_tok[q][slot] = tok
        self.ops[q].append([waits, fn, (sem, 16)])
        self._commit(tok, reads, writes)
        return tok

    def _all_tokens(self):
        toks = []
        for e in ("pe", "act", "dve", "pool"):
            for idx in range(len(self.ops[e]) - 1, -1, -1):
                ent = self.ops[e][idx]
                if ent[1] is not None and ent[2] is None:
                    toks.append(("c", e, idx))
                    break
        for q in ("act", "pool", "sp"):
            for t in self.ring_tok[q]:
                if t is not None:
                    toks.append(t)
        return toks

    def barrier(self):
        toks = self._all_tokens()
        for e in self.ENG:
            waits = []
            for t in toks:
                k = self._key(t)
                if self.seen[e].get(k, -1) >= t[2]:
                    continue
                self.seen[e][k] = t[2]
                waits.append(t)
            if waits:
                self.ops[e].append([waits, None, None])

    def emit(self):
        nc = self.nc
        final_waits = []
        for t in self._all_tokens():
            self._need("sp", t, final_waits)
        signal = {e: set() for e in self.ENG}
        allw = [final_waits]
        for e in self.ENG:
            for ent in self.ops[e]:
                allw.append(ent[0])
        for ws in allw:
            for t in ws:
                if t[0] == "c":
                    signal[t[1]].add(t[2])
        semval = {e: {} for e in self.ENG}
        for e in self.ENG:
            n = 0
            for idx in sorted(signal[e]):
                n += 1
                semval[e][idx] = n

        def resolve(t):
            if t[0] == "c":
                return self.sem[t[1]], semval[t[1]][t[2]]
            return t[1], t[2]

        def run(e, handle, extra=None):
            for idx, (waits, fn, dinc) in enumerate(self.ops[e]):
                for t in waits:
                    sem, val = resolve(t)
                    handle.wait_ge(sem, val)
                if fn is not None:
                    ins = fn(handle)
                    if dinc is not None:
                        ins.then_inc(dinc[0], dinc[1])
                    elif idx in signal[e]:
                        ins.then_inc(self.sem[e], 1)
            if extra:
                for t in extra:
                    sem, val = resolve(t)
                    handle.wait_ge(sem, val)

        with nc.Block() as block:
            @block.tensor
            def _(h):
                run("pe", h)

            @block.scalar
            def _(h):
                run("act", h)

            @block.vector
            def _(h):
                run("dve", h)

            @block.gpsimd
            def _(h):
                run("pool", h)

            @block.sync
            def _(h):
                run("sp", h, final_waits)


class Ctx:
    pass


def bufs(n, name=""):
    return [Buf("%s%d" % (name, i)) for i in range(n)]


def phase_a(c):
    nc, P = c.nc, c.P
    with ExitStack() as st:
        sb = lambda n, s, d: st.enter_context(nc.sbuf_tensor(n, s, d))
        ps = lambda n, s, d: st.enter_context(nc.psum_tensor(n, s, d))
        Wbf = sb("a_Wbf", [128, 8, INW], BF16)
        stage = [sb("a_stage%d" % i, [128, INW], F32) for i in range(2)]
        w1 = sb("a_w1", [128, 8], F32)
        identb = sb("a_identb", [128, 128], BF16)
        gq = sb("a_gq", [128, 64], F32)
        gk = sb("a_gk", [128, 64], F32)
        xt = [sb("a_xt%d" % i, [128, D], F32) for i in range(2)]
        junk = sb("a_junk", [128, D], F32)
        ss = sb("a_ss", [128, 4], F32)
        xn = sb("a_xn", [128, D], BF16)
        xnT = sb("a_xnT", [128, 8, 128], BF16)
        sq = sb("a_sq", [128, 512], F32)
        tmp = sb("a_tmp", [128, 512], F32)
        s8 = sb("a_s8", [128, 24], F32)
        qn = sb("a_qn", [128, 512], BF16)
        qTs = sb("a_qTs", [128, 4, 128], BF16)
        ob = [sb("a_ob%d" % i, [128, 512], BF16) for i in range(2)]
        fm = [sb("a_fm%d" % i, [128, 4, 128], F32) for i in range(2)]
        gsb = sb("a_gsb", [4, 4, 128], F32)
        ps_t = ps("a_ps_t", [128, D], BF16)
        ps_g = [ps("a_ps_g%d" % i, [128, 512], F32) for i in range(2)]
        ps_q = ps("a_ps_q", [128, 4, 128], BF16)
        ps_f = [ps("a_ps_f%d" % i, [128, 4, 128], F32) for i in range(2)]
        ps_gt = ps("a_ps_gt", [4, 4, 128], F32)

        bW = bufs(8, "W")
        bst = bufs(2, "st")
        bc = Buf("consts")
        bxt = bufs(2, "xt")
        bjunk, bss, bxn, bxnT, bsq, btmp, bs8, bqn, bqTs = [Buf(n) for n in
            ("junk", "ss", "xn", "xnT", "sq", "tmp", "s8", "qn", "qTs")]
        bob = bufs(2, "ob")
        bfm = bufs(2, "fm")
        bgsb = Buf("gsb")
        bps_t, bps_q, bps_gt = Buf("ps_t"), Buf("ps_q"), Buf("ps_gt")
        bps_g = bufs(2, "ps_g")
        bps_f = bufs(2, "ps_f")

        P.dma("sp", lambda h: h.dma_start(out=w1[:], in_=c.norm1_w), writes=[bc])
        P.dma("sp", lambda h: h.dma_start(out=identb[:], in_=c.identb), writes=[bc])
        P.dma("sp", lambda h: h.dma_start(out=gq[:], in_=c.gq), writes=[bc])
        P.dma("sp", lambda h: h.dma_start(out=gk[:], in_=c.gk), writes=[bc])
        for kc in range(8):
            s_ = stage[kc % 2]
            P.dma("sp", lambda h, kc=kc, s_=s_: h.dma_start(out=s_[:], in_=c.w_in[kc * 128:(kc + 1) * 128, :]),
                  writes=[bst[kc % 2]])
            P.op("dve" if kc % 2 == 0 else "pool",
                 lambda h, kc=kc, s_=s_: h.tensor_scalar(out=Wbf[:, kc, :], in0=s_[:], scalar1=w1[:, kc:kc + 1],
                                                         scalar2=None, op0=ALU.mult),
                 reads=[bst[kc % 2], bc], writes=[bW[kc]])

        gi = [0]

        def mm_group_tok(cols, sub=None):
            k = gi[0] % 2
            gi[0] += 1
            c0, c1 = cols
            for kc in range(8):
                P.op("pe", lambda h, kc=kc, k=k: h.matmul(ps_g[k][:, 0:c1 - c0], lhsT=xnT[:, kc, :],
                                                       rhs=Wbf[:, kc, c0:c1], start=(kc == 0), stop=(kc == 7)),
                     reads=[bxnT, bW[kc]], writes=[bps_g[k]])
            return k

        oi = [0]
        fi = [0]
        for i in range(NT):
            x_ = xt[i % 2]
            bx_ = bxt[i % 2]
            P.dma("sp", lambda h, i=i, x_=x_: h.dma_start(out=x_[:], in_=c.x[i * 128:(i + 1) * 128, :]), writes=[bx_])
            P.op("act", lambda h, x_=x_: h.activation(out=junk[:], in_=x_[:], func=AF.Square, accum_out=ss[:, 0:1]),
                 reads=[bx_], writes=[bjunk, bss])
            P.op("act", lambda h: h.activation(out=ss[:, 1:2], in_=ss[:, 0:1], func=AF.Sqrt, scale=1.0 / D, bias=EPS),
                 reads=[bss], writes=[bss])
            P.op("dve", lambda h: h.reciprocal(out=ss[:, 2:3], in_=ss[:, 1:2]), reads=[bss], writes=[bss])
            P.op("dve", lambda h, x_=x_: h.tensor_scalar(out=xn[:], in0=x_[:], scalar1=ss[:, 2:3], scalar2=None,
                                                          op0=ALU.mult), reads=[bx_, bss], writes=[bxn])
            for kc in range(8):
                P.op("pe", lambda h, kc=kc: h.transpose(out=ps_t[:, kc * 128:(kc + 1) * 128],
                                                        in_=xn[:, kc * 128:(kc + 1) * 128], identity=identb[:]),
                     reads=[bxn, bc], writes=[bps_t])
            P.op("act", lambda h: h.copy(out=xnT[:].rearrange("p k t -> p (k t)"), in_=ps_t[:]),
                 reads=[bps_t], writes=[bxnT])

            for which, (c0, gain, dst) in enumerate(((0, gq, c.QT), (512, gk, c.KT))):
                k = mm_group_tok((c0, c0 + 512))
                P.op("act", lambda h, k=k: h.activation(out=sq[:], in_=ps_g[k][:], func=AF.Square),
                     reads=[bps_g[k]], writes=[bsq])
                P.op("dve", lambda h: h.tensor_reduce(out=s8[:, 0:8], in_=sq[:].rearrange("p (a b) -> p a b", b=64),
                                                      axis=AX.X, op=ALU.add), reads=[bsq], writes=[bs8])
                P.op("act", lambda h: h.activation(out=s8[:, 8:16], in_=s8[:, 0:8], func=AF.Sqrt, scale=1.0 / 64,
                                                   bias=EPS), reads=[bs8], writes=[bs8])
                P.op("dve", lambda h: h.reciprocal(out=s8[:, 16:24], in_=s8[:, 8:16]), reads=[bs8], writes=[bs8])
                P.op("dve", lambda h, k=k: h.tensor_tensor(
                    out=tmp[:].rearrange("p (a b) -> p a b", b=64),
                    in0=ps_g[k][:].rearrange("p (a b) -> p a b", b=64),
                    in1=s8[:, 16:24].unsqueeze(2).to_broadcast([128, 8, 64]), op=ALU.mult),
                    reads=[bps_g[k], bs8], writes=[btmp])
                P.op("pool", lambda h, gain=gain: h.tensor_tensor(
                    out=qn[:].rearrange("p (a b) -> p a b", b=64),
                    in0=tmp[:].rearrange("p (a b) -> p a b", b=64),
                    in1=gain[:].unsqueeze(1).to_broadcast([128, 8, 64]), op=ALU.mult),
                    reads=[btmp, bc], writes=[bqn])
                for hp in range(4):
                    P.op("pe", lambda h, hp=hp: h.transpose(out=ps_q[:, hp, :], in_=qn[:, hp * 128:(hp + 1) * 128],
                                                            identity=identb[:]), reads=[bqn, bc], writes=[bps_q])
                P.op("act", lambda h: h.copy(out=qTs[:], in_=ps_q[:]), reads=[bps_q], writes=[bqTs])
                P.dma("sp", lambda h, i=i, dst=dst: h.dma_start(
                    out=dst[:, :, i * 128:(i + 1) * 128].rearrange("a p t -> p a t"), in_=qTs[:]),
                    reads=[bqTs], writes=[c.bQKT[which][i]])

            for (c0, dst, bdst, fn) in ((1024, c.V, c.bV, AF.Copy), (2560, c.MV, c.bMV, AF.Copy),
                                        (3072, c.SIGO, c.bSIGO, AF.Sigmoid)):
                k = mm_group_tok((c0, c0 + 512))
                o = oi[0] % 2
                oi[0] += 1
                P.op("act", lambda h, k=k, o=o, fn=fn: h.activation(out=ob[o][:], in_=ps_g[k][:], func=fn),
                     reads=[bps_g[k]], writes=[bob[o]])
                P.dma("sp", lambda h, i=i, o=o, dst=dst: h.dma_start(out=dst[i * 128:(i + 1) * 128, :], in_=ob[o][:]),
                      reads=[bob[o]], writes=[bdst[i]])

            for half in range(2):
                f = fi[0] % 2
                fi[0] += 1
                for cc in range(4):
                    ch = half * 4 + cc
                    col = 1536 + ch * 128
                    for kc in range(8):
                        P.op("pe", lambda h, kc=kc, f=f, cc=cc, col=col: h.matmul(
                            ps_f[f][:, cc, :], lhsT=Wbf[:, kc, col:col + 128], rhs=xnT[:, kc, :],
                            start=(kc == 0), stop=(kc == 7)), reads=[bxnT, bW[kc]], writes=[bps_f[f]])
                P.op("dve", lambda h, f=f: h.tensor_copy(out=fm[f][:], in_=ps_f[f][:]), reads=[bps_f[f]], writes=[bfm[f]])
                P.dma("sp", lambda h, i=i, f=f, half=half: h.dma_start(
                    out=c.MQKT[half * 512:(half + 1) * 512, i * 128:(i + 1) * 128].rearrange("(a p) t -> p a t", p=128),
                    in_=fm[f][:]), reads=[bfm[f]], writes=[c.bMQKT[i]])
            for g in range(4):
                col = 3584 + 4 * g
                for kc in range(8):
                    P.op("pe", lambda h, kc=kc, g=g, col=col: h.matmul(
                        ps_gt[:, g, :], lhsT=Wbf[:, kc, col:col + 4], rhs=xnT[:, kc, :],
                        start=(kc == 0), stop=(kc == 7)), reads=[bxnT, bW[kc]], writes=[bps_gt])
            P.op("dve", lambda h: h.tensor_copy(out=gsb[:], in_=ps_gt[:]), reads=[bps_gt], writes=[bgsb])
            P.dma("sp", lambda h, i=i: h.dma_start(out=c.GT[:, :, i * 128:(i + 1) * 128].rearrange("g a t -> a g t"),
                                                   in_=gsb[:]), reads=[bgsb], writes=[c.bGT[i]])
    P.barrier()


def phase_b(c):
    nc, P = c.nc, c.P
    with ExitStack() as st:
        sb = lambda n, s, d: st.enter_context(nc.sbuf_tensor(n, s, d))
        ps = lambda n, s, d: st.enter_context(nc.psum_tensor(n, s, d))
        QT = sb("b_QT", [128, 4, S], BF16)
        KT = sb("b_KT", [128, 4, S], BF16)
        V = sb("b_V", [128, NT, 8, 65], BF16)
        TBI = sb("b_TBI", [128, 8, 896], F32)
        TBA = sb("b_TBA", [128, 8, 896], F32)
        MI = sb("b_MI", [128, 896], F32)
        MA = sb("b_MA", [128, 896], F32)
        gao = sb("b_gao", [128, 512], F32)
        sT = [sb("b_sT%d" % i, [128, 640], F32) for i in range(2)]
        pT = [sb("b_pT%d" % i, [128, 640], BF16) for i in range(2)]
        ao = sb("b_ao", [128, 512], F32)
        junk = sb("b_junk", [128, 512], F32)
        rc = [sb("b_rc%d" % i, [128, 1], F32) for i in range(2)]
        ss = sb("b_ss", [128, 4], F32)
        aob = [sb("b_aob%d" % i, [128, 512], BF16) for i in range(2)]
        ps_s = [ps("b_ps_s%d" % i, [128, 1024], F32) for i in range(2)]
        ps_o = [ps("b_ps_o%d" % i, [128, 128], F32) for i in range(2)]

        bQT, bKT = bufs(4, "bQT"), bufs(4, "bKT")
        bVt = bufs(NT, "bV")
        bones, btb, bm, bgao = Buf("ones"), Buf("tb"), Buf("m"), Buf("gao")
        bsT, bpT, brc, baob = bufs(2, "sT"), bufs(2, "pT"), bufs(2, "rc"), bufs(2, "aob")
        bao, bjunk, bss = Buf("ao"), Buf("junk"), Buf("ss")
        bps_s, bps_o = bufs(2, "ps_s"), bufs(2, "ps_o")

        P.dma("sp", lambda h: h.dma_start(out=TBI[:], in_=c.rpbg), writes=[btb])
        P.dma("sp", lambda h: h.dma_start(out=MI[:], in_=c.mask_i), writes=[bm])
        P.dma("sp", lambda h: h.dma_start(out=MA[:], in_=c.mask_a), writes=[bm])
        P.dma("sp", lambda h: h.dma_start(out=gao[:], in_=c.gao), writes=[bgao])
        for hp in range(4):
            P.dma("sp", lambda h, hp=hp: h.dma_start(out=QT[:, hp, :], in_=c.QT[hp]),
                  reads=c.bQKT[0], writes=[bQT[hp]])
            P.dma("act", lambda h, hp=hp: h.dma_start(out=KT[:, hp, :], in_=c.KT[hp]),
                  reads=c.bQKT[1], writes=[bKT[hp]])
        P.op("pool", lambda h: h.memset(V[:, :, :, 64:65], 1.0), writes=[bones])
        for i in range(NT):
            P.dma("sp" if i % 2 == 0 else "act", lambda h, i=i: h.dma_start(
                out=V[:, i, :, 0:64], in_=c.V[i * 128:(i + 1) * 128, :].rearrange("p (a b) -> p a b", b=64)),
                reads=[c.bV[i]], writes=[bVt[i]])
        for hd in range(8):
            P.op("dve", lambda h, hd=hd: h.tensor_tensor(out=TBA[:, hd, :], in0=TBI[:, hd, :], in1=MA[:], op=ALU.add),
                 reads=[btb, bm], writes=[btb])
        for hd in range(8):
            P.op("dve", lambda h, hd=hd: h.tensor_tensor(out=TBI[:, hd, :], in0=TBI[:, hd, :], in1=MI[:], op=ALU.add),
                 reads=[btb, bm], writes=[btb])

        it = 0
        for j in range(NT):
            if 2 <= j <= 29:
                kts = list(range(j - 2, j + 3)); tb = TBI; s0 = 1
            elif j == 0:
                kts = [0, 1, 2, 3]; tb = TBA; s0 = 3
            elif j == 1:
                kts = [0, 1, 2, 3]; tb = TBA; s0 = 2
            elif j == 30:
                kts = [28, 29, 30, 31]; tb = TBA; s0 = 1
            else:
                kts = [28, 29, 30, 31]; tb = TBA; s0 = 0
            n = len(kts)
            for hd in range(8):
                hp, hh = hd // 2, hd % 2
                k = it % 2
                it += 1
                p0, p1 = hh * 64, hh * 64 + 64
                for idx, kt in enumerate(kts):
                    P.op("pe", lambda h, k=k, idx=idx, kt=kt, hp=hp, p0=p0, p1=p1, j=j: h.matmul(
                        ps_s[k][:, idx * 128:(idx + 1) * 128], lhsT=KT[p0:p1, hp, kt * 128:(kt + 1) * 128],
                        rhs=QT[p0:p1, hp, j * 128:(j + 1) * 128], start=True, stop=True),
                        reads=[bKT[hp], bQT[hp]], writes=[bps_s[k]])
                P.op("dve", lambda h, k=k, n=n, tb=tb, s0=s0, hd=hd: h.scalar_tensor_tensor(
                    out=sT[k][:, 0:n * 128], in0=ps_s[k][:, 0:n * 128], scalar=0.125,
                    in1=tb[:, hd, s0 * 128:(s0 + n) * 128], op0=ALU.mult, op1=ALU.add),
                    reads=[bps_s[k], btb], writes=[bsT[k]])
                P.op("act", lambda h, k=k, n=n: h.activation(out=pT[k][:, 0:n * 128], in_=sT[k][:, 0:n * 128],
                                                             func=AF.Exp), reads=[bsT[k]], writes=[bpT[k]])
                for idx, kt in enumerate(kts):
                    P.op("pe", lambda h, k=k, idx=idx, kt=kt, hd=hd, n=n: h.matmul(
                        ps_o[k][:, 0:65], lhsT=pT[k][:, idx * 128:(idx + 1) * 128], rhs=V[:, kt, hd, :],
                        start=(idx == 0), stop=(idx == n - 1)),
                        reads=[bpT[k], bVt[kt], bones], writes=[bps_o[k]])
                P.op("dve", lambda h, k=k: h.reciprocal(out=rc[k][:], in_=ps_o[k][:, 64:65]),
                     reads=[bps_o[k]], writes=[brc[k]])
                P.op("dve", lambda h, k=k, hd=hd: h.tensor_scalar(
                    out=ao[:, hd * 64:(hd + 1) * 64], in0=ps_o[k][:, 0:64], scalar1=rc[k][:], scalar2=None,
                    op0=ALU.mult), reads=[bps_o[k], brc[k]], writes=[bao])
            o = j % 2
            P.op("act", lambda h: h.activation(out=junk[:], in_=ao[:], func=AF.Square, accum_out=ss[:, 0:1]),
                 reads=[bao], writes=[bjunk, bss])
            P.op("act", lambda h: h.activation(out=ss[:, 1:2], in_=ss[:, 0:1], func=AF.Sqrt, scale=1.0 / 512, bias=EPS),
                 reads=[bss], writes=[bss])
            P.op("dve", lambda h: h.reciprocal(out=ss[:, 2:3], in_=ss[:, 1:2]), reads=[bss], writes=[bss])
            P.op("dve", lambda h, o=o: h.scalar_tensor_tensor(out=aob[o][:], in0=ao[:], scalar=ss[:, 2:3], in1=gao[:],
                                                          op0=ALU.mult, op1=ALU.mult),
                 reads=[bao, bss, bgao], writes=[baob[o]])
            P.dma("sp", lambda h, j=j, o=o: h.dma_start(out=c.AOUT[j * 128:(j + 1) * 128, :], in_=aob[o][:]),
                  reads=[baob[o]], writes=[c.bAOUT[j]])
    P.barrier()


def phase_c(c):
    nc, P = c.nc, c.P
    with ExitStack() as st0:
        sb0 = lambda n, s, d: st0.enter_context(nc.sbuf_tensor(n, s, d))
        COLS = sb0("c_COLS", [128, NT, 24], F32)
        bCOLS = Buf("COLS")
        with ExitStack() as st:
            sb = lambda n, s, d: st.enter_context(nc.sbuf_tensor(n, s, d))
            ps = lambda n, s, d: st.enter_context(nc.psum_tensor(n, s, d))
            G1, G2, CL, Aa, AA, ZER, T1 = [sb("c1_" + n, [4, S], F32) for n in ("G1", "G2", "CL", "Aa", "AA", "ZER", "T1")]
            bG1, bG2, bCL, bAa, bAA, bZER, bT1 = [Buf(n) for n in ("G1", "G2", "CL", "Aa", "AA", "ZER", "T1")]
            BCR = sb("c1_BCR", [4, NT, 257], F32)
            ROWS = sb("c1_ROWS", [24, S], F32)
            gb = sb("c1_gb", [4, 4], F32)
            ngb = sb("c1_ngb", [4, 4], F32)
            AE = sb("c1_AE", [4, NT], F32)
            APv = sb("c1_AP", [4, NT], F32)
            dd = sb("c1_dd", [4, NT], F32)
            identf = sb("c1_identf", [128, 128], F32)
            ps_c = ps("c1_ps_c", [128, NT, 32], F32)
            bBCR, bgb, bAE, bAPv, bdd, bid, bps_c = [Buf(n) for n in ("BCR", "gb", "AE", "AP", "dd", "id", "ps_c")]
            bROWS = bufs(6, "ROWS")
            P.dma("sp", lambda h: h.dma_start(out=gb[:], in_=c.gate_b), writes=[bgb])
            P.dma("sp", lambda h: h.dma_start(out=identf[:], in_=c.identf), writes=[bid])
            P.op("dve", lambda h: h.tensor_scalar(out=ngb[:], in0=gb[:], scalar1=-1.0, scalar2=None, op0=ALU.mult),
                 reads=[bgb], writes=[bgb])
            P.op("pool", lambda h: h.memset(ZER[:], 0.0), writes=[bZER])
            for d in range(2):
                rv = (lambda t: t[:, :]) if d == 0 else (lambda t: t[:, ::-1])
                gi_, gf_ = 2 * d, 2 * d + 1
                P.dma("sp", lambda h, gi_=gi_: h.dma_start(out=G1[:], in_=c.GT[gi_]), reads=c.bGT, writes=[bG1])
                P.dma("sp", lambda h, gf_=gf_: h.dma_start(out=G2[:], in_=c.GT[gf_]), reads=c.bGT, writes=[bG2])
                P.op("act", lambda h, gf_=gf_: h.activation(out=G2[:], in_=G2[:], func=AF.Exp, scale=-1.0,
                                                            bias=ngb[:, gf_:gf_ + 1]), reads=[bG2, bgb], writes=[bG2])
                P.op("act", lambda h: h.activation(out=G2[:], in_=G2[:], func=AF.Ln, bias=1.0), reads=[bG2], writes=[bG2])
                P.op("dve", lambda h, rv=rv: h.tensor_tensor_scan(out=rv(CL), data0=rv(G2), data1=ZER[:], initial=0.0,
                                                                  op0=ALU.add, op1=ALU.add),
                     reads=[bG2, bZER], writes=[bCL])
                P.op("dve", lambda h, gi_=gi_: h.scalar_tensor_tensor(out=Aa[:], in0=G1[:], scalar=gb[:, gi_:gi_ + 1],
                                                                      in1=CL[:], op0=ALU.add, op1=ALU.add),
                     reads=[bG1, bgb, bCL], writes=[bAa])
                P.op("dve", lambda h, rv=rv: h.tensor_tensor_scan(out=rv(AA), data0=rv(Aa), data1=ZER[:], initial=0.0,
                                                                  op0=ALU.max, op1=ALU.add),
                     reads=[bAa, bZER], writes=[bAA])
                P.op("dve", lambda h: h.tensor_tensor(out=T1[:], in0=CL[:], in1=AA[:], op=ALU.subtract),
                     reads=[bCL, bAA], writes=[bT1])
                P.op("act", lambda h: h.activation(out=T1[:], in_=T1[:], func=AF.Exp), reads=[bT1], writes=[bT1])
                P.dma("sp", lambda h, d=d: h.dma_start(out=ROWS[12 * d + 8:12 * d + 12, :], in_=T1[:]),
                      reads=[bT1], writes=[bROWS[3 * d + 2]])
                P.dma("sp", lambda h, d=d: h.dma_start(out=ROWS[12 * d:12 * d + 4, :], in_=Aa[:]),
                      reads=[bAa], writes=[bROWS[3 * d]])
                AAv = AA[:].rearrange("p (c t) -> p c t", t=128)
                epos = 127 if d == 0 else 0
                P.op("dve", lambda h, AAv=AAv, epos=epos: h.tensor_copy(out=AE[:], in_=AAv[:, :, epos]),
                     reads=[bAA], writes=[bAE])
                P.op("dve", lambda h: h.memset(APv[:], 0.0), writes=[bAPv])
                if d == 0:
                    P.op("dve", lambda h: h.tensor_copy(out=APv[:, 1:NT], in_=AE[:, 0:NT - 1]), reads=[bAE], writes=[bAPv])
                else:
                    P.op("dve", lambda h: h.tensor_copy(out=APv[:, 0:NT - 1], in_=AE[:, 1:NT]), reads=[bAE], writes=[bAPv])
                G1v = G1[:].rearrange("p (c t) -> p c t", t=128)
                G2v = G2[:].rearrange("p (c t) -> p c t", t=128)
                Aav = Aa[:].rearrange("p (c t) -> p c t", t=128)
                P.op("dve", lambda h, G1v=G1v, Aav=Aav: h.tensor_tensor(
                    out=G1v, in0=Aav, in1=AE[:].unsqueeze(2).to_broadcast([4, NT, 128]), op=ALU.subtract),
                    reads=[bAa, bAE], writes=[bG1])
                P.op("act", lambda h: h.activation(out=G1[:], in_=G1[:], func=AF.Exp), reads=[bG1], writes=[bG1])
                P.dma("sp", lambda h, d=d: h.dma_start(out=ROWS[12 * d + 4:12 * d + 8, :], in_=G1[:]),
                      reads=[bG1], writes=[bROWS[3 * d + 1]])
                P.op("dve", lambda h, AAv=AAv: h.tensor_scalar(out=BCR[:, :, 0:128], in0=AAv, scalar1=-1.0, scalar2=None,
                                                               op0=ALU.mult), reads=[bAA], writes=[bBCR])
                P.op("dve", lambda h, G2v=G2v, AAv=AAv: h.tensor_tensor(
                    out=G2v, in0=APv[:].unsqueeze(2).to_broadcast([4, NT, 128]), in1=AAv, op=ALU.subtract),
                    reads=[bAA, bAPv], writes=[bG2])
                P.op("act", lambda h, G2v=G2v: h.activation(out=BCR[:, :, 128:256], in_=G2v, func=AF.Exp),
                     reads=[bG2], writes=[bBCR])
                P.op("dve", lambda h: h.tensor_tensor(out=dd[:], in0=APv[:], in1=AE[:], op=ALU.subtract),
                     reads=[bAPv, bAE], writes=[bdd])
                P.op("act", lambda h: h.activation(out=BCR[:, :, 256], in_=dd[:], func=AF.Exp), reads=[bdd], writes=[bBCR])
                P.dma("sp", lambda h, d=d: h.dma_start(out=c.BCRD[d], in_=BCR[:]), reads=[bBCR], writes=[c.bBCRD[d]])
            for ch in range(NT):
                P.op("pe", lambda h, ch=ch: h.transpose(out=ps_c[:, ch, 0:24], in_=ROWS[0:24, ch * 128:(ch + 1) * 128],
                                                        identity=identf[0:24, 0:24]), reads=bROWS + [bid], writes=[bps_c])
            P.op("dve", lambda h: h.tensor_copy(out=COLS[:], in_=ps_c[:, :, 0:24]), reads=[bps_c], writes=[bCOLS])
        P.barrier()

        with ExitStack() as st:
            sb = lambda n, s, d: st.enter_context(nc.sbuf_tensor(n, s, d))
            ps = lambda n, s, d: st.enter_context(nc.psum_tensor(n, s, d))
            QKT = sb("c_QKT", [128, 8, S], BF16)
            bQKT = bufs(8, "cQKT")
            cw = sb("c_cw", [128, 8, 5], F32)
            cb = sb("c_cb", [128, 8], F32)
            bcw = Buf("cw")
            P.dma("sp", lambda h: h.dma_start(out=cw[:], in_=c.conv_w), writes=[bcw])
            P.dma("sp", lambda h: h.dma_start(out=cb[:], in_=c.conv_b), writes=[bcw])
            with ExitStack() as st2:
                sb2 = lambda n, s, d: st2.enter_context(nc.sbuf_tensor(n, s, d))
                xpad = [sb2("c2_xpad%d" % i, [128, S + 4], F32) for i in range(2)]
                acc = [sb2("c2_acc%d" % i, [128, S], F32) for i in range(2)]
                bxp, bacc = bufs(2, "xpad"), bufs(2, "acc")
                for i in range(2):
                    P.op("pool", lambda h, i=i: h.memset(xpad[i][:, 0:2], 0.0), writes=[bxp[i]])
                    P.op("pool", lambda h, i=i: h.memset(xpad[i][:, S + 2:S + 4], 0.0), writes=[bxp[i]])
                for cc in range(8):
                    k = cc % 2
                    P.dma("sp", lambda h, cc=cc, k=k: h.dma_start(out=xpad[k][:, 2:S + 2], in_=c.MQKT[cc * 128:(cc + 1) * 128, :]),
                          reads=c.bMQKT, writes=[bxp[k]])
                    P.op("dve", lambda h, cc=cc, k=k: h.tensor_scalar(out=acc[k][:], in0=xpad[k][:, 0:S], scalar1=cw[:, cc, 0:1],
                                                                      scalar2=cb[:, cc:cc + 1], op0=ALU.mult, op1=ALU.add),
                         reads=[bxp[k], bcw], writes=[bacc[k]])
                    for j in range(1, 5):
                        P.op("dve", lambda h, cc=cc, k=k, j=j: h.scalar_tensor_tensor(
                            out=acc[k][:], in0=xpad[k][:, j:j + S], scalar=cw[:, cc, j:j + 1], in1=acc[k][:],
                            op0=ALU.mult, op1=ALU.add), reads=[bxp[k], bcw, bacc[k]], writes=[bacc[k]])
                    if cc < 4:
                        P.op("act", lambda h, cc=cc, k=k: h.activation(out=QKT[:, cc, :], in_=acc[k][:], func=AF.Silu),
                             reads=[bacc[k]], writes=[bQKT[cc]])
                    else:
                        P.op("act", lambda h, cc=cc, k=k: h.activation(out=acc[k][:], in_=acc[k][:], func=AF.Silu),
                             reads=[bacc[k]], writes=[bacc[k]])
                        P.op("pool", lambda h, cc=cc, k=k: h.tensor_scalar(out=QKT[:, cc, :], in0=acc[k][:], scalar1=128.0 ** -0.5,
                                                                           scalar2=None, op0=ALU.mult),
                             reads=[bacc[k]], writes=[bQKT[cc]])
            P.barrier()

            MVs = sb("c_MVs", [128, NT, 4, 129], BF16)
            bMVs = bufs(NT, "cMV")
            bones = Buf("ones")
            SEL = sb("c_SEL", [4, 4, 128], F32)
            MSK = sb("c_MSK", [128, 2, 128], F32)
            identb = sb("c_identb", [128, 128], BF16)
            gmn = sb("c_gmn", [128, 512], F32)
            bconst = Buf("const")
            P.dma("sp", lambda h: h.dma_start(out=SEL[:], in_=c.sel), writes=[bconst])
            P.dma("sp", lambda h: h.dma_start(out=MSK[:], in_=c.msk), writes=[bconst])
            P.dma("sp", lambda h: h.dma_start(out=identb[:], in_=c.identb), writes=[bconst])
            P.dma("sp", lambda h: h.dma_start(out=gmn[:], in_=c.gmn), writes=[bconst])
            P.op("pool", lambda h: h.memset(MVs[:, :, :, 128:129], 1.0), writes=[bones])
            for i in range(NT):
                P.dma("sp" if i % 2 == 0 else "act", lambda h, i=i: h.dma_start(
                    out=MVs[:, i, :, 0:128], in_=c.MV[i * 128:(i + 1) * 128, :].rearrange("p (a b) -> p a b", b=128)),
                    reads=[c.bMV[i]], writes=[bMVs[i]])
            Cst = [sb("c_C%d" % i, [128, 129], F32) for i in range(4)]
            Cbf = [sb("c_Cbf%d" % i, [128, 129], BF16) for i in range(4)]
            bC, bCbf = bufs(4, "C"), bufs(4, "Cbf")
            bcr = [sb("c_bcr%d" % i, [4, 257], F32) for i in range(2)]
            bbcr = bufs(2, "bcr")
            Gt = [sb("c_G%d" % i, [128, 128], F32) for i in range(2)]
            Wt = [sb("c_W%d" % i, [128, 128], F32) for i in range(2)]
            PT = [sb("c_PT%d" % i, [128, 128], BF16) for i in range(2)]
            qs = [sb("c_qs%d" % i, [128, 128], BF16) for i in range(2)]
            kw = [sb("c_kw%d" % i, [128, 128], BF16) for i in range(2)]
            dec = [sb("c_dec%d" % i, [128, 1], F32) for i in range(2)]
            dn = [sb("c_dn%d" % i, [128, 2], F32) for i in range(2)]
            bG, bW, bPT, bqs, bkw, bdec, bdn = [bufs(2, n) for n in ("G", "W", "PT", "qs", "kw", "dec", "dn")]
            hbuf = [sb("c_hbuf%d" % i, [128, 512], F32) for i in range(2)]
            bhbuf = bufs(2, "hbuf")
            hf = sb("c_hf", [128, 512], F32)
            sg = sb("c_sg", [128, 512], BF16)
            sq = sb("c_sq", [128, 512], F32)
            s4 = sb("c_s4", [128, 12], F32)
            hmo = [sb("c_hmo%d" % i, [128, 512], BF16) for i in range(2)]
            bhf, bsg, bsq, bs4 = Buf("hf"), Buf("sg"), Buf("sq"), Buf("s4")
            bhmo = bufs(2, "hmo")
            ps_bc = [ps("c_ps_bc%d" % i, [128, 512], F32) for i in range(2)]
            ps_st = [ps("c_ps_st%d" % i, [128, 128], F32) for i in range(2)]
            ps_n = [ps("c_ps_n%d" % i, [128, 512], F32) for i in range(2)]
            ps_kt = ps("c_ps_kt", [128, 128], BF16)
            ps_dc = ps("c_ps_dc", [128, 512], F32)
            bps_bc, bps_st, bps_n = bufs(2, "ps_bc"), bufs(2, "ps_st"), bufs(2, "ps_n")
            bps_kt, bps_dc = Buf("ps_kt"), Buf("ps_dc")

            it = 0
            for d in range(2):
                for hd in range(4):
                    P.op("pool", lambda h, hd=hd: h.memset(Cst[hd][:], 0.0), writes=[bC[hd]])
                    P.op("pool", lambda h, hd=hd: h.memset(Cbf[hd][:], 0.0), writes=[bCbf[hd]])
                order = list(range(NT)) if d == 0 else list(range(NT - 1, -1, -1))
                for ci, ch in enumerate(order):
                    kb = ci % 2
                    P.dma("sp", lambda h, d=d, ch=ch, kb=kb: h.dma_start(out=bcr[kb][:], in_=c.BCRD[d, :, ch, :]),
                          reads=[c.bBCRD[d]], writes=[bbcr[kb]])
                    hb = hbuf[ci % 2]
                    bhb = bhbuf[ci % 2]
                    tsl = slice(ch * 128, (ch + 1) * 128)
                    for hd in range(4):
                        k = it % 2
                        it += 1
                        a_col = COLS[:, ch, 12 * d + hd:12 * d + hd + 1]
                        wk_col = COLS[:, ch, 12 * d + 4 + hd:12 * d + 5 + hd]
                        emt_col = COLS[:, ch, 12 * d + 8 + hd:12 * d + 9 + hd]
                        P.op("pe", lambda h, k=k, kb=kb, hd=hd: h.matmul(ps_bc[k][:, 0:257], lhsT=SEL[:, hd, :], rhs=bcr[kb][:],
                                                                         start=True, stop=True),
                             reads=[bconst, bbcr[kb]], writes=[bps_bc[k]])
                        P.op("pe", lambda h, k=k, hd=hd, tsl=tsl: h.matmul(ps_st[k][:], lhsT=QKT[:, 4 + hd, tsl], rhs=QKT[:, hd, tsl],
                                                                          start=True, stop=True),
                             reads=[bQKT[4 + hd], bQKT[hd]], writes=[bps_st[k]])
                        P.op("dve", lambda h, k=k, d=d: h.tensor_tensor(out=Gt[k][:], in0=ps_bc[k][:, 0:128], in1=MSK[:, d, :],
                                                                       op=ALU.add), reads=[bps_bc[k], bconst], writes=[bG[k]])
                        P.op("act", lambda h, k=k, a_col=a_col: h.activation(out=Wt[k][:], in_=Gt[k][:], func=AF.Exp, bias=a_col),
                             reads=[bG[k], bCOLS], writes=[bW[k]])
                        P.op("dve", lambda h, k=k: h.tensor_tensor(out=PT[k][:], in0=ps_st[k][:], in1=Wt[k][:], op=ALU.mult),
                             reads=[bps_st[k], bW[k]], writes=[bPT[k]])
                        P.op("dve", lambda h, k=k, hd=hd, tsl=tsl: h.tensor_tensor(out=qs[k][:], in0=ps_bc[k][:, 128:256],
                                                                                  in1=QKT[:, hd, tsl], op=ALU.mult),
                             reads=[bps_bc[k], bQKT[hd]], writes=[bqs[k]])
                        P.op("act", lambda h, k=k: h.copy(out=dec[k][:], in_=ps_bc[k][:, 256:257]),
                             reads=[bps_bc[k]], writes=[bdec[k]])
                        P.op("pe", lambda h, k=k, ch=ch, hd=hd: h.matmul(ps_n[k][:, 0:129], lhsT=PT[k][:], rhs=MVs[:, ch, hd, :],
                                                                         start=True, stop=False),
                             reads=[bPT[k], bMVs[ch], bones], writes=[bps_n[k]])
                        P.op("pe", lambda h, k=k, hd=hd: h.matmul(ps_n[k][:, 0:129], lhsT=qs[k][:], rhs=Cbf[hd][:],
                                                                  start=False, stop=True),
                             reads=[bqs[k], bCbf[hd]], writes=[bps_n[k]])
                        P.op("pe", lambda h, hd=hd, tsl=tsl: h.transpose(out=ps_kt[:], in_=QKT[:, 4 + hd, tsl], identity=identb[:]),
                             reads=[bQKT[4 + hd], bconst], writes=[bps_kt])
                        P.op("act", lambda h, k=k, wk_col=wk_col: h.activation(out=kw[k][:], in_=ps_kt[:], func=AF.Copy, scale=wk_col),
                             reads=[bps_kt, bCOLS], writes=[bkw[k]])
                        P.op("pe", lambda h, k=k, ch=ch, hd=hd: h.matmul(ps_dc[:, 0:129], lhsT=kw[k][:], rhs=MVs[:, ch, hd, :],
                                                                         start=True, stop=True),
                             reads=[bkw[k], bMVs[ch], bones], writes=[bps_dc])
                        P.op("dve", lambda h, k=k, hd=hd: h.scalar_tensor_tensor(out=Cst[hd][:], in0=Cst[hd][:], scalar=dec[k][:],
                                                                                 in1=ps_dc[:, 0:129], op0=ALU.mult, op1=ALU.add),
                             reads=[bC[hd], bdec[k], bps_dc], writes=[bC[hd]])
                        P.op("act", lambda h, hd=hd: h.copy(out=Cbf[hd][:], in_=Cst[hd][:]), reads=[bC[hd]], writes=[bCbf[hd]])
                        P.op("act", lambda h, k=k: h.activation(out=dn[k][:, 1:2], in_=ps_n[k][:, 128:129], func=AF.Abs),
                             reads=[bps_n[k]], writes=[bdn[k]])
                        P.op("dve", lambda h, k=k, emt_col=emt_col: h.tensor_scalar(out=dn[k][:, 0:1], in0=dn[k][:, 1:2],
                                                                                    scalar1=emt_col, scalar2=None, op0=ALU.max),
                             reads=[bdn[k], bCOLS], writes=[bdn[k]])
                        P.op("dve", lambda h, k=k: h.reciprocal(out=dn[k][:, 1:2], in_=dn[k][:, 0:1]), reads=[bdn[k]], writes=[bdn[k]])
                        P.op("dve", lambda h, k=k, hd=hd, hb=hb: h.tensor_scalar(out=hb[:, hd * 128:(hd + 1) * 128], in0=ps_n[k][:, 0:128],
                                                                                 scalar1=dn[k][:, 1:2], scalar2=None, op0=ALU.mult),
                             reads=[bps_n[k], bdn[k]], writes=[bhb])
                    if d == 0:
                        P.dma("sp", lambda h, ch=ch, hb=hb: h.dma_start(out=c.HF[ch * 128:(ch + 1) * 128, :], in_=hb[:]),
                              reads=[bhb], writes=[c.bHF[ch]])
                    else:
                        o = ci % 2
                        P.dma("sp", lambda h, ch=ch: h.dma_start(out=hf[:], in_=c.HF[ch * 128:(ch + 1) * 128, :]),
                              reads=[c.bHF[ch]], writes=[bhf])
                        P.dma("sp", lambda h, ch=ch: h.dma_start(out=sg[:], in_=c.SIGO[ch * 128:(ch + 1) * 128, :]),
                              reads=[c.bSIGO[ch]], writes=[bsg])
                        P.op("pool", lambda h, hb=hb: h.tensor_tensor(out=hf[:], in0=hf[:], in1=hb[:], op=ALU.add),
                             reads=[bhb, bhf], writes=[bhf])
                        P.op("act", lambda h: h.activation(out=sq[:], in_=hf[:], func=AF.Square), reads=[bhf], writes=[bsq])
                        P.op("dve", lambda h: h.tensor_reduce(out=s4[:, 0:4], in_=sq[:].rearrange("p (a b) -> p a b", b=128),
                                                              axis=AX.X, op=ALU.add), reads=[bsq], writes=[bs4])
                        P.op("act", lambda h: h.activation(out=s4[:, 4:8], in_=s4[:, 0:4], func=AF.Sqrt, scale=1.0 / 128, bias=EPS),
                             reads=[bs4], writes=[bs4])
                        P.op("dve", lambda h: h.reciprocal(out=s4[:, 8:12], in_=s4[:, 4:8]), reads=[bs4], writes=[bs4])
                        P.op("dve", lambda h: h.tensor_tensor(out=sq[:].rearrange("p (a b) -> p a b", b=128),
                                                              in0=hf[:].rearrange("p (a b) -> p a b", b=128),
                                                              in1=s4[:, 8:12].unsqueeze(2).to_broadcast([128, 4, 128]), op=ALU.mult),
                             reads=[bhf, bs4, bsq], writes=[bsq])
                        P.op("pool", lambda h: h.tensor_tensor(out=sq[:], in0=sq[:], in1=gmn[:], op=ALU.mult),
                             reads=[bsq, bconst], writes=[bsq])
                        P.op("pool", lambda h, o=o: h.tensor_tensor(out=hmo[o][:], in0=sq[:], in1=sg[:], op=ALU.mult),
                             reads=[bsq, bsg], writes=[bhmo[o]])
                        P.dma("sp", lambda h, ch=ch, o=o: h.dma_start(out=c.HM[ch * 128:(ch + 1) * 128, :], in_=hmo[o][:]),
                              reads=[bhmo[o]], writes=[c.bHM[ch]])
    P.barrier()


def phase_t(c):
    nc, P = c.nc, c.P
    JB = 4
    with ExitStack() as st:
        sb = lambda n, s, d: st.enter_context(nc.sbuf_tensor(n, s, d))
        tin = [sb("t_in%d" % i, [128, JB * 2 * D], F32) for i in range(2)]
        tout = [sb("t_out%d" % i, [128, JB * 2 * D], BF16) for i in range(2)]
        bin_, bout = bufs(2, "tin"), bufs(2, "tout")
        src = c.peer_uv.rearrange("(p j) d -> p (j d)", p=128)
        dst = c.UVB.rearrange("(p j) d -> p (j d)", p=128)
        W = JB * 2 * D
        third = W // 4
        for stp in range(128 // JB):
            k = stp % 2
            P.dma("sp", lambda h, stp=stp, k=k: h.dma_start(out=tin[k][:], in_=src[:, stp * W:(stp + 1) * W]), writes=[bin_[k]])
            P.op("dve", lambda h, k=k: h.tensor_copy(out=tout[k][:, 0:2 * third], in_=tin[k][:, 0:2 * third]), reads=[bin_[k]], writes=[bout[k]])
            P.op("act", lambda h, k=k: h.copy(out=tout[k][:, 2 * third:3 * third], in_=tin[k][:, 2 * third:3 * third]), reads=[bin_[k]], writes=[bout[k]])
            P.op("pool", lambda h, k=k: h.tensor_copy(out=tout[k][:, 3 * third:W], in_=tin[k][:, 3 * third:W]), reads=[bin_[k]], writes=[bout[k]])
            P.dma("act", lambda h, stp=stp, k=k: h.dma_start(out=dst[:, stp * W:(stp + 1) * W], in_=tout[k][:]), reads=[bout[k]])
    P.barrier()


def phase_d(c):
    nc, P = c.nc, c.P
    with ExitStack() as st:
        sb = lambda n, s, d: st.enter_context(nc.sbuf_tensor(n, s, d))
        ps = lambda n, s, d: st.enter_context(nc.psum_tensor(n, s, d))
        Wo = sb("d_Wo", [128, 8, D], BF16)
        Wq = sb("d_Wq", [128, 8, 2048], BF16)
        SKT = sb("d_SKT", [128, 16, 128], BF16)
        gn2 = sb("d_gn2", [128, D], F32)
        identb = sb("d_identb", [128, 128], BF16)
        identf = sb("d_identf", [128, 128], F32)
        THR = sb("d_THR", [128, 16], F32)
        IOT = sb("d_IOT", [128, 16], F32)
        st_stage = ExitStack()
        stage = [st_stage.enter_context(nc.sbuf_tensor("d_stage%d" % i, [128, 2048], F32)) for i in range(2)]
        bconst = Buf("dconst")
        bst = bufs(2, "dst")
        bWo, bWq = bufs(8, "Wo"), bufs(8, "Wq")
        bSKT = Buf("SKT")
        for (t, src) in ((gn2, c.gn2), (identb, c.identb), (THR, c.thr), (IOT, c.iot), (identf, c.identf)):
            P.dma("sp", lambda h, t=t, src=src: h.dma_start(out=t[:], in_=src), writes=[bconst])
        n = 0
        for kc in range(8):
            k = n % 2; n += 1
            P.dma("sp", lambda h, kc=kc, k=k: h.dma_start(out=stage[k][:, 0:D], in_=c.w_out[kc * 128:(kc + 1) * 128, :]), writes=[bst[k]])
            P.op("dve", lambda h, kc=kc, k=k: h.tensor_copy(out=Wo[:, kc, :], in_=stage[k][:, 0:D]), reads=[bst[k]], writes=[bWo[kc]])
        for kc in range(8):
            k = n % 2; n += 1
            P.dma("sp", lambda h, kc=kc, k=k: h.dma_start(out=stage[k][:], in_=c.w_q[kc * 128:(kc + 1) * 128, :]), writes=[bst[k]])
            P.op("dve", lambda h, kc=kc, k=k: h.tensor_copy(out=Wq[:, kc, :], in_=stage[k][:]), reads=[bst[k]], writes=[bWq[kc]])
        k = n % 2; n += 1
        P.dma("sp", lambda h, k=k: h.dma_start(out=stage[k][:].rearrange("p (a b) -> p a b", b=128), in_=c.skt), writes=[bst[k]])
        P.op("dve", lambda h, k=k: h.tensor_copy(out=SKT[:].rearrange("p a b -> p (a b)"), in_=stage[k][:]), reads=[bst[k]], writes=[bSKT])
        P.barrier()
        st_stage.close()

        cat = sb("d_cat", [128, D], BF16)
        catT = sb("d_catT", [128, 8, 128], BF16)
        xt = sb("d_xt", [128, D], F32)
        junk = sb("d_junk", [128, D], F32)
        junk2 = sb("d_junk2", [128, D], F32)
        ss = sb("d_ss", [128, 4], F32)
        xn2b = sb("d_xn2b", [128, D], BF16)
        xn2T = sb("d_xn2T", [128, 8, 128], BF16)
        qT = sb("d_qT", [128, 16, 128], BF16)
        sc = sb("d_sc", [128, 16, 128], F32)
        m8 = sb("d_m8", [128, 16, 16], F32)
        i8 = sb("d_i8", [128, 16, 16], U32)
        i8f = sb("d_i8f", [128, 16, 16], F32)
        cand = sb("d_cand", [128, 8, 256], F32)
        t8 = sb("d_t8", [128, 8, 16], F32)
        j8 = sb("d_j8", [128, 8, 16], U32)
        jf = sb("d_jf", [128, 128], F32)
        T4 = sb("d_T4", [128, 128, 16], F32)
        af = sb("d_af", [128, 128], F32)
        bf_ = sb("d_bf", [128, 128], F32)
        E1 = sb("d_E1", [128, 128], F32)
        E2 = sb("d_E2", [128, 128], F32)
        g8 = sb("d_g8", [128, 16], F32)
        x1 = [sb("d_x1_%d" % i, [128, D], F32) for i in range(2)]
        xn2 = [sb("d_xn2_%d" % i, [128, D], F32) for i in range(2)]
        eidx = [sb("d_eidx%d" % i, [128, 128], I32) for i in range(2)]
        gts = [sb("d_gts%d" % i, [128, 8, 16], F32) for i in range(2)]
        adot = sb("d_adot", [128, 128], F32)
        ga = sb("d_ga", [128, 128], F32)
        NG = 12
        GS = 4
        NGRP = 128 // GS
        uv = [sb("d_uv%d" % i, [128, 2 * D], BF16) for i in range(NG)]
        dg = [sb("d_dg%d" % i, [128, 128], BF16) for i in range(4)]
        yacc = sb("d_y", [128, D], F32)
        ps_t = ps("d_ps_t", [128, D], BF16)
        ps_o = [ps("d_ps_o%d" % i, [128, 512], F32) for i in range(2)]
        ps_y = [ps("d_ps_y%d" % i, [128, 512], F32) for i in range(2)]
        ps_q = [ps("d_ps_q%d" % i, [128, 4, 128], F32) for i in range(2)]
        (bcat, bcatT, bxt, bss, bxn2b, bxn2T, bqT, bsc, bm8, bi8, bi8f, bcand, bt8, bj8,
         bjf, bT4, baf, bbf, bE1, bE2, bg8, by, bps_t) = [Buf(n_) for n_ in (
            "cat", "catT", "xt", "ss", "xn2b", "xn2T", "qT", "sc", "m8", "i8", "i8f", "cand",
            "t8", "j8", "jf", "T4", "af", "bf", "E1", "E2", "g8", "y", "ps_t")]
        bx1, bxn2, beidx, bgts = bufs(2, "x1"), bufs(2, "xn2"), bufs(2, "eidx"), bufs(2, "gts")
        badot, bga = bufs(NGRP, "adot"), bufs(NGRP, "ga")
        buv, bdg = bufs(NG, "uv"), bufs(4, "dg")
        bps_o, bps_q, bps_y = bufs(2, "ps_o"), bufs(2, "ps_q"), bufs(2, "ps_y")
        qi = [0]

        def routing(i):
            par = i % 2
            x1_, xn2_, eidx_, gts_ = x1[par], xn2[par], eidx[par], gts[par]
            bx1_, bxn2_, beidx_, bgts_ = bx1[par], bxn2[par], beidx[par], bgts[par]
            rows = slice(i * 128, (i + 1) * 128)
            P.dma("sp", lambda h: h.dma_start(out=cat[:, 0:512], in_=c.AOUT[rows, :]), reads=[c.bAOUT[i]], writes=[bcat])
            P.dma("sp", lambda h: h.dma_start(out=cat[:, 512:1024], in_=c.HM[rows, :]), reads=[c.bHM[i]], writes=[bcat])
            P.dma("sp", lambda h: h.dma_start(out=xt[:], in_=c.x[rows, :]), writes=[bxt])
            for kc in range(8):
                P.op("pe", lambda h, kc=kc: h.transpose(out=ps_t[:, kc * 128:(kc + 1) * 128], in_=cat[:, kc * 128:(kc + 1) * 128],
                                                        identity=identb[:]), reads=[bcat, bconst], writes=[bps_t])
            P.op("act", lambda h: h.copy(out=catT[:].rearrange("p k t -> p (k t)"), in_=ps_t[:]), reads=[bps_t], writes=[bcatT])
            yield
            for g in range(2):
                for kc in range(8):
                    P.op("pe", lambda h, g=g, kc=kc: h.matmul(ps_o[g][:], lhsT=catT[:, kc, :], rhs=Wo[:, kc, g * 512:(g + 1) * 512],
                                                             start=(kc == 0), stop=(kc == 7)), reads=[bcatT, bWo[kc]], writes=[bps_o[g]])
                P.op("dve", lambda h, g=g: h.tensor_tensor(out=x1_[:, g * 512:(g + 1) * 512], in0=ps_o[g][:], in1=xt[:, g * 512:(g + 1) * 512],
                                                          op=ALU.add), reads=[bps_o[g], bxt], writes=[bx1_])
                yield
            P.op("act", lambda h: h.activation(out=junk[:], in_=x1_[:], func=AF.Square, accum_out=ss[:, 0:1]), reads=[bx1_], writes=[bss])
            P.op("act", lambda h: h.activation(out=ss[:, 1:2], in_=ss[:, 0:1], func=AF.Sqrt, scale=1.0 / D, bias=EPS), reads=[bss], writes=[bss])
            P.op("dve", lambda h: h.reciprocal(out=ss[:, 2:3], in_=ss[:, 1:2]), reads=[bss], writes=[bss])
            P.op("dve", lambda h: h.scalar_tensor_tensor(out=xn2_[:], in0=x1_[:], scalar=ss[:, 2:3], in1=gn2[:], op0=ALU.mult, op1=ALU.mult),
                 reads=[bx1_, bss, bconst], writes=[bxn2_])
            P.op("act", lambda h: h.copy(out=xn2b[:], in_=xn2_[:]), reads=[bxn2_], writes=[bxn2b])
            yield
            for kc in range(8):
                P.op("pe", lambda h, kc=kc: h.transpose(out=ps_t[:, kc * 128:(kc + 1) * 128], in_=xn2b[:, kc * 128:(kc + 1) * 128],
                                                        identity=identb[:]), reads=[bxn2b, bconst], writes=[bps_t])
            P.op("act", lambda h: h.copy(out=xn2T[:].rearrange("p k t -> p (k t)"), in_=ps_t[:]), reads=[bps_t], writes=[bxn2T])
            yield
            for qg in range(4):
                k = qi[0] % 2
                qi[0] += 1
                for cc in range(4):
                    hp = qg * 4 + cc
                    for kc in range(8):
                        P.op("pe", lambda h, k=k, cc=cc, hp=hp, kc=kc: h.matmul(ps_q[k][:, cc, :], lhsT=Wq[:, kc, hp * 128:(hp + 1) * 128],
                                                                              rhs=xn2T[:, kc, :], start=(kc == 0), stop=(kc == 7)),
                             reads=[bWq[kc], bxn2T], writes=[bps_q[k]])
                P.op("act", lambda h, k=k, qg=qg: h.copy(out=qT[:, qg * 4:(qg + 1) * 4, :], in_=ps_q[k][:]), reads=[bps_q[k]], writes=[bqT])
                yield
            for qg in range(4):
                k = qi[0] % 2
                qi[0] += 1
                for cc in range(4):
                    hp = qg * 4 + cc
                    P.op("pe", lambda h, k=k, cc=cc, hp=hp: h.matmul(ps_q[k][:, cc, :], lhsT=qT[:, hp, :], rhs=SKT[:, hp, :],
                                                                   start=True, stop=True), reads=[bqT, bSKT], writes=[bps_q[k]])
                P.op("act", lambda h, k=k, qg=qg: h.copy(out=sc[:, qg * 4:(qg + 1) * 4, :], in_=ps_q[k][:]), reads=[bps_q[k]], writes=[bsc])
            yield
            for g in range(16):
                P.op("dve", lambda h, g=g: h.max(out=m8[:, g, 0:8], in_=sc[:, g, :]), reads=[bsc], writes=[bm8])
                P.op("dve", lambda h, g=g: h.max_index(out=i8[:, g, 0:8], in_max=m8[:, g, 0:8], in_values=sc[:, g, :]),
                     reads=[bsc, bm8], writes=[bi8])
                P.op("dve", lambda h, g=g: h.match_replace(out=sc[:, g, :], in_to_replace=m8[:, g, 0:8], in_values=sc[:, g, :],
                                                           imm_value=-1e30), reads=[bm8], writes=[bsc])
                P.op("dve", lambda h, g=g: h.max(out=m8[:, g, 8:16], in_=sc[:, g, :]), reads=[bsc], writes=[bm8])
                P.op("dve", lambda h, g=g: h.max_index(out=i8[:, g, 8:16], in_max=m8[:, g, 8:16], in_values=sc[:, g, :]),
                     reads=[bsc, bm8], writes=[bi8])
                yield
            m8v = m8[:].rearrange("p (a b) k -> p a b k", b=2)
            P.op("dve", lambda h: h.tensor_tensor(
                out=cand[:].rearrange("p a (x y) -> p a x y", y=16),
                in0=m8v[:, :, 0, :].unsqueeze(3).to_broadcast([128, 8, 16, 16]),
                in1=m8v[:, :, 1, :].unsqueeze(2).to_broadcast([128, 8, 16, 16]), op=ALU.add), reads=[bm8], writes=[bcand])
            yield
            for hd in range(8):
                P.op("dve", lambda h, hd=hd: h.max(out=t8[:, hd, 0:8], in_=cand[:, hd, :]), reads=[bcand], writes=[bt8])
                P.op("dve", lambda h, hd=hd: h.max_index(out=j8[:, hd, 0:8], in_max=t8[:, hd, 0:8], in_values=cand[:, hd, :]),
                     reads=[bcand, bt8], writes=[bj8])
                P.op("dve", lambda h, hd=hd: h.match_replace(out=cand[:, hd, :], in_to_replace=t8[:, hd, 0:8], in_values=cand[:, hd, :],
                                                             imm_value=-1e30), reads=[bt8], writes=[bcand])
                P.op("dve", lambda h, hd=hd: h.max(out=t8[:, hd, 8:16], in_=cand[:, hd, :]), reads=[bcand], writes=[bt8])
                P.op("dve", lambda h, hd=hd: h.max_index(out=j8[:, hd, 8:16], in_max=t8[:, hd, 8:16], in_values=cand[:, hd, :]),
                     reads=[bcand, bt8], writes=[bj8])
                yield
            P.op("dve", lambda h: h.tensor_copy(out=i8f[:], in_=i8[:]), reads=[bi8], writes=[bi8f])
            P.op("dve", lambda h: h.tensor_copy(out=jf[:], in_=j8[:].rearrange("p a k -> p (a k)")), reads=[bj8], writes=[bjf])
            P.op("dve", lambda h: h.tensor_tensor(out=T4[:], in0=jf[:].unsqueeze(2).to_broadcast([128, 128, 16]),
                                                  in1=THR[:].unsqueeze(1).to_broadcast([128, 128, 16]), op=ALU.is_ge),
                 reads=[bjf, bconst], writes=[bT4])
            yield
            P.op("dve", lambda h: h.tensor_reduce(out=af[:], in_=T4[:], axis=AX.X, op=ALU.add), reads=[bT4], writes=[baf])
            P.op("dve", lambda h: h.scalar_tensor_tensor(out=bf_[:], in0=af[:], scalar=-16.0, in1=jf[:], op0=ALU.mult, op1=ALU.add),
                 reads=[baf, bjf], writes=[bbf])
            yield
            i8v = i8f[:].rearrange("p (a b) k -> p a b k", b=2)
            for side, (idxt, Et, bE) in enumerate(((af, E1, bE1), (bf_, E2, bE2))):
                P.op("dve", lambda h, idxt=idxt: h.tensor_tensor(out=T4[:], in0=idxt[:].unsqueeze(2).to_broadcast([128, 128, 16]),
                                                                in1=IOT[:].unsqueeze(1).to_broadcast([128, 128, 16]), op=ALU.is_equal),
                     reads=[baf, bbf, bconst], writes=[bT4])
                yield
                P.op("dve", lambda h, side=side: h.tensor_tensor(
                    out=T4[:].rearrange("p (a k) x -> p a k x", k=16), in0=T4[:].rearrange("p (a k) x -> p a k x", k=16),
                    in1=i8v[:, :, side, :].unsqueeze(2).to_broadcast([128, 8, 16, 16]), op=ALU.mult),
                    reads=[bi8f], writes=[bT4])
                yield
                P.op("dve", lambda h, Et=Et: h.tensor_reduce(out=Et[:], in_=T4[:], axis=AX.X, op=ALU.add), reads=[bT4], writes=[bE])
                yield
            P.op("dve", lambda h: h.scalar_tensor_tensor(out=E1[:], in0=E1[:], scalar=128.0, in1=E2[:], op0=ALU.mult, op1=ALU.add),
                 reads=[bE2], writes=[bE1])
            P.op("dve", lambda h: h.tensor_copy(out=eidx_[:], in_=E1[:]), reads=[bE1], writes=[beidx_])
            P.op("dve", lambda h: h.tensor_tensor(out=gts_[:], in0=t8[:], in1=t8[:, :, 0:1].to_broadcast([128, 8, 16]), op=ALU.subtract),
                 reads=[bt8], writes=[bgts_])
            P.op("act", lambda h: h.activation(out=gts_[:], in_=gts_[:], func=AF.Exp), reads=[], writes=[bgts_])
            P.op("dve", lambda h: h.tensor_reduce(out=g8[:, 0:8], in_=gts_[:], axis=AX.X, op=ALU.add), reads=[bgts_], writes=[bg8])
            P.op("dve", lambda h: h.reciprocal(out=g8[:, 8:16], in_=g8[:, 0:8]), reads=[], writes=[bg8])
            P.op("dve", lambda h: h.tensor_tensor(out=gts_[:], in0=gts_[:], in1=g8[:, 8:16].unsqueeze(2).to_broadcast([128, 8, 16]),
                                                  op=ALU.mult), reads=[bg8], writes=[bgts_])
            if "EIDX" in c.dbg:
                P.dma("sp", lambda h: h.dma_start(out=c.EIDX[rows, :], in_=eidx_[:]), reads=[beidx_])
                P.dma("sp", lambda h: h.dma_start(out=c.GTS[rows, :], in_=gts_[:].rearrange("p a k -> p (a k)")), reads=[bgts_])
                P.dma("sp", lambda h: h.dma_start(out=c.X1[rows, :], in_=x1_[:]), reads=[bx1_])
            yield

        def experts(i, nxt):
            par = i % 2
            x1_, xn2_, eidx_, gts_ = x1[par], xn2[par], eidx[par], gts[par]
            bx1_, bxn2_, beidx_, bgts_ = bx1[par], bxn2[par], beidx[par], bgts[par]
            rows = slice(i * 128, (i + 1) * 128)
            gts_f = gts_[:].rearrange("p a k -> p (a k)")

            def stage_a(grp):
                for sl in range(grp * GS, (grp + 1) * GS):
                    k = sl % NG
                    P.dma("pool", lambda h, sl=sl, k=k: h.indirect_dma_start(
                        out=uv[k][:], out_offset=None, in_=c.UVB,
                        in_offset=bass.IndirectOffsetOnAxis(ap=eidx_[:, sl:sl + 1], axis=0)), reads=[beidx_], writes=[buv[k]])
                    P.op("dve", lambda h, sl=sl, k=k: h.scalar_tensor_tensor(out=junk2[:], in0=uv[k][:, 0:D], scalar=1.0, in1=xn2_[:],
                                                                            op0=ALU.mult, op1=ALU.mult, accum_out=adot[:, sl:sl + 1]),
                         reads=[buv[k], bxn2_], writes=[badot[grp]])

            def stage_b(grp):
                g0, g1 = grp * GS, (grp + 1) * GS
                P.op("act", lambda h: h.activation(out=ga[:, g0:g1], in_=adot[:, g0:g1], func=AF.Gelu),
                     reads=[badot[grp]], writes=[bga[grp]])
                for sl in range(g0, g1):
                    k = sl % NG
                    kd = sl % 4
                    P.op("act", lambda h, sl=sl: h.activation(out=ga[:, sl:sl + 1], in_=ga[:, sl:sl + 1], func=AF.Copy,
                                                              scale=gts_f[:, sl:sl + 1]), reads=[bgts_], writes=[bga[grp]])
                    P.op("act", lambda h, sl=sl, kd=kd: h.activation(out=dg[kd][:], in_=identf[:], func=AF.Copy, scale=ga[:, sl:sl + 1]),
                         reads=[bga[grp], bconst], writes=[bdg[kd]])
                    for g in range(2):
                        P.op("pe", lambda h, sl=sl, k=k, kd=kd, g=g: h.matmul(ps_y[g][:], lhsT=dg[kd][:],
                                                                             rhs=uv[k][:, D + g * 512:D + (g + 1) * 512],
                                                                             start=(sl == 0), stop=(sl == 127)),
                             reads=[bdg[kd], buv[k]], writes=[bps_y[g]])

            def adv(nsteps):
                if nxt is None:
                    return
                for _ in range(nsteps):
                    try:
                        next(nxt)
                    except StopIteration:
                        return

            stage_a(0)
            for grp in range(NGRP):
                if grp + 1 < NGRP:
                    stage_a(grp + 1)
                stage_b(grp)
                adv(2)
            for g in range(2):
                P.op("dve", lambda h, g=g: h.tensor_tensor(out=yacc[:, g * 512:(g + 1) * 512], in0=ps_y[g][:], in1=x1_[:, g * 512:(g + 1) * 512],
                                                          op=ALU.add), reads=[bps_y[g], bx1_], writes=[by])
            P.dma("sp", lambda h: h.dma_start(out=c.out[rows, :], in_=yacc[:]), reads=[by], writes=[c.bOUT[i]])
            adv(1000)

        r0 = routing(0)
        for _ in r0:
            pass
        for i in range(NT):
            nxt = routing(i + 1) if i + 1 < NT else None
            if "noexp" in c.dbg or i >= c.nexp:
                par = i % 2
                P.dma("sp", lambda h, i=i, par=par: h.dma_start(out=c.out[i * 128:(i + 1) * 128, :], in_=x1[par][:]),
                      reads=[bx1[par]], writes=[c.bOUT[i]])
                if nxt is not None:
                    for _ in nxt:
                        pass
            else:
                if "noil" in c.dbg and nxt is not None:
                    for _ in nxt:
                        pass
                experts(i, nxt)
    P.barrier()


def build(dbg=(), phases="abcd"):
    nc = bass.Bass("TRN2", target_bir_lowering=False)
    c = Ctx()
    c.nc = nc
    ext_in = lambda n, s, d: nc.dram_tensor(n, s, d, kind="ExternalInput").ap()

    def scratch(n, s, d):
        kind = "ExternalOutput" if n in dbg else "Internal"
        return nc.dram_tensor(n, s, d, kind=kind).ap()

    c.x = ext_in("x", [S, D], F32)
    c.w_in = ext_in("w_in", [D, INW], F32)
    c.norm1_w = ext_in("norm1_w", [128, 8], F32)
    c.identb = ext_in("identb", [128, 128], BF16)
    c.gq = ext_in("gq", [128, 64], F32)
    c.gk = ext_in("gk", [128, 64], F32)
    c.rpbg = ext_in("rpbg", [128, 8, 896], F32)
    c.mask_i = ext_in("mask_i", [128, 896], F32)
    c.mask_a = ext_in("mask_a", [128, 896], F32)
    c.gao = ext_in("gao", [128, 512], F32)
    c.gate_b = ext_in("gate_b", [4, 4], F32)
    c.identf = ext_in("identf", [128, 128], F32)
    c.conv_w = ext_in("conv_w", [128, 8, 5], F32)
    c.conv_b = ext_in("conv_b", [128, 8], F32)
    c.sel = ext_in("sel", [4, 4, 128], F32)
    c.msk = ext_in("msk", [128, 2, 128], F32)
    c.gmn = ext_in("gmn", [128, 512], F32)
    c.w_out = ext_in("w_out", [D, D], F32)
    c.w_q = ext_in("w_q", [D, 2048], F32)
    c.skt = ext_in("skt", [128, 16, 128], F32)
    c.gn2 = ext_in("gn2", [128, D], F32)
    c.thr = ext_in("thr", [128, 16], F32)
    c.iot = ext_in("iot", [128, 16], F32)
    c.peer_uv = ext_in("peer_uv", [16384, 2 * D], F32)
    c.UVB = scratch("UVB", [16384, 2 * D], BF16)
    c.out = nc.dram_tensor("out", [S, D], F32, kind="ExternalOutput").ap()
    c.bOUT = bufs(NT, "OUT")
    c.dbg = dbg
    c.nexp = NT
    for d_ in dbg:
        if d_.startswith("exp"):
            c.nexp = int(d_[3:])
    if "EIDX" in dbg:
        c.EIDX = scratch("EIDX", [S, 128], I32)
        c.GTS = scratch("GTS", [S, 128], F32)
        c.X1 = scratch("X1", [S, D], F32)

    c.QT = scratch("QT", [4, 128, S], BF16)
    c.KT = scratch("KT", [4, 128, S], BF16)
    c.V = scratch("V", [S, 512], BF16)
    c.MV = scratch("MV", [S, 512], BF16)
    c.SIGO = scratch("SIGO", [S, 512], BF16)
    c.MQKT = scratch("MQKT", [1024, S], F32)
    c.GT = scratch("GT", [4, 4, S], F32)
    c.AOUT = scratch("AOUT", [S, 512], BF16)
    c.BCRD = scratch("BCRD", [2, 4, NT, 257], F32)
    c.bBCRD = bufs(2, "BCRD")
    c.HF = scratch("HF", [S, 512], F32)
    c.bHF = bufs(NT, "HF")
    c.HM = scratch("HM", [S, 512], BF16)
    c.bHM = bufs(NT, "HM")
    c.bAOUT = bufs(NT, "AOUT")
    c.bQKT = [bufs(NT, "QT"), bufs(NT, "KT")]
    c.bV, c.bMV, c.bSIGO, c.bMQKT, c.bGT = (bufs(NT, n) for n in ("V", "MV", "SIGO", "MQKT", "GT"))

    with ExitStack() as st:
        c.P = Prog(nc, st)
        if "a" in phases:
            phase_a(c)
        if "b" in phases:
            phase_b(c)
        if "c" in phases:
            phase_c(c)
        if "d" in phases:
            phase_t(c)
            phase_d(c)
        c.P.emit()
    return nc


def host_inputs(inputs, b):
    f32 = np.float32
    m = {}
    m["x"] = np.ascontiguousarray(inputs["x"][b], dtype=f32)
    m["w_in"] = np.ascontiguousarray(inputs["w_in"][0], dtype=f32)
    m["norm1_w"] = np.ascontiguousarray(inputs["norm1_w"][0].reshape(8, 128).T, dtype=f32)
    m["identb"] = np.eye(128, dtype=f32).astype(ml_dtypes.bfloat16)
    m["gq"] = np.ascontiguousarray(np.broadcast_to(inputs["q_norm_w"][0][None, :], (128, 64)), dtype=f32)
    m["gk"] = np.ascontiguousarray(np.broadcast_to(inputs["k_norm_w"][0][None, :], (128, 64)), dtype=f32)
    p = np.arange(128); kr = p // 64; kc = p % 64
    col = np.arange(128); rq = col // 64; cc = col % 64
    dt = np.arange(-3, 4)
    drow = 2 * dt[None, :, None] + kr[:, None, None] - rq[None, None, :]
    dcol = kc[:, None, None] - cc[None, None, :] + 0 * dt[None, :, None]
    rpb = inputs["attn_rpb"][0]
    g = rpb[:, np.clip(drow + 7, 0, 14), np.clip(dcol + 15, 0, 30)]
    m["rpbg"] = np.ascontiguousarray(g.transpose(1, 0, 2, 3).reshape(128, 8, 896), dtype=f32)
    cs = np.clip(cc - 8, 0, 48)
    colvalid = (kc[:, None, None] >= cs[None, None, :]) & (kc[:, None, None] < cs[None, None, :] + 16)
    colvalid = colvalid & (dt[None, :, None] > -100)
    m["mask_a"] = np.where(colvalid & (np.abs(drow) <= 7), 0.0, NEG).astype(f32).reshape(128, 896)
    m["mask_i"] = np.where(colvalid & (drow >= -4) & (drow <= 3), 0.0, NEG).astype(f32).reshape(128, 896)
    m["gate_b"] = np.ascontiguousarray(inputs["mlstm_gate_b"][0].T, dtype=f32)
    m["identf"] = np.eye(128, dtype=f32)
    m["conv_w"] = np.ascontiguousarray(inputs["mlstm_conv_w"][0].reshape(5, 8, 128).transpose(2, 1, 0), dtype=f32)
    m["conv_b"] = np.ascontiguousarray(inputs["mlstm_conv_b"][0].reshape(8, 128).T, dtype=f32)
    sel = np.zeros((4, 4, 128), f32)
    for hh in range(4):
        sel[hh, hh, :] = 1.0
    m["sel"] = sel
    ii = np.arange(128)
    msk = np.zeros((128, 2, 128), f32)
    msk[:, 0, :] = np.where(ii[:, None] <= ii[None, :], 0.0, NEG)
    msk[:, 1, :] = np.where(ii[:, None] >= ii[None, :], 0.0, NEG)
    m["msk"] = msk
    m["gmn"] = np.ascontiguousarray(np.broadcast_to(inputs["mlstm_norm_w"][0][None, :], (128, 512)), dtype=f32)
    m["w_out"] = np.ascontiguousarray(inputs["w_out"][0], dtype=f32)
    m["w_q"] = np.ascontiguousarray(inputs["peer_w_q"][0], dtype=f32)
    m["skt"] = np.ascontiguousarray(inputs["peer_sub_keys"][0].reshape(16, 128, 128).transpose(2, 0, 1), dtype=f32)
    m["gn2"] = np.ascontiguousarray(np.broadcast_to(inputs["norm2_w"][0][None, :], (128, D)), dtype=f32)
    thr = (np.arange(16, dtype=f32) + 1.0) * 16.0
    thr[15] = 1e9
    m["thr"] = np.ascontiguousarray(np.broadcast_to(thr[None, :], (128, 16)), dtype=f32)
    m["iot"] = np.ascontiguousarray(np.broadcast_to(np.arange(16, dtype=f32)[None, :], (128, 16)), dtype=f32)
    m["peer_uv"] = np.ascontiguousarray(np.concatenate([inputs["peer_u"][0], inputs["peer_v"][0]], axis=1), dtype=f32)
    m["gao"] = np.ascontiguousarray(np.broadcast_to(inputs["attn_out_norm_w"][0][None, :], (128, 512)), dtype=f32)
    return m


def kernel(**inputs):
    nc = build()
    in_maps = [host_inputs(inputs, b) for b in range(8)]
    res = run_bass_kernel_spmd(nc, in_maps, core_ids=list(range(8)))
    return np.stack([r["out"] for r in res.results], axis=0).astype(np.float32)
```

```python
import numpy as np
import ml_dtypes
import concourse.bass as bass
import concourse.mybir as mybir
from concourse.bass_utils import run_bass_kernel_spmd
from contextlib import ExitStack

F32 = mybir.dt.float32
BF16 = mybir.dt.bfloat16
I32 = mybir.dt.int32
U32 = mybir.dt.uint32
ALU = mybir.AluOpType
AF = mybir.ActivationFunctionType
AX = mybir.AxisListType

S = 4096
D = 1024
NT = 32
INW = 3600
EPS = 1e-6
NEG = -30000.0


class Buf:
    __slots__ = ("name", "w", "r")

    def __init__(self, name=""):
        self.name = name
        self.w = {}
        self.r = {}


class Prog:
    ENG = ("pe", "act", "dve", "pool", "sp")
    NRINGS = {"sp": 12, "act": 6, "pool": 48}

    def __init__(self, nc, stack):
        self.nc = nc
        self.sem = {e: stack.enter_context(nc.semaphore("s_" + e)) for e in self.ENG}
        self.ops = {e: [] for e in self.ENG}
        self.seen = {e: {} for e in self.ENG}
        self.ring = {}
        self.ring_i = {}
        self.ring_tok = {}
        for q in ("sp", "act", "pool"):
            self.ring[q] = [stack.enter_context(nc.semaphore("r_%s%d" % (q, i)))
                            for i in range(self.NRINGS[q])]
            self.ring_i[q] = 0
            self.ring_tok[q] = [None] * self.NRINGS[q]

    @staticmethod
    def _key(tok):
        return ("c", tok[1]) if tok[0] == "c" else ("d", id(tok[1]))

    @staticmethod
    def _val(tok):
        return tok[2]

    def _need(self, eng, tok, waits, same_ok=False):
        if tok[0] == "c" and tok[1] == eng and (eng == "pe" or same_ok):
            return
        k = self._key(tok)
        if self.seen[eng].get(k, -1) >= tok[2]:
            return
        self.seen[eng][k] = tok[2]
        waits.append(tok)

    def _deps(self, eng, reads, writes):
        waits = []
        for b in reads:
            for t in b.w.values():
                self._need(eng, t, waits)
        for b in writes:
            for t in b.w.values():
                self._need(eng, t, waits)
            for t in b.r.values():
                self._need(eng, t, waits)
        return waits

    def _commit(self, tok, reads, writes):
        k = self._key(tok)
        for b in reads:
            b.r[k] = tok
        for b in writes:
            b.w = {k: tok}
            b.r = {}

    def op(self, eng, fn, reads=(), writes=()):
        waits = self._deps(eng, reads, writes)
        tok = ("c", eng, len(self.ops[eng]))
        self.ops[eng].append([waits, fn, None])
        self._commit(tok, reads, writes)
        return tok

    def dma(self, q, fn, reads=(), writes=(), final=False):
        waits = self._deps(q, reads, writes)
        i = self.ring_i[q]
        nr = self.NRINGS[q]
        slot = i % nr
        prev = self.ring_tok[q][slot]
        if prev is not None:
            self._need(q, prev, waits)
        sem = self.ring[q][slot]
        val = 16 * (i // nr + 1)
        self.ring_i[q] = i + 1
        tok = ("d", sem, val, q)
        self.ring_tok[q][slot] = tok
        self.ops[q].append([waits, fn, (sem, 16)])
        self._commit(tok, reads, writes)
        return tok

    def _all_tokens(self):
        toks = []
        for e in ("pe", "act", "dve", "pool"):
            for idx in range(len(self.ops[e]) - 1, -1, -1):
                ent = self.ops[e][idx]
                if ent[1] is not None and ent[2] is None:
                    toks.append(("c", e, idx))
                    break
        for q in ("act", "pool", "sp"):
            for t in self.ring_tok[q]:
                if t is not None:
                    toks.append(t)
        return toks

    def barrier(self):
        toks = self._all_tokens()
        for e in self.ENG:
            waits = []
            for t in toks:
                k = self._key(t)
                if self.seen[e].get(k, -1) >= t[2]:
                    continue
                self.seen[e][k] = t[2]
                waits.append(t)
            if waits:
                self.ops[e].append([waits, None, None])

    def emit(self):
        nc = self.nc
        final_waits = []
        for t in self._all_tokens():
            self._need("sp", t, final_waits)
        signal = {e: set() for e in self.ENG}
        allw = [final_waits]
        for e in self.ENG:
            for ent in self.ops[e]:
                allw.append(ent[0])
        for ws in allw:
            for t in ws:
                if t[0] == "c":
                    signal[t[1]].add(t[2])
        semval = {e: {} for e in self.ENG}
        for e in self.ENG:
            n = 0
            for idx in sorted(signal[e]):
                n += 1
                semval[e][idx] = n

        def resolve(t):
            if t[0] == "c":
                return self.sem[t[1]], semval[t[1]][t[2]]
            return t[1], t[2]

        def run(e, handle, extra=None):
            for idx, (waits, fn, dinc) in enumerate(self.ops[e]):
                for t in waits:
                    sem, val = resolve(t)
                    handle.wait_ge(sem, val)
                if fn is not None:
                    ins = fn(handle)
                    if dinc is not None:
                        ins.then_inc(dinc[0], dinc[1])
                    elif idx in signal[e]:
                        ins.then_inc(self.sem[e], 1)
            if extra:
                for t in extra:
                    sem, val = resolve(t)
                    handle.wait_ge(sem, val)

        with nc.Block() as block:
            @block.tensor
            def _(h):
                run("pe", h)

            @block.scalar
            def _(h):
                run("act", h)

            @block.vector
            def _(h):
                run("dve", h)

            @block.gpsimd
            def _(h):
                run("pool", h)

            @block.sync
            def _(h):
                run("sp", h, final_waits)


class Ctx:
    pass


def bufs(n, name=""):
    return [Buf("%s%d" % (name, i)) for i in range(n)]


def phase_a(c):
    nc, P = c.nc, c.P
    with ExitStack() as st:
        sb = lambda n, s, d: st.enter_context(nc.sbuf_tensor(n, s, d))
        ps = lambda n, s, d: st.enter_context(nc.psum_tensor(n, s, d))
        Wbf = sb("a_Wbf", [128, 8, INW], BF16)
        stage = [sb("a_stage%d" % i, [128, INW], F32) for i in range(2)]
        w1 = sb("a_w1", [128, 8], F32)
        identb = sb("a_identb", [128, 128], BF16)
        gq = sb("a_gq", [128, 64], F32)
        gk = sb("a_gk", [128, 64], F32)
        xt = [sb("a_xt%d" % i, [128, D], F32) for i in range(2)]
        junk = sb("a_junk", [128, D], F32)
        ss = sb("a_ss", [128, 4], F32)
        xn = sb("a_xn", [128, D], BF16)
        xnT = sb("a_xnT", [128, 8, 128], BF16)
        sq = sb("a_sq", [128, 512], F32)
        tmp = sb("a_tmp", [128, 512], F32)
        s8 = sb("a_s8", [128, 24], F32)
        qn = sb("a_qn", [128, 512], BF16)
        qTs = sb("a_qTs", [128, 4, 128], BF16)
        ob = [sb("a_ob%d" % i, [128, 512], BF16) for i in range(2)]
        fm = [sb("a_fm%d" % i, [128, 4, 128], F32) for i in range(2)]
        gsb = sb("a_gsb", [4, 4, 128], F32)
        ps_t = ps("a_ps_t", [128, D], BF16)
        ps_g = [ps("a_ps_g%d" % i, [128, 512], F32) for i in range(2)]
        ps_q = ps("a_ps_q", [128, 4, 128], BF16)
        ps_f = [ps("a_ps_f%d" % i, [128, 4, 128], F32) for i in range(2)]
        ps_gt = ps("a_ps_gt", [4, 4, 128], F32)

        bW = bufs(8, "W")
        bst = bufs(2, "st")
        bc = Buf("consts")
        bxt = bufs(2, "xt")
        bjunk, bss, bxn, bxnT, bsq, btmp, bs8, bqn, bqTs = [Buf(n) for n in
            ("junk", "ss", "xn", "xnT", "sq", "tmp", "s8", "qn", "qTs")]
        bob = bufs(2, "ob")
        bfm = bufs(2, "fm")
        bgsb = Buf("gsb")
        bps_t, bps_q, bps_gt = Buf("ps_t"), Buf("ps_q"), Buf("ps_gt")
        bps_g = bufs(2, "ps_g")
        bps_f = bufs(2, "ps_f")

        P.dma("sp", lambda h: h.dma_start(out=w1[:], in_=c.norm1_w), writes=[bc])
        P.dma("sp", lambda h: h.dma_start(out=identb[:], in_=c.identb), writes=[bc])
        P.dma("sp", lambda h: h.dma_start(out=gq[:], in_=c.gq), writes=[bc])
        P.dma("sp", lambda h: h.dma_start(out=gk[:], in_=c.gk), writes=[bc])
        for kc in range(8):
            s_ = stage[kc % 2]
            P.dma("sp", lambda h, kc=kc, s_=s_: h.dma_start(out=s_[:], in_=c.w_in[kc * 128:(kc + 1) * 128, :]),
                  writes=[bst[kc % 2]])
            P.op("dve" if kc % 2 == 0 else "pool",
                 lambda h, kc=kc, s_=s_: h.tensor_scalar(out=Wbf[:, kc, :], in0=s_[:], scalar1=w1[:, kc:kc + 1],
                                                         scalar2=None, op0=ALU.mult),
                 reads=[bst[kc % 2], bc], writes=[bW[kc]])

        gi = [0]

        def mm_group_tok(cols, sub=None):
            k = gi[0] % 2
            gi[0] += 1
            c0, c1 = cols
            for kc in range(8):
                P.op("pe", lambda h, kc=kc, k=k: h.matmul(ps_g[k][:, 0:c1 - c0], lhsT=xnT[:, kc, :],
                                                       rhs=Wbf[:, kc, c0:c1], start=(kc == 0), stop=(kc == 7)),
                     reads=[bxnT, bW[kc]], writes=[bps_g[k]])
            return k

        oi = [0]
        fi = [0]
        xn2_ = [xn, sb("a_xn_b", [128, D], BF16)]
        xnT2 = [xnT, sb("a_xnT_b", [128, 8, 128], BF16)]
        ps_t2 = [ps_t, ps("a_ps_t_b", [128, D], BF16)]
        bxn2, bxnT2, bps_t2 = [bxn, Buf("xn_b")], [bxnT, Buf("xnT_b")], [bps_t, Buf("ps_t_b")]
        sq2 = [sq, sb("a_sq_b", [128, 512], F32)]
        tmp2 = [tmp, sb("a_tmp_b", [128, 512], F32)]
        s82 = [s8, sb("a_s8_b", [128, 24], F32)]
        qn2 = [qn, sb("a_qn_b", [128, 512], BF16)]
        qTs2 = [qTs, sb("a_qTs_b", [128, 4, 128], BF16)]
        bsq2, btmp2, bs82, bqn2, bqTs2 = [bsq, Buf("sq_b")], [btmp, Buf("tmp_b")], [bs8, Buf("s8_b")], [bqn, Buf("qn_b")], [bqTs, Buf("qTs_b")]

        def front(i):
            p = i % 2
            x_, bx_ = xt[p], bxt[p]
            P.dma("sp", lambda h: h.dma_start(out=x_[:], in_=c.x[i * 128:(i + 1) * 128, :]), writes=[bx_])
            P.op("act", lambda h: h.activation(out=junk[:], in_=x_[:], func=AF.Square, accum_out=ss[:, 0:1]),
                 reads=[bx_], writes=[bss])
            P.op("act", lambda h: h.activation(out=ss[:, 1:2], in_=ss[:, 0:1], func=AF.Sqrt, scale=1.0 / D, bias=EPS),
                 reads=[bss], writes=[bss])
            P.op("dve", lambda h: h.reciprocal(out=ss[:, 2:3], in_=ss[:, 1:2]), reads=[bss], writes=[bss])
            P.op("dve", lambda h: h.tensor_scalar(out=xn2_[p][:], in0=x_[:], scalar1=ss[:, 2:3], scalar2=None,
                                                  op0=ALU.mult), reads=[bx_, bss], writes=[bxn2[p]])
            for kc in range(8):
                P.op("pe", lambda h, kc=kc: h.transpose(out=ps_t2[p][:, kc * 128:(kc + 1) * 128],
                                                        in_=xn2_[p][:, kc * 128:(kc + 1) * 128], identity=identb[:]),
                     reads=[bxn2[p], bc], writes=[bps_t2[p]])
            P.op("act", lambda h: h.copy(out=xnT2[p][:].rearrange("p k t -> p (k t)"), in_=ps_t2[p][:]),
                 reads=[bps_t2[p]], writes=[bxnT2[p]])

        def body(i):
            p = i % 2
            xT, bxT = xnT2[p], bxnT2[p]

            def mm_tok(c0):
                k = gi[0] % 2
                gi[0] += 1
                for kc in range(8):
                    P.op("pe", lambda h, kc=kc, k=k: h.matmul(ps_g[k][:], lhsT=xT[:, kc, :], rhs=Wbf[:, kc, c0:c0 + 512],
                                                           start=(kc == 0), stop=(kc == 7)),
                         reads=[bxT, bW[kc]], writes=[bps_g[k]])
                return k

            def qk_chain(w, k, gain):
                P.op("act", lambda h: h.activation(out=sq2[w][:], in_=ps_g[k][:], func=AF.Square),
                     reads=[bps_g[k]], writes=[bsq2[w]])
                P.op("dve", lambda h: h.tensor_reduce(out=s82[w][:, 0:8], in_=sq2[w][:].rearrange("p (a b) -> p a b", b=64),
                                                      axis=AX.X, op=ALU.add), reads=[bsq2[w]], writes=[bs82[w]])
                P.op("act", lambda h: h.activation(out=s82[w][:, 8:16], in_=s82[w][:, 0:8], func=AF.Sqrt, scale=1.0 / 64,
                                                   bias=EPS), reads=[], writes=[bs82[w]])
                P.op("dve", lambda h: h.reciprocal(out=s82[w][:, 16:24], in_=s82[w][:, 8:16]), reads=[], writes=[bs82[w]])
                P.op("dve", lambda h: h.tensor_tensor(
                    out=tmp2[w][:].rearrange("p (a b) -> p a b", b=64),
                    in0=ps_g[k][:].rearrange("p (a b) -> p a b", b=64),
                    in1=s82[w][:, 16:24].unsqueeze(2).to_broadcast([128, 8, 64]), op=ALU.mult),
                    reads=[bps_g[k], bs82[w]], writes=[btmp2[w]])
                P.op("pool", lambda h: h.tensor_tensor(
                    out=qn2[w][:].rearrange("p (a b) -> p a b", b=64),
                    in0=tmp2[w][:].rearrange("p (a b) -> p a b", b=64),
                    in1=gain[:].unsqueeze(1).to_broadcast([128, 8, 64]), op=ALU.mult),
                    reads=[btmp2[w], bc], writes=[bqn2[w]])

            def qk_tr(w, dst):
                for hp in range(4):
                    P.op("pe", lambda h, hp=hp: h.transpose(out=ps_q[:, hp, :], in_=qn2[w][:, hp * 128:(hp + 1) * 128],
                                                            identity=identb[:]), reads=[bqn2[w], bc], writes=[bps_q])
                P.op("act", lambda h: h.copy(out=qTs2[w][:], in_=ps_q[:]), reads=[bps_q], writes=[bqTs2[w]])
                P.dma("sp", lambda h: h.dma_start(
                    out=dst[:, :, i * 128:(i + 1) * 128].rearrange("a p t -> p a t"), in_=qTs2[w][:]),
                    reads=[bqTs2[w]], writes=[c.bQKT[w][i]])

            def tok_out(c0, dst, bdst, fn):
                k = mm_tok(c0)
                o = oi[0] % 2
                oi[0] += 1
                P.op("act", lambda h: h.activation(out=ob[o][:], in_=ps_g[k][:], func=fn),
                     reads=[bps_g[k]], writes=[bob[o]])
                P.dma("sp", lambda h: h.dma_start(out=dst[i * 128:(i + 1) * 128, :], in_=ob[o][:]),
                      reads=[bob[o]], writes=[bdst[i]])

            def fm_half(half):
                f = fi[0] % 2
                fi[0] += 1
                for cc in range(4):
                    ch = half * 4 + cc
                    col = 1536 + ch * 128
                    for kc in range(8):
                        P.op("pe", lambda h, kc=kc, cc=cc, col=col: h.matmul(
                            ps_f[f][:, cc, :], lhsT=Wbf[:, kc, col:col + 128], rhs=xT[:, kc, :],
                            start=(kc == 0), stop=(kc == 7)), reads=[bxT, bW[kc]], writes=[bps_f[f]])
                P.op("dve", lambda h: h.tensor_copy(out=fm[f][:], in_=ps_f[f][:]), reads=[bps_f[f]], writes=[bfm[f]])
                P.dma("sp", lambda h: h.dma_start(
                    out=c.MQKT[half * 512:(half + 1) * 512, i * 128:(i + 1) * 128].rearrange("(a p) t -> p a t", p=128),
                    in_=fm[f][:]), reads=[bfm[f]], writes=[c.bMQKT[i]])

            kq = mm_tok(0)
            qk_chain(0, kq, gq)
            kk = mm_tok(512)
            qk_chain(1, kk, gk)
            tok_out(1024, c.V, c.bV, AF.Copy)
            qk_tr(0, c.QT)
            tok_out(2560, c.MV, c.bMV, AF.Copy)
            qk_tr(1, c.KT)
            tok_out(3072, c.SIGO, c.bSIGO, AF.Sigmoid)
            fm_half(0)
            fm_half(1)
            for g in range(4):
                col = 3584 + 4 * g
                for kc in range(8):
                    P.op("pe", lambda h, kc=kc, g=g, col=col: h.matmul(
                        ps_gt[:, g, :], lhsT=Wbf[:, kc, col:col + 4], rhs=xT[:, kc, :],
                        start=(kc == 0), stop=(kc == 7)), reads=[bxT, bW[kc]], writes=[bps_gt])
            P.op("dve", lambda h: h.tensor_copy(out=gsb[:], in_=ps_gt[:]), reads=[bps_gt], writes=[bgsb])
            P.dma("sp", lambda h: h.dma_start(out=c.GT[:, :, i * 128:(i + 1) * 128].rearrange("g a t -> a g t"),
                                              in_=gsb[:]), reads=[bgsb], writes=[c.bGT[i]])

        front(0)
        for i in range(NT):
            if i + 1 < NT:
                front(i + 1)
            body(i)
    P.barrier()


def phase_b(c):
    nc, P = c.nc, c.P
    with ExitStack() as st:
        sb = lambda n, s, d: st.enter_context(nc.sbuf_tensor(n, s, d))
        ps = lambda n, s, d: st.enter_context(nc.psum_tensor(n, s, d))
        QT = sb("b_QT", [128, 4, S], BF16)
        KT = sb("b_KT", [128, 4, S], BF16)
        V = sb("b_V", [128, NT, 8, 65], BF16)
        TBI = sb("b_TBI", [128, 8, 896], F32)
        TBA = sb("b_TBA", [128, 8, 896], F32)
        MI = sb("b_MI", [128, 896], F32)
        MA = sb("b_MA", [128, 896], F32)
        gao = sb("b_gao", [128, 512], F32)
        sT = [sb("b_sT%d" % i, [128, 640], F32) for i in range(2)]
        pT = [sb("b_pT%d" % i, [128, 640], BF16) for i in range(2)]
        ao = sb("b_ao", [128, 512], F32)
        junk = sb("b_junk", [128, 512], F32)
        rc = [sb("b_rc%d" % i, [128, 1], F32) for i in range(2)]
        ss = sb("b_ss", [128, 4], F32)
        aob = [sb("b_aob%d" % i, [128, 512], BF16) for i in range(2)]
        ps_s = [ps("b_ps_s%d" % i, [128, 1024], F32) for i in range(2)]
        ps_o = [ps("b_ps_o%d" % i, [128, 128], F32) for i in range(2)]

        bQT, bKT = bufs(4, "bQT"), bufs(4, "bKT")
        bVt = bufs(NT, "bV")
        bones, btb, bm, bgao = Buf("ones"), Buf("tb"), Buf("m"), Buf("gao")
        bsT, bpT, brc, baob = bufs(2, "sT"), bufs(2, "pT"), bufs(2, "rc"), bufs(2, "aob")
        bao, bjunk, bss = Buf("ao"), Buf("junk"), Buf("ss")
        bps_s, bps_o = bufs(2, "ps_s"), bufs(2, "ps_o")

        P.dma("sp", lambda h: h.dma_start(out=TBI[:], in_=c.rpbg), writes=[btb])
        P.dma("sp", lambda h: h.dma_start(out=MI[:], in_=c.mask_i), writes=[bm])
        P.dma("sp", lambda h: h.dma_start(out=MA[:], in_=c.mask_a), writes=[bm])
        P.dma("sp", lambda h: h.dma_start(out=gao[:], in_=c.gao), writes=[bgao])
        for hp in range(4):
            P.dma("sp", lambda h, hp=hp: h.dma_start(out=QT[:, hp, :], in_=c.QT[hp]),
                  reads=c.bQKT[0], writes=[bQT[hp]])
            P.dma("act", lambda h, hp=hp: h.dma_start(out=KT[:, hp, :], in_=c.KT[hp]),
                  reads=c.bQKT[1], writes=[bKT[hp]])
        P.op("pool", lambda h: h.memset(V[:, :, :, 64:65], 1.0), writes=[bones])
        for i in range(NT):
            P.dma("sp" if i % 2 == 0 else "act", lambda h, i=i: h.dma_start(
                out=V[:, i, :, 0:64], in_=c.V[i * 128:(i + 1) * 128, :].rearrange("p (a b) -> p a b", b=64)),
                reads=[c.bV[i]], writes=[bVt[i]])
        for hd in range(8):
            P.op("dve", lambda h, hd=hd: h.tensor_tensor(out=TBA[:, hd, :], in0=TBI[:, hd, :], in1=MA[:], op=ALU.add),
                 reads=[btb, bm], writes=[btb])
        for hd in range(8):
            P.op("dve", lambda h, hd=hd: h.tensor_tensor(out=TBI[:, hd, :], in0=TBI[:, hd, :], in1=MI[:], op=ALU.add),
                 reads=[btb, bm], writes=[btb])

        it = 0
        for j in range(NT):
            if 2 <= j <= 29:
                kts = list(range(j - 2, j + 3)); tb = TBI; s0 = 1
            elif j == 0:
                kts = [0, 1, 2, 3]; tb = TBA; s0 = 3
            elif j == 1:
                kts = [0, 1, 2, 3]; tb = TBA; s0 = 2
            elif j == 30:
                kts = [28, 29, 30, 31]; tb = TBA; s0 = 1
            else:
                kts = [28, 29, 30, 31]; tb = TBA; s0 = 0
            n = len(kts)
            def st_mm(hd, k):
                hp, hh = hd // 2, hd % 2
                p0, p1 = hh * 64, hh * 64 + 64
                for idx, kt in enumerate(kts):
                    P.op("pe", lambda h, k=k, idx=idx, kt=kt, hp=hp, p0=p0, p1=p1, j=j: h.matmul(
                        ps_s[k][:, idx * 128:(idx + 1) * 128], lhsT=KT[p0:p1, hp, kt * 128:(kt + 1) * 128],
                        rhs=QT[p0:p1, hp, j * 128:(j + 1) * 128], start=True, stop=True),
                        reads=[bKT[hp], bQT[hp]], writes=[bps_s[k]])

            def post(hd, k):
                P.op("dve", lambda h, k=k, n=n, tb=tb, s0=s0, hd=hd: h.scalar_tensor_tensor(
                    out=sT[k][:, 0:n * 128], in0=ps_s[k][:, 0:n * 128], scalar=0.125,
                    in1=tb[:, hd, s0 * 128:(s0 + n) * 128], op0=ALU.mult, op1=ALU.add),
                    reads=[bps_s[k], btb], writes=[bsT[k]])
                P.op("act", lambda h, k=k, n=n: h.activation(out=pT[k][:, 0:n * 128], in_=sT[k][:, 0:n * 128],
                                                             func=AF.Exp), reads=[bsT[k]], writes=[bpT[k]])

            def pv(hd, k):
                for idx, kt in enumerate(kts):
                    P.op("pe", lambda h, k=k, idx=idx, kt=kt, hd=hd, n=n: h.matmul(
                        ps_o[k][:, 0:65], lhsT=pT[k][:, idx * 128:(idx + 1) * 128], rhs=V[:, kt, hd, :],
                        start=(idx == 0), stop=(idx == n - 1)),
                        reads=[bpT[k], bVt[kt], bones], writes=[bps_o[k]])
                P.op("dve", lambda h, k=k: h.reciprocal(out=rc[k][:], in_=ps_o[k][:, 64:65]),
                     reads=[bps_o[k]], writes=[brc[k]])
                P.op("dve", lambda h, k=k, hd=hd: h.tensor_scalar(
                    out=ao[:, hd * 64:(hd + 1) * 64], in0=ps_o[k][:, 0:64], scalar1=rc[k][:], scalar2=None,
                    op0=ALU.mult), reads=[bps_o[k], brc[k]], writes=[bao])

            st_mm(0, it % 2)
            for hd in range(8):
                k = it % 2
                it += 1
                post(hd, k)
                if hd + 1 < 8:
                    st_mm(hd + 1, it % 2)
                pv(hd, k)
            o = j % 2
            P.op("act", lambda h: h.activation(out=junk[:], in_=ao[:], func=AF.Square, accum_out=ss[:, 0:1]),
                 reads=[bao], writes=[bjunk, bss])
            P.op("act", lambda h: h.activation(out=ss[:, 1:2], in_=ss[:, 0:1], func=AF.Sqrt, scale=1.0 / 512, bias=EPS),
                 reads=[bss], writes=[bss])
            P.op("dve", lambda h: h.reciprocal(out=ss[:, 2:3], in_=ss[:, 1:2]), reads=[bss], writes=[bss])
            P.op("dve", lambda h, o=o: h.scalar_tensor_tensor(out=aob[o][:], in0=ao[:], scalar=ss[:, 2:3], in1=gao[:],
                                                          op0=ALU.mult, op1=ALU.mult),
                 reads=[bao, bss, bgao], writes=[baob[o]])
            P.dma("sp", lambda h, j=j, o=o: h.dma_start(out=c.AOUT[j * 128:(j + 1) * 128, :], in_=aob[o][:]),
                  reads=[baob[o]], writes=[c.bAOUT[j]])
    P.barrier()


def phase_c(c):
    nc, P = c.nc, c.P
    with ExitStack() as st0:
        sb0 = lambda n, s, d: st0.enter_context(nc.sbuf_tensor(n, s, d))
        COLS = sb0("c_COLS", [128, NT, 24], F32)
        bCOLS = Buf("COLS")
        with ExitStack() as st:
            sb = lambda n, s, d: st.enter_context(nc.sbuf_tensor(n, s, d))
            ps = lambda n, s, d: st.enter_context(nc.psum_tensor(n, s, d))
            G1, G2, CL, Aa, AA, ZER, T1 = [sb("c1_" + n, [4, S], F32) for n in ("G1", "G2", "CL", "Aa", "AA", "ZER", "T1")]
            bG1, bG2, bCL, bAa, bAA, bZER, bT1 = [Buf(n) for n in ("G1", "G2", "CL", "Aa", "AA", "ZER", "T1")]
            BCR = sb("c1_BCR", [4, NT, 257], F32)
            ROWS = sb("c1_ROWS", [24, S], F32)
            gb = sb("c1_gb", [4, 4], F32)
            ngb = sb("c1_ngb", [4, 4], F32)
            AE = sb("c1_AE", [4, NT], F32)
            APv = sb("c1_AP", [4, NT], F32)
            dd = sb("c1_dd", [4, NT], F32)
            identf = sb("c1_identf", [128, 128], F32)
            ps_c = ps("c1_ps_c", [128, NT, 32], F32)
            bBCR, bgb, bAE, bAPv, bdd, bid, bps_c = [Buf(n) for n in ("BCR", "gb", "AE", "AP", "dd", "id", "ps_c")]
            bROWS = bufs(6, "ROWS")
            P.dma("sp", lambda h: h.dma_start(out=gb[:], in_=c.gate_b), writes=[bgb])
            P.dma("sp", lambda h: h.dma_start(out=identf[:], in_=c.identf), writes=[bid])
            P.op("dve", lambda h: h.tensor_scalar(out=ngb[:], in0=gb[:], scalar1=-1.0, scalar2=None, op0=ALU.mult),
                 reads=[bgb], writes=[bgb])
            P.op("pool", lambda h: h.memset(ZER[:], 0.0), writes=[bZER])
            for d in range(2):
                rv = (lambda t: t[:, :]) if d == 0 else (lambda t: t[:, ::-1])
                gi_, gf_ = 2 * d, 2 * d + 1
                P.dma("sp", lambda h, gi_=gi_: h.dma_start(out=G1[:], in_=c.GT[gi_]), reads=c.bGT, writes=[bG1])
                P.dma("sp", lambda h, gf_=gf_: h.dma_start(out=G2[:], in_=c.GT[gf_]), reads=c.bGT, writes=[bG2])
                P.op("act", lambda h, gf_=gf_: h.activation(out=G2[:], in_=G2[:], func=AF.Exp, scale=-1.0,
                                                            bias=ngb[:, gf_:gf_ + 1]), reads=[bG2, bgb], writes=[bG2])
                P.op("act", lambda h: h.activation(out=G2[:], in_=G2[:], func=AF.Ln, bias=1.0), reads=[bG2], writes=[bG2])
                P.op("dve", lambda h, rv=rv: h.tensor_tensor_scan(out=rv(CL), data0=rv(G2), data1=ZER[:], initial=0.0,
                                                                  op0=ALU.add, op1=ALU.add),
                     reads=[bG2, bZER], writes=[bCL])
                P.op("dve", lambda h, gi_=gi_: h.scalar_tensor_tensor(out=Aa[:], in0=G1[:], scalar=gb[:, gi_:gi_ + 1],
                                                                      in1=CL[:], op0=ALU.add, op1=ALU.add),
                     reads=[bG1, bgb, bCL], writes=[bAa])
                P.op("dve", lambda h, rv=rv: h.tensor_tensor_scan(out=rv(AA), data0=rv(Aa), data1=ZER[:], initial=0.0,
                                                                  op0=ALU.max, op1=ALU.add),
                     reads=[bAa, bZER], writes=[bAA])
                P.op("dve", lambda h: h.tensor_tensor(out=T1[:], in0=CL[:], in1=AA[:], op=ALU.subtract),
                     reads=[bCL, bAA], writes=[bT1])
                P.op("act", lambda h: h.activation(out=T1[:], in_=T1[:], func=AF.Exp), reads=[bT1], writes=[bT1])
                P.dma("sp", lambda h, d=d: h.dma_start(out=ROWS[12 * d + 8:12 * d + 12, :], in_=T1[:]),
                      reads=[bT1], writes=[bROWS[3 * d + 2]])
                P.dma("sp", lambda h, d=d: h.dma_start(out=ROWS[12 * d:12 * d + 4, :], in_=Aa[:]),
                      reads=[bAa], writes=[bROWS[3 * d]])
                AAv = AA[:].rearrange("p (c t) -> p c t", t=128)
                epos = 127 if d == 0 else 0
                P.op("dve", lambda h, AAv=AAv, epos=epos: h.tensor_copy(out=AE[:], in_=AAv[:, :, epos]),
                     reads=[bAA], writes=[bAE])
                P.op("dve", lambda h: h.memset(APv[:], 0.0), writes=[bAPv])
                if d == 0:
                    P.op("dve", lambda h: h.tensor_copy(out=APv[:, 1:NT], in_=AE[:, 0:NT - 1]), reads=[bAE], writes=[bAPv])
                else:
                    P.op("dve", lambda h: h.tensor_copy(out=APv[:, 0:NT - 1], in_=AE[:, 1:NT]), reads=[bAE], writes=[bAPv])
                G1v = G1[:].rearrange("p (c t) -> p c t", t=128)
                G2v = G2[:].rearrange("p (c t) -> p c t", t=128)
                Aav = Aa[:].rearrange("p (c t) -> p c t", t=128)
                P.op("dve", lambda h, G1v=G1v, Aav=Aav: h.tensor_tensor(
                    out=G1v, in0=Aav, in1=AE[:].unsqueeze(2).to_broadcast([4, NT, 128]), op=ALU.subtract),
                    reads=[bAa, bAE], writes=[bG1])
                P.op("act", lambda h: h.activation(out=G1[:], in_=G1[:], func=AF.Exp), reads=[bG1], writes=[bG1])
                P.dma("sp", lambda h, d=d: h.dma_start(out=ROWS[12 * d + 4:12 * d + 8, :], in_=G1[:]),
                      reads=[bG1], writes=[bROWS[3 * d + 1]])
                P.op("dve", lambda h, AAv=AAv: h.tensor_scalar(out=BCR[:, :, 0:128], in0=AAv, scalar1=-1.0, scalar2=None,
                                                               op0=ALU.mult), reads=[bAA], writes=[bBCR])
                P.op("dve", lambda h, G2v=G2v, AAv=AAv: h.tensor_tensor(
                    out=G2v, in0=APv[:].unsqueeze(2).to_broadcast([4, NT, 128]), in1=AAv, op=ALU.subtract),
                    reads=[bAA, bAPv], writes=[bG2])
                P.op("act", lambda h, G2v=G2v: h.activation(out=BCR[:, :, 128:256], in_=G2v, func=AF.Exp),
                     reads=[bG2], writes=[bBCR])
                P.op("dve", lambda h: h.tensor_tensor(out=dd[:], in0=APv[:], in1=AE[:], op=ALU.subtract),
                     reads=[bAPv, bAE], writes=[bdd])
                P.op("act", lambda h: h.activation(out=BCR[:, :, 256], in_=dd[:], func=AF.Exp), reads=[bdd], writes=[bBCR])
                P.dma("sp", lambda h, d=d: h.dma_start(out=c.BCRD[d], in_=BCR[:]), reads=[bBCR], writes=[c.bBCRD[d]])
            for ch in range(NT):
                P.op("pe", lambda h, ch=ch: h.transpose(out=ps_c[:, ch, 0:24], in_=ROWS[0:24, ch * 128:(ch + 1) * 128],
                                                        identity=identf[0:24, 0:24]), reads=bROWS + [bid], writes=[bps_c])
            P.op("dve", lambda h: h.tensor_copy(out=COLS[:], in_=ps_c[:, :, 0:24]), reads=[bps_c], writes=[bCOLS])
        P.barrier()

        with ExitStack() as st:
            sb = lambda n, s, d: st.enter_context(nc.sbuf_tensor(n, s, d))
            ps = lambda n, s, d: st.enter_context(nc.psum_tensor(n, s, d))
            QKT = sb("c_QKT", [128, 8, S], BF16)
            bQKT = bufs(8, "cQKT")
            cw = sb("c_cw", [128, 8, 5], F32)
            cb = sb("c_cb", [128, 8], F32)
            bcw = Buf("cw")
            P.dma("sp", lambda h: h.dma_start(out=cw[:], in_=c.conv_w), writes=[bcw])
            P.dma("sp", lambda h: h.dma_start(out=cb[:], in_=c.conv_b), writes=[bcw])
            with ExitStack() as st2:
                sb2 = lambda n, s, d: st2.enter_context(nc.sbuf_tensor(n, s, d))
                xpad = [sb2("c2_xpad%d" % i, [128, S + 4], F32) for i in range(2)]
                acc = [sb2("c2_acc%d" % i, [128, S], F32) for i in range(2)]
                bxp, bacc = bufs(2, "xpad"), bufs(2, "acc")
                for i in range(2):
                    P.op("pool", lambda h, i=i: h.memset(xpad[i][:, 0:2], 0.0), writes=[bxp[i]])
                    P.op("pool", lambda h, i=i: h.memset(xpad[i][:, S + 2:S + 4], 0.0), writes=[bxp[i]])
                for cc in range(8):
                    k = cc % 2
                    P.dma("sp", lambda h, cc=cc, k=k: h.dma_start(out=xpad[k][:, 2:S + 2], in_=c.MQKT[cc * 128:(cc + 1) * 128, :]),
                          reads=c.bMQKT, writes=[bxp[k]])
                    P.op("dve", lambda h, cc=cc, k=k: h.tensor_scalar(out=acc[k][:], in0=xpad[k][:, 0:S], scalar1=cw[:, cc, 0:1],
                                                                      scalar2=cb[:, cc:cc + 1], op0=ALU.mult, op1=ALU.add),
                         reads=[bxp[k], bcw], writes=[bacc[k]])
                    for j in range(1, 5):
                        P.op("dve", lambda h, cc=cc, k=k, j=j: h.scalar_tensor_tensor(
                            out=acc[k][:], in0=xpad[k][:, j:j + S], scalar=cw[:, cc, j:j + 1], in1=acc[k][:],
                            op0=ALU.mult, op1=ALU.add), reads=[bxp[k], bcw, bacc[k]], writes=[bacc[k]])
                    if cc < 4:
                        P.op("act", lambda h, cc=cc, k=k: h.activation(out=QKT[:, cc, :], in_=acc[k][:], func=AF.Silu),
                             reads=[bacc[k]], writes=[bQKT[cc]])
                    else:
                        P.op("act", lambda h, cc=cc, k=k: h.activation(out=acc[k][:], in_=acc[k][:], func=AF.Silu),
                             reads=[bacc[k]], writes=[bacc[k]])
                        P.op("pool", lambda h, cc=cc, k=k: h.tensor_scalar(out=QKT[:, cc, :], in0=acc[k][:], scalar1=128.0 ** -0.5,
                                                                           scalar2=None, op0=ALU.mult),
                             reads=[bacc[k]], writes=[bQKT[cc]])
            P.barrier()

            MVs = sb("c_MVs", [128, NT, 4, 129], BF16)
            bMVs = bufs(NT, "cMV")
            bones = Buf("ones")
            SEL = sb("c_SEL", [4, 4, 128], F32)
            MSK = sb("c_MSK", [128, 2, 128], F32)
            identb = sb("c_identb", [128, 128], BF16)
            gmn = sb("c_gmn", [128, 512], F32)
            bconst = Buf("const")
            P.dma("sp", lambda h: h.dma_start(out=SEL[:], in_=c.sel), writes=[bconst])
            P.dma("sp", lambda h: h.dma_start(out=MSK[:], in_=c.msk), writes=[bconst])
            P.dma("sp", lambda h: h.dma_start(out=identb[:], in_=c.identb), writes=[bconst])
            P.dma("sp", lambda h: h.dma_start(out=gmn[:], in_=c.gmn), writes=[bconst])
            P.op("pool", lambda h: h.memset(MVs[:, :, :, 128:129], 1.0), writes=[bones])
            for i in range(NT):
                P.dma("sp" if i % 2 == 0 else "act", lambda h, i=i: h.dma_start(
                    out=MVs[:, i, :, 0:128], in_=c.MV[i * 128:(i + 1) * 128, :].rearrange("p (a b) -> p a b", b=128)),
                    reads=[c.bMV[i]], writes=[bMVs[i]])
            Cst = [sb("c_C%d" % i, [128, 129], F32) for i in range(4)]
            Cbf = [sb("c_Cbf%d" % i, [128, 129], BF16) for i in range(4)]
            bC, bCbf = bufs(4, "C"), bufs(4, "Cbf")
            bcr = [sb("c_bcr%d" % i, [4, 257], F32) for i in range(2)]
            bbcr = bufs(2, "bcr")
            Gt = [sb("c_G%d" % i, [128, 128], F32) for i in range(2)]
            Wt = [sb("c_W%d" % i, [128, 128], F32) for i in range(2)]
            PT = [sb("c_PT%d" % i, [128, 128], BF16) for i in range(2)]
            qs = [sb("c_qs%d" % i, [128, 128], BF16) for i in range(2)]
            kw = [sb("c_kw%d" % i, [128, 128], BF16) for i in range(2)]
            dec = [sb("c_dec%d" % i, [128, 1], F32) for i in range(2)]
            dn = [sb("c_dn%d" % i, [128, 2], F32) for i in range(2)]
            bG, bW, bPT, bqs, bkw, bdec, bdn = [bufs(2, n) for n in ("G", "W", "PT", "qs", "kw", "dec", "dn")]
            hbuf = [sb("c_hbuf%d" % i, [128, 512], F32) for i in range(2)]
            bhbuf = bufs(2, "hbuf")
            hf = sb("c_hf", [128, 512], F32)
            sg = sb("c_sg", [128, 512], BF16)
            sq = sb("c_sq", [128, 512], F32)
            s4 = sb("c_s4", [128, 12], F32)
            hmo = [sb("c_hmo%d" % i, [128, 512], BF16) for i in range(2)]
            bhf, bsg, bsq, bs4 = Buf("hf"), Buf("sg"), Buf("sq"), Buf("s4")
            bhmo = bufs(2, "hmo")
            ps_bc = [ps("c_ps_bc%d" % i, [128, 512], F32) for i in range(2)]
            ps_st = [ps("c_ps_st%d" % i, [128, 128], F32) for i in range(2)]
            ps_n = [ps("c_ps_n%d" % i, [128, 512], F32) for i in range(2)]
            ps_kt = ps("c_ps_kt", [128, 128], BF16)
            ps_dc = ps("c_ps_dc", [128, 512], F32)
            bps_bc, bps_st, bps_n = bufs(2, "ps_bc"), bufs(2, "ps_st"), bufs(2, "ps_n")
            bps_kt, bps_dc = Buf("ps_kt"), Buf("ps_dc")

            it = 0
            for d in range(2):
                for hd in range(4):
                    P.op("pool", lambda h, hd=hd: h.memset(Cst[hd][:], 0.0), writes=[bC[hd]])
                    P.op("pool", lambda h, hd=hd: h.memset(Cbf[hd][:], 0.0), writes=[bCbf[hd]])
                order = list(range(NT)) if d == 0 else list(range(NT - 1, -1, -1))
                for ci, ch in enumerate(order):
                    kb = ci % 2
                    P.dma("sp", lambda h, d=d, ch=ch, kb=kb: h.dma_start(out=bcr[kb][:], in_=c.BCRD[d, :, ch, :]),
                          reads=[c.bBCRD[d]], writes=[bbcr[kb]])
                    hb = hbuf[ci % 2]
                    bhb = bhbuf[ci % 2]
                    tsl = slice(ch * 128, (ch + 1) * 128)
                    for hd in range(4):
                        k = it % 2
                        it += 1
                        a_col = COLS[:, ch, 12 * d + hd:12 * d + hd + 1]
                        wk_col = COLS[:, ch, 12 * d + 4 + hd:12 * d + 5 + hd]
                        emt_col = COLS[:, ch, 12 * d + 8 + hd:12 * d + 9 + hd]
                        P.op("pe", lambda h, k=k, kb=kb, hd=hd: h.matmul(ps_bc[k][:, 0:257], lhsT=SEL[:, hd, :], rhs=bcr[kb][:],
                                                                         start=True, stop=True),
                             reads=[bconst, bbcr[kb]], writes=[bps_bc[k]])
                        P.op("pe", lambda h, k=k, hd=hd, tsl=tsl: h.matmul(ps_st[k][:], lhsT=QKT[:, 4 + hd, tsl], rhs=QKT[:, hd, tsl],
                                                                          start=True, stop=True),
                             reads=[bQKT[4 + hd], bQKT[hd]], writes=[bps_st[k]])
                        P.op("dve", lambda h, k=k, d=d: h.tensor_tensor(out=Gt[k][:], in0=ps_bc[k][:, 0:128], in1=MSK[:, d, :],
                                                                       op=ALU.add), reads=[bps_bc[k], bconst], writes=[bG[k]])
                        P.op("act", lambda h, k=k, a_col=a_col: h.activation(out=Wt[k][:], in_=Gt[k][:], func=AF.Exp, bias=a_col),
                             reads=[bG[k], bCOLS], writes=[bW[k]])
                        P.op("dve", lambda h, k=k: h.tensor_tensor(out=PT[k][:], in0=ps_st[k][:], in1=Wt[k][:], op=ALU.mult),
                             reads=[bps_st[k], bW[k]], writes=[bPT[k]])
                        P.op("dve", lambda h, k=k, hd=hd, tsl=tsl: h.tensor_tensor(out=qs[k][:], in0=ps_bc[k][:, 128:256],
                                                                                  in1=QKT[:, hd, tsl], op=ALU.mult),
                             reads=[bps_bc[k], bQKT[hd]], writes=[bqs[k]])
                        P.op("act", lambda h, k=k: h.copy(out=dec[k][:], in_=ps_bc[k][:, 256:257]),
                             reads=[bps_bc[k]], writes=[bdec[k]])
                        P.op("pe", lambda h, k=k, ch=ch, hd=hd: h.matmul(ps_n[k][:, 0:129], lhsT=PT[k][:], rhs=MVs[:, ch, hd, :],
                                                                         start=True, stop=False),
                             reads=[bPT[k], bMVs[ch], bones], writes=[bps_n[k]])
                        P.op("pe", lambda h, k=k, hd=hd: h.matmul(ps_n[k][:, 0:129], lhsT=qs[k][:], rhs=Cbf[hd][:],
                                                                  start=False, stop=True),
                             reads=[bqs[k], bCbf[hd]], writes=[bps_n[k]])
                        P.op("pe", lambda h, hd=hd, tsl=tsl: h.transpose(out=ps_kt[:], in_=QKT[:, 4 + hd, tsl], identity=identb[:]),
                             reads=[bQKT[4 + hd], bconst], writes=[bps_kt])
                        P.op("act", lambda h, k=k, wk_col=wk_col: h.activation(out=kw[k][:], in_=ps_kt[:], func=AF.Copy, scale=wk_col),
                             reads=[bps_kt, bCOLS], writes=[bkw[k]])
                        P.op("pe", lambda h, k=k, ch=ch, hd=hd: h.matmul(ps_dc[:, 0:129], lhsT=kw[k][:], rhs=MVs[:, ch, hd, :],
                                                                         start=True, stop=True),
                             reads=[bkw[k], bMVs[ch], bones], writes=[bps_dc])
                        P.op("dve", lambda h, k=k, hd=hd: h.scalar_tensor_tensor(out=Cst[hd][:], in0=Cst[hd][:], scalar=dec[k][:],
                                                                                 in1=ps_dc[:, 0:129], op0=ALU.mult, op1=ALU.add),
                             reads=[bC[hd], bdec[k], bps_dc], writes=[bC[hd]])
                        P.op("act", lambda h, hd=hd: h.copy(out=Cbf[hd][:], in_=Cst[hd][:]), reads=[bC[hd]], writes=[bCbf[hd]])
                        P.op("act", lambda h, k=k: h.activation(out=dn[k][:, 1:2], in_=ps_n[k][:, 128:129], func=AF.Abs),
                             reads=[bps_n[k]], writes=[bdn[k]])
                        P.op("dve", lambda h, k=k, emt_col=emt_col: h.tensor_scalar(out=dn[k][:, 0:1], in0=dn[k][:, 1:2],
                                                                                    scalar1=emt_col, scalar2=None, op0=ALU.max),
                             reads=[bdn[k], bCOLS], writes=[bdn[k]])
                        P.op("dve", lambda h, k=k: h.reciprocal(out=dn[k][:, 1:2], in_=dn[k][:, 0:1]), reads=[bdn[k]], writes=[bdn[k]])
                        P.op("dve", lambda h, k=k, hd=hd, hb=hb: h.tensor_scalar(out=hb[:, hd * 128:(hd + 1) * 128], in0=ps_n[k][:, 0:128],
                                                                                 scalar1=dn[k][:, 1:2], scalar2=None, op0=ALU.mult),
                             reads=[bps_n[k], bdn[k]], writes=[bhb])
                    if d == 0:
                        P.dma("sp", lambda h, ch=ch, hb=hb: h.dma_start(out=c.HF[ch * 128:(ch + 1) * 128, :], in_=hb[:]),
                              reads=[bhb], writes=[c.bHF[ch]])
                    else:
                        o = ci % 2
                        P.dma("sp", lambda h, ch=ch: h.dma_start(out=hf[:], in_=c.HF[ch * 128:(ch + 1) * 128, :]),
                              reads=[c.bHF[ch]], writes=[bhf])
                        P.dma("sp", lambda h, ch=ch: h.dma_start(out=sg[:], in_=c.SIGO[ch * 128:(ch + 1) * 128, :]),
                              reads=[c.bSIGO[ch]], writes=[bsg])
                        P.op("pool", lambda h, hb=hb: h.tensor_tensor(out=hf[:], in0=hf[:], in1=hb[:], op=ALU.add),
                             reads=[bhb, bhf], writes=[bhf])
                        P.op("act", lambda h: h.activation(out=sq[:], in_=hf[:], func=AF.Square), reads=[bhf], writes=[bsq])
                        P.op("dve", lambda h: h.tensor_reduce(out=s4[:, 0:4], in_=sq[:].rearrange("p (a b) -> p a b", b=128),
                                                              axis=AX.X, op=ALU.add), reads=[bsq], writes=[bs4])
                        P.op("act", lambda h: h.activation(out=s4[:, 4:8], in_=s4[:, 0:4], func=AF.Sqrt, scale=1.0 / 128, bias=EPS),
                             reads=[bs4], writes=[bs4])
                        P.op("dve", lambda h: h.reciprocal(out=s4[:, 8:12], in_=s4[:, 4:8]), reads=[bs4], writes=[bs4])
                        P.op("dve", lambda h: h.tensor_tensor(out=sq[:].rearrange("p (a b) -> p a b", b=128),
                                                              in0=hf[:].rearrange("p (a b) -> p a b", b=128),
                                                              in1=s4[:, 8:12].unsqueeze(2).to_broadcast([128, 4, 128]), op=ALU.mult),
                             reads=[bhf, bs4, bsq], writes=[bsq])
                        P.op("pool", lambda h: h.tensor_tensor(out=sq[:], in0=sq[:], in1=gmn[:], op=ALU.mult),
                             reads=[bsq, bconst], writes=[bsq])
                        P.op("pool", lambda h, o=o: h.tensor_tensor(out=hmo[o][:], in0=sq[:], in1=sg[:], op=ALU.mult),
                             reads=[bsq, bsg], writes=[bhmo[o]])
                        P.dma("sp", lambda h, ch=ch, o=o: h.dma_start(out=c.HM[ch * 128:(ch + 1) * 128, :], in_=hmo[o][:]),
                              reads=[bhmo[o]], writes=[c.bHM[ch]])
    P.barrier()


def phase_t(c):
    nc, P = c.nc, c.P
    JB = 4
    with ExitStack() as st:
        sb = lambda n, s, d: st.enter_context(nc.sbuf_tensor(n, s, d))
        tin = [sb("t_in%d" % i, [128, JB * 2 * D], F32) for i in range(2)]
        tout = [sb("t_out%d" % i, [128, JB * 2 * D], BF16) for i in range(2)]
        bin_, bout, bout2 = bufs(2, "tin"), bufs(2, "tout"), bufs(2, "tout2")
        src = c.peer_uv.rearrange("(p j) d -> p (j d)", p=128)
        dst = c.UVB.rearrange("(p j) d -> p (j d)", p=128)
        W = JB * 2 * D
        third = W // 4
        for stp in range(128 // JB):
            k = stp % 2
            P.dma("sp", lambda h, stp=stp, k=k: h.dma_start(out=tin[k][:], in_=src[:, stp * W:(stp + 1) * W]), writes=[bin_[k]])
            cut = (W * 5) // 8
            P.op("dve", lambda h, k=k, cut=cut: h.tensor_copy(out=tout[k][:, 0:cut], in_=tin[k][:, 0:cut]), reads=[bin_[k]], writes=[bout[k]])
            P.op("act", lambda h, k=k, cut=cut: h.copy(out=tout[k][:, cut:W], in_=tin[k][:, cut:W]), reads=[bin_[k]], writes=[bout2[k]])
            P.dma("sp", lambda h, stp=stp, k=k: h.dma_start(out=dst[:, stp * W:(stp + 1) * W], in_=tout[k][:]), reads=[bout[k], bout2[k]])
    P.barrier()


def phase_d(c):
    nc, P = c.nc, c.P
    with ExitStack() as st:
        sb = lambda n, s, d: st.enter_context(nc.sbuf_tensor(n, s, d))
        ps = lambda n, s, d: st.enter_context(nc.psum_tensor(n, s, d))
        Wo = sb("d_Wo", [128, 8, D], BF16)
        Wq = sb("d_Wq", [128, 8, 2048], BF16)
        SKT = sb("d_SKT", [128, 16, 128], BF16)
        gn2 = sb("d_gn2", [128, D], F32)
        identb = sb("d_identb", [128, 128], BF16)
        identf = sb("d_identf", [128, 128], F32)
        THR = sb("d_THR", [128, 16], F32)
        IOT = sb("d_IOT", [128, 16], F32)
        st_stage = ExitStack()
        stage = [st_stage.enter_context(nc.sbuf_tensor("d_stage%d" % i, [128, 2048], F32)) for i in range(2)]
        bconst = Buf("dconst")
        bst = bufs(2, "dst")
        bWo, bWq = bufs(8, "Wo"), bufs(8, "Wq")
        bSKT = Buf("SKT")
        for (t, src) in ((gn2, c.gn2), (identb, c.identb), (THR, c.thr), (IOT, c.iot), (identf, c.identf)):
            P.dma("sp", lambda h, t=t, src=src: h.dma_start(out=t[:], in_=src), writes=[bconst])
        n = 0
        for kc in range(8):
            k = n % 2; n += 1
            P.dma("sp", lambda h, kc=kc, k=k: h.dma_start(out=stage[k][:, 0:D], in_=c.w_out[kc * 128:(kc + 1) * 128, :]), writes=[bst[k]])
            P.op("dve", lambda h, kc=kc, k=k: h.tensor_copy(out=Wo[:, kc, :], in_=stage[k][:, 0:D]), reads=[bst[k]], writes=[bWo[kc]])
        for kc in range(8):
            k = n % 2; n += 1
            P.dma("sp", lambda h, kc=kc, k=k: h.dma_start(out=stage[k][:], in_=c.w_q[kc * 128:(kc + 1) * 128, :]), writes=[bst[k]])
            P.op("dve", lambda h, kc=kc, k=k: h.tensor_copy(out=Wq[:, kc, :], in_=stage[k][:]), reads=[bst[k]], writes=[bWq[kc]])
        k = n % 2; n += 1
        P.dma("sp", lambda h, k=k: h.dma_start(out=stage[k][:].rearrange("p (a b) -> p a b", b=128), in_=c.skt), writes=[bst[k]])
        P.op("dve", lambda h, k=k: h.tensor_copy(out=SKT[:].rearrange("p a b -> p (a b)"), in_=stage[k][:]), reads=[bst[k]], writes=[bSKT])
        P.barrier()
        st_stage.close()

        cat = sb("d_cat", [128, D], BF16)
        catT = sb("d_catT", [128, 8, 128], BF16)
        xt = sb("d_xt", [128, D], F32)
        junk = sb("d_junk", [128, D], F32)
        junk2 = sb("d_junk2", [128, D], F32)
        ss = sb("d_ss", [128, 4], F32)
        xn2b = sb("d_xn2b", [128, D], BF16)
        xn2T = sb("d_xn2T", [128, 8, 128], BF16)
        qT = sb("d_qT", [128, 16, 128], BF16)
        sc = sb("d_sc", [128, 16, 128], F32)
        m8 = sb("d_m8", [128, 16, 16], F32)
        i8 = sb("d_i8", [128, 16, 16], U32)
        i8f = sb("d_i8f", [128, 16, 16], F32)
        cand = sb("d_cand", [128, 8, 256], F32)
        t8 = sb("d_t8", [128, 8, 16], F32)
        j8 = sb("d_j8", [128, 8, 16], U32)
        jf = sb("d_jf", [128, 128], F32)
        T4 = sb("d_T4", [128, 128, 16], F32)
        af = sb("d_af", [128, 128], F32)
        bf_ = sb("d_bf", [128, 128], F32)
        E1 = sb("d_E1", [128, 128], F32)
        E2 = sb("d_E2", [128, 128], F32)
        g8 = sb("d_g8", [128, 16], F32)
        x1 = [sb("d_x1_%d" % i, [128, D], F32) for i in range(2)]
        xn2 = [sb("d_xn2_%d" % i, [128, D], F32) for i in range(2)]
        eidx = [sb("d_eidx%d" % i, [128, 128], I32) for i in range(2)]
        gts = [sb("d_gts%d" % i, [128, 8, 16], F32) for i in range(2)]
        adot = sb("d_adot", [128, 128], F32)
        ga = sb("d_ga", [128, 128], F32)
        NG = 16
        GS = 4
        NGRP = 128 // GS
        uv = [sb("d_uv%d" % i, [128, 2 * D], BF16) for i in range(NG)]
        dg = [sb("d_dg%d" % i, [128, 128], BF16) for i in range(4)]
        yacc = sb("d_y", [128, D], F32)
        ps_t = ps("d_ps_t", [128, D], BF16)
        ps_o = [ps("d_ps_o%d" % i, [128, 512], F32) for i in range(2)]
        ps_y = [ps("d_ps_y%d" % i, [128, 512], F32) for i in range(2)]
        ps_q = [ps("d_ps_q%d" % i, [128, 4, 128], F32) for i in range(2)]
        (bcat, bcatT, bxt, bss, bxn2b, bxn2T, bqT, bsc, bm8, bi8, bi8f, bcand, bt8, bj8,
         bjf, bT4, baf, bbf, bE1, bE2, bg8, by, bps_t) = [Buf(n_) for n_ in (
            "cat", "catT", "xt", "ss", "xn2b", "xn2T", "qT", "sc", "m8", "i8", "i8f", "cand",
            "t8", "j8", "jf", "T4", "af", "bf", "E1", "E2", "g8", "y", "ps_t")]
        bx1, bxn2, beidx, bgts = bufs(2, "x1"), bufs(2, "xn2"), bufs(2, "eidx"), bufs(2, "gts")
        badot, bga = bufs(NGRP, "adot"), bufs(NGRP, "ga")
        buv, bdg = bufs(NG, "uv"), bufs(4, "dg")
        bps_o, bps_q, bps_y = bufs(2, "ps_o"), bufs(2, "ps_q"), bufs(2, "ps_y")
        qi = [0]

        def routing(i):
            par = i % 2
            x1_, xn2_, eidx_, gts_ = x1[par], xn2[par], eidx[par], gts[par]
            bx1_, bxn2_, beidx_, bgts_ = bx1[par], bxn2[par], beidx[par], bgts[par]
            rows = slice(i * 128, (i + 1) * 128)
            P.dma("sp", lambda h: h.dma_start(out=cat[:, 0:512], in_=c.AOUT[rows, :]), reads=[c.bAOUT[i]], writes=[bcat])
            P.dma("sp", lambda h: h.dma_start(out=cat[:, 512:1024], in_=c.HM[rows, :]), reads=[c.bHM[i]], writes=[bcat])
            P.dma("sp", lambda h: h.dma_start(out=xt[:], in_=c.x[rows, :]), writes=[bxt])
            for kc in range(8):
                P.op("pe", lambda h, kc=kc: h.transpose(out=ps_t[:, kc * 128:(kc + 1) * 128], in_=cat[:, kc * 128:(kc + 1) * 128],
                                                        identity=identb[:]), reads=[bcat, bconst], writes=[bps_t])
            P.op("act", lambda h: h.copy(out=catT[:].rearrange("p k t -> p (k t)"), in_=ps_t[:]), reads=[bps_t], writes=[bcatT])
            yield
            for g in range(2):
                for kc in range(8):
                    P.op("pe", lambda h, g=g, kc=kc: h.matmul(ps_o[g][:], lhsT=catT[:, kc, :], rhs=Wo[:, kc, g * 512:(g + 1) * 512],
                                                             start=(kc == 0), stop=(kc == 7)), reads=[bcatT, bWo[kc]], writes=[bps_o[g]])
                P.op("dve", lambda h, g=g: h.tensor_tensor(out=x1_[:, g * 512:(g + 1) * 512], in0=ps_o[g][:], in1=xt[:, g * 512:(g + 1) * 512],
                                                          op=ALU.add), reads=[bps_o[g], bxt], writes=[bx1_])
                yield
            P.op("act", lambda h: h.activation(out=junk[:], in_=x1_[:], func=AF.Square, accum_out=ss[:, 0:1]), reads=[bx1_], writes=[bss])
            P.op("act", lambda h: h.activation(out=ss[:, 1:2], in_=ss[:, 0:1], func=AF.Sqrt, scale=1.0 / D, bias=EPS), reads=[bss], writes=[bss])
            P.op("dve", lambda h: h.reciprocal(out=ss[:, 2:3], in_=ss[:, 1:2]), reads=[bss], writes=[bss])
            P.op("dve", lambda h: h.scalar_tensor_tensor(out=xn2_[:], in0=x1_[:], scalar=ss[:, 2:3], in1=gn2[:], op0=ALU.mult, op1=ALU.mult),
                 reads=[bx1_, bss, bconst], writes=[bxn2_])
            P.op("act", lambda h: h.copy(out=xn2b[:], in_=xn2_[:]), reads=[bxn2_], writes=[bxn2b])
            yield
            for kc in range(8):
                P.op("pe", lambda h, kc=kc: h.transpose(out=ps_t[:, kc * 128:(kc + 1) * 128], in_=xn2b[:, kc * 128:(kc + 1) * 128],
                                                        identity=identb[:]), reads=[bxn2b, bconst], writes=[bps_t])
            P.op("act", lambda h: h.copy(out=xn2T[:].rearrange("p k t -> p (k t)"), in_=ps_t[:]), reads=[bps_t], writes=[bxn2T])
            yield
            for qg in range(4):
                k = qi[0] % 2
                qi[0] += 1
                for cc in range(4):
                    hp = qg * 4 + cc
                    for kc in range(8):
                        P.op("pe", lambda h, k=k, cc=cc, hp=hp, kc=kc: h.matmul(ps_q[k][:, cc, :], lhsT=Wq[:, kc, hp * 128:(hp + 1) * 128],
                                                                              rhs=xn2T[:, kc, :], start=(kc == 0), stop=(kc == 7)),
                             reads=[bWq[kc], bxn2T], writes=[bps_q[k]])
                P.op("act", lambda h, k=k, qg=qg: h.copy(out=qT[:, qg * 4:(qg + 1) * 4, :], in_=ps_q[k][:]), reads=[bps_q[k]], writes=[bqT])
                yield
            for qg in range(4):
                k = qi[0] % 2
                qi[0] += 1
                for cc in range(4):
                    hp = qg * 4 + cc
                    P.op("pe", lambda h, k=k, cc=cc, hp=hp: h.matmul(ps_q[k][:, cc, :], lhsT=qT[:, hp, :], rhs=SKT[:, hp, :],
                                                                   start=True, stop=True), reads=[bqT, bSKT], writes=[bps_q[k]])
                P.op("act", lambda h, k=k, qg=qg: h.copy(out=sc[:, qg * 4:(qg + 1) * 4, :], in_=ps_q[k][:]), reads=[bps_q[k]], writes=[bsc])
            yield
            for g in range(16):
                P.op("dve", lambda h, g=g: h.max(out=m8[:, g, 0:8], in_=sc[:, g, :]), reads=[bsc], writes=[bm8])
                P.op("dve", lambda h, g=g: h.max_index(out=i8[:, g, 0:8], in_max=m8[:, g, 0:8], in_values=sc[:, g, :]),
                     reads=[bsc, bm8], writes=[bi8])
                P.op("dve", lambda h, g=g: h.match_replace(out=sc[:, g, :], in_to_replace=m8[:, g, 0:8], in_values=sc[:, g, :],
                                                           imm_value=-1e30), reads=[bm8], writes=[bsc])
                P.op("dve", lambda h, g=g: h.max(out=m8[:, g, 8:16], in_=sc[:, g, :]), reads=[bsc], writes=[bm8])
                P.op("dve", lambda h, g=g: h.max_index(out=i8[:, g, 8:16], in_max=m8[:, g, 8:16], in_values=sc[:, g, :]),
                     reads=[bsc, bm8], writes=[bi8])
                yield
            m8v = m8[:].rearrange("p (a b) k -> p a b k", b=2)
            P.op("dve", lambda h: h.tensor_tensor(
                out=cand[:].rearrange("p a (x y) -> p a x y", y=16),
                in0=m8v[:, :, 0, :].unsqueeze(3).to_broadcast([128, 8, 16, 16]),
                in1=m8v[:, :, 1, :].unsqueeze(2).to_broadcast([128, 8, 16, 16]), op=ALU.add), reads=[bm8], writes=[bcand])
            yield
            for hd in range(8):
                P.op("dve", lambda h, hd=hd: h.max(out=t8[:, hd, 0:8], in_=cand[:, hd, :]), reads=[bcand], writes=[bt8])
                P.op("dve", lambda h, hd=hd: h.max_index(out=j8[:, hd, 0:8], in_max=t8[:, hd, 0:8], in_values=cand[:, hd, :]),
                     reads=[bcand, bt8], writes=[bj8])
                P.op("dve", lambda h, hd=hd: h.match_replace(out=cand[:, hd, :], in_to_replace=t8[:, hd, 0:8], in_values=cand[:, hd, :],
                                                             imm_value=-1e30), reads=[bt8], writes=[bcand])
                P.op("dve", lambda h, hd=hd: h.max(out=t8[:, hd, 8:16], in_=cand[:, hd, :]), reads=[bcand], writes=[bt8])
                P.op("dve", lambda h, hd=hd: h.max_index(out=j8[:, hd, 8:16], in_max=t8[:, hd, 8:16], in_values=cand[:, hd, :]),
                     reads=[bcand, bt8], writes=[bj8])
                yield
            P.op("dve", lambda h: h.tensor_copy(out=i8f[:], in_=i8[:]), reads=[bi8], writes=[bi8f])
            P.op("dve", lambda h: h.tensor_copy(out=jf[:], in_=j8[:].rearrange("p a k -> p (a k)")), reads=[bj8], writes=[bjf])
            P.op("dve", lambda h: h.tensor_tensor(out=T4[:], in0=jf[:].unsqueeze(2).to_broadcast([128, 128, 16]),
                                                  in1=THR[:].unsqueeze(1).to_broadcast([128, 128, 16]), op=ALU.is_ge),
                 reads=[bjf, bconst], writes=[bT4])
            yield
            P.op("dve", lambda h: h.tensor_reduce(out=af[:], in_=T4[:], axis=AX.X, op=ALU.add), reads=[bT4], writes=[baf])
            P.op("dve", lambda h: h.scalar_tensor_tensor(out=bf_[:], in0=af[:], scalar=-16.0, in1=jf[:], op0=ALU.mult, op1=ALU.add),
                 reads=[baf, bjf], writes=[bbf])
            yield
            i8v = i8f[:].rearrange("p (a b) k -> p a b k", b=2)
            for side, (idxt, Et, bE) in enumerate(((af, E1, bE1), (bf_, E2, bE2))):
                P.op("dve", lambda h, idxt=idxt: h.tensor_tensor(out=T4[:], in0=idxt[:].unsqueeze(2).to_broadcast([128, 128, 16]),
                                                                in1=IOT[:].unsqueeze(1).to_broadcast([128, 128, 16]), op=ALU.is_equal),
                     reads=[baf, bbf, bconst], writes=[bT4])
                yield
                P.op("dve", lambda h, side=side: h.tensor_tensor(
                    out=T4[:].rearrange("p (a k) x -> p a k x", k=16), in0=T4[:].rearrange("p (a k) x -> p a k x", k=16),
                    in1=i8v[:, :, side, :].unsqueeze(2).to_broadcast([128, 8, 16, 16]), op=ALU.mult),
                    reads=[bi8f], writes=[bT4])
                yield
                P.op("dve", lambda h, Et=Et: h.tensor_reduce(out=Et[:], in_=T4[:], axis=AX.X, op=ALU.add), reads=[bT4], writes=[bE])
                yield
            P.op("dve", lambda h: h.scalar_tensor_tensor(out=E1[:], in0=E1[:], scalar=128.0, in1=E2[:], op0=ALU.mult, op1=ALU.add),
                 reads=[bE2], writes=[bE1])
            P.op("dve", lambda h: h.tensor_copy(out=eidx_[:], in_=E1[:]), reads=[bE1], writes=[beidx_])
            P.op("dve", lambda h: h.tensor_tensor(out=gts_[:], in0=t8[:], in1=t8[:, :, 0:1].to_broadcast([128, 8, 16]), op=ALU.subtract),
                 reads=[bt8], writes=[bgts_])
            P.op("act", lambda h: h.activation(out=gts_[:], in_=gts_[:], func=AF.Exp), reads=[], writes=[bgts_])
            P.op("dve", lambda h: h.tensor_reduce(out=g8[:, 0:8], in_=gts_[:], axis=AX.X, op=ALU.add), reads=[bgts_], writes=[bg8])
            P.op("dve", lambda h: h.reciprocal(out=g8[:, 8:16], in_=g8[:, 0:8]), reads=[], writes=[bg8])
            P.op("dve", lambda h: h.tensor_tensor(out=gts_[:], in0=gts_[:], in1=g8[:, 8:16].unsqueeze(2).to_broadcast([128, 8, 16]),
                                                  op=ALU.mult), reads=[bg8], writes=[bgts_])
            if "EIDX" in c.dbg:
                P.dma("sp", lambda h: h.dma_start(out=c.EIDX[rows, :], in_=eidx_[:]), reads=[beidx_])
                P.dma("sp", lambda h: h.dma_start(out=c.GTS[rows, :], in_=gts_[:].rearrange("p a k -> p (a k)")), reads=[bgts_])
                P.dma("sp", lambda h: h.dma_start(out=c.X1[rows, :], in_=x1_[:]), reads=[bx1_])
            yield

        def experts(i, nxt):
            par = i % 2
            x1_, xn2_, eidx_, gts_ = x1[par], xn2[par], eidx[par], gts[par]
            bx1_, bxn2_, beidx_, bgts_ = bx1[par], bxn2[par], beidx[par], bgts[par]
            rows = slice(i * 128, (i + 1) * 128)
            gts_f = gts_[:].rearrange("p a k -> p (a k)")

            def stage_a(grp):
                for sl in range(grp * GS, (grp + 1) * GS):
                    k = sl % NG
                    P.dma("pool", lambda h, sl=sl, k=k: h.indirect_dma_start(
                        out=uv[k][:], out_offset=None, in_=c.UVB,
                        in_offset=bass.IndirectOffsetOnAxis(ap=eidx_[:, sl:sl + 1], axis=0)), reads=[beidx_], writes=[buv[k]])
                    P.op("dve", lambda h, sl=sl, k=k: h.scalar_tensor_tensor(out=junk2[:], in0=uv[k][:, 0:D], scalar=1.0, in1=xn2_[:],
                                                                            op0=ALU.mult, op1=ALU.mult, accum_out=adot[:, sl:sl + 1]),
                         reads=[buv[k], bxn2_], writes=[badot[grp]])

            def stage_b(grp):
                g0, g1 = grp * GS, (grp + 1) * GS
                P.op("act", lambda h: h.activation(out=ga[:, g0:g1], in_=adot[:, g0:g1], func=AF.Gelu),
                     reads=[badot[grp]], writes=[bga[grp]])
                for sl in range(g0, g1):
                    k = sl % NG
                    kd = sl % 4
                    P.op("act", lambda h, sl=sl: h.activation(out=ga[:, sl:sl + 1], in_=ga[:, sl:sl + 1], func=AF.Copy,
                                                              scale=gts_f[:, sl:sl + 1]), reads=[bgts_], writes=[bga[grp]])
                    P.op("act", lambda h, sl=sl, kd=kd: h.activation(out=dg[kd][:], in_=identf[:], func=AF.Copy, scale=ga[:, sl:sl + 1]),
                         reads=[bga[grp], bconst], writes=[bdg[kd]])
                    for g in range(2):
                        P.op("pe", lambda h, sl=sl, k=k, kd=kd, g=g: h.matmul(ps_y[g][:], lhsT=dg[kd][:],
                                                                             rhs=uv[k][:, D + g * 512:D + (g + 1) * 512],
                                                                             start=(sl == 0), stop=(sl == 127)),
                             reads=[bdg[kd], buv[k]], writes=[bps_y[g]])

            def adv(nsteps):
                if nxt is None:
                    return
                for _ in range(nsteps):
                    try:
                        next(nxt)
                    except StopIteration:
                        return

            stage_a(0)
            for grp in range(NGRP):
                if grp + 1 < NGRP:
                    stage_a(grp + 1)
                stage_b(grp)
                adv(2)
            for g in range(2):
                P.op("dve", lambda h, g=g: h.tensor_tensor(out=yacc[:, g * 512:(g + 1) * 512], in0=ps_y[g][:], in1=x1_[:, g * 512:(g + 1) * 512],
                                                          op=ALU.add), reads=[bps_y[g], bx1_], writes=[by])
            P.dma("sp", lambda h: h.dma_start(out=c.out[rows, :], in_=yacc[:]), reads=[by], writes=[c.bOUT[i]])
            adv(1000)

        r0 = routing(0)
        for _ in r0:
            pass
        for i in range(NT):
            nxt = routing(i + 1) if i + 1 < NT else None
            if "noexp" in c.dbg or i >= c.nexp:
                par = i % 2
                P.dma("sp", lambda h, i=i, par=par: h.dma_start(out=c.out[i * 128:(i + 1) * 128, :], in_=x1[par][:]),
                      reads=[bx1[par]], writes=[c.bOUT[i]])
                if nxt is not None:
                    for _ in nxt:
                        pass
            else:
                if "noil" in c.dbg and nxt is not None:
                    for _ in nxt:
                        pass
                experts(i, nxt)
    P.barrier()


def build(dbg=(), phases="abcd"):
    nc = bass.Bass("TRN2", target_bir_lowering=False)
    c = Ctx()
    c.nc = nc
    ext_in = lambda n, s, d: nc.dram_tensor(n, s, d, kind="ExternalInput").ap()

    def scratch(n, s, d):
        kind = "ExternalOutput" if n in dbg else "Internal"
        return nc.dram_tensor(n, s, d, kind=kind).ap()

    c.x = ext_in("x", [S, D], F32)
    c.w_in = ext_in("w_in", [D, INW], F32)
    c.norm1_w = ext_in("norm1_w", [128, 8], F32)
    c.identb = ext_in("identb", [128, 128], BF16)
    c.gq = ext_in("gq", [128, 64], F32)
    c.gk = ext_in("gk", [128, 64], F32)
    c.rpbg = ext_in("rpbg", [128, 8, 896], F32)
    c.mask_i = ext_in("mask_i", [128, 896], F32)
    c.mask_a = ext_in("mask_a", [128, 896], F32)
    c.gao = ext_in("gao", [128, 512], F32)
    c.gate_b = ext_in("gate_b", [4, 4], F32)
    c.identf = ext_in("identf", [128, 128], F32)
    c.conv_w = ext_in("conv_w", [128, 8, 5], F32)
    c.conv_b = ext_in("conv_b", [128, 8], F32)
    c.sel = ext_in("sel", [4, 4, 128], F32)
    c.msk = ext_in("msk", [128, 2, 128], F32)
    c.gmn = ext_in("gmn", [128, 512], F32)
    c.w_out = ext_in("w_out", [D, D], F32)
    c.w_q = ext_in("w_q", [D, 2048], F32)
    c.skt = ext_in("skt", [128, 16, 128], F32)
    c.gn2 = ext_in("gn2", [128, D], F32)
    c.thr = ext_in("thr", [128, 16], F32)
    c.iot = ext_in("iot", [128, 16], F32)
    c.peer_uv = ext_in("peer_uv", [16384, 2 * D], F32)
    c.UVB = scratch("UVB", [16384, 2 * D], BF16)
    c.out = nc.dram_tensor("out", [S, D], F32, kind="ExternalOutput").ap()
    c.bOUT = bufs(NT, "OUT")
    c.dbg = dbg
    c.nexp = NT
    for d_ in dbg:
        if d_.startswith("exp"):
            c.nexp = int(d_[3:])
    if "EIDX" in dbg:
        c.EIDX = scratch("EIDX", [S, 128], I32)
        c.GTS = scratch("GTS", [S, 128], F32)
        c.X1 = scratch("X1", [S, D], F32)

    c.QT = scratch("QT", [4, 128, S], BF16)
    c.KT = scratch("KT", [4, 128, S], BF16)
    c.V = scratch("V", [S, 512], BF16)
    c.MV = scratch("MV", [S, 512], BF16)
    c.SIGO = scratch("SIGO", [S, 512], BF16)
    c.MQKT = scratch("MQKT", [1024, S], F32)
    c.GT = scratch("GT", [4, 4, S], F32)
    c.AOUT = scratch("AOUT", [S, 512], BF16)
    c.BCRD = scratch("BCRD", [2, 4, NT, 257], F32)
    c.bBCRD = bufs(2, "BCRD")
    c.HF = scratch("HF", [S, 512], F32)
    c.bHF = bufs(NT, "HF")
    c.HM = scratch("HM", [S, 512], BF16)
    c.bHM = bufs(NT, "HM")
    c.bAOUT = bufs(NT, "AOUT")
    c.bQKT = [bufs(NT, "QT"), bufs(NT, "KT")]
    c.bV, c.bMV, c.bSIGO, c.bMQKT, c.bGT = (bufs(NT, n) for n in ("V", "MV", "SIGO", "MQKT", "GT"))

    with ExitStack() as st:
        c.P = Prog(nc, st)
        if "a" in phases:
            phase_a(c)
        if "b" in phases:
            phase_b(c)
        if "c" in phases:
            phase_c(c)
        if "d" in phases:
            phase_t(c)
            phase_d(c)
        c.P.emit()
    return nc


def host_inputs(inputs, b):
    f32 = np.float32
    m = {}
    m["x"] = np.ascontiguousarray(inputs["x"][b], dtype=f32)
    m["w_in"] = np.ascontiguousarray(inputs["w_in"][0], dtype=f32)
    m["norm1_w"] = np.ascontiguousarray(inputs["norm1_w"][0].reshape(8, 128).T, dtype=f32)
    m["identb"] = np.eye(128, dtype=f32).astype(ml_dtypes.bfloat16)
    m["gq"] = np.ascontiguousarray(np.broadcast_to(inputs["q_norm_w"][0][None, :], (128, 64)), dtype=f32)
    m["gk"] = np.ascontiguousarray(np.broadcast_to(inputs["k_norm_w"][0][None, :], (128, 64)), dtype=f32)
    p = np.arange(128); kr = p // 64; kc = p % 64
    col = np.arange(128); rq = col // 64; cc = col % 64
    dt = np.arange(-3, 4)
    drow = 2 * dt[None, :, None] + kr[:, None, None] - rq[None, None, :]
    dcol = kc[:, None, None] - cc[None, None, :] + 0 * dt[None, :, None]
    rpb = inputs["attn_rpb"][0]
    g = rpb[:, np.clip(drow + 7, 0, 14), np.clip(dcol + 15, 0, 30)]
    m["rpbg"] = np.ascontiguousarray(g.transpose(1, 0, 2, 3).reshape(128, 8, 896), dtype=f32)
    cs = np.clip(cc - 8, 0, 48)
    colvalid = (kc[:, None, None] >= cs[None, None, :]) & (kc[:, None, None] < cs[None, None, :] + 16)
    colvalid = colvalid & (dt[None, :, None] > -100)
    m["mask_a"] = np.where(colvalid & (np.abs(drow) <= 7), 0.0, NEG).astype(f32).reshape(128, 896)
    m["mask_i"] = np.where(colvalid & (drow >= -4) & (drow <= 3), 0.0, NEG).astype(f32).reshape(128, 896)
    m["gate_b"] = np.ascontiguousarray(inputs["mlstm_gate_b"][0].T, dtype=f32)
    m["identf"] = np.eye(128, dtype=f32)
    m["conv_w"] = np.ascontiguousarray(inputs["mlstm_conv_w"][0].reshape(5, 8, 128).transpose(2, 1, 0), dtype=f32)
    m["conv_b"] = np.ascontiguousarray(inputs["mlstm_conv_b"][0].reshape(8, 128).T, dtype=f32)
    sel = np.zeros((4, 4, 128), f32)
    for hh in range(4):
        sel[hh, hh, :] = 1.0
    m["sel"] = sel
    ii = np.arange(128)
    msk = np.zeros((128, 2, 128), f32)
    msk[:, 0, :] = np.where(ii[:, None] <= ii[None, :], 0.0, NEG)
    msk[:, 1, :] = np.where(ii[:, None] >= ii[None, :], 0.0, NEG)
    m["msk"] = msk
    m["gmn"] = np.ascontiguousarray(np.broadcast_to(inputs["mlstm_norm_w"][0][None, :], (128, 512)), dtype=f32)
    m["w_out"] = np.ascontiguousarray(inputs["w_out"][0], dtype=f32)
    m["w_q"] = np.ascontiguousarray(inputs["peer_w_q"][0], dtype=f32)
    m["skt"] = np.ascontiguousarray(inputs["peer_sub_keys"][0].reshape(16, 128, 128).transpose(2, 0, 1), dtype=f32)
    m["gn2"] = np.ascontiguousarray(np.broadcast_to(inputs["norm2_w"][0][None, :], (128, D)), dtype=f32)
    thr = (np.arange(16, dtype=f32) + 1.0) * 16.0
    thr[15] = 1e9
    m["thr"] = np.ascontiguousarray(np.broadcast_to(thr[None, :], (128, 16)), dtype=f32)
    m["iot"] = np.ascontiguousarray(np.broadcast_to(np.arange(16, dtype=f32)[None, :], (128, 16)), dtype=f32)
    m["peer_uv"] = np.ascontiguousarray(np.concatenate([inputs["peer_u"][0], inputs["peer_v"][0]], axis=1), dtype=f32)
    m["gao"] = np.ascontiguousarray(np.broadcast_to(inputs["attn_out_norm_w"][0][None, :], (128, 512)), dtype=f32)
    return m


def kernel(**inputs):
    nc = build()
    in_maps = [host_inputs(inputs, b) for b in range(8)]
    res = run_bass_kernel_spmd(nc, in_maps, core_ids=list(range(8)))
    return np.stack([r["out"] for r in res.results], axis=0).astype(np.float32)
```

```python
import numpy as np
import ml_dtypes
import concourse.bass as bass
import concourse.mybir as mybir
from concourse.bass_utils import run_bass_kernel_spmd
from contextlib import ExitStack

F32 = mybir.dt.float32
BF16 = mybir.dt.bfloat16
I32 = mybir.dt.int32
U32 = mybir.dt.uint32
ALU = mybir.AluOpType
AF = mybir.ActivationFunctionType
AX = mybir.AxisListType

S = 4096
D = 1024
NT = 32
INW = 3600
EPS = 1e-6
NEG = -30000.0


class Buf:
    __slots__ = ("name", "w", "r")

    def __init__(self, name=""):
        self.name = name
        self.w = {}
        self.r = {}


class Prog:
    ENG = ("pe", "act", "dve", "pool", "sp")
    NRINGS = {"sp": 12, "act": 6, "pool": 48}

    def __init__(self, nc, stack):
        self.nc = nc
        self.sem = {e: stack.enter_context(nc.semaphore("s_" + e)) for e in self.ENG}
        self.ops = {e: [] for e in self.ENG}
        self.seen = {e: {} for e in self.ENG}
        self.ring = {}
        self.ring_i = {}
        self.ring_tok = {}
        for q in ("sp", "act", "pool"):
            self.ring[q] = [stack.enter_context(nc.semaphore("r_%s%d" % (q, i)))
                            for i in range(self.NRINGS[q])]
            self.ring_i[q] = 0
            self.ring_tok[q] = [None] * self.NRINGS[q]

    @staticmethod
    def _key(tok):
        return ("c", tok[1]) if tok[0] == "c" else ("d", id(tok[1]))

    @staticmethod
    def _val(tok):
        return tok[2]

    def _need(self, eng, tok, waits, same_ok=False):
        if tok[0] == "c" and tok[1] == eng and (eng == "pe" or same_ok):
            return
        k = self._key(tok)
        if self.seen[eng].get(k, -1) >= tok[2]:
            return
        self.seen[eng][k] = tok[2]
        waits.append(tok)

    def _deps(self, eng, reads, writes):
        waits = []
        for b in reads:
            for t in b.w.values():
                self._need(eng, t, waits)
        for b in writes:
            for t in b.w.values():
                self._need(eng, t, waits)
            for t in b.r.values():
                self._need(eng, t, waits)
        return waits

    def _commit(self, tok, reads, writes):
        k = self._key(tok)
        for b in reads:
            b.r[k] = tok
        for b in writes:
            b.w = {k: tok}
            b.r = {}

    def op(self, eng, fn, reads=(), writes=()):
        waits = self._deps(eng, reads, writes)
        tok = ("c", eng, len(self.ops[eng]))
        self.ops[eng].append([waits, fn, None])
        self._commit(tok, reads, writes)
        return tok

    def dma(self, q, fn, reads=(), writes=(), final=False):
        waits = self._deps(q, reads, writes)
        i = self.ring_i[q]
        nr = self.NRINGS[q]
        slot = i % nr
        prev = self.ring_tok[q][slot]
        if prev is not None:
            self._need(q, prev, waits)
        sem = self.ring[q][slot]
        val = 16 * (i // nr + 1)
        self.ring_i[q] = i + 1
        tok = ("d", sem, val, q)
        self.ring_tok[q][slot] = tok
        self.ops[q].append([waits, fn, (sem, 16)])
        self._commit(tok, reads, writes)
        return tok

    def _all_tokens(self):
        toks = []
        for e in ("pe", "act", "dve", "pool"):
            for idx in range(len(self.ops[e]) - 1, -1, -1):
                ent = self.ops[e][idx]
                if ent[1] is not None and ent[2] is None:
                    toks.append(("c", e, idx))
                    break
        for q in ("act", "pool", "sp"):
            for t in self.ring_tok[q]:
                if t is not None:
                    toks.append(t)
        return toks

    def barrier(self):
        toks = self._all_tokens()
        for e in self.ENG:
            waits = []
            for t in toks:
                k = self._key(t)
                if self.seen[e].get(k, -1) >= t[2]:
                    continue
                self.seen[e][k] = t[2]
                waits.append(t)
            if waits:
                self.ops[e].append([waits, None, None])

    def emit(self):
        nc = self.nc
        final_waits = []
        for t in self._all_tokens():
            self._need("sp", t, final_waits)
        signal = {e: set() for e in self.ENG}
        allw = [final_waits]
        for e in self.ENG:
            for ent in self.ops[e]:
                allw.append(ent[0])
        for ws in allw:
            for t in ws:
                if t[0] == "c":
                    signal[t[1]].add(t[2])
        semval = {e: {} for e in self.ENG}
        for e in self.ENG:
            n = 0
            for idx in sorted(signal[e]):
                n += 1
                semval[e][idx] = n

        def resolve(t):
            if t[0] == "c":
                return self.sem[t[1]], semval[t[1]][t[2]]
            return t[1], t[2]

        def run(e, handle, extra=None):
            for idx, (waits, fn, dinc) in enumerate(self.ops[e]):
                for t in waits:
                    sem, val = resolve(t)
                    handle.wait_ge(sem, val)
                if fn is not None:
                    ins = fn(handle)
                    if dinc is not None:
                        ins.then_inc(dinc[0], dinc[1])
                    elif idx in signal[e]:
                        ins.then_inc(self.sem[e], 1)
            if extra:
                for t in extra:
                    sem, val = resolve(t)
                    handle.wait_ge(sem, val)

        with nc.Block() as block:
            @block.tensor
            def _(h):
                run("pe", h)

            @block.scalar
            def _(h):
                run("act", h)

            @block.vector
            def _(h):
                run("dve", h)

            @block.gpsimd
            def _(h):
                run("pool", h)

            @block.sync
            def _(h):
                run("sp", h, final_waits)


class Ctx:
    pass


def bufs(n, name=""):
    return [Buf("%s%d" % (name, i)) for i in range(n)]


def phase_a(c):
    nc, P = c.nc, c.P
    with ExitStack() as st:
        sb = lambda n, s, d: st.enter_context(nc.sbuf_tensor(n, s, d))
        ps = lambda n, s, d: st.enter_context(nc.psum_tensor(n, s, d))
        Wbf = sb("a_Wbf", [128, 8, INW], BF16)
        stage = [sb("a_stage%d" % i, [128, INW], F32) for i in range(2)]
        w1 = sb("a_w1", [128, 8], F32)
        identb = sb("a_identb", [128, 128], BF16)
        gq = sb("a_gq", [128, 64], F32)
        gk = sb("a_gk", [128, 64], F32)
        xt = [sb("a_xt%d" % i, [128, D], F32) for i in range(2)]
        junk = sb("a_junk", [128, D], F32)
        ss = sb("a_ss", [128, 4], F32)
        xn = sb("a_xn", [128, D], BF16)
        xnT = sb("a_xnT", [128, 8, 128], BF16)
        sq = sb("a_sq", [128, 512], F32)
        tmp = sb("a_tmp", [128, 512], F32)
        s8 = sb("a_s8", [128, 24], F32)
        qn = sb("a_qn", [128, 512], BF16)
        qTs = sb("a_qTs", [128, 4, 128], BF16)
        ob = [sb("a_ob%d" % i, [128, 512], BF16) for i in range(2)]
        fm = [sb("a_fm%d" % i, [128, 4, 128], F32) for i in range(2)]
        gsb = sb("a_gsb", [4, 4, 128], F32)
        ps_t = ps("a_ps_t", [128, D], BF16)
        ps_g = [ps("a_ps_g%d" % i, [128, 512], F32) for i in range(2)]
        ps_q = ps("a_ps_q", [128, 4, 128], BF16)
        ps_f = [ps("a_ps_f%d" % i, [128, 4, 128], F32) for i in range(2)]
        ps_gt = ps("a_ps_gt", [4, 4, 128], F32)

        bW = bufs(8, "W")
        bst = bufs(2, "st")
        bc = Buf("consts")
        bxt = bufs(2, "xt")
        bjunk, bss, bxn, bxnT, bsq, btmp, bs8, bqn, bqTs = [Buf(n) for n in
            ("junk", "ss", "xn", "xnT", "sq", "tmp", "s8", "qn", "qTs")]
        bob = bufs(2, "ob")
        bfm = bufs(2, "fm")
        bgsb = Buf("gsb")
        bps_t, bps_q, bps_gt = Buf("ps_t"), Buf("ps_q"), Buf("ps_gt")
        bps_g = bufs(2, "ps_g")
        bps_f = bufs(2, "ps_f")

        P.dma("sp", lambda h: h.dma_start(out=w1[:], in_=c.norm1_w), writes=[bc])
        P.dma("sp", lambda h: h.dma_start(out=identb[:], in_=c.identb), writes=[bc])
        P.dma("sp", lambda h: h.dma_start(out=gq[:], in_=c.gq), writes=[bc])
        P.dma("sp", lambda h: h.dma_start(out=gk[:], in_=c.gk), writes=[bc])
        for kc in range(8):
            s_ = stage[kc % 2]
            P.dma("sp", lambda h, kc=kc, s_=s_: h.dma_start(out=s_[:], in_=c.w_in[kc * 128:(kc + 1) * 128, :]),
                  writes=[bst[kc % 2]])
            P.op("dve" if kc % 2 == 0 else "pool",
                 lambda h, kc=kc, s_=s_: h.tensor_scalar(out=Wbf[:, kc, :], in0=s_[:], scalar1=w1[:, kc:kc + 1],
                                                         scalar2=None, op0=ALU.mult),
                 reads=[bst[kc % 2], bc], writes=[bW[kc]])

        gi = [0]

        def mm_group_tok(cols, sub=None):
            k = gi[0] % 2
            gi[0] += 1
            c0, c1 = cols
            for kc in range(8):
                P.op("pe", lambda h, kc=kc, k=k: h.matmul(ps_g[k][:, 0:c1 - c0], lhsT=xnT[:, kc, :],
                                                       rhs=Wbf[:, kc, c0:c1], start=(kc == 0), stop=(kc == 7)),
                     reads=[bxnT, bW[kc]], writes=[bps_g[k]])
            return k

        oi = [0]
        fi = [0]
        xn2_ = [xn, sb("a_xn_b", [128, D], BF16)]
        xnT2 = [xnT, sb("a_xnT_b", [128, 8, 128], BF16)]
        ps_t2 = [ps_t, ps("a_ps_t_b", [128, D], BF16)]
        bxn2, bxnT2, bps_t2 = [bxn, Buf("xn_b")], [bxnT, Buf("xnT_b")], [bps_t, Buf("ps_t_b")]
        sq2 = [sq, sb("a_sq_b", [128, 512], F32)]
        tmp2 = [tmp, sb("a_tmp_b", [128, 512], F32)]
        s82 = [s8, sb("a_s8_b", [128, 24], F32)]
        qn2 = [qn, sb("a_qn_b", [128, 512], BF16)]
        qTs2 = [qTs, sb("a_qTs_b", [128, 4, 128], BF16)]
        bsq2, btmp2, bs82, bqn2, bqTs2 = [bsq, Buf("sq_b")], [btmp, Buf("tmp_b")], [bs8, Buf("s8_b")], [bqn, Buf("qn_b")], [bqTs, Buf("qTs_b")]

        def front(i):
            p = i % 2
            x_, bx_ = xt[p], bxt[p]
            P.dma("sp", lambda h: h.dma_start(out=x_[:], in_=c.x[i * 128:(i + 1) * 128, :]), writes=[bx_])
            P.op("act", lambda h: h.activation(out=junk[:], in_=x_[:], func=AF.Square, accum_out=ss[:, 0:1]),
                 reads=[bx_], writes=[bss])
            P.op("act", lambda h: h.activation(out=ss[:, 1:2], in_=ss[:, 0:1], func=AF.Sqrt, scale=1.0 / D, bias=EPS),
                 reads=[bss], writes=[bss])
            P.op("dve", lambda h: h.reciprocal(out=ss[:, 2:3], in_=ss[:, 1:2]), reads=[bss], writes=[bss])
            P.op("dve", lambda h: h.tensor_scalar(out=xn2_[p][:], in0=x_[:], scalar1=ss[:, 2:3], scalar2=None,
                                                  op0=ALU.mult), reads=[bx_, bss], writes=[bxn2[p]])
            for kc in range(8):
                P.op("pe", lambda h, kc=kc: h.transpose(out=ps_t2[p][:, kc * 128:(kc + 1) * 128],
                                                        in_=xn2_[p][:, kc * 128:(kc + 1) * 128], identity=identb[:]),
                     reads=[bxn2[p], bc], writes=[bps_t2[p]])
            P.op("act", lambda h: h.copy(out=xnT2[p][:].rearrange("p k t -> p (k t)"), in_=ps_t2[p][:]),
                 reads=[bps_t2[p]], writes=[bxnT2[p]])

        def body(i):
            p = i % 2
            xT, bxT = xnT2[p], bxnT2[p]

            def mm_tok(c0):
                k = gi[0] % 2
                gi[0] += 1
                for kc in range(8):
                    P.op("pe", lambda h, kc=kc, k=k: h.matmul(ps_g[k][:], lhsT=xT[:, kc, :], rhs=Wbf[:, kc, c0:c0 + 512],
                                                           start=(kc == 0), stop=(kc == 7)),
                         reads=[bxT, bW[kc]], writes=[bps_g[k]])
                return k

            def qk_chain(w, k, gain):
                P.op("act", lambda h: h.activation(out=sq2[w][:], in_=ps_g[k][:], func=AF.Square),
                     reads=[bps_g[k]], writes=[bsq2[w]])
                P.op("dve", lambda h: h.tensor_reduce(out=s82[w][:, 0:8], in_=sq2[w][:].rearrange("p (a b) -> p a b", b=64),
                                                      axis=AX.X, op=ALU.add), reads=[bsq2[w]], writes=[bs82[w]])
                P.op("act", lambda h: h.activation(out=s82[w][:, 8:16], in_=s82[w][:, 0:8], func=AF.Sqrt, scale=1.0 / 64,
                                                   bias=EPS), reads=[], writes=[bs82[w]])
                P.op("dve", lambda h: h.reciprocal(out=s82[w][:, 16:24], in_=s82[w][:, 8:16]), reads=[], writes=[bs82[w]])
                P.op("dve", lambda h: h.tensor_tensor(
                    out=tmp2[w][:].rearrange("p (a b) -> p a b", b=64),
                    in0=ps_g[k][:].rearrange("p (a b) -> p a b", b=64),
                    in1=s82[w][:, 16:24].unsqueeze(2).to_broadcast([128, 8, 64]), op=ALU.mult),
                    reads=[bps_g[k], bs82[w]], writes=[btmp2[w]])
                P.op("pool", lambda h: h.tensor_tensor(
                    out=qn2[w][:].rearrange("p (a b) -> p a b", b=64),
                    in0=tmp2[w][:].rearrange("p (a b) -> p a b", b=64),
                    in1=gain[:].unsqueeze(1).to_broadcast([128, 8, 64]), op=ALU.mult),
                    reads=[btmp2[w], bc], writes=[bqn2[w]])

            def qk_tr(w, dst):
                for hp in range(4):
                    P.op("pe", lambda h, hp=hp: h.transpose(out=ps_q[:, hp, :], in_=qn2[w][:, hp * 128:(hp + 1) * 128],
                                                            identity=identb[:]), reads=[bqn2[w], bc], writes=[bps_q])
                P.op("act", lambda h: h.copy(out=qTs2[w][:], in_=ps_q[:]), reads=[bps_q], writes=[bqTs2[w]])
                P.dma("sp", lambda h: h.dma_start(
                    out=dst[:, :, i * 128:(i + 1) * 128].rearrange("a p t -> p a t"), in_=qTs2[w][:]),
                    reads=[bqTs2[w]], writes=[c.bQKT[w][i]])

            def tok_out(c0, dst, bdst, fn):
                k = mm_tok(c0)
                o = oi[0] % 2
                oi[0] += 1
                P.op("act", lambda h: h.activation(out=ob[o][:], in_=ps_g[k][:], func=fn),
                     reads=[bps_g[k]], writes=[bob[o]])
                P.dma("sp", lambda h: h.dma_start(out=dst[i * 128:(i + 1) * 128, :], in_=ob[o][:]),
                      reads=[bob[o]], writes=[bdst[i]])

            def fm_half(half):
                f = fi[0] % 2
                fi[0] += 1
                for cc in range(4):
                    ch = half * 4 + cc
                    col = 1536 + ch * 128
                    for kc in range(8):
                        P.op("pe", lambda h, kc=kc, cc=cc, col=col: h.matmul(
                            ps_f[f][:, cc, :], lhsT=Wbf[:, kc, col:col + 128], rhs=xT[:, kc, :],
                            start=(kc == 0), stop=(kc == 7)), reads=[bxT, bW[kc]], writes=[bps_f[f]])
                P.op("dve", lambda h: h.tensor_copy(out=fm[f][:], in_=ps_f[f][:]), reads=[bps_f[f]], writes=[bfm[f]])
                P.dma("sp", lambda h: h.dma_start(
                    out=c.MQKT[half * 512:(half + 1) * 512, i * 128:(i + 1) * 128].rearrange("(a p) t -> p a t", p=128),
                    in_=fm[f][:]), reads=[bfm[f]], writes=[c.bMQKT[i]])

            kq = mm_tok(0)
            qk_chain(0, kq, gq)
            kk = mm_tok(512)
            qk_chain(1, kk, gk)
            tok_out(1024, c.V, c.bV, AF.Copy)
            qk_tr(0, c.QT)
            tok_out(2560, c.MV, c.bMV, AF.Copy)
            qk_tr(1, c.KT)
            tok_out(3072, c.SIGO, c.bSIGO, AF.Sigmoid)
            fm_half(0)
            fm_half(1)
            for g in range(4):
                col = 3584 + 4 * g
                for kc in range(8):
                    P.op("pe", lambda h, kc=kc, g=g, col=col: h.matmul(
                        ps_gt[:, g, :], lhsT=Wbf[:, kc, col:col + 4], rhs=xT[:, kc, :],
                        start=(kc == 0), stop=(kc == 7)), reads=[bxT, bW[kc]], writes=[bps_gt])
            P.op("dve", lambda h: h.tensor_copy(out=gsb[:], in_=ps_gt[:]), reads=[bps_gt], writes=[bgsb])
            P.dma("sp", lambda h: h.dma_start(out=c.GT[:, :, i * 128:(i + 1) * 128].rearrange("g a t -> a g t"),
                                              in_=gsb[:]), reads=[bgsb], writes=[c.bGT[i]])

        front(0)
        for i in range(NT):
            if i + 1 < NT:
                front(i + 1)
            body(i)
    P.barrier()


def phase_b(c):
    nc, P = c.nc, c.P
    with ExitStack() as st:
        sb = lambda n, s, d: st.enter_context(nc.sbuf_tensor(n, s, d))
        ps = lambda n, s, d: st.enter_context(nc.psum_tensor(n, s, d))
        QT = sb("b_QT", [128, 4, S], BF16)
        KT = sb("b_KT", [128, 4, S], BF16)
        V = sb("b_V", [128, NT, 8, 65], BF16)
        TBI = sb("b_TBI", [128, 8, 896], F32)
        TBA = sb("b_TBA", [128, 8, 896], F32)
        MI = sb("b_MI", [128, 896], F32)
        MA = sb("b_MA", [128, 896], F32)
        gao = sb("b_gao", [128, 512], F32)
        sT = [sb("b_sT%d" % i, [128, 640], F32) for i in range(2)]
        pT = [sb("b_pT%d" % i, [128, 640], BF16) for i in range(2)]
        ao = sb("b_ao", [128, 512], F32)
        junk = sb("b_junk", [128, 512], F32)
        rc = [sb("b_rc%d" % i, [128, 1], F32) for i in range(2)]
        ss = sb("b_ss", [128, 4], F32)
        aob = [sb("b_aob%d" % i, [128, 512], BF16) for i in range(2)]
        ps_s = [ps("b_ps_s%d" % i, [128, 1024], F32) for i in range(2)]
        ps_o = [ps("b_ps_o%d" % i, [128, 128], F32) for i in range(2)]

        bQT, bKT = bufs(4, "bQT"), bufs(4, "bKT")
        bVt = bufs(NT, "bV")
        bones, btb, bm, bgao = Buf("ones"), Buf("tb"), Buf("m"), Buf("gao")
        bsT, bpT, brc, baob = bufs(2, "sT"), bufs(2, "pT"), bufs(2, "rc"), bufs(2, "aob")
        bao, bjunk, bss = Buf("ao"), Buf("junk"), Buf("ss")
        bps_s, bps_o = bufs(2, "ps_s"), bufs(2, "ps_o")

        P.dma("sp", lambda h: h.dma_start(out=TBI[:], in_=c.rpbg), writes=[btb])
        P.dma("sp", lambda h: h.dma_start(out=MI[:], in_=c.mask_i), writes=[bm])
        P.dma("sp", lambda h: h.dma_start(out=MA[:], in_=c.mask_a), writes=[bm])
        P.dma("sp", lambda h: h.dma_start(out=gao[:], in_=c.gao), writes=[bgao])
        for hp in range(4):
            P.dma("sp", lambda h, hp=hp: h.dma_start(out=QT[:, hp, :], in_=c.QT[hp]),
                  reads=c.bQKT[0], writes=[bQT[hp]])
            P.dma("act", lambda h, hp=hp: h.dma_start(out=KT[:, hp, :], in_=c.KT[hp]),
                  reads=c.bQKT[1], writes=[bKT[hp]])
        P.op("pool", lambda h: h.memset(V[:, :, :, 64:65], 1.0), writes=[bones])
        for i in range(NT):
            P.dma("sp" if i % 2 == 0 else "act", lambda h, i=i: h.dma_start(
                out=V[:, i, :, 0:64], in_=c.V[i * 128:(i + 1) * 128, :].rearrange("p (a b) -> p a b", b=64)),
                reads=[c.bV[i]], writes=[bVt[i]])
        for hd in range(8):
            P.op("dve", lambda h, hd=hd: h.tensor_tensor(out=TBA[:, hd, :], in0=TBI[:, hd, :], in1=MA[:], op=ALU.add),
                 reads=[btb, bm], writes=[btb])
        for hd in range(8):
            P.op("dve", lambda h, hd=hd: h.tensor_tensor(out=TBI[:, hd, :], in0=TBI[:, hd, :], in1=MI[:], op=ALU.add),
                 reads=[btb, bm], writes=[btb])

        it = 0
        for j in range(NT):
            if 2 <= j <= 29:
                kts = list(range(j - 2, j + 3)); tb = TBI; s0 = 1
            elif j == 0:
                kts = [0, 1, 2, 3]; tb = TBA; s0 = 3
            elif j == 1:
                kts = [0, 1, 2, 3]; tb = TBA; s0 = 2
            elif j == 30:
                kts = [28, 29, 30, 31]; tb = TBA; s0 = 1
            else:
                kts = [28, 29, 30, 31]; tb = TBA; s0 = 0
            n = len(kts)
            def st_mm(hd, k):
                hp, hh = hd // 2, hd % 2
                p0, p1 = hh * 64, hh * 64 + 64
                for idx, kt in enumerate(kts):
                    P.op("pe", lambda h, k=k, idx=idx, kt=kt, hp=hp, p0=p0, p1=p1, j=j: h.matmul(
                        ps_s[k][:, idx * 128:(idx + 1) * 128], lhsT=KT[p0:p1, hp, kt * 128:(kt + 1) * 128],
                        rhs=QT[p0:p1, hp, j * 128:(j + 1) * 128], start=True, stop=True),
                        reads=[bKT[hp], bQT[hp]], writes=[bps_s[k]])

            def post(hd, k):
                P.op("dve", lambda h, k=k, n=n, tb=tb, s0=s0, hd=hd: h.scalar_tensor_tensor(
                    out=sT[k][:, 0:n * 128], in0=ps_s[k][:, 0:n * 128], scalar=0.125,
                    in1=tb[:, hd, s0 * 128:(s0 + n) * 128], op0=ALU.mult, op1=ALU.add),
                    reads=[bps_s[k], btb], writes=[bsT[k]])
                P.op("act", lambda h, k=k, n=n: h.activation(out=pT[k][:, 0:n * 128], in_=sT[k][:, 0:n * 128],
                                                             func=AF.Exp), reads=[bsT[k]], writes=[bpT[k]])

            def pv(hd, k):
                for idx, kt in enumerate(kts):
                    P.op("pe", lambda h, k=k, idx=idx, kt=kt, hd=hd, n=n: h.matmul(
                        ps_o[k][:, 0:65], lhsT=pT[k][:, idx * 128:(idx + 1) * 128], rhs=V[:, kt, hd, :],
                        start=(idx == 0), stop=(idx == n - 1)),
                        reads=[bpT[k], bVt[kt], bones], writes=[bps_o[k]])
                P.op("dve", lambda h, k=k: h.reciprocal(out=rc[k][:], in_=ps_o[k][:, 64:65]),
                     reads=[bps_o[k]], writes=[brc[k]])
                P.op("dve", lambda h, k=k, hd=hd: h.tensor_scalar(
                    out=ao[:, hd * 64:(hd + 1) * 64], in0=ps_o[k][:, 0:64], scalar1=rc[k][:], scalar2=None,
                    op0=ALU.mult), reads=[bps_o[k], brc[k]], writes=[bao])

            st_mm(0, it % 2)
            for hd in range(8):
                k = it % 2
                it += 1
                post(hd, k)
                if hd + 1 < 8:
                    st_mm(hd + 1, it % 2)
                pv(hd, k)
            o = j % 2
            P.op("act", lambda h: h.activation(out=junk[:], in_=ao[:], func=AF.Square, accum_out=ss[:, 0:1]),
                 reads=[bao], writes=[bjunk, bss])
            P.op("act", lambda h: h.activation(out=ss[:, 1:2], in_=ss[:, 0:1], func=AF.Sqrt, scale=1.0 / 512, bias=EPS),
                 reads=[bss], writes=[bss])
            P.op("dve", lambda h: h.reciprocal(out=ss[:, 2:3], in_=ss[:, 1:2]), reads=[bss], writes=[bss])
            P.op("dve", lambda h, o=o: h.scalar_tensor_tensor(out=aob[o][:], in0=ao[:], scalar=ss[:, 2:3], in1=gao[:],
                                                          op0=ALU.mult, op1=ALU.mult),
                 reads=[bao, bss, bgao], writes=[baob[o]])
            P.dma("sp", lambda h, j=j, o=o: h.dma_start(out=c.AOUT[j * 128:(j + 1) * 128, :], in_=aob[o][:]),
                  reads=[baob[o]], writes=[c.bAOUT[j]])
    P.barrier()


def phase_c(c):
    nc, P = c.nc, c.P
    with ExitStack() as st0:
        sb0 = lambda n, s, d: st0.enter_context(nc.sbuf_tensor(n, s, d))
        COLS = sb0("c_COLS", [128, NT, 24], F32)
        bCOLS = Buf("COLS")
        with ExitStack() as st:
            sb = lambda n, s, d: st.enter_context(nc.sbuf_tensor(n, s, d))
            ps = lambda n, s, d: st.enter_context(nc.psum_tensor(n, s, d))
            G1, G2, CL, Aa, AA, ZER, T1 = [sb("c1_" + n, [4, S], F32) for n in ("G1", "G2", "CL", "Aa", "AA", "ZER", "T1")]
            bG1, bG2, bCL, bAa, bAA, bZER, bT1 = [Buf(n) for n in ("G1", "G2", "CL", "Aa", "AA", "ZER", "T1")]
            BCR = sb("c1_BCR", [4, NT, 257], F32)
            ROWS = sb("c1_ROWS", [24, S], F32)
            gb = sb("c1_gb", [4, 4], F32)
            ngb = sb("c1_ngb", [4, 4], F32)
            AE = sb("c1_AE", [4, NT], F32)
            APv = sb("c1_AP", [4, NT], F32)
            dd = sb("c1_dd", [4, NT], F32)
            identf = sb("c1_identf", [128, 128], F32)
            ps_c = ps("c1_ps_c", [128, NT, 32], F32)
            bBCR, bgb, bAE, bAPv, bdd, bid, bps_c = [Buf(n) for n in ("BCR", "gb", "AE", "AP", "dd", "id", "ps_c")]
            bROWS = bufs(6, "ROWS")
            P.dma("sp", lambda h: h.dma_start(out=gb[:], in_=c.gate_b), writes=[bgb])
            P.dma("sp", lambda h: h.dma_start(out=identf[:], in_=c.identf), writes=[bid])
            P.op("dve", lambda h: h.tensor_scalar(out=ngb[:], in0=gb[:], scalar1=-1.0, scalar2=None, op0=ALU.mult),
                 reads=[bgb], writes=[bgb])
            P.op("pool", lambda h: h.memset(ZER[:], 0.0), writes=[bZER])
            for d in range(2):
                rv = (lambda t: t[:, :]) if d == 0 else (lambda t: t[:, ::-1])
                gi_, gf_ = 2 * d, 2 * d + 1
                P.dma("sp", lambda h, gi_=gi_: h.dma_start(out=G1[:], in_=c.GT[gi_]), reads=c.bGT, writes=[bG1])
                P.dma("sp", lambda h, gf_=gf_: h.dma_start(out=G2[:], in_=c.GT[gf_]), reads=c.bGT, writes=[bG2])
                P.op("act", lambda h, gf_=gf_: h.activation(out=G2[:], in_=G2[:], func=AF.Exp, scale=-1.0,
                                                            bias=ngb[:, gf_:gf_ + 1]), reads=[bG2, bgb], writes=[bG2])
                P.op("act", lambda h: h.activation(out=G2[:], in_=G2[:], func=AF.Ln, bias=1.0), reads=[bG2], writes=[bG2])
                P.op("dve", lambda h, rv=rv: h.tensor_tensor_scan(out=rv(CL), data0=rv(G2), data1=ZER[:], initial=0.0,
                                                                  op0=ALU.add, op1=ALU.add),
                     reads=[bG2, bZER], writes=[bCL])
                P.op("dve", lambda h, gi_=gi_: h.scalar_tensor_tensor(out=Aa[:], in0=G1[:], scalar=gb[:, gi_:gi_ + 1],
                                                                      in1=CL[:], op0=ALU.add, op1=ALU.add),
                     reads=[bG1, bgb, bCL], writes=[bAa])
                P.op("dve", lambda h, rv=rv: h.tensor_tensor_scan(out=rv(AA), data0=rv(Aa), data1=ZER[:], initial=0.0,
                                                                  op0=ALU.max, op1=ALU.add),
                     reads=[bAa, bZER], writes=[bAA])
                P.op("dve", lambda h: h.tensor_tensor(out=T1[:], in0=CL[:], in1=AA[:], op=ALU.subtract),
                     reads=[bCL, bAA], writes=[bT1])
                P.op("act", lambda h: h.activation(out=T1[:], in_=T1[:], func=AF.Exp), reads=[bT1], writes=[bT1])
                P.dma("sp", lambda h, d=d: h.dma_start(out=ROWS[12 * d + 8:12 * d + 12, :], in_=T1[:]),
                      reads=[bT1], writes=[bROWS[3 * d + 2]])
                P.dma("sp", lambda h, d=d: h.dma_start(out=ROWS[12 * d:12 * d + 4, :], in_=Aa[:]),
                      reads=[bAa], writes=[bROWS[3 * d]])
                AAv = AA[:].rearrange("p (c t) -> p c t", t=128)
                epos = 127 if d == 0 else 0
                P.op("dve", lambda h, AAv=AAv, epos=epos: h.tensor_copy(out=AE[:], in_=AAv[:, :, epos]),
                     reads=[bAA], writes=[bAE])
                P.op("dve", lambda h: h.memset(APv[:], 0.0), writes=[bAPv])
                if d == 0:
                    P.op("dve", lambda h: h.tensor_copy(out=APv[:, 1:NT], in_=AE[:, 0:NT - 1]), reads=[bAE], writes=[bAPv])
                else:
                    P.op("dve", lambda h: h.tensor_copy(out=APv[:, 0:NT - 1], in_=AE[:, 1:NT]), reads=[bAE], writes=[bAPv])
                G1v = G1[:].rearrange("p (c t) -> p c t", t=128)
                G2v = G2[:].rearrange("p (c t) -> p c t", t=128)
                Aav = Aa[:].rearrange("p (c t) -> p c t", t=128)
                P.op("dve", lambda h, G1v=G1v, Aav=Aav: h.tensor_tensor(
                    out=G1v, in0=Aav, in1=AE[:].unsqueeze(2).to_broadcast([4, NT, 128]), op=ALU.subtract),
                    reads=[bAa, bAE], writes=[bG1])
                P.op("act", lambda h: h.activation(out=G1[:], in_=G1[:], func=AF.Exp), reads=[bG1], writes=[bG1])
                P.dma("sp", lambda h, d=d: h.dma_start(out=ROWS[12 * d + 4:12 * d + 8, :], in_=G1[:]),
                      reads=[bG1], writes=[bROWS[3 * d + 1]])
                P.op("dve", lambda h, AAv=AAv: h.tensor_scalar(out=BCR[:, :, 0:128], in0=AAv, scalar1=-1.0, scalar2=None,
                                                               op0=ALU.mult), reads=[bAA], writes=[bBCR])
                P.op("dve", lambda h, G2v=G2v, AAv=AAv: h.tensor_tensor(
                    out=G2v, in0=APv[:].unsqueeze(2).to_broadcast([4, NT, 128]), in1=AAv, op=ALU.subtract),
                    reads=[bAA, bAPv], writes=[bG2])
                P.op("act", lambda h, G2v=G2v: h.activation(out=BCR[:, :, 128:256], in_=G2v, func=AF.Exp),
                     reads=[bG2], writes=[bBCR])
                P.op("dve", lambda h: h.tensor_tensor(out=dd[:], in0=APv[:], in1=AE[:], op=ALU.subtract),
                     reads=[bAPv, bAE], writes=[bdd])
                P.op("act", lambda h: h.activation(out=BCR[:, :, 256], in_=dd[:], func=AF.Exp), reads=[bdd], writes=[bBCR])
                P.dma("sp", lambda h, d=d: h.dma_start(out=c.BCRD[d], in_=BCR[:]), reads=[bBCR], writes=[c.bBCRD[d]])
            for ch in range(NT):
                P.op("pe", lambda h, ch=ch: h.transpose(out=ps_c[:, ch, 0:24], in_=ROWS[0:24, ch * 128:(ch + 1) * 128],
                                                        identity=identf[0:24, 0:24]), reads=bROWS + [bid], writes=[bps_c])
            P.op("dve", lambda h: h.tensor_copy(out=COLS[:], in_=ps_c[:, :, 0:24]), reads=[bps_c], writes=[bCOLS])
        P.barrier()

        with ExitStack() as st:
            sb = lambda n, s, d: st.enter_context(nc.sbuf_tensor(n, s, d))
            ps = lambda n, s, d: st.enter_context(nc.psum_tensor(n, s, d))
            QKT = sb("c_QKT", [128, 8, S], BF16)
            bQKT = bufs(8, "cQKT")
            cw = sb("c_cw", [128, 8, 5], F32)
            cb = sb("c_cb", [128, 8], F32)
            bcw = Buf("cw")
            P.dma("sp", lambda h: h.dma_start(out=cw[:], in_=c.conv_w), writes=[bcw])
            P.dma("sp", lambda h: h.dma_start(out=cb[:], in_=c.conv_b), writes=[bcw])
            with ExitStack() as st2:
                sb2 = lambda n, s, d: st2.enter_context(nc.sbuf_tensor(n, s, d))
                xpad = [sb2("c2_xpad%d" % i, [128, S + 4], F32) for i in range(2)]
                acc = [sb2("c2_acc%d" % i, [128, S], F32) for i in range(2)]
                bxp, bacc = bufs(2, "xpad"), bufs(2, "acc")
                for i in range(2):
                    P.op("pool", lambda h, i=i: h.memset(xpad[i][:, 0:2], 0.0), writes=[bxp[i]])
                    P.op("pool", lambda h, i=i: h.memset(xpad[i][:, S + 2:S + 4], 0.0), writes=[bxp[i]])
                for cc in range(8):
                    k = cc % 2
                    P.dma("sp", lambda h, cc=cc, k=k: h.dma_start(out=xpad[k][:, 2:S + 2], in_=c.MQKT[cc * 128:(cc + 1) * 128, :]),
                          reads=c.bMQKT, writes=[bxp[k]])
                    P.op("dve", lambda h, cc=cc, k=k: h.tensor_scalar(out=acc[k][:], in0=xpad[k][:, 0:S], scalar1=cw[:, cc, 0:1],
                                                                      scalar2=cb[:, cc:cc + 1], op0=ALU.mult, op1=ALU.add),
                         reads=[bxp[k], bcw], writes=[bacc[k]])
                    for j in range(1, 5):
                        P.op("dve", lambda h, cc=cc, k=k, j=j: h.scalar_tensor_tensor(
                            out=acc[k][:], in0=xpad[k][:, j:j + S], scalar=cw[:, cc, j:j + 1], in1=acc[k][:],
                            op0=ALU.mult, op1=ALU.add), reads=[bxp[k], bcw, bacc[k]], writes=[bacc[k]])
                    if cc < 4:
                        P.op("act", lambda h, cc=cc, k=k: h.activation(out=QKT[:, cc, :], in_=acc[k][:], func=AF.Silu),
                             reads=[bacc[k]], writes=[bQKT[cc]])
                    else:
                        P.op("act", lambda h, cc=cc, k=k: h.activation(out=acc[k][:], in_=acc[k][:], func=AF.Silu),
                             reads=[bacc[k]], writes=[bacc[k]])
                        P.op("pool", lambda h, cc=cc, k=k: h.tensor_scalar(out=QKT[:, cc, :], in0=acc[k][:], scalar1=128.0 ** -0.5,
                                                                           scalar2=None, op0=ALU.mult),
                             reads=[bacc[k]], writes=[bQKT[cc]])
            P.barrier()

            MVs = sb("c_MVs", [128, NT, 4, 129], BF16)
            bMVs = bufs(NT, "cMV")
            bones = Buf("ones")
            SEL = sb("c_SEL", [4, 4, 128], F32)
            MSK = sb("c_MSK", [128, 2, 128], F32)
            identb = sb("c_identb", [128, 128], BF16)
            gmn = sb("c_gmn", [128, 512], F32)
            bconst = Buf("const")
            P.dma("sp", lambda h: h.dma_start(out=SEL[:], in_=c.sel), writes=[bconst])
            P.dma("sp", lambda h: h.dma_start(out=MSK[:], in_=c.msk), writes=[bconst])
            P.dma("sp", lambda h: h.dma_start(out=identb[:], in_=c.identb), writes=[bconst])
            P.dma("sp", lambda h: h.dma_start(out=gmn[:], in_=c.gmn), writes=[bconst])
            P.op("pool", lambda h: h.memset(MVs[:, :, :, 128:129], 1.0), writes=[bones])
            for i in range(NT):
                P.dma("sp" if i % 2 == 0 else "act", lambda h, i=i: h.dma_start(
                    out=MVs[:, i, :, 0:128], in_=c.MV[i * 128:(i + 1) * 128, :].rearrange("p (a b) -> p a b", b=128)),
                    reads=[c.bMV[i]], writes=[bMVs[i]])
            Cst = [sb("c_C%d" % i, [128, 129], F32) for i in range(4)]
            Cbf = [sb("c_Cbf%d" % i, [128, 129], BF16) for i in range(4)]
            bC, bCbf = bufs(4, "C"), bufs(4, "Cbf")
            bcr = [sb("c_bcr%d" % i, [4, 257], F32) for i in range(2)]
            bbcr = bufs(2, "bcr")
            Gt = [sb("c_G%d" % i, [128, 128], F32) for i in range(2)]
            Wt = [sb("c_W%d" % i, [128, 128], F32) for i in range(2)]
            PT = [sb("c_PT%d" % i, [128, 128], BF16) for i in range(2)]
            qs = [sb("c_qs%d" % i, [128, 128], BF16) for i in range(2)]
            kw = [sb("c_kw%d" % i, [128, 128], BF16) for i in range(2)]
            dec = [sb("c_dec%d" % i, [128, 1], F32) for i in range(2)]
            dn = [sb("c_dn%d" % i, [128, 2], F32) for i in range(2)]
            bG, bW, bPT, bqs, bkw, bdec, bdn = [bufs(2, n) for n in ("G", "W", "PT", "qs", "kw", "dec", "dn")]
            hbuf = [sb("c_hbuf%d" % i, [128, 512], F32) for i in range(2)]
            bhbuf = bufs(2, "hbuf")
            hf = sb("c_hf", [128, 512], F32)
            sg = sb("c_sg", [128, 512], BF16)
            sq = sb("c_sq", [128, 512], F32)
            s4 = sb("c_s4", [128, 12], F32)
            hmo = [sb("c_hmo%d" % i, [128, 512], BF16) for i in range(2)]
            bhf, bsg, bsq, bs4 = Buf("hf"), Buf("sg"), Buf("sq"), Buf("s4")
            bhmo = bufs(2, "hmo")
            ps_bc = [ps("c_ps_bc%d" % i, [128, 512], F32) for i in range(2)]
            ps_st = [ps("c_ps_st%d" % i, [128, 128], F32) for i in range(2)]
            ps_n = [ps("c_ps_n%d" % i, [128, 512], F32) for i in range(2)]
            ps_kt = ps("c_ps_kt", [128, 128], BF16)
            ps_dc = ps("c_ps_dc", [128, 512], F32)
            bps_bc, bps_st, bps_n = bufs(2, "ps_bc"), bufs(2, "ps_st"), bufs(2, "ps_n")
            bps_kt, bps_dc = Buf("ps_kt"), Buf("ps_dc")

            it = 0
            for d in range(2):
                for hd in range(4):
                    P.op("pool", lambda h, hd=hd: h.memset(Cst[hd][:], 0.0), writes=[bC[hd]])
                    P.op("pool", lambda h, hd=hd: h.memset(Cbf[hd][:], 0.0), writes=[bCbf[hd]])
                order = list(range(NT)) if d == 0 else list(range(NT - 1, -1, -1))
                for ci, ch in enumerate(order):
                    kb = ci % 2
                    P.dma("sp", lambda h, d=d, ch=ch, kb=kb: h.dma_start(out=bcr[kb][:], in_=c.BCRD[d, :, ch, :]),
                          reads=[c.bBCRD[d]], writes=[bbcr[kb]])
                    hb = hbuf[ci % 2]
                    bhb = bhbuf[ci % 2]
                    tsl = slice(ch * 128, (ch + 1) * 128)
                    for hd in range(4):
                        k = it % 2
                        it += 1
                        a_col = COLS[:, ch, 12 * d + hd:12 * d + hd + 1]
                        wk_col = COLS[:, ch, 12 * d + 4 + hd:12 * d + 5 + hd]
                        emt_col = COLS[:, ch, 12 * d + 8 + hd:12 * d + 9 + hd]
                        P.op("pe", lambda h, k=k, kb=kb, hd=hd: h.matmul(ps_bc[k][:, 0:257], lhsT=SEL[:, hd, :], rhs=bcr[kb][:],
                                                                         start=True, stop=True),
                             reads=[bconst, bbcr[kb]], writes=[bps_bc[k]])
                        P.op("pe", lambda h, k=k, hd=hd, tsl=tsl: h.matmul(ps_st[k][:], lhsT=QKT[:, 4 + hd, tsl], rhs=QKT[:, hd, tsl],
                                                                          start=True, stop=True),
                             reads=[bQKT[4 + hd], bQKT[hd]], writes=[bps_st[k]])
                        P.op("dve", lambda h, k=k, d=d: h.tensor_tensor(out=Gt[k][:], in0=ps_bc[k][:, 0:128], in1=MSK[:, d, :],
                                                                       op=ALU.add), reads=[bps_bc[k], bconst], writes=[bG[k]])
                        P.op("act", lambda h, k=k, a_col=a_col: h.activation(out=Wt[k][:], in_=Gt[k][:], func=AF.Exp, bias=a_col),
                             reads=[bG[k], bCOLS], writes=[bW[k]])
                        P.op("dve", lambda h, k=k: h.tensor_tensor(out=PT[k][:], in0=ps_st[k][:], in1=Wt[k][:], op=ALU.mult),
                             reads=[bps_st[k], bW[k]], writes=[bPT[k]])
                        P.op("dve", lambda h, k=k, hd=hd, tsl=tsl: h.tensor_tensor(out=qs[k][:], in0=ps_bc[k][:, 128:256],
                                                                                  in1=QKT[:, hd, tsl], op=ALU.mult),
                             reads=[bps_bc[k], bQKT[hd]], writes=[bqs[k]])
                        P.op("act", lambda h, k=k: h.copy(out=dec[k][:], in_=ps_bc[k][:, 256:257]),
                             reads=[bps_bc[k]], writes=[bdec[k]])
                        P.op("pe", lambda h, k=k, ch=ch, hd=hd: h.matmul(ps_n[k][:, 0:129], lhsT=PT[k][:], rhs=MVs[:, ch, hd, :],
                                                                         start=True, stop=False),
                             reads=[bPT[k], bMVs[ch], bones], writes=[bps_n[k]])
                        P.op("pe", lambda h, k=k, hd=hd: h.matmul(ps_n[k][:, 0:129], lhsT=qs[k][:], rhs=Cbf[hd][:],
                                                                  start=False, stop=True),
                             reads=[bqs[k], bCbf[hd]], writes=[bps_n[k]])
                        P.op("pe", lambda h, hd=hd, tsl=tsl: h.transpose(out=ps_kt[:], in_=QKT[:, 4 + hd, tsl], identity=identb[:]),
                             reads=[bQKT[4 + hd], bconst], writes=[bps_kt])
                        P.op("act", lambda h, k=k, wk_col=wk_col: h.activation(out=kw[k][:], in_=ps_kt[:], func=AF.Copy, scale=wk_col),
                             reads=[bps_kt, bCOLS], writes=[bkw[k]])
                        P.op("pe", lambda h, k=k, ch=ch, hd=hd: h.matmul(ps_dc[:, 0:129], lhsT=kw[k][:], rhs=MVs[:, ch, hd, :],
                                                                         start=True, stop=True),
                             reads=[bkw[k], bMVs[ch], bones], writes=[bps_dc])
                        P.op("dve", lambda h, k=k, hd=hd: h.scalar_tensor_tensor(out=Cst[hd][:], in0=Cst[hd][:], scalar=dec[k][:],
                                                                                 in1=ps_dc[:, 0:129], op0=ALU.mult, op1=ALU.add),
                             reads=[bC[hd], bdec[k], bps_dc], writes=[bC[hd]])
                        P.op("act", lambda h, hd=hd: h.copy(out=Cbf[hd][:], in_=Cst[hd][:]), reads=[bC[hd]], writes=[bCbf[hd]])
                        P.op("act", lambda h, k=k: h.activation(out=dn[k][:, 1:2], in_=ps_n[k][:, 128:129], func=AF.Abs),
                             reads=[bps_n[k]], writes=[bdn[k]])
                        P.op("dve", lambda h, k=k, emt_col=emt_col: h.tensor_scalar(out=dn[k][:, 0:1], in0=dn[k][:, 1:2],
                                                                                    scalar1=emt_col, scalar2=None, op0=ALU.max),
                             reads=[bdn[k], bCOLS], writes=[bdn[k]])
                        P.op("dve", lambda h, k=k: h.reciprocal(out=dn[k][:, 1:2], in_=dn[k][:, 0:1]), reads=[bdn[k]], writes=[bdn[k]])
                        P.op("dve", lambda h, k=k, hd=hd, hb=hb: h.tensor_scalar(out=hb[:, hd * 128:(hd + 1) * 128], in0=ps_n[k][:, 0:128],
                                                                                 scalar1=dn[k][:, 1:2], scalar2=None, op0=ALU.mult),
                             reads=[bps_n[k], bdn[k]], writes=[bhb])
                    if d == 0:
                        P.dma("sp", lambda h, ch=ch, hb=hb: h.dma_start(out=c.HF[ch * 128:(ch + 1) * 128, :], in_=hb[:]),
                              reads=[bhb], writes=[c.bHF[ch]])
                    else:
                        o = ci % 2
                        P.dma("sp", lambda h, ch=ch: h.dma_start(out=hf[:], in_=c.HF[ch * 128:(ch + 1) * 128, :]),
                              reads=[c.bHF[ch]], writes=[bhf])
                        P.dma("sp", lambda h, ch=ch: h.dma_start(out=sg[:], in_=c.SIGO[ch * 128:(ch + 1) * 128, :]),
                              reads=[c.bSIGO[ch]], writes=[bsg])
                        P.op("pool", lambda h, hb=hb: h.tensor_tensor(out=hf[:], in0=hf[:], in1=hb[:], op=ALU.add),
                             reads=[bhb, bhf], writes=[bhf])
                        P.op("act", lambda h: h.activation(out=sq[:], in_=hf[:], func=AF.Square), reads=[bhf], writes=[bsq])
                        P.op("dve", lambda h: h.tensor_reduce(out=s4[:, 0:4], in_=sq[:].rearrange("p (a b) -> p a b", b=128),
                                                              axis=AX.X, op=ALU.add), reads=[bsq], writes=[bs4])
                        P.op("act", lambda h: h.activation(out=s4[:, 4:8], in_=s4[:, 0:4], func=AF.Sqrt, scale=1.0 / 128, bias=EPS),
                             reads=[bs4], writes=[bs4])
                        P.op("dve", lambda h: h.reciprocal(out=s4[:, 8:12], in_=s4[:, 4:8]), reads=[bs4], writes=[bs4])
                        P.op("dve", lambda h: h.tensor_tensor(out=sq[:].rearrange("p (a b) -> p a b", b=128),
                                                              in0=hf[:].rearrange("p (a b) -> p a b", b=128),
                                                              in1=s4[:, 8:12].unsqueeze(2).to_broadcast([128, 4, 128]), op=ALU.mult),
                             reads=[bhf, bs4, bsq], writes=[bsq])
                        P.op("pool", lambda h: h.tensor_tensor(out=sq[:], in0=sq[:], in1=gmn[:], op=ALU.mult),
                             reads=[bsq, bconst], writes=[bsq])
                        P.op("pool", lambda h, o=o: h.tensor_tensor(out=hmo[o][:], in0=sq[:], in1=sg[:], op=ALU.mult),
                             reads=[bsq, bsg], writes=[bhmo[o]])
                        P.dma("sp", lambda h, ch=ch, o=o: h.dma_start(out=c.HM[ch * 128:(ch + 1) * 128, :], in_=hmo[o][:]),
                              reads=[bhmo[o]], writes=[c.bHM[ch]])
    P.barrier()


def phase_t(c):
    nc, P = c.nc, c.P
    JB = 4
    with ExitStack() as st:
        sb = lambda n, s, d: st.enter_context(nc.sbuf_tensor(n, s, d))
        tin = [sb("t_in%d" % i, [128, JB * 2 * D], F32) for i in range(2)]
        tout = [sb("t_out%d" % i, [128, JB * 2 * D], BF16) for i in range(2)]
        bin_, bout, bout2 = bufs(2, "tin"), bufs(2, "tout"), bufs(2, "tout2")
        src = c.peer_uv.rearrange("(p j) d -> p (j d)", p=128)
        dst = c.UVB.rearrange("(p j) d -> p (j d)", p=128)
        W = JB * 2 * D
        third = W // 4
        for stp in range(128 // JB):
            k = stp % 2
            P.dma("sp", lambda h, stp=stp, k=k: h.dma_start(out=tin[k][:], in_=src[:, stp * W:(stp + 1) * W]), writes=[bin_[k]])
            cut = (W * 5) // 8
            P.op("dve", lambda h, k=k, cut=cut: h.tensor_copy(out=tout[k][:, 0:cut], in_=tin[k][:, 0:cut]), reads=[bin_[k]], writes=[bout[k]])
            P.op("act", lambda h, k=k, cut=cut: h.copy(out=tout[k][:, cut:W], in_=tin[k][:, cut:W]), reads=[bin_[k]], writes=[bout2[k]])
            P.dma("sp", lambda h, stp=stp, k=k: h.dma_start(out=dst[:, stp * W:(stp + 1) * W], in_=tout[k][:]), reads=[bout[k], bout2[k]])
    P.barrier()


def phase_d(c):
    nc, P = c.nc, c.P
    with ExitStack() as st:
        sb = lambda n, s, d: st.enter_context(nc.sbuf_tensor(n, s, d))
        ps = lambda n, s, d: st.enter_context(nc.psum_tensor(n, s, d))
        Wo = sb("d_Wo", [128, 8, D], BF16)
        Wq = sb("d_Wq", [128, 8, 2048], BF16)
        SKT = sb("d_SKT", [128, 16, 128], BF16)
        gn2 = sb("d_gn2", [128, D], F32)
        identb = sb("d_identb", [128, 128], BF16)
        identf = sb("d_identf", [128, 128], F32)
        THR = sb("d_THR", [128, 16], F32)
        IOT = sb("d_IOT", [128, 16], F32)
        st_stage = ExitStack()
        stage = [st_stage.enter_context(nc.sbuf_tensor("d_stage%d" % i, [128, 2048], F32)) for i in range(2)]
        bconst = Buf("dconst")
        bst = bufs(2, "dst")
        bWo, bWq = bufs(8, "Wo"), bufs(8, "Wq")
        bSKT = Buf("SKT")
        for (t, src) in ((gn2, c.gn2), (identb, c.identb), (THR, c.thr), (IOT, c.iot), (identf, c.identf)):
            P.dma("sp", lambda h, t=t, src=src: h.dma_start(out=t[:], in_=src), writes=[bconst])
        n = 0
        for kc in range(8):
            k = n % 2; n += 1
            P.dma("sp", lambda h, kc=kc, k=k: h.dma_start(out=stage[k][:, 0:D], in_=c.w_out[kc * 128:(kc + 1) * 128, :]), writes=[bst[k]])
            P.op("dve", lambda h, kc=kc, k=k: h.tensor_copy(out=Wo[:, kc, :], in_=stage[k][:, 0:D]), reads=[bst[k]], writes=[bWo[kc]])
        for kc in range(8):
            k = n % 2; n += 1
            P.dma("sp", lambda h, kc=kc, k=k: h.dma_start(out=stage[k][:], in_=c.w_q[kc * 128:(kc + 1) * 128, :]), writes=[bst[k]])
            P.op("dve", lambda h, kc=kc, k=k: h.tensor_copy(out=Wq[:, kc, :], in_=stage[k][:]), reads=[bst[k]], writes=[bWq[kc]])
        k = n % 2; n += 1
        P.dma("sp", lambda h, k=k: h.dma_start(out=stage[k][:].rearrange("p (a b) -> p a b", b=128), in_=c.skt), writes=[bst[k]])
        P.op("dve", lambda h, k=k: h.tensor_copy(out=SKT[:].rearrange("p a b -> p (a b)"), in_=stage[k][:]), reads=[bst[k]], writes=[bSKT])
        P.barrier()
        st_stage.close()

        cat = sb("d_cat", [128, D], BF16)
        catT = sb("d_catT", [128, 8, 128], BF16)
        xt = sb("d_xt", [128, D], F32)
        junk = sb("d_junk", [128, D], F32)
        junk2 = sb("d_junk2", [128, D], F32)
        ss = sb("d_ss", [128, 4], F32)
        xn2b = sb("d_xn2b", [128, D], BF16)
        xn2T = sb("d_xn2T", [128, 8, 128], BF16)
        qT = sb("d_qT", [128, 16, 128], BF16)
        sc = sb("d_sc", [128, 16, 128], F32)
        m8 = sb("d_m8", [128, 16, 16], F32)
        i8 = sb("d_i8", [128, 16, 16], U32)
        i8f = sb("d_i8f", [128, 16, 16], F32)
        cand = sb("d_cand", [128, 8, 256], F32)
        t8 = sb("d_t8", [128, 8, 16], F32)
        j8 = sb("d_j8", [128, 8, 16], U32)
        jf = sb("d_jf", [128, 128], F32)
        T4 = sb("d_T4", [128, 128, 16], F32)
        af = sb("d_af", [128, 128], F32)
        bf_ = sb("d_bf", [128, 128], F32)
        E1 = sb("d_E1", [128, 128], F32)
        E2 = sb("d_E2", [128, 128], F32)
        g8 = sb("d_g8", [128, 16], F32)
        x1 = [sb("d_x1_%d" % i, [128, D], F32) for i in range(2)]
        xn2 = [sb("d_xn2_%d" % i, [128, D], F32) for i in range(2)]
        eidx = [sb("d_eidx%d" % i, [128, 128], I32) for i in range(2)]
        gts = [sb("d_gts%d" % i, [128, 8, 16], F32) for i in range(2)]
        adot = sb("d_adot", [128, 128], F32)
        ga = sb("d_ga", [128, 128], F32)
        NG = 16
        GS = c.gs
        NGRP = 128 // GS
        uv = [sb("d_uv%d" % i, [128, 2 * D], BF16) for i in range(NG)]
        dg = [sb("d_dg%d" % i, [128, 128], BF16) for i in range(4)]
        yacc = sb("d_y", [128, D], F32)
        ps_t = ps("d_ps_t", [128, D], BF16)
        ps_o = [ps("d_ps_o%d" % i, [128, 512], F32) for i in range(2)]
        ps_y = [ps("d_ps_y%d" % i, [128, 512], F32) for i in range(2)]
        ps_q = [ps("d_ps_q%d" % i, [128, 4, 128], F32) for i in range(2)]
        (bcat, bcatT, bxt, bss, bxn2b, bxn2T, bqT, bsc, bm8, bi8, bi8f, bcand, bt8, bj8,
         bjf, bT4, baf, bbf, bE1, bE2, bg8, by, bps_t) = [Buf(n_) for n_ in (
            "cat", "catT", "xt", "ss", "xn2b", "xn2T", "qT", "sc", "m8", "i8", "i8f", "cand",
            "t8", "j8", "jf", "T4", "af", "bf", "E1", "E2", "g8", "y", "ps_t")]
        bx1, bxn2, beidx, bgts = bufs(2, "x1"), bufs(2, "xn2"), bufs(2, "eidx"), bufs(2, "gts")
        badot, bga = bufs(NGRP, "adot"), bufs(NGRP, "ga")
        buv, bdg = bufs(NG, "uv"), bufs(4, "dg")
        bps_o, bps_q, bps_y = bufs(2, "ps_o"), bufs(2, "ps_q"), bufs(2, "ps_y")
        qi = [0]

        def routing(i):
            par = i % 2
            x1_, xn2_, eidx_, gts_ = x1[par], xn2[par], eidx[par], gts[par]
            bx1_, bxn2_, beidx_, bgts_ = bx1[par], bxn2[par], beidx[par], bgts[par]
            rows = slice(i * 128, (i + 1) * 128)
            P.dma("sp", lambda h: h.dma_start(out=cat[:, 0:512], in_=c.AOUT[rows, :]), reads=[c.bAOUT[i]], writes=[bcat])
            P.dma("sp", lambda h: h.dma_start(out=cat[:, 512:1024], in_=c.HM[rows, :]), reads=[c.bHM[i]], writes=[bcat])
            P.dma("sp", lambda h: h.dma_start(out=xt[:], in_=c.x[rows, :]), writes=[bxt])
            for kc in range(8):
                P.op("pe", lambda h, kc=kc: h.transpose(out=ps_t[:, kc * 128:(kc + 1) * 128], in_=cat[:, kc * 128:(kc + 1) * 128],
                                                        identity=identb[:]), reads=[bcat, bconst], writes=[bps_t])
            P.op("act", lambda h: h.copy(out=catT[:].rearrange("p k t -> p (k t)"), in_=ps_t[:]), reads=[bps_t], writes=[bcatT])
            yield
            for g in range(2):
                for kc in range(8):
                    P.op("pe", lambda h, g=g, kc=kc: h.matmul(ps_o[g][:], lhsT=catT[:, kc, :], rhs=Wo[:, kc, g * 512:(g + 1) * 512],
                                                             start=(kc == 0), stop=(kc == 7)), reads=[bcatT, bWo[kc]], writes=[bps_o[g]])
                P.op("dve", lambda h, g=g: h.tensor_tensor(out=x1_[:, g * 512:(g + 1) * 512], in0=ps_o[g][:], in1=xt[:, g * 512:(g + 1) * 512],
                                                          op=ALU.add), reads=[bps_o[g], bxt], writes=[bx1_])
                yield
            P.op("act", lambda h: h.activation(out=junk[:], in_=x1_[:], func=AF.Square, accum_out=ss[:, 0:1]), reads=[bx1_], writes=[bss])
            P.op("act", lambda h: h.activation(out=ss[:, 1:2], in_=ss[:, 0:1], func=AF.Sqrt, scale=1.0 / D, bias=EPS), reads=[bss], writes=[bss])
            P.op("dve", lambda h: h.reciprocal(out=ss[:, 2:3], in_=ss[:, 1:2]), reads=[bss], writes=[bss])
            P.op("dve", lambda h: h.scalar_tensor_tensor(out=xn2_[:], in0=x1_[:], scalar=ss[:, 2:3], in1=gn2[:], op0=ALU.mult, op1=ALU.mult),
                 reads=[bx1_, bss, bconst], writes=[bxn2_])
            P.op("act", lambda h: h.copy(out=xn2b[:], in_=xn2_[:]), reads=[bxn2_], writes=[bxn2b])
            yield
            for kc in range(8):
                P.op("pe", lambda h, kc=kc: h.transpose(out=ps_t[:, kc * 128:(kc + 1) * 128], in_=xn2b[:, kc * 128:(kc + 1) * 128],
                                                        identity=identb[:]), reads=[bxn2b, bconst], writes=[bps_t])
            P.op("act", lambda h: h.copy(out=xn2T[:].rearrange("p k t -> p (k t)"), in_=ps_t[:]), reads=[bps_t], writes=[bxn2T])
            yield
            for qg in range(4):
                k = qi[0] % 2
                qi[0] += 1
                for cc in range(4):
                    hp = qg * 4 + cc
                    for kc in range(8):
                        P.op("pe", lambda h, k=k, cc=cc, hp=hp, kc=kc: h.matmul(ps_q[k][:, cc, :], lhsT=Wq[:, kc, hp * 128:(hp + 1) * 128],
                                                                              rhs=xn2T[:, kc, :], start=(kc == 0), stop=(kc == 7)),
                             reads=[bWq[kc], bxn2T], writes=[bps_q[k]])
                P.op("act", lambda h, k=k, qg=qg: h.copy(out=qT[:, qg * 4:(qg + 1) * 4, :], in_=ps_q[k][:]), reads=[bps_q[k]], writes=[bqT])
                yield
            for qg in range(4):
                k = qi[0] % 2
                qi[0] += 1
                for cc in range(4):
                    hp = qg * 4 + cc
                    P.op("pe", lambda h, k=k, cc=cc, hp=hp: h.matmul(ps_q[k][:, cc, :], lhsT=qT[:, hp, :], rhs=SKT[:, hp, :],
                                                                   start=True, stop=True), reads=[bqT, bSKT], writes=[bps_q[k]])
                P.op("act", lambda h, k=k, qg=qg: h.copy(out=sc[:, qg * 4:(qg + 1) * 4, :], in_=ps_q[k][:]), reads=[bps_q[k]], writes=[bsc])
            yield
            for g in range(16):
                P.op("dve", lambda h, g=g: h.max(out=m8[:, g, 0:8], in_=sc[:, g, :]), reads=[bsc], writes=[bm8])
                P.op("dve", lambda h, g=g: h.max_index(out=i8[:, g, 0:8], in_max=m8[:, g, 0:8], in_values=sc[:, g, :]),
                     reads=[bsc, bm8], writes=[bi8])
                P.op("dve", lambda h, g=g: h.match_replace(out=sc[:, g, :], in_to_replace=m8[:, g, 0:8], in_values=sc[:, g, :],
                                                           imm_value=-1e30), reads=[bm8], writes=[bsc])
                P.op("dve", lambda h, g=g: h.max(out=m8[:, g, 8:16], in_=sc[:, g, :]), reads=[bsc], writes=[bm8])
                P.op("dve", lambda h, g=g: h.max_index(out=i8[:, g, 8:16], in_max=m8[:, g, 8:16], in_values=sc[:, g, :]),
                     reads=[bsc, bm8], writes=[bi8])
                yield
            m8v = m8[:].rearrange("p (a b) k -> p a b k", b=2)
            P.op("dve", lambda h: h.tensor_tensor(
                out=cand[:].rearrange("p a (x y) -> p a x y", y=16),
                in0=m8v[:, :, 0, :].unsqueeze(3).to_broadcast([128, 8, 16, 16]),
                in1=m8v[:, :, 1, :].unsqueeze(2).to_broadcast([128, 8, 16, 16]), op=ALU.add), reads=[bm8], writes=[bcand])
            yield
            for hd in range(8):
                P.op("dve", lambda h, hd=hd: h.max(out=t8[:, hd, 0:8], in_=cand[:, hd, :]), reads=[bcand], writes=[bt8])
                P.op("dve", lambda h, hd=hd: h.max_index(out=j8[:, hd, 0:8], in_max=t8[:, hd, 0:8], in_values=cand[:, hd, :]),
                     reads=[bcand, bt8], writes=[bj8])
                P.op("dve", lambda h, hd=hd: h.match_replace(out=cand[:, hd, :], in_to_replace=t8[:, hd, 0:8], in_values=cand[:, hd, :],
                                                             imm_value=-1e30), reads=[bt8], writes=[bcand])
                P.op("dve", lambda h, hd=hd: h.max(out=t8[:, hd, 8:16], in_=cand[:, hd, :]), reads=[bcand], writes=[bt8])
                P.op("dve", lambda h, hd=hd: h.max_index(out=j8[:, hd, 8:16], in_max=t8[:, hd, 8:16], in_values=cand[:, hd, :]),
                     reads=[bcand, bt8], writes=[bj8])
                yield
            P.op("dve", lambda h: h.tensor_copy(out=i8f[:], in_=i8[:]), reads=[bi8], writes=[bi8f])
            P.op("dve", lambda h: h.tensor_copy(out=jf[:], in_=j8[:].rearrange("p a k -> p (a k)")), reads=[bj8], writes=[bjf])
            P.op("dve", lambda h: h.tensor_tensor(out=T4[:], in0=jf[:].unsqueeze(2).to_broadcast([128, 128, 16]),
                                                  in1=THR[:].unsqueeze(1).to_broadcast([128, 128, 16]), op=ALU.is_ge),
                 reads=[bjf, bconst], writes=[bT4])
            yield
            P.op("dve", lambda h: h.tensor_reduce(out=af[:], in_=T4[:], axis=AX.X, op=ALU.add), reads=[bT4], writes=[baf])
            P.op("dve", lambda h: h.scalar_tensor_tensor(out=bf_[:], in0=af[:], scalar=-16.0, in1=jf[:], op0=ALU.mult, op1=ALU.add),
                 reads=[baf, bjf], writes=[bbf])
            yield
            i8v = i8f[:].rearrange("p (a b) k -> p a b k", b=2)
            for side, (idxt, Et, bE) in enumerate(((af, E1, bE1), (bf_, E2, bE2))):
                P.op("dve", lambda h, idxt=idxt: h.tensor_tensor(out=T4[:], in0=idxt[:].unsqueeze(2).to_broadcast([128, 128, 16]),
                                                                in1=IOT[:].unsqueeze(1).to_broadcast([128, 128, 16]), op=ALU.is_equal),
                     reads=[baf, bbf, bconst], writes=[bT4])
                yield
                P.op("dve", lambda h, side=side: h.tensor_tensor(
                    out=T4[:].rearrange("p (a k) x -> p a k x", k=16), in0=T4[:].rearrange("p (a k) x -> p a k x", k=16),
                    in1=i8v[:, :, side, :].unsqueeze(2).to_broadcast([128, 8, 16, 16]), op=ALU.mult),
                    reads=[bi8f], writes=[bT4])
                yield
                P.op("dve", lambda h, Et=Et: h.tensor_reduce(out=Et[:], in_=T4[:], axis=AX.X, op=ALU.add), reads=[bT4], writes=[bE])
                yield
            P.op("dve", lambda h: h.scalar_tensor_tensor(out=E1[:], in0=E1[:], scalar=128.0, in1=E2[:], op0=ALU.mult, op1=ALU.add),
                 reads=[bE2], writes=[bE1])
            P.op("dve", lambda h: h.tensor_copy(out=eidx_[:], in_=E1[:]), reads=[bE1], writes=[beidx_])
            P.op("dve", lambda h: h.tensor_tensor(out=gts_[:], in0=t8[:], in1=t8[:, :, 0:1].to_broadcast([128, 8, 16]), op=ALU.subtract),
                 reads=[bt8], writes=[bgts_])
            P.op("act", lambda h: h.activation(out=gts_[:], in_=gts_[:], func=AF.Exp), reads=[], writes=[bgts_])
            P.op("dve", lambda h: h.tensor_reduce(out=g8[:, 0:8], in_=gts_[:], axis=AX.X, op=ALU.add), reads=[bgts_], writes=[bg8])
            P.op("dve", lambda h: h.reciprocal(out=g8[:, 8:16], in_=g8[:, 0:8]), reads=[], writes=[bg8])
            P.op("dve", lambda h: h.tensor_tensor(out=gts_[:], in0=gts_[:], in1=g8[:, 8:16].unsqueeze(2).to_broadcast([128, 8, 16]),
                                                  op=ALU.mult), reads=[bg8], writes=[bgts_])
            if "EIDX" in c.dbg:
                P.dma("sp", lambda h: h.dma_start(out=c.EIDX[rows, :], in_=eidx_[:]), reads=[beidx_])
                P.dma("sp", lambda h: h.dma_start(out=c.GTS[rows, :], in_=gts_[:].rearrange("p a k -> p (a k)")), reads=[bgts_])
                P.dma("sp", lambda h: h.dma_start(out=c.X1[rows, :], in_=x1_[:]), reads=[bx1_])
            yield

        def experts(i, nxt):
            par = i % 2
            x1_, xn2_, eidx_, gts_ = x1[par], xn2[par], eidx[par], gts[par]
            bx1_, bxn2_, beidx_, bgts_ = bx1[par], bxn2[par], beidx[par], bgts[par]
            rows = slice(i * 128, (i + 1) * 128)
            gts_f = gts_[:].rearrange("p a k -> p (a k)")

            def stage_a(grp):
                for sl in range(grp * GS, (grp + 1) * GS):
                    k = sl % NG
                    P.dma("pool", lambda h, sl=sl, k=k: h.indirect_dma_start(
                        out=uv[k][:], out_offset=None, in_=c.UVB,
                        in_offset=bass.IndirectOffsetOnAxis(ap=eidx_[:, sl:sl + 1], axis=0)), reads=[beidx_], writes=[buv[k]])
                    P.op("dve", lambda h, sl=sl, k=k: h.scalar_tensor_tensor(out=junk2[:], in0=uv[k][:, 0:D], scalar=1.0, in1=xn2_[:],
                                                                            op0=ALU.mult, op1=ALU.mult, accum_out=adot[:, sl:sl + 1]),
                         reads=[buv[k], bxn2_], writes=[badot[grp]])

            def stage_b(grp):
                g0, g1 = grp * GS, (grp + 1) * GS
                P.op("act", lambda h: h.activation(out=ga[:, g0:g1], in_=adot[:, g0:g1], func=AF.Gelu),
                     reads=[badot[grp]], writes=[bga[grp]])
                for sl in range(g0, g1):
                    k = sl % NG
                    kd = sl % 4
                    P.op("act", lambda h, sl=sl: h.activation(out=ga[:, sl:sl + 1], in_=ga[:, sl:sl + 1], func=AF.Copy,
                                                              scale=gts_f[:, sl:sl + 1]), reads=[bgts_], writes=[bga[grp]])
                    P.op("act", lambda h, sl=sl, kd=kd: h.activation(out=dg[kd][:], in_=identf[:], func=AF.Copy, scale=ga[:, sl:sl + 1]),
                         reads=[bga[grp], bconst], writes=[bdg[kd]])
                    for g in range(2):
                        P.op("pe", lambda h, sl=sl, k=k, kd=kd, g=g: h.matmul(ps_y[g][:], lhsT=dg[kd][:],
                                                                             rhs=uv[k][:, D + g * 512:D + (g + 1) * 512],
                                                                             start=(sl == 0), stop=(sl == 127)),
                             reads=[bdg[kd], buv[k]], writes=[bps_y[g]])

            def adv(nsteps):
                if nxt is None:
                    return
                for _ in range(nsteps):
                    try:
                        next(nxt)
                    except StopIteration:
                        return

            stage_a(0)
            for grp in range(NGRP):
                if grp + 1 < NGRP:
                    stage_a(grp + 1)
                stage_b(grp)
                adv(c.advn)
            for g in range(2):
                P.op("dve", lambda h, g=g: h.tensor_tensor(out=yacc[:, g * 512:(g + 1) * 512], in0=ps_y[g][:], in1=x1_[:, g * 512:(g + 1) * 512],
                                                          op=ALU.add), reads=[bps_y[g], bx1_], writes=[by])
            P.dma("sp", lambda h: h.dma_start(out=c.out[rows, :], in_=yacc[:]), reads=[by], writes=[c.bOUT[i]])
            adv(1000)

        r0 = routing(0)
        for _ in r0:
            pass
        for i in range(NT):
            nxt = routing(i + 1) if i + 1 < NT else None
            if "noexp" in c.dbg or i >= c.nexp:
                par = i % 2
                P.dma("sp", lambda h, i=i, par=par: h.dma_start(out=c.out[i * 128:(i + 1) * 128, :], in_=x1[par][:]),
                      reads=[bx1[par]], writes=[c.bOUT[i]])
                if nxt is not None:
                    for _ in nxt:
                        pass
            else:
                if "noil" in c.dbg and nxt is not None:
                    for _ in nxt:
                        pass
                experts(i, nxt)
    P.barrier()


def build(dbg=(), phases="abcd"):
    nc = bass.Bass("TRN2", target_bir_lowering=False)
    c = Ctx()
    c.nc = nc
    ext_in = lambda n, s, d: nc.dram_tensor(n, s, d, kind="ExternalInput").ap()

    def scratch(n, s, d):
        kind = "ExternalOutput" if n in dbg else "Internal"
        return nc.dram_tensor(n, s, d, kind=kind).ap()

    c.x = ext_in("x", [S, D], F32)
    c.w_in = ext_in("w_in", [D, INW], F32)
    c.norm1_w = ext_in("norm1_w", [128, 8], F32)
    c.identb = ext_in("identb", [128, 128], BF16)
    c.gq = ext_in("gq", [128, 64], F32)
    c.gk = ext_in("gk", [128, 64], F32)
    c.rpbg = ext_in("rpbg", [128, 8, 896], F32)
    c.mask_i = ext_in("mask_i", [128, 896], F32)
    c.mask_a = ext_in("mask_a", [128, 896], F32)
    c.gao = ext_in("gao", [128, 512], F32)
    c.gate_b = ext_in("gate_b", [4, 4], F32)
    c.identf = ext_in("identf", [128, 128], F32)
    c.conv_w = ext_in("conv_w", [128, 8, 5], F32)
    c.conv_b = ext_in("conv_b", [128, 8], F32)
    c.sel = ext_in("sel", [4, 4, 128], F32)
    c.msk = ext_in("msk", [128, 2, 128], F32)
    c.gmn = ext_in("gmn", [128, 512], F32)
    c.w_out = ext_in("w_out", [D, D], F32)
    c.w_q = ext_in("w_q", [D, 2048], F32)
    c.skt = ext_in("skt", [128, 16, 128], F32)
    c.gn2 = ext_in("gn2", [128, D], F32)
    c.thr = ext_in("thr", [128, 16], F32)
    c.iot = ext_in("iot", [128, 16], F32)
    c.peer_uv = ext_in("peer_uv", [16384, 2 * D], F32)
    c.UVB = scratch("UVB", [16384, 2 * D], BF16)
    c.out = nc.dram_tensor("out", [S, D], F32, kind="ExternalOutput").ap()
    c.bOUT = bufs(NT, "OUT")
    c.dbg = dbg
    c.nexp = NT
    c.advn = 1
    c.gs = 4
    for d_ in dbg:
        if d_.startswith("gs"):
            c.gs = int(d_[2:])
    for d_ in dbg:
        if d_.startswith("adv"):
            c.advn = int(d_[3:])
    for d_ in dbg:
        if d_.startswith("exp"):
            c.nexp = int(d_[3:])
    if "EIDX" in dbg:
        c.EIDX = scratch("EIDX", [S, 128], I32)
        c.GTS = scratch("GTS", [S, 128], F32)
        c.X1 = scratch("X1", [S, D], F32)

    c.QT = scratch("QT", [4, 128, S], BF16)
    c.KT = scratch("KT", [4, 128, S], BF16)
    c.V = scratch("V", [S, 512], BF16)
    c.MV = scratch("MV", [S, 512], BF16)
    c.SIGO = scratch("SIGO", [S, 512], BF16)
    c.MQKT = scratch("MQKT", [1024, S], F32)
    c.GT = scratch("GT", [4, 4, S], F32)
    c.AOUT = scratch("AOUT", [S, 512], BF16)
    c.BCRD = scratch("BCRD", [2, 4, NT, 257], F32)
    c.bBCRD = bufs(2, "BCRD")
    c.HF = scratch("HF", [S, 512], F32)
    c.bHF = bufs(NT, "HF")
    c.HM = scratch("HM", [S, 512], BF16)
    c.bHM = bufs(NT, "HM")
    c.bAOUT = bufs(NT, "AOUT")
    c.bQKT = [bufs(NT, "QT"), bufs(NT, "KT")]
    c.bV, c.bMV, c.bSIGO, c.bMQKT, c.bGT = (bufs(NT, n) for n in ("V", "MV", "SIGO", "MQKT", "GT"))

    with ExitStack() as st:
        c.P = Prog(nc, st)
        if "a" in phases:
            phase_a(c)
        if "b" in phases:
            phase_b(c)
        if "c" in phases:
            phase_c(c)
        if "d" in phases:
            phase_t(c)
            phase_d(c)
        c.P.emit()
    return nc


def host_inputs(inputs, b):
    f32 = np.float32
    m = {}
    m["x"] = np.ascontiguousarray(inputs["x"][b], dtype=f32)
    m["w_in"] = np.ascontiguousarray(inputs["w_in"][0], dtype=f32)
    m["norm1_w"] = np.ascontiguousarray(inputs["norm1_w"][0].reshape(8, 128).T, dtype=f32)
    m["identb"] = np.eye(128, dtype=f32).astype(ml_dtypes.bfloat16)
    m["gq"] = np.ascontiguousarray(np.broadcast_to(inputs["q_norm_w"][0][None, :], (128, 64)), dtype=f32)
    m["gk"] = np.ascontiguousarray(np.broadcast_to(inputs["k_norm_w"][0][None, :], (128, 64)), dtype=f32)
    p = np.arange(128); kr = p // 64; kc = p % 64
    col = np.arange(128); rq = col // 64; cc = col % 64
    dt = np.arange(-3, 4)
    drow = 2 * dt[None, :, None] + kr[:, None, None] - rq[None, None, :]
    dcol = kc[:, None, None] - cc[None, None, :] + 0 * dt[None, :, None]
    rpb = inputs["attn_rpb"][0]
    g = rpb[:, np.clip(drow + 7, 0, 14), np.clip(dcol + 15, 0, 30)]
    m["rpbg"] = np.ascontiguousarray(g.transpose(1, 0, 2, 3).reshape(128, 8, 896), dtype=f32)
    cs = np.clip(cc - 8, 0, 48)
    colvalid = (kc[:, None, None] >= cs[None, None, :]) & (kc[:, None, None] < cs[None, None, :] + 16)
    colvalid = colvalid & (dt[None, :, None] > -100)
    m["mask_a"] = np.where(colvalid & (np.abs(drow) <= 7), 0.0, NEG).astype(f32).reshape(128, 896)
    m["mask_i"] = np.where(colvalid & (drow >= -4) & (drow <= 3), 0.0, NEG).astype(f32).reshape(128, 896)
    m["gate_b"] = np.ascontiguousarray(inputs["mlstm_gate_b"][0].T, dtype=f32)
    m["identf"] = np.eye(128, dtype=f32)
    m["conv_w"] = np.ascontiguousarray(inputs["mlstm_conv_w"][0].reshape(5, 8, 128).transpose(2, 1, 0), dtype=f32)
    m["conv_b"] = np.ascontiguousarray(inputs["mlstm_conv_b"][0].reshape(8, 128).T, dtype=f32)
    sel = np.zeros((4, 4, 128), f32)
    for hh in range(4):
        sel[hh, hh, :] = 1.0
    m["sel"] = sel
    ii = np.arange(128)
    msk = np.zeros((128, 2, 128), f32)
    msk[:, 0, :] = np.where(ii[:, None] <= ii[None, :], 0.0, NEG)
    msk[:, 1, :] = np.where(ii[:, None] >= ii[None, :], 0.0, NEG)
    m["msk"] = msk
    m["gmn"] = np.ascontiguousarray(np.broadcast_to(inputs["mlstm_norm_w"][0][None, :], (128, 512)), dtype=f32)
    m["w_out"] = np.ascontiguousarray(inputs["w_out"][0], dtype=f32)
    m["w_q"] = np.ascontiguousarray(inputs["peer_w_q"][0], dtype=f32)
    m["skt"] = np.ascontiguousarray(inputs["peer_sub_keys"][0].reshape(16, 128, 128).transpose(2, 0, 1), dtype=f32)
    m["gn2"] = np.ascontiguousarray(np.broadcast_to(inputs["norm2_w"][0][None, :], (128, D)), dtype=f32)
    thr = (np.arange(16, dtype=f32) + 1.0) * 16.0
    thr[15] = 1e9
    m["thr"] = np.ascontiguousarray(np.broadcast_to(thr[None, :], (128, 16)), dtype=f32)
    m["iot"] = np.ascontiguousarray(np.broadcast_to(np.arange(16, dtype=f32)[None, :], (128, 16)), dtype=f32)
    m["peer_uv"] = np.ascontiguousarray(np.concatenate([inputs["peer_u"][0], inputs["peer_v"][0]], axis=1), dtype=f32)
    m["gao"] = np.ascontiguousarray(np.broadcast_to(inputs["attn_out_norm_w"][0][None, :], (128, 512)), dtype=f32)
    return m


def kernel(**inputs):
    nc = build()
    in_maps = [host_inputs(inputs, b) for b in range(8)]
    res = run_bass_kernel_spmd(nc, in_maps, core_ids=list(range(8)))
    return np.stack([r["out"] for r in res.results], axis=0).astype(np.float32)
```

```python
import numpy as np
import ml_dtypes
import concourse.bass as bass
import concourse.mybir as mybir
from concourse.bass_utils import run_bass_kernel_spmd
from contextlib import ExitStack

F32 = mybir.dt.float32
BF16 = mybir.dt.bfloat16
I32 = mybir.dt.int32
U32 = mybir.dt.uint32
ALU = mybir.AluOpType
AF = mybir.ActivationFunctionType
AX = mybir.AxisListType

S = 4096
D = 1024
NT = 32
INW = 3600
EPS = 1e-6
NEG = -30000.0


class Buf:
    __slots__ = ("name", "w", "r")

    def __init__(self, name=""):
        self.name = name
        self.w = {}
        self.r = {}


class Prog:
    ENG = ("pe", "act", "dve", "pool", "sp")
    NRINGS = {"sp": 12, "act": 6, "pool": 48}

    def __init__(self, nc, stack):
        self.nc = nc
        self.sem = {e: stack.enter_context(nc.semaphore("s_" + e)) for e in self.ENG}
        self.ops = {e: [] for e in self.ENG}
        self.seen = {e: {} for e in self.ENG}
        self.ring = {}
        self.ring_i = {}
        self.ring_tok = {}
        for q in ("sp", "act", "pool"):
            self.ring[q] = [stack.enter_context(nc.semaphore("r_%s%d" % (q, i)))
                            for i in range(self.NRINGS[q])]
            self.ring_i[q] = 0
            self.ring_tok[q] = [None] * self.NRINGS[q]

    @staticmethod
    def _key(tok):
        return ("c", tok[1]) if tok[0] == "c" else ("d", id(tok[1]))

    @staticmethod
    def _val(tok):
        return tok[2]

    def _need(self, eng, tok, waits, same_ok=False):
        if tok[0] == "c" and tok[1] == eng and (eng == "pe" or same_ok):
            return
        k = self._key(tok)
        if self.seen[eng].get(k, -1) >= tok[2]:
            return
        self.seen[eng][k] = tok[2]
        waits.append(tok)

    def _deps(self, eng, reads, writes):
        waits = []
        for b in reads:
            for t in b.w.values():
                self._need(eng, t, waits)
        for b in writes:
            for t in b.w.values():
                self._need(eng, t, waits)
            for t in b.r.values():
                self._need(eng, t, waits)
        return waits

    def _commit(self, tok, reads, writes):
        k = self._key(tok)
        for b in reads:
            b.r[k] = tok
        for b in writes:
            b.w = {k: tok}
            b.r = {}

    def op(self, eng, fn, reads=(), writes=()):
        waits = self._deps(eng, reads, writes)
        tok = ("c", eng, len(self.ops[eng]))
        self.ops[eng].append([waits, fn, None])
        self._commit(tok, reads, writes)
        return tok

    def dma(self, q, fn, reads=(), writes=(), final=False):
        waits = self._deps(q, reads, writes)
        i = self.ring_i[q]
        nr = self.NRINGS[q]
        slot = i % nr
        prev = self.ring_tok[q][slot]
        if prev is not None:
            self._need(q, prev, waits)
        sem = self.ring[q][slot]
        val = 16 * (i // nr + 1)
        self.ring_i[q] = i + 1
        tok = ("d", sem, val, q)
        self.ring_tok[q][slot] = tok
        self.ops[q].append([waits, fn, (sem, 16)])
        self._commit(tok, reads, writes)
        return tok

    def _all_tokens(self):
        toks = []
        for e in ("pe", "act", "dve", "pool"):
            for idx in range(len(self.ops[e]) - 1, -1, -1):
                ent = self.ops[e][idx]
                if ent[1] is not None and ent[2] is None:
                    toks.append(("c", e, idx))
                    break
        for q in ("act", "pool", "sp"):
            for t in self.ring_tok[q]:
                if t is not None:
                    toks.append(t)
        return toks

    def barrier(self):
        toks = self._all_tokens()
        for e in self.ENG:
            waits = []
            for t in toks:
                k = self._key(t)
                if self.seen[e].get(k, -1) >= t[2]:
                    continue
                self.seen[e][k] = t[2]
                waits.append(t)
            if waits:
                self.ops[e].append([waits, None, None])

    def emit(self):
        nc = self.nc
        final_waits = []
        for t in self._all_tokens():
            self._need("sp", t, final_waits)
        signal = {e: set() for e in self.ENG}
        allw = [final_waits]
        for e in self.ENG:
            for ent in self.ops[e]:
                allw.append(ent[0])
        for ws in allw:
            for t in ws:
                if t[0] == "c":
                    signal[t[1]].add(t[2])
        semval = {e: {} for e in self.ENG}
        for e in self.ENG:
            n = 0
            for idx in sorted(signal[e]):
                n += 1
                semval[e][idx] = n

        def resolve(t):
            if t[0] == "c":
                return self.sem[t[1]], semval[t[1]][t[2]]
            return t[1], t[2]

        def run(e, handle, extra=None):
            for idx, (waits, fn, dinc) in enumerate(self.ops[e]):
                for t in waits:
                    sem, val = resolve(t)
                    handle.wait_ge(sem, val)
                if fn is not None:
                    ins = fn(handle)
                    if dinc is not None:
                        ins.then_inc(dinc[0], dinc[1])
                    elif idx in signal[e]:
                        ins.then_inc(self.sem[e], 1)
            if extra:
                for t in extra:
                    sem, val = resolve(t)
                    handle.wait_ge(sem, val)

        with nc.Block() as block:
            @block.tensor
            def _(h):
                run("pe", h)

            @block.scalar
            def _(h):
                run("act", h)

            @block.vector
            def _(h):
                run("dve", h)

            @block.gpsimd
            def _(h):
                run("pool", h)

            @block.sync
            def _(h):
                run("sp", h, final_waits)


class Ctx:
    pass


def bufs(n, name=""):
    return [Buf("%s%d" % (name, i)) for i in range(n)]


def phase_a(c):
    nc, P = c.nc, c.P
    with ExitStack() as st:
        sb = lambda n, s, d: st.enter_context(nc.sbuf_tensor(n, s, d))
        ps = lambda n, s, d: st.enter_context(nc.psum_tensor(n, s, d))
        Wbf = sb("a_Wbf", [128, 8, INW], BF16)
        stage = [sb("a_stage%d" % i, [128, INW], F32) for i in range(2)]
        w1 = sb("a_w1", [128, 8], F32)
        identb = sb("a_identb", [128, 128], BF16)
        gq = sb("a_gq", [128, 64], F32)
        gk = sb("a_gk", [128, 64], F32)
        xt = [sb("a_xt%d" % i, [128, D], F32) for i in range(2)]
        junk = sb("a_junk", [128, D], F32)
        ss = sb("a_ss", [128, 4], F32)
        xn = sb("a_xn", [128, D], BF16)
        xnT = sb("a_xnT", [128, 8, 128], BF16)
        sq = sb("a_sq", [128, 512], F32)
        tmp = sb("a_tmp", [128, 512], F32)
        s8 = sb("a_s8", [128, 24], F32)
        qn = sb("a_qn", [128, 512], BF16)
        qTs = sb("a_qTs", [128, 4, 128], BF16)
        ob = [sb("a_ob%d" % i, [128, 512], BF16) for i in range(2)]
        fm = [sb("a_fm%d" % i, [128, 4, 128], F32) for i in range(2)]
        gsb = sb("a_gsb", [4, 4, 128], F32)
        ps_t = ps("a_ps_t", [128, D], BF16)
        ps_g = [ps("a_ps_g%d" % i, [128, 512], F32) for i in range(2)]
        ps_q = ps("a_ps_q", [128, 4, 128], BF16)
        ps_f = [ps("a_ps_f%d" % i, [128, 4, 128], F32) for i in range(2)]
        ps_gt = ps("a_ps_gt", [4, 4, 128], F32)

        bW = bufs(8, "W")
        bst = bufs(2, "st")
        bc = Buf("consts")
        bxt = bufs(2, "xt")
        bjunk, bss, bxn, bxnT, bsq, btmp, bs8, bqn, bqTs = [Buf(n) for n in
            ("junk", "ss", "xn", "xnT", "sq", "tmp", "s8", "qn", "qTs")]
        bob = bufs(2, "ob")
        bfm = bufs(2, "fm")
        bgsb = Buf("gsb")
        bps_t, bps_q, bps_gt = Buf("ps_t"), Buf("ps_q"), Buf("ps_gt")
        bps_g = bufs(2, "ps_g")
        bps_f = bufs(2, "ps_f")

        P.dma("sp", lambda h: h.dma_start(out=w1[:], in_=c.norm1_w), writes=[bc])
        P.dma("sp", lambda h: h.dma_start(out=identb[:], in_=c.identb), writes=[bc])
        P.dma("sp", lambda h: h.dma_start(out=gq[:], in_=c.gq), writes=[bc])
        P.dma("sp", lambda h: h.dma_start(out=gk[:], in_=c.gk), writes=[bc])
        for kc in range(8):
            s_ = stage[kc % 2]
            P.dma("sp", lambda h, kc=kc, s_=s_: h.dma_start(out=s_[:], in_=c.w_in[kc * 128:(kc + 1) * 128, :]),
                  writes=[bst[kc % 2]])
            P.op("dve" if kc % 2 == 0 else "pool",
                 lambda h, kc=kc, s_=s_: h.tensor_scalar(out=Wbf[:, kc, :], in0=s_[:], scalar1=w1[:, kc:kc + 1],
                                                         scalar2=None, op0=ALU.mult),
                 reads=[bst[kc % 2], bc], writes=[bW[kc]])

        gi = [0]

        def mm_group_tok(cols, sub=None):
            k = gi[0] % 2
            gi[0] += 1
            c0, c1 = cols
            for kc in range(8):
                P.op("pe", lambda h, kc=kc, k=k: h.matmul(ps_g[k][:, 0:c1 - c0], lhsT=xnT[:, kc, :],
                                                       rhs=Wbf[:, kc, c0:c1], start=(kc == 0), stop=(kc == 7)),
                     reads=[bxnT, bW[kc]], writes=[bps_g[k]])
            return k

        oi = [0]
        fi = [0]
        xn2_ = [xn, sb("a_xn_b", [128, D], BF16)]
        xnT2 = [xnT, sb("a_xnT_b", [128, 8, 128], BF16)]
        ps_t2 = [ps_t, ps("a_ps_t_b", [128, D], BF16)]
        bxn2, bxnT2, bps_t2 = [bxn, Buf("xn_b")], [bxnT, Buf("xnT_b")], [bps_t, Buf("ps_t_b")]
        sq2 = [sq, sb("a_sq_b", [128, 512], F32)]
        tmp2 = [tmp, sb("a_tmp_b", [128, 512], F32)]
        s82 = [s8, sb("a_s8_b", [128, 24], F32)]
        qn2 = [qn, sb("a_qn_b", [128, 512], BF16)]
        qTs2 = [qTs, sb("a_qTs_b", [128, 4, 128], BF16)]
        bsq2, btmp2, bs82, bqn2, bqTs2 = [bsq, Buf("sq_b")], [btmp, Buf("tmp_b")], [bs8, Buf("s8_b")], [bqn, Buf("qn_b")], [bqTs, Buf("qTs_b")]

        def front(i):
            p = i % 2
            x_, bx_ = xt[p], bxt[p]
            P.dma("sp", lambda h: h.dma_start(out=x_[:], in_=c.x[i * 128:(i + 1) * 128, :]), writes=[bx_])
            P.op("act", lambda h: h.activation(out=junk[:], in_=x_[:], func=AF.Square, accum_out=ss[:, 0:1]),
                 reads=[bx_], writes=[bss])
            P.op("act", lambda h: h.activation(out=ss[:, 1:2], in_=ss[:, 0:1], func=AF.Sqrt, scale=1.0 / D, bias=EPS),
                 reads=[bss], writes=[bss])
            P.op("dve", lambda h: h.reciprocal(out=ss[:, 2:3], in_=ss[:, 1:2]), reads=[bss], writes=[bss])
            P.op("dve", lambda h: h.tensor_scalar(out=xn2_[p][:], in0=x_[:], scalar1=ss[:, 2:3], scalar2=None,
                                                  op0=ALU.mult), reads=[bx_, bss], writes=[bxn2[p]])
            for kc in range(8):
                P.op("pe", lambda h, kc=kc: h.transpose(out=ps_t2[p][:, kc * 128:(kc + 1) * 128],
                                                        in_=xn2_[p][:, kc * 128:(kc + 1) * 128], identity=identb[:]),
                     reads=[bxn2[p], bc], writes=[bps_t2[p]])
            P.op("act", lambda h: h.copy(out=xnT2[p][:].rearrange("p k t -> p (k t)"), in_=ps_t2[p][:]),
                 reads=[bps_t2[p]], writes=[bxnT2[p]])

        def body(i):
            p = i % 2
            xT, bxT = xnT2[p], bxnT2[p]

            def mm_tok(c0):
                k = gi[0] % 2
                gi[0] += 1
                for kc in range(8):
                    P.op("pe", lambda h, kc=kc, k=k: h.matmul(ps_g[k][:], lhsT=xT[:, kc, :], rhs=Wbf[:, kc, c0:c0 + 512],
                                                           start=(kc == 0), stop=(kc == 7)),
                         reads=[bxT, bW[kc]], writes=[bps_g[k]])
                return k

            def qk_chain(w, k, gain):
                P.op("act", lambda h: h.activation(out=sq2[w][:], in_=ps_g[k][:], func=AF.Square),
                     reads=[bps_g[k]], writes=[bsq2[w]])
                P.op("dve", lambda h: h.tensor_reduce(out=s82[w][:, 0:8], in_=sq2[w][:].rearrange("p (a b) -> p a b", b=64),
                                                      axis=AX.X, op=ALU.add), reads=[bsq2[w]], writes=[bs82[w]])
                P.op("act", lambda h: h.activation(out=s82[w][:, 8:16], in_=s82[w][:, 0:8], func=AF.Sqrt, scale=1.0 / 64,
                                                   bias=EPS), reads=[], writes=[bs82[w]])
                P.op("dve", lambda h: h.reciprocal(out=s82[w][:, 16:24], in_=s82[w][:, 8:16]), reads=[], writes=[bs82[w]])
                P.op("dve", lambda h: h.tensor_tensor(
                    out=tmp2[w][:].rearrange("p (a b) -> p a b", b=64),
                    in0=ps_g[k][:].rearrange("p (a b) -> p a b", b=64),
                    in1=s82[w][:, 16:24].unsqueeze(2).to_broadcast([128, 8, 64]), op=ALU.mult),
                    reads=[bps_g[k], bs82[w]], writes=[btmp2[w]])
                P.op("pool", lambda h: h.tensor_tensor(
                    out=qn2[w][:].rearrange("p (a b) -> p a b", b=64),
                    in0=tmp2[w][:].rearrange("p (a b) -> p a b", b=64),
                    in1=gain[:].unsqueeze(1).to_broadcast([128, 8, 64]), op=ALU.mult),
                    reads=[btmp2[w], bc], writes=[bqn2[w]])

            def qk_tr(w, dst):
                for hp in range(4):
                    P.op("pe", lambda h, hp=hp: h.transpose(out=ps_q[:, hp, :], in_=qn2[w][:, hp * 128:(hp + 1) * 128],
                                                            identity=identb[:]), reads=[bqn2[w], bc], writes=[bps_q])
                P.op("act", lambda h: h.copy(out=qTs2[w][:], in_=ps_q[:]), reads=[bps_q], writes=[bqTs2[w]])
                P.dma("sp", lambda h: h.dma_start(
                    out=dst[:, :, i * 128:(i + 1) * 128].rearrange("a p t -> p a t"), in_=qTs2[w][:]),
                    reads=[bqTs2[w]], writes=[c.bQKT[w][i]])

            def tok_out(c0, dst, bdst, fn):
                k = mm_tok(c0)
                o = oi[0] % 2
                oi[0] += 1
                P.op("act", lambda h: h.activation(out=ob[o][:], in_=ps_g[k][:], func=fn),
                     reads=[bps_g[k]], writes=[bob[o]])
                P.dma("sp", lambda h: h.dma_start(out=dst[i * 128:(i + 1) * 128, :], in_=ob[o][:]),
                      reads=[bob[o]], writes=[bdst[i]])

            def fm_half(half):
                f = fi[0] % 2
                fi[0] += 1
                for cc in range(4):
                    ch = half * 4 + cc
                    col = 1536 + ch * 128
                    for kc in range(8):
                        P.op("pe", lambda h, kc=kc, cc=cc, col=col: h.matmul(
                            ps_f[f][:, cc, :], lhsT=Wbf[:, kc, col:col + 128], rhs=xT[:, kc, :],
                            start=(kc == 0), stop=(kc == 7)), reads=[bxT, bW[kc]], writes=[bps_f[f]])
                P.op("dve", lambda h: h.tensor_copy(out=fm[f][:], in_=ps_f[f][:]), reads=[bps_f[f]], writes=[bfm[f]])
                P.dma("sp", lambda h: h.dma_start(
                    out=c.MQKT[half * 512:(half + 1) * 512, i * 128:(i + 1) * 128].rearrange("(a p) t -> p a t", p=128),
                    in_=fm[f][:]), reads=[bfm[f]], writes=[c.bMQKT[i]])

            kq = mm_tok(0)
            qk_chain(0, kq, gq)
            kk = mm_tok(512)
            qk_chain(1, kk, gk)
            tok_out(1024, c.V, c.bV, AF.Copy)
            qk_tr(0, c.QT)
            tok_out(2560, c.MV, c.bMV, AF.Copy)
            qk_tr(1, c.KT)
            tok_out(3072, c.SIGO, c.bSIGO, AF.Sigmoid)
            fm_half(0)
            fm_half(1)
            for g in range(4):
                col = 3584 + 4 * g
                for kc in range(8):
                    P.op("pe", lambda h, kc=kc, g=g, col=col: h.matmul(
                        ps_gt[:, g, :], lhsT=Wbf[:, kc, col:col + 4], rhs=xT[:, kc, :],
                        start=(kc == 0), stop=(kc == 7)), reads=[bxT, bW[kc]], writes=[bps_gt])
            P.op("dve", lambda h: h.tensor_copy(out=gsb[:], in_=ps_gt[:]), reads=[bps_gt], writes=[bgsb])
            P.dma("sp", lambda h: h.dma_start(out=c.GT[:, :, i * 128:(i + 1) * 128].rearrange("g a t -> a g t"),
                                              in_=gsb[:]), reads=[bgsb], writes=[c.bGT[i]])

        front(0)
        for i in range(NT):
            if i + 1 < NT:
                front(i + 1)
            body(i)
    P.barrier()


def phase_b(c):
    nc, P = c.nc, c.P
    with ExitStack() as st:
        sb = lambda n, s, d: st.enter_context(nc.sbuf_tensor(n, s, d))
        ps = lambda n, s, d: st.enter_context(nc.psum_tensor(n, s, d))
        QT = sb("b_QT", [128, 4, S], BF16)
        KT = sb("b_KT", [128, 4, S], BF16)
        V = sb("b_V", [128, NT, 8, 65], BF16)
        TBI = sb("b_TBI", [128, 8, 896], F32)
        TBA = sb("b_TBA", [128, 8, 896], F32)
        MI = sb("b_MI", [128, 896], F32)
        MA = sb("b_MA", [128, 896], F32)
        gao = sb("b_gao", [128, 512], F32)
        sT = [sb("b_sT%d" % i, [128, 640], F32) for i in range(2)]
        pT = [sb("b_pT%d" % i, [128, 640], BF16) for i in range(2)]
        ao = sb("b_ao", [128, 512], F32)
        junk = sb("b_junk", [128, 512], F32)
        rc = [sb("b_rc%d" % i, [128, 1], F32) for i in range(2)]
        ss = sb("b_ss", [128, 4], F32)
        aob = [sb("b_aob%d" % i, [128, 512], BF16) for i in range(2)]
        ps_s = [ps("b_ps_s%d" % i, [128, 1024], F32) for i in range(2)]
        ps_o = [ps("b_ps_o%d" % i, [128, 128], F32) for i in range(2)]

        bQT, bKT = bufs(4, "bQT"), bufs(4, "bKT")
        bVt = bufs(NT, "bV")
        bones, btb, bm, bgao = Buf("ones"), Buf("tb"), Buf("m"), Buf("gao")
        bsT, bpT, brc, baob = bufs(2, "sT"), bufs(2, "pT"), bufs(2, "rc"), bufs(2, "aob")
        bao, bjunk, bss = Buf("ao"), Buf("junk"), Buf("ss")
        bps_s, bps_o = bufs(2, "ps_s"), bufs(2, "ps_o")

        P.dma("sp", lambda h: h.dma_start(out=TBI[:], in_=c.rpbg), writes=[btb])
        P.dma("sp", lambda h: h.dma_start(out=MI[:], in_=c.mask_i), writes=[bm])
        P.dma("sp", lambda h: h.dma_start(out=MA[:], in_=c.mask_a), writes=[bm])
        P.dma("sp", lambda h: h.dma_start(out=gao[:], in_=c.gao), writes=[bgao])
        for hp in range(4):
            P.dma("sp", lambda h, hp=hp: h.dma_start(out=QT[:, hp, :], in_=c.QT[hp]),
                  reads=c.bQKT[0], writes=[bQT[hp]])
            P.dma("act", lambda h, hp=hp: h.dma_start(out=KT[:, hp, :], in_=c.KT[hp]),
                  reads=c.bQKT[1], writes=[bKT[hp]])
        P.op("pool", lambda h: h.memset(V[:, :, :, 64:65], 1.0), writes=[bones])
        for i in range(NT):
            P.dma("sp" if i % 2 == 0 else "act", lambda h, i=i: h.dma_start(
                out=V[:, i, :, 0:64], in_=c.V[i * 128:(i + 1) * 128, :].rearrange("p (a b) -> p a b", b=64)),
                reads=[c.bV[i]], writes=[bVt[i]])
        for hd in range(8):
            P.op("dve", lambda h, hd=hd: h.tensor_tensor(out=TBA[:, hd, :], in0=TBI[:, hd, :], in1=MA[:], op=ALU.add),
                 reads=[btb, bm], writes=[btb])
        for hd in range(8):
            P.op("dve", lambda h, hd=hd: h.tensor_tensor(out=TBI[:, hd, :], in0=TBI[:, hd, :], in1=MI[:], op=ALU.add),
                 reads=[btb, bm], writes=[btb])

        it = 0
        for j in range(NT):
            if 2 <= j <= 29:
                kts = list(range(j - 2, j + 3)); tb = TBI; s0 = 1
            elif j == 0:
                kts = [0, 1, 2, 3]; tb = TBA; s0 = 3
            elif j == 1:
                kts = [0, 1, 2, 3]; tb = TBA; s0 = 2
            elif j == 30:
                kts = [28, 29, 30, 31]; tb = TBA; s0 = 1
            else:
                kts = [28, 29, 30, 31]; tb = TBA; s0 = 0
            n = len(kts)
            def st_mm(hd, k):
                hp, hh = hd // 2, hd % 2
                p0, p1 = hh * 64, hh * 64 + 64
                for idx, kt in enumerate(kts):
                    P.op("pe", lambda h, k=k, idx=idx, kt=kt, hp=hp, p0=p0, p1=p1, j=j: h.matmul(
                        ps_s[k][:, idx * 128:(idx + 1) * 128], lhsT=KT[p0:p1, hp, kt * 128:(kt + 1) * 128],
                        rhs=QT[p0:p1, hp, j * 128:(j + 1) * 128], start=True, stop=True),
                        reads=[bKT[hp], bQT[hp]], writes=[bps_s[k]])

            def post(hd, k):
                P.op("dve", lambda h, k=k, n=n, tb=tb, s0=s0, hd=hd: h.scalar_tensor_tensor(
                    out=sT[k][:, 0:n * 128], in0=ps_s[k][:, 0:n * 128], scalar=0.125,
                    in1=tb[:, hd, s0 * 128:(s0 + n) * 128], op0=ALU.mult, op1=ALU.add),
                    reads=[bps_s[k], btb], writes=[bsT[k]])
                P.op("act", lambda h, k=k, n=n: h.activation(out=pT[k][:, 0:n * 128], in_=sT[k][:, 0:n * 128],
                                                             func=AF.Exp), reads=[bsT[k]], writes=[bpT[k]])

            def pv(hd, k):
                for idx, kt in enumerate(kts):
                    P.op("pe", lambda h, k=k, idx=idx, kt=kt, hd=hd, n=n: h.matmul(
                        ps_o[k][:, 0:65], lhsT=pT[k][:, idx * 128:(idx + 1) * 128], rhs=V[:, kt, hd, :],
                        start=(idx == 0), stop=(idx == n - 1)),
                        reads=[bpT[k], bVt[kt], bones], writes=[bps_o[k]])
                P.op("dve", lambda h, k=k: h.reciprocal(out=rc[k][:], in_=ps_o[k][:, 64:65]),
                     reads=[bps_o[k]], writes=[brc[k]])
                P.op("dve", lambda h, k=k, hd=hd: h.tensor_scalar(
                    out=ao[:, hd * 64:(hd + 1) * 64], in0=ps_o[k][:, 0:64], scalar1=rc[k][:], scalar2=None,
                    op0=ALU.mult), reads=[bps_o[k], brc[k]], writes=[bao])

            st_mm(0, it % 2)
            for hd in range(8):
                k = it % 2
                it += 1
                post(hd, k)
                if hd + 1 < 8:
                    st_mm(hd + 1, it % 2)
                pv(hd, k)
            o = j % 2
            P.op("act", lambda h: h.activation(out=junk[:], in_=ao[:], func=AF.Square, accum_out=ss[:, 0:1]),
                 reads=[bao], writes=[bjunk, bss])
            P.op("act", lambda h: h.activation(out=ss[:, 1:2], in_=ss[:, 0:1], func=AF.Sqrt, scale=1.0 / 512, bias=EPS),
                 reads=[bss], writes=[bss])
            P.op("dve", lambda h: h.reciprocal(out=ss[:, 2:3], in_=ss[:, 1:2]), reads=[bss], writes=[bss])
            P.op("dve", lambda h, o=o: h.scalar_tensor_tensor(out=aob[o][:], in0=ao[:], scalar=ss[:, 2:3], in1=gao[:],
                                                          op0=ALU.mult, op1=ALU.mult),
                 reads=[bao, bss, bgao], writes=[baob[o]])
            P.dma("sp", lambda h, j=j, o=o: h.dma_start(out=c.AOUT[j * 128:(j + 1) * 128, :], in_=aob[o][:]),
                  reads=[baob[o]], writes=[c.bAOUT[j]])
    P.barrier()


def phase_c(c):
    nc, P = c.nc, c.P
    with ExitStack() as st0:
        sb0 = lambda n, s, d: st0.enter_context(nc.sbuf_tensor(n, s, d))
        COLS = sb0("c_COLS", [128, NT, 24], F32)
        bCOLS = Buf("COLS")
        with ExitStack() as st:
            sb = lambda n, s, d: st.enter_context(nc.sbuf_tensor(n, s, d))
            ps = lambda n, s, d: st.enter_context(nc.psum_tensor(n, s, d))
            G1, G2, CL, Aa, AA, ZER, T1 = [sb("c1_" + n, [4, S], F32) for n in ("G1", "G2", "CL", "Aa", "AA", "ZER", "T1")]
            bG1, bG2, bCL, bAa, bAA, bZER, bT1 = [Buf(n) for n in ("G1", "G2", "CL", "Aa", "AA", "ZER", "T1")]
            BCR = sb("c1_BCR", [4, NT, 257], F32)
            ROWS = sb("c1_ROWS", [24, S], F32)
            gb = sb("c1_gb", [4, 4], F32)
            ngb = sb("c1_ngb", [4, 4], F32)
            AE = sb("c1_AE", [4, NT], F32)
            APv = sb("c1_AP", [4, NT], F32)
            dd = sb("c1_dd", [4, NT], F32)
            identf = sb("c1_identf", [128, 128], F32)
            ps_c = ps("c1_ps_c", [128, NT, 32], F32)
            bBCR, bgb, bAE, bAPv, bdd, bid, bps_c = [Buf(n) for n in ("BCR", "gb", "AE", "AP", "dd", "id", "ps_c")]
            bROWS = bufs(6, "ROWS")
            P.dma("sp", lambda h: h.dma_start(out=gb[:], in_=c.gate_b), writes=[bgb])
            P.dma("sp", lambda h: h.dma_start(out=identf[:], in_=c.identf), writes=[bid])
            P.op("dve", lambda h: h.tensor_scalar(out=ngb[:], in0=gb[:], scalar1=-1.0, scalar2=None, op0=ALU.mult),
                 reads=[bgb], writes=[bgb])
            P.op("pool", lambda h: h.memset(ZER[:], 0.0), writes=[bZER])
            for d in range(2):
                rv = (lambda t: t[:, :]) if d == 0 else (lambda t: t[:, ::-1])
                gi_, gf_ = 2 * d, 2 * d + 1
                P.dma("sp", lambda h, gi_=gi_: h.dma_start(out=G1[:], in_=c.GT[gi_]), reads=c.bGT, writes=[bG1])
                P.dma("sp", lambda h, gf_=gf_: h.dma_start(out=G2[:], in_=c.GT[gf_]), reads=c.bGT, writes=[bG2])
                P.op("act", lambda h, gf_=gf_: h.activation(out=G2[:], in_=G2[:], func=AF.Exp, scale=-1.0,
                                                            bias=ngb[:, gf_:gf_ + 1]), reads=[bG2, bgb], writes=[bG2])
                P.op("act", lambda h: h.activation(out=G2[:], in_=G2[:], func=AF.Ln, bias=1.0), reads=[bG2], writes=[bG2])
                P.op("dve", lambda h, rv=rv: h.tensor_tensor_scan(out=rv(CL), data0=rv(G2), data1=ZER[:], initial=0.0,
                                                                  op0=ALU.add, op1=ALU.add),
                     reads=[bG2, bZER], writes=[bCL])
                P.op("dve", lambda h, gi_=gi_: h.scalar_tensor_tensor(out=Aa[:], in0=G1[:], scalar=gb[:, gi_:gi_ + 1],
                                                                      in1=CL[:], op0=ALU.add, op1=ALU.add),
                     reads=[bG1, bgb, bCL], writes=[bAa])
                P.op("dve", lambda h, rv=rv: h.tensor_tensor_scan(out=rv(AA), data0=rv(Aa), data1=ZER[:], initial=0.0,
                                                                  op0=ALU.max, op1=ALU.add),
                     reads=[bAa, bZER], writes=[bAA])
                P.op("dve", lambda h: h.tensor_tensor(out=T1[:], in0=CL[:], in1=AA[:], op=ALU.subtract),
                     reads=[bCL, bAA], writes=[bT1])
                P.op("act", lambda h: h.activation(out=T1[:], in_=T1[:], func=AF.Exp), reads=[bT1], writes=[bT1])
                P.dma("sp", lambda h, d=d: h.dma_start(out=ROWS[12 * d + 8:12 * d + 12, :], in_=T1[:]),
                      reads=[bT1], writes=[bROWS[3 * d + 2]])
                P.dma("sp", lambda h, d=d: h.dma_start(out=ROWS[12 * d:12 * d + 4, :], in_=Aa[:]),
                      reads=[bAa], writes=[bROWS[3 * d]])
                AAv = AA[:].rearrange("p (c t) -> p c t", t=128)
                epos = 127 if d == 0 else 0
                P.op("dve", lambda h, AAv=AAv, epos=epos: h.tensor_copy(out=AE[:], in_=AAv[:, :, epos]),
                     reads=[bAA], writes=[bAE])
                P.op("dve", lambda h: h.memset(APv[:], 0.0), writes=[bAPv])
                if d == 0:
                    P.op("dve", lambda h: h.tensor_copy(out=APv[:, 1:NT], in_=AE[:, 0:NT - 1]), reads=[bAE], writes=[bAPv])
                else:
                    P.op("dve", lambda h: h.tensor_copy(out=APv[:, 0:NT - 1], in_=AE[:, 1:NT]), reads=[bAE], writes=[bAPv])
                G1v = G1[:].rearrange("p (c t) -> p c t", t=128)
                G2v = G2[:].rearrange("p (c t) -> p c t", t=128)
                Aav = Aa[:].rearrange("p (c t) -> p c t", t=128)
                P.op("dve", lambda h, G1v=G1v, Aav=Aav: h.tensor_tensor(
                    out=G1v, in0=Aav, in1=AE[:].unsqueeze(2).to_broadcast([4, NT, 128]), op=ALU.subtract),
                    reads=[bAa, bAE], writes=[bG1])
                P.op("act", lambda h: h.activation(out=G1[:], in_=G1[:], func=AF.Exp), reads=[bG1], writes=[bG1])
                P.dma("sp", lambda h, d=d: h.dma_start(out=ROWS[12 * d + 4:12 * d + 8, :], in_=G1[:]),
                      reads=[bG1], writes=[bROWS[3 * d + 1]])
                P.op("dve", lambda h, AAv=AAv: h.tensor_scalar(out=BCR[:, :, 0:128], in0=AAv, scalar1=-1.0, scalar2=None,
                                                               op0=ALU.mult), reads=[bAA], writes=[bBCR])
                P.op("dve", lambda h, G2v=G2v, AAv=AAv: h.tensor_tensor(
                    out=G2v, in0=APv[:].unsqueeze(2).to_broadcast([4, NT, 128]), in1=AAv, op=ALU.subtract),
                    reads=[bAA, bAPv], writes=[bG2])
                P.op("act", lambda h, G2v=G2v: h.activation(out=BCR[:, :, 128:256], in_=G2v, func=AF.Exp),
                     reads=[bG2], writes=[bBCR])
                P.op("dve", lambda h: h.tensor_tensor(out=dd[:], in0=APv[:], in1=AE[:], op=ALU.subtract),
                     reads=[bAPv, bAE], writes=[bdd])
                P.op("act", lambda h: h.activation(out=BCR[:, :, 256], in_=dd[:], func=AF.Exp), reads=[bdd], writes=[bBCR])
                P.dma("sp", lambda h, d=d: h.dma_start(out=c.BCRD[d], in_=BCR[:]), reads=[bBCR], writes=[c.bBCRD[d]])
            for ch in range(NT):
                P.op("pe", lambda h, ch=ch: h.transpose(out=ps_c[:, ch, 0:24], in_=ROWS[0:24, ch * 128:(ch + 1) * 128],
                                                        identity=identf[0:24, 0:24]), reads=bROWS + [bid], writes=[bps_c])
            P.op("dve", lambda h: h.tensor_copy(out=COLS[:], in_=ps_c[:, :, 0:24]), reads=[bps_c], writes=[bCOLS])
        P.barrier()

        with ExitStack() as st:
            sb = lambda n, s, d: st.enter_context(nc.sbuf_tensor(n, s, d))
            ps = lambda n, s, d: st.enter_context(nc.psum_tensor(n, s, d))
            QKT = sb("c_QKT", [128, 8, S], BF16)
            bQKT = bufs(8, "cQKT")
            cw = sb("c_cw", [128, 8, 5], F32)
            cb = sb("c_cb", [128, 8], F32)
            bcw = Buf("cw")
            P.dma("sp", lambda h: h.dma_start(out=cw[:], in_=c.conv_w), writes=[bcw])
            P.dma("sp", lambda h: h.dma_start(out=cb[:], in_=c.conv_b), writes=[bcw])
            with ExitStack() as st2:
                sb2 = lambda n, s, d: st2.enter_context(nc.sbuf_tensor(n, s, d))
                xpad = [sb2("c2_xpad%d" % i, [128, S + 4], F32) for i in range(2)]
                acc = [sb2("c2_acc%d" % i, [128, S], F32) for i in range(2)]
                bxp, bacc = bufs(2, "xpad"), bufs(2, "acc")
                for i in range(2):
                    P.op("pool", lambda h, i=i: h.memset(xpad[i][:, 0:2], 0.0), writes=[bxp[i]])
                    P.op("pool", lambda h, i=i: h.memset(xpad[i][:, S + 2:S + 4], 0.0), writes=[bxp[i]])
                for cc in range(8):
                    k = cc % 2
                    P.dma("sp", lambda h, cc=cc, k=k: h.dma_start(out=xpad[k][:, 2:S + 2], in_=c.MQKT[cc * 128:(cc + 1) * 128, :]),
                          reads=c.bMQKT, writes=[bxp[k]])
                    P.op("dve", lambda h, cc=cc, k=k: h.tensor_scalar(out=acc[k][:], in0=xpad[k][:, 0:S], scalar1=cw[:, cc, 0:1],
                                                                      scalar2=cb[:, cc:cc + 1], op0=ALU.mult, op1=ALU.add),
                         reads=[bxp[k], bcw], writes=[bacc[k]])
                    for j in range(1, 5):
                        P.op("dve", lambda h, cc=cc, k=k, j=j: h.scalar_tensor_tensor(
                            out=acc[k][:], in0=xpad[k][:, j:j + S], scalar=cw[:, cc, j:j + 1], in1=acc[k][:],
                            op0=ALU.mult, op1=ALU.add), reads=[bxp[k], bcw, bacc[k]], writes=[bacc[k]])
                    if cc < 4:
                        P.op("act", lambda h, cc=cc, k=k: h.activation(out=QKT[:, cc, :], in_=acc[k][:], func=AF.Silu),
                             reads=[bacc[k]], writes=[bQKT[cc]])
                    else:
                        P.op("act", lambda h, cc=cc, k=k: h.activation(out=acc[k][:], in_=acc[k][:], func=AF.Silu),
                             reads=[bacc[k]], writes=[bacc[k]])
                        P.op("pool", lambda h, cc=cc, k=k: h.tensor_scalar(out=QKT[:, cc, :], in0=acc[k][:], scalar1=128.0 ** -0.5,
                                                                           scalar2=None, op0=ALU.mult),
                             reads=[bacc[k]], writes=[bQKT[cc]])
            P.barrier()

            MVs = sb("c_MVs", [128, NT, 4, 129], BF16)
            bMVs = bufs(NT, "cMV")
            bones = Buf("ones")
            SEL = sb("c_SEL", [4, 4, 128], F32)
            MSK = sb("c_MSK", [128, 2, 128], F32)
            identb = sb("c_identb", [128, 128], BF16)
            gmn = sb("c_gmn", [128, 512], F32)
            bconst = Buf("const")
            P.dma("sp", lambda h: h.dma_start(out=SEL[:], in_=c.sel), writes=[bconst])
            P.dma("sp", lambda h: h.dma_start(out=MSK[:], in_=c.msk), writes=[bconst])
            P.dma("sp", lambda h: h.dma_start(out=identb[:], in_=c.identb), writes=[bconst])
            P.dma("sp", lambda h: h.dma_start(out=gmn[:], in_=c.gmn), writes=[bconst])
            P.op("pool", lambda h: h.memset(MVs[:, :, :, 128:129], 1.0), writes=[bones])
            for i in range(NT):
                P.dma("sp" if i % 2 == 0 else "act", lambda h, i=i: h.dma_start(
                    out=MVs[:, i, :, 0:128], in_=c.MV[i * 128:(i + 1) * 128, :].rearrange("p (a b) -> p a b", b=128)),
                    reads=[c.bMV[i]], writes=[bMVs[i]])
            Cst = [sb("c_C%d" % i, [128, 129], F32) for i in range(4)]
            Cbf = [sb("c_Cbf%d" % i, [128, 129], BF16) for i in range(4)]
            bC, bCbf = bufs(4, "C"), bufs(4, "Cbf")
            bcr = [sb("c_bcr%d" % i, [4, 257], F32) for i in range(2)]
            bbcr = bufs(2, "bcr")
            Gt = [sb("c_G%d" % i, [128, 128], F32) for i in range(2)]
            Wt = [sb("c_W%d" % i, [128, 128], F32) for i in range(2)]
            PT = [sb("c_PT%d" % i, [128, 128], BF16) for i in range(2)]
            qs = [sb("c_qs%d" % i, [128, 128], BF16) for i in range(2)]
            kw = [sb("c_kw%d" % i, [128, 128], BF16) for i in range(2)]
            dec = [sb("c_dec%d" % i, [128, 1], F32) for i in range(2)]
            dn = [sb("c_dn%d" % i, [128, 2], F32) for i in range(2)]
            bG, bW, bPT, bqs, bkw, bdec, bdn = [bufs(2, n) for n in ("G", "W", "PT", "qs", "kw", "dec", "dn")]
            hbuf = [sb("c_hbuf%d" % i, [128, 512], F32) for i in range(2)]
            bhbuf = bufs(2, "hbuf")
            hf = sb("c_hf", [128, 512], F32)
            sg = sb("c_sg", [128, 512], BF16)
            sq = sb("c_sq", [128, 512], F32)
            s4 = sb("c_s4", [128, 12], F32)
            hmo = [sb("c_hmo%d" % i, [128, 512], BF16) for i in range(2)]
            bhf, bsg, bsq, bs4 = Buf("hf"), Buf("sg"), Buf("sq"), Buf("s4")
            bhmo = bufs(2, "hmo")
            ps_bc = [ps("c_ps_bc%d" % i, [128, 512], F32) for i in range(2)]
            ps_st = [ps("c_ps_st%d" % i, [128, 128], F32) for i in range(2)]
            ps_n = [ps("c_ps_n%d" % i, [128, 512], F32) for i in range(2)]
            ps_kt = ps("c_ps_kt", [128, 128], BF16)
            ps_dc = ps("c_ps_dc", [128, 512], F32)
            bps_bc, bps_st, bps_n = bufs(2, "ps_bc"), bufs(2, "ps_st"), bufs(2, "ps_n")
            bps_kt, bps_dc = Buf("ps_kt"), Buf("ps_dc")

            it = 0
            for d in range(2):
                for hd in range(4):
                    P.op("pool", lambda h, hd=hd: h.memset(Cst[hd][:], 0.0), writes=[bC[hd]])
                    P.op("pool", lambda h, hd=hd: h.memset(Cbf[hd][:], 0.0), writes=[bCbf[hd]])
                order = list(range(NT)) if d == 0 else list(range(NT - 1, -1, -1))
                for ci, ch in enumerate(order):
                    kb = ci % 2
                    P.dma("sp", lambda h, d=d, ch=ch, kb=kb: h.dma_start(out=bcr[kb][:], in_=c.BCRD[d, :, ch, :]),
                          reads=[c.bBCRD[d]], writes=[bbcr[kb]])
                    hb = hbuf[ci % 2]
                    bhb = bhbuf[ci % 2]
                    tsl = slice(ch * 128, (ch + 1) * 128)
                    def front(hd, k, ch=ch, tsl=tsl, kb=kb, d=d, hb=hb, bhb=bhb):
                        P.op("pe", lambda h: h.matmul(ps_bc[k][:, 0:257], lhsT=SEL[:, hd, :], rhs=bcr[kb][:], start=True, stop=True),
                             reads=[bconst, bbcr[kb]], writes=[bps_bc[k]])
                        P.op("pe", lambda h: h.matmul(ps_st[k][:], lhsT=QKT[:, 4 + hd, tsl], rhs=QKT[:, hd, tsl], start=True, stop=True),
                             reads=[bQKT[4 + hd], bQKT[hd]], writes=[bps_st[k]])
                        P.op("pe", lambda h: h.transpose(out=ps_kt[:], in_=QKT[:, 4 + hd, tsl], identity=identb[:]),
                             reads=[bQKT[4 + hd], bconst], writes=[bps_kt])

                    def mid(hd, k, ch=ch, tsl=tsl, kb=kb, d=d, hb=hb, bhb=bhb):
                        a_col = COLS[:, ch, 12 * d + hd:12 * d + hd + 1]
                        wk_col = COLS[:, ch, 12 * d + 4 + hd:12 * d + 5 + hd]
                        P.op("dve", lambda h: h.tensor_tensor(out=Gt[k][:], in0=ps_bc[k][:, 0:128], in1=MSK[:, d, :], op=ALU.add),
                             reads=[bps_bc[k], bconst], writes=[bG[k]])
                        P.op("act", lambda h: h.activation(out=kw[k][:], in_=ps_kt[:], func=AF.Copy, scale=wk_col),
                             reads=[bps_kt, bCOLS], writes=[bkw[k]])
                        P.op("act", lambda h: h.activation(out=Wt[k][:], in_=Gt[k][:], func=AF.Exp, bias=a_col),
                             reads=[bG[k], bCOLS], writes=[bW[k]])
                        P.op("dve", lambda h: h.tensor_tensor(out=qs[k][:], in0=ps_bc[k][:, 128:256], in1=QKT[:, hd, tsl], op=ALU.mult),
                             reads=[bps_bc[k], bQKT[hd]], writes=[bqs[k]])
                        P.op("act", lambda h: h.copy(out=dec[k][:], in_=ps_bc[k][:, 256:257]), reads=[bps_bc[k]], writes=[bdec[k]])
                        P.op("dve", lambda h: h.tensor_tensor(out=PT[k][:], in0=ps_st[k][:], in1=Wt[k][:], op=ALU.mult),
                             reads=[bps_st[k], bW[k]], writes=[bPT[k]])

                    def back(hd, k, ch=ch, tsl=tsl, kb=kb, d=d, hb=hb, bhb=bhb):
                        emt_col = COLS[:, ch, 12 * d + 8 + hd:12 * d + 9 + hd]
                        P.op("pe", lambda h: h.matmul(ps_dc[:, 0:129], lhsT=kw[k][:], rhs=MVs[:, ch, hd, :], start=True, stop=True),
                             reads=[bkw[k], bMVs[ch], bones], writes=[bps_dc])
                        P.op("pe", lambda h: h.matmul(ps_n[k][:, 0:129], lhsT=PT[k][:], rhs=MVs[:, ch, hd, :], start=True, stop=False),
                             reads=[bPT[k], bMVs[ch], bones], writes=[bps_n[k]])
                        P.op("pe", lambda h: h.matmul(ps_n[k][:, 0:129], lhsT=qs[k][:], rhs=Cbf[hd][:], start=False, stop=True),
                             reads=[bqs[k], bCbf[hd]], writes=[bps_n[k]])
                        P.op("dve", lambda h: h.scalar_tensor_tensor(out=Cst[hd][:], in0=Cst[hd][:], scalar=dec[k][:],
                                                                     in1=ps_dc[:, 0:129], op0=ALU.mult, op1=ALU.add),
                             reads=[bC[hd], bdec[k], bps_dc], writes=[bC[hd]])
                        P.op("act", lambda h: h.copy(out=Cbf[hd][:], in_=Cst[hd][:]), reads=[bC[hd]], writes=[bCbf[hd]])
                        P.op("act", lambda h: h.activation(out=dn[k][:, 1:2], in_=ps_n[k][:, 128:129], func=AF.Abs),
                             reads=[bps_n[k]], writes=[bdn[k]])
                        P.op("dve", lambda h: h.tensor_scalar(out=dn[k][:, 0:1], in0=dn[k][:, 1:2], scalar1=emt_col, scalar2=None, op0=ALU.max),
                             reads=[bdn[k], bCOLS], writes=[bdn[k]])
                        P.op("dve", lambda h: h.reciprocal(out=dn[k][:, 1:2], in_=dn[k][:, 0:1]), reads=[bdn[k]], writes=[bdn[k]])
                        P.op("dve", lambda h: h.tensor_scalar(out=hb[:, hd * 128:(hd + 1) * 128], in0=ps_n[k][:, 0:128],
                                                              scalar1=dn[k][:, 1:2], scalar2=None, op0=ALU.mult),
                             reads=[bps_n[k], bdn[k]], writes=[bhb])

                    front(0, it % 2)
                    for hd in range(4):
                        k = it % 2
                        it += 1
                        mid(hd, k)
                        if hd + 1 < 4:
                            front(hd + 1, it % 2)
                        back(hd, k)
                    if d == 0:
                        P.dma("sp", lambda h, ch=ch, hb=hb: h.dma_start(out=c.HF[ch * 128:(ch + 1) * 128, :], in_=hb[:]),
                              reads=[bhb], writes=[c.bHF[ch]])
                    else:
                        o = ci % 2
                        P.dma("sp", lambda h, ch=ch: h.dma_start(out=hf[:], in_=c.HF[ch * 128:(ch + 1) * 128, :]),
                              reads=[c.bHF[ch]], writes=[bhf])
                        P.dma("sp", lambda h, ch=ch: h.dma_start(out=sg[:], in_=c.SIGO[ch * 128:(ch + 1) * 128, :]),
                              reads=[c.bSIGO[ch]], writes=[bsg])
                        P.op("pool", lambda h, hb=hb: h.tensor_tensor(out=hf[:], in0=hf[:], in1=hb[:], op=ALU.add),
                             reads=[bhb, bhf], writes=[bhf])
                        P.op("act", lambda h: h.activation(out=sq[:], in_=hf[:], func=AF.Square), reads=[bhf], writes=[bsq])
                        P.op("dve", lambda h: h.tensor_reduce(out=s4[:, 0:4], in_=sq[:].rearrange("p (a b) -> p a b", b=128),
                                                              axis=AX.X, op=ALU.add), reads=[bsq], writes=[bs4])
                        P.op("act", lambda h: h.activation(out=s4[:, 4:8], in_=s4[:, 0:4], func=AF.Sqrt, scale=1.0 / 128, bias=EPS),
                             reads=[bs4], writes=[bs4])
                        P.op("dve", lambda h: h.reciprocal(out=s4[:, 8:12], in_=s4[:, 4:8]), reads=[bs4], writes=[bs4])
                        P.op("dve", lambda h: h.tensor_tensor(out=sq[:].rearrange("p (a b) -> p a b", b=128),
                                                              in0=hf[:].rearrange("p (a b) -> p a b", b=128),
                                                              in1=s4[:, 8:12].unsqueeze(2).to_broadcast([128, 4, 128]), op=ALU.mult),
                             reads=[bhf, bs4, bsq], writes=[bsq])
                        P.op("pool", lambda h: h.tensor_tensor(out=sq[:], in0=sq[:], in1=gmn[:], op=ALU.mult),
                             reads=[bsq, bconst], writes=[bsq])
                        P.op("pool", lambda h, o=o: h.tensor_tensor(out=hmo[o][:], in0=sq[:], in1=sg[:], op=ALU.mult),
                             reads=[bsq, bsg], writes=[bhmo[o]])
                        P.dma("sp", lambda h, ch=ch, o=o: h.dma_start(out=c.HM[ch * 128:(ch + 1) * 128, :], in_=hmo[o][:]),
                              reads=[bhmo[o]], writes=[c.bHM[ch]])
    P.barrier()


def phase_t(c):
    nc, P = c.nc, c.P
    JB = 4
    with ExitStack() as st:
        sb = lambda n, s, d: st.enter_context(nc.sbuf_tensor(n, s, d))
        tin = [sb("t_in%d" % i, [128, JB * 2 * D], F32) for i in range(2)]
        tout = [sb("t_out%d" % i, [128, JB * 2 * D], BF16) for i in range(2)]
        bin_, bout, bout2 = bufs(2, "tin"), bufs(2, "tout"), bufs(2, "tout2")
        src = c.peer_uv.rearrange("(p j) d -> p (j d)", p=128)
        dst = c.UVB.rearrange("(p j) d -> p (j d)", p=128)
        W = JB * 2 * D
        third = W // 4
        for stp in range(128 // JB):
            k = stp % 2
            P.dma("sp", lambda h, stp=stp, k=k: h.dma_start(out=tin[k][:], in_=src[:, stp * W:(stp + 1) * W]), writes=[bin_[k]])
            cut = (W * 5) // 8
            P.op("dve", lambda h, k=k, cut=cut: h.tensor_copy(out=tout[k][:, 0:cut], in_=tin[k][:, 0:cut]), reads=[bin_[k]], writes=[bout[k]])
            P.op("act", lambda h, k=k, cut=cut: h.copy(out=tout[k][:, cut:W], in_=tin[k][:, cut:W]), reads=[bin_[k]], writes=[bout2[k]])
            P.dma("sp", lambda h, stp=stp, k=k: h.dma_start(out=dst[:, stp * W:(stp + 1) * W], in_=tout[k][:]), reads=[bout[k], bout2[k]])
    P.barrier()


def phase_d(c):
    nc, P = c.nc, c.P
    with ExitStack() as st:
        sb = lambda n, s, d: st.enter_context(nc.sbuf_tensor(n, s, d))
        ps = lambda n, s, d: st.enter_context(nc.psum_tensor(n, s, d))
        Wo = sb("d_Wo", [128, 8, D], BF16)
        Wq = sb("d_Wq", [128, 8, 2048], BF16)
        SKT = sb("d_SKT", [128, 16, 128], BF16)
        gn2 = sb("d_gn2", [128, D], F32)
        identb = sb("d_identb", [128, 128], BF16)
        identf = sb("d_identf", [128, 128], F32)
        THR = sb("d_THR", [128, 16], F32)
        IOT = sb("d_IOT", [128, 16], F32)
        st_stage = ExitStack()
        stage = [st_stage.enter_context(nc.sbuf_tensor("d_stage%d" % i, [128, 2048], F32)) for i in range(2)]
        bconst = Buf("dconst")
        bst = bufs(2, "dst")
        bWo, bWq = bufs(8, "Wo"), bufs(8, "Wq")
        bSKT = Buf("SKT")
        for (t, src) in ((gn2, c.gn2), (identb, c.identb), (THR, c.thr), (IOT, c.iot), (identf, c.identf)):
            P.dma("sp", lambda h, t=t, src=src: h.dma_start(out=t[:], in_=src), writes=[bconst])
        n = 0
        for kc in range(8):
            k = n % 2; n += 1
            P.dma("sp", lambda h, kc=kc, k=k: h.dma_start(out=stage[k][:, 0:D], in_=c.w_out[kc * 128:(kc + 1) * 128, :]), writes=[bst[k]])
            P.op("dve", lambda h, kc=kc, k=k: h.tensor_copy(out=Wo[:, kc, :], in_=stage[k][:, 0:D]), reads=[bst[k]], writes=[bWo[kc]])
        for kc in range(8):
            k = n % 2; n += 1
            P.dma("sp", lambda h, kc=kc, k=k: h.dma_start(out=stage[k][:], in_=c.w_q[kc * 128:(kc + 1) * 128, :]), writes=[bst[k]])
            P.op("dve", lambda h, kc=kc, k=k: h.tensor_copy(out=Wq[:, kc, :], in_=stage[k][:]), reads=[bst[k]], writes=[bWq[kc]])
        k = n % 2; n += 1
        P.dma("sp", lambda h, k=k: h.dma_start(out=stage[k][:].rearrange("p (a b) -> p a b", b=128), in_=c.skt), writes=[bst[k]])
        P.op("dve", lambda h, k=k: h.tensor_copy(out=SKT[:].rearrange("p a b -> p (a b)"), in_=stage[k][:]), reads=[bst[k]], writes=[bSKT])
        P.barrier()
        st_stage.close()

        cat = sb("d_cat", [128, D], BF16)
        catT = sb("d_catT", [128, 8, 128], BF16)
        xt = sb("d_xt", [128, D], F32)
        junk = sb("d_junk", [128, D], F32)
        junk2 = sb("d_junk2", [128, D], F32)
        ss = sb("d_ss", [128, 4], F32)
        xn2b = sb("d_xn2b", [128, D], BF16)
        xn2T = sb("d_xn2T", [128, 8, 128], BF16)
        qT = sb("d_qT", [128, 16, 128], BF16)
        sc = sb("d_sc", [128, 16, 128], F32)
        m8 = sb("d_m8", [128, 16, 16], F32)
        i8 = sb("d_i8", [128, 16, 16], U32)
        i8f = sb("d_i8f", [128, 16, 16], F32)
        cand = sb("d_cand", [128, 8, 256], F32)
        t8 = sb("d_t8", [128, 8, 16], F32)
        j8 = sb("d_j8", [128, 8, 16], U32)
        jf = sb("d_jf", [128, 128], F32)
        T4 = sb("d_T4", [128, 128, 16], F32)
        af = sb("d_af", [128, 128], F32)
        bf_ = sb("d_bf", [128, 128], F32)
        E1 = sb("d_E1", [128, 128], F32)
        E2 = sb("d_E2", [128, 128], F32)
        g8 = sb("d_g8", [128, 16], F32)
        x1 = [sb("d_x1_%d" % i, [128, D], F32) for i in range(2)]
        xn2 = [sb("d_xn2_%d" % i, [128, D], F32) for i in range(2)]
        eidx = [sb("d_eidx%d" % i, [128, 128], I32) for i in range(2)]
        gts = [sb("d_gts%d" % i, [128, 8, 16], F32) for i in range(2)]
        adot = sb("d_adot", [128, 128], F32)
        ga = sb("d_ga", [128, 128], F32)
        NG = 16
        GS = c.gs
        NGRP = 128 // GS
        uv = [sb("d_uv%d" % i, [128, 2 * D], BF16) for i in range(NG)]
        dg = [sb("d_dg%d" % i, [128, 128], BF16) for i in range(4)]
        yacc = sb("d_y", [128, D], F32)
        ps_t = ps("d_ps_t", [128, D], BF16)
        ps_o = [ps("d_ps_o%d" % i, [128, 512], F32) for i in range(2)]
        ps_y = [ps("d_ps_y%d" % i, [128, 512], F32) for i in range(2)]
        ps_q = [ps("d_ps_q%d" % i, [128, 4, 128], F32) for i in range(2)]
        (bcat, bcatT, bxt, bss, bxn2b, bxn2T, bqT, bsc, bm8, bi8, bi8f, bcand, bt8, bj8,
         bjf, bT4, baf, bbf, bE1, bE2, bg8, by, bps_t) = [Buf(n_) for n_ in (
            "cat", "catT", "xt", "ss", "xn2b", "xn2T", "qT", "sc", "m8", "i8", "i8f", "cand",
            "t8", "j8", "jf", "T4", "af", "bf", "E1", "E2", "g8", "y", "ps_t")]
        bx1, bxn2, beidx, bgts = bufs(2, "x1"), bufs(2, "xn2"), bufs(2, "eidx"), bufs(2, "gts")
        badot, bga = bufs(NGRP, "adot"), bufs(NGRP, "ga")
        buv, bdg = bufs(NG, "uv"), bufs(4, "dg")
        bps_o, bps_q, bps_y = bufs(2, "ps_o"), bufs(2, "ps_q"), bufs(2, "ps_y")
        qi = [0]

        def routing(i):
            par = i % 2
            x1_, xn2_, eidx_, gts_ = x1[par], xn2[par], eidx[par], gts[par]
            bx1_, bxn2_, beidx_, bgts_ = bx1[par], bxn2[par], beidx[par], bgts[par]
            rows = slice(i * 128, (i + 1) * 128)
            P.dma("sp", lambda h: h.dma_start(out=cat[:, 0:512], in_=c.AOUT[rows, :]), reads=[c.bAOUT[i]], writes=[bcat])
            P.dma("sp", lambda h: h.dma_start(out=cat[:, 512:1024], in_=c.HM[rows, :]), reads=[c.bHM[i]], writes=[bcat])
            P.dma("sp", lambda h: h.dma_start(out=xt[:], in_=c.x[rows, :]), writes=[bxt])
            for kc in range(8):
                P.op("pe", lambda h, kc=kc: h.transpose(out=ps_t[:, kc * 128:(kc + 1) * 128], in_=cat[:, kc * 128:(kc + 1) * 128],
                                                        identity=identb[:]), reads=[bcat, bconst], writes=[bps_t])
            P.op("act", lambda h: h.copy(out=catT[:].rearrange("p k t -> p (k t)"), in_=ps_t[:]), reads=[bps_t], writes=[bcatT])
            yield
            for g in range(2):
                for kc in range(8):
                    P.op("pe", lambda h, g=g, kc=kc: h.matmul(ps_o[g][:], lhsT=catT[:, kc, :], rhs=Wo[:, kc, g * 512:(g + 1) * 512],
                                                             start=(kc == 0), stop=(kc == 7)), reads=[bcatT, bWo[kc]], writes=[bps_o[g]])
                P.op("dve", lambda h, g=g: h.tensor_tensor(out=x1_[:, g * 512:(g + 1) * 512], in0=ps_o[g][:], in1=xt[:, g * 512:(g + 1) * 512],
                                                          op=ALU.add), reads=[bps_o[g], bxt], writes=[bx1_])
                yield
            P.op("act", lambda h: h.activation(out=junk[:], in_=x1_[:], func=AF.Square, accum_out=ss[:, 0:1]), reads=[bx1_], writes=[bss])
            P.op("act", lambda h: h.activation(out=ss[:, 1:2], in_=ss[:, 0:1], func=AF.Sqrt, scale=1.0 / D, bias=EPS), reads=[bss], writes=[bss])
            P.op("dve", lambda h: h.reciprocal(out=ss[:, 2:3], in_=ss[:, 1:2]), reads=[bss], writes=[bss])
            P.op("dve", lambda h: h.scalar_tensor_tensor(out=xn2_[:], in0=x1_[:], scalar=ss[:, 2:3], in1=gn2[:], op0=ALU.mult, op1=ALU.mult),
                 reads=[bx1_, bss, bconst], writes=[bxn2_])
            P.op("act", lambda h: h.copy(out=xn2b[:], in_=xn2_[:]), reads=[bxn2_], writes=[bxn2b])
            yield
            for kc in range(8):
                P.op("pe", lambda h, kc=kc: h.transpose(out=ps_t[:, kc * 128:(kc + 1) * 128], in_=xn2b[:, kc * 128:(kc + 1) * 128],
                                                        identity=identb[:]), reads=[bxn2b, bconst], writes=[bps_t])
            P.op("act", lambda h: h.copy(out=xn2T[:].rearrange("p k t -> p (k t)"), in_=ps_t[:]), reads=[bps_t], writes=[bxn2T])
            yield
            for qg in range(4):
                k = qi[0] % 2
                qi[0] += 1
                for cc in range(4):
                    hp = qg * 4 + cc
                    for kc in range(8):
                        P.op("pe", lambda h, k=k, cc=cc, hp=hp, kc=kc: h.matmul(ps_q[k][:, cc, :], lhsT=Wq[:, kc, hp * 128:(hp + 1) * 128],
                                                                              rhs=xn2T[:, kc, :], start=(kc == 0), stop=(kc == 7)),
                             reads=[bWq[kc], bxn2T], writes=[bps_q[k]])
                P.op("act", lambda h, k=k, qg=qg: h.copy(out=qT[:, qg * 4:(qg + 1) * 4, :], in_=ps_q[k][:]), reads=[bps_q[k]], writes=[bqT])
                yield
            for qg in range(4):
                k = qi[0] % 2
                qi[0] += 1
                for cc in range(4):
                    hp = qg * 4 + cc
                    P.op("pe", lambda h, k=k, cc=cc, hp=hp: h.matmul(ps_q[k][:, cc, :], lhsT=qT[:, hp, :], rhs=SKT[:, hp, :],
                                                                   start=True, stop=True), reads=[bqT, bSKT], writes=[bps_q[k]])
                P.op("act", lambda h, k=k, qg=qg: h.copy(out=sc[:, qg * 4:(qg + 1) * 4, :], in_=ps_q[k][:]), reads=[bps_q[k]], writes=[bsc])
            yield
            for g in range(16):
                P.op("dve", lambda h, g=g: h.max(out=m8[:, g, 0:8], in_=sc[:, g, :]), reads=[bsc], writes=[bm8])
                P.op("dve", lambda h, g=g: h.max_index(out=i8[:, g, 0:8], in_max=m8[:, g, 0:8], in_values=sc[:, g, :]),
                     reads=[bsc, bm8], writes=[bi8])
                P.op("dve", lambda h, g=g: h.match_replace(out=sc[:, g, :], in_to_replace=m8[:, g, 0:8], in_values=sc[:, g, :],
                                                           imm_value=-1e30), reads=[bm8], writes=[bsc])
                P.op("dve", lambda h, g=g: h.max(out=m8[:, g, 8:16], in_=sc[:, g, :]), reads=[bsc], writes=[bm8])
                P.op("dve", lambda h, g=g: h.max_index(out=i8[:, g, 8:16], in_max=m8[:, g, 8:16], in_values=sc[:, g, :]),
                     reads=[bsc, bm8], writes=[bi8])
                yield
            m8v = m8[:].rearrange("p (a b) k -> p a b k", b=2)
            P.op("dve", lambda h: h.tensor_tensor(
                out=cand[:].rearrange("p a (x y) -> p a x y", y=16),
                in0=m8v[:, :, 0, :].unsqueeze(3).to_broadcast([128, 8, 16, 16]),
                in1=m8v[:, :, 1, :].unsqueeze(2).to_broadcast([128, 8, 16, 16]), op=ALU.add), reads=[bm8], writes=[bcand])
            yield
            for hd in range(8):
                P.op("dve", lambda h, hd=hd: h.max(out=t8[:, hd, 0:8], in_=cand[:, hd, :]), reads=[bcand], writes=[bt8])
                P.op("dve", lambda h, hd=hd: h.max_index(out=j8[:, hd, 0:8], in_max=t8[:, hd, 0:8], in_values=cand[:, hd, :]),
                     reads=[bcand, bt8], writes=[bj8])
                P.op("dve", lambda h, hd=hd: h.match_replace(out=cand[:, hd, :], in_to_replace=t8[:, hd, 0:8], in_values=cand[:, hd, :],
                                                             imm_value=-1e30), reads=[bt8], writes=[bcand])
                P.op("dve", lambda h, hd=hd: h.max(out=t8[:, hd, 8:16], in_=cand[:, hd, :]), reads=[bcand], writes=[bt8])
                P.op("dve", lambda h, hd=hd: h.max_index(out=j8[:, hd, 8:16], in_max=t8[:, hd, 8:16], in_values=cand[:, hd, :]),
                     reads=[bcand, bt8], writes=[bj8])
                yield
            P.op("dve", lambda h: h.tensor_copy(out=i8f[:], in_=i8[:]), reads=[bi8], writes=[bi8f])
            P.op("dve", lambda h: h.tensor_copy(out=jf[:], in_=j8[:].rearrange("p a k -> p (a k)")), reads=[bj8], writes=[bjf])
            P.op("dve", lambda h: h.tensor_tensor(out=T4[:], in0=jf[:].unsqueeze(2).to_broadcast([128, 128, 16]),
                                                  in1=THR[:].unsqueeze(1).to_broadcast([128, 128, 16]), op=ALU.is_ge),
                 reads=[bjf, bconst], writes=[bT4])
            yield
            P.op("dve", lambda h: h.tensor_reduce(out=af[:], in_=T4[:], axis=AX.X, op=ALU.add), reads=[bT4], writes=[baf])
            P.op("dve", lambda h: h.scalar_tensor_tensor(out=bf_[:], in0=af[:], scalar=-16.0, in1=jf[:], op0=ALU.mult, op1=ALU.add),
                 reads=[baf, bjf], writes=[bbf])
            yield
            i8v = i8f[:].rearrange("p (a b) k -> p a b k", b=2)
            for side, (idxt, Et, bE) in enumerate(((af, E1, bE1), (bf_, E2, bE2))):
                P.op("dve", lambda h, idxt=idxt: h.tensor_tensor(out=T4[:], in0=idxt[:].unsqueeze(2).to_broadcast([128, 128, 16]),
                                                                in1=IOT[:].unsqueeze(1).to_broadcast([128, 128, 16]), op=ALU.is_equal),
                     reads=[baf, bbf, bconst], writes=[bT4])
                yield
                P.op("dve", lambda h, side=side: h.tensor_tensor(
                    out=T4[:].rearrange("p (a k) x -> p a k x", k=16), in0=T4[:].rearrange("p (a k) x -> p a k x", k=16),
                    in1=i8v[:, :, side, :].unsqueeze(2).to_broadcast([128, 8, 16, 16]), op=ALU.mult),
                    reads=[bi8f], writes=[bT4])
                yield
                P.op("dve", lambda h, Et=Et: h.tensor_reduce(out=Et[:], in_=T4[:], axis=AX.X, op=ALU.add), reads=[bT4], writes=[bE])
                yield
            P.op("dve", lambda h: h.scalar_tensor_tensor(out=E1[:], in0=E1[:], scalar=128.0, in1=E2[:], op0=ALU.mult, op1=ALU.add),
                 reads=[bE2], writes=[bE1])
            P.op("dve", lambda h: h.tensor_copy(out=eidx_[:], in_=E1[:]), reads=[bE1], writes=[beidx_])
            P.op("dve", lambda h: h.tensor_tensor(out=gts_[:], in0=t8[:], in1=t8[:, :, 0:1].to_broadcast([128, 8, 16]), op=ALU.subtract),
                 reads=[bt8], writes=[bgts_])
            P.op("act", lambda h: h.activation(out=gts_[:], in_=gts_[:], func=AF.Exp), reads=[], writes=[bgts_])
            P.op("dve", lambda h: h.tensor_reduce(out=g8[:, 0:8], in_=gts_[:], axis=AX.X, op=ALU.add), reads=[bgts_], writes=[bg8])
            P.op("dve", lambda h: h.reciprocal(out=g8[:, 8:16], in_=g8[:, 0:8]), reads=[], writes=[bg8])
            P.op("dve", lambda h: h.tensor_tensor(out=gts_[:], in0=gts_[:], in1=g8[:, 8:16].unsqueeze(2).to_broadcast([128, 8, 16]),
                                                  op=ALU.mult), reads=[bg8], writes=[bgts_])
            if "EIDX" in c.dbg:
                P.dma("sp", lambda h: h.dma_start(out=c.EIDX[rows, :], in_=eidx_[:]), reads=[beidx_])
                P.dma("sp", lambda h: h.dma_start(out=c.GTS[rows, :], in_=gts_[:].rearrange("p a k -> p (a k)")), reads=[bgts_])
                P.dma("sp", lambda h: h.dma_start(out=c.X1[rows, :], in_=x1_[:]), reads=[bx1_])
            yield

        def experts(i, nxt):
            par = i % 2
            x1_, xn2_, eidx_, gts_ = x1[par], xn2[par], eidx[par], gts[par]
            bx1_, bxn2_, beidx_, bgts_ = bx1[par], bxn2[par], beidx[par], bgts[par]
            rows = slice(i * 128, (i + 1) * 128)
            gts_f = gts_[:].rearrange("p a k -> p (a k)")

            def stage_a(grp):
                for sl in range(grp * GS, (grp + 1) * GS):
                    k = sl % NG
                    P.dma("pool", lambda h, sl=sl, k=k: h.indirect_dma_start(
                        out=uv[k][:], out_offset=None, in_=c.UVB,
                        in_offset=bass.IndirectOffsetOnAxis(ap=eidx_[:, sl:sl + 1], axis=0)), reads=[beidx_], writes=[buv[k]])
                    P.op("dve", lambda h, sl=sl, k=k: h.scalar_tensor_tensor(out=junk2[:], in0=uv[k][:, 0:D], scalar=1.0, in1=xn2_[:],
                                                                            op0=ALU.mult, op1=ALU.mult, accum_out=adot[:, sl:sl + 1]),
                         reads=[buv[k], bxn2_], writes=[badot[grp]])

            def stage_b(grp):
                g0, g1 = grp * GS, (grp + 1) * GS
                P.op("act", lambda h: h.activation(out=ga[:, g0:g1], in_=adot[:, g0:g1], func=AF.Gelu),
                     reads=[badot[grp]], writes=[bga[grp]])
                for sl in range(g0, g1):
                    k = sl % NG
                    kd = sl % 4
                    P.op("act", lambda h, sl=sl: h.activation(out=ga[:, sl:sl + 1], in_=ga[:, sl:sl + 1], func=AF.Copy,
                                                              scale=gts_f[:, sl:sl + 1]), reads=[bgts_], writes=[bga[grp]])
                    P.op("act", lambda h, sl=sl, kd=kd: h.activation(out=dg[kd][:], in_=identf[:], func=AF.Copy, scale=ga[:, sl:sl + 1]),
                         reads=[bga[grp], bconst], writes=[bdg[kd]])
                    for g in range(2):
                        P.op("pe", lambda h, sl=sl, k=k, kd=kd, g=g: h.matmul(ps_y[g][:], lhsT=dg[kd][:],
                                                                             rhs=uv[k][:, D + g * 512:D + (g + 1) * 512],
                                                                             start=(sl == 0), stop=(sl == 127)),
                             reads=[bdg[kd], buv[k]], writes=[bps_y[g]])

            def adv(nsteps):
                if nxt is None:
                    return
                for _ in range(nsteps):
                    try:
                        next(nxt)
                    except StopIteration:
                        return

            stage_a(0)
            for grp in range(NGRP):
                if grp + 1 < NGRP:
                    stage_a(grp + 1)
                stage_b(grp)
                adv(c.advn)
            for g in range(2):
                P.op("dve", lambda h, g=g: h.tensor_tensor(out=yacc[:, g * 512:(g + 1) * 512], in0=ps_y[g][:], in1=x1_[:, g * 512:(g + 1) * 512],
                                                          op=ALU.add), reads=[bps_y[g], bx1_], writes=[by])
            P.dma("sp", lambda h: h.dma_start(out=c.out[rows, :], in_=yacc[:]), reads=[by], writes=[c.bOUT[i]])
            adv(1000)

        r0 = routing(0)
        for _ in r0:
            pass
        for i in range(NT):
            nxt = routing(i + 1) if i + 1 < NT else None
            if "noexp" in c.dbg or i >= c.nexp:
                par = i % 2
                P.dma("sp", lambda h, i=i, par=par: h.dma_start(out=c.out[i * 128:(i + 1) * 128, :], in_=x1[par][:]),
                      reads=[bx1[par]], writes=[c.bOUT[i]])
                if nxt is not None:
                    for _ in nxt:
                        pass
            else:
                if "noil" in c.dbg and nxt is not None:
                    for _ in nxt:
                        pass
                experts(i, nxt)
    P.barrier()


def build(dbg=(), phases="abcd"):
    nc = bass.Bass("TRN2", target_bir_lowering=False)
    c = Ctx()
    c.nc = nc
    ext_in = lambda n, s, d: nc.dram_tensor(n, s, d, kind="ExternalInput").ap()

    def scratch(n, s, d):
        kind = "ExternalOutput" if n in dbg else "Internal"
        return nc.dram_tensor(n, s, d, kind=kind).ap()

    c.x = ext_in("x", [S, D], F32)
    c.w_in = ext_in("w_in", [D, INW], F32)
    c.norm1_w = ext_in("norm1_w", [128, 8], F32)
    c.identb = ext_in("identb", [128, 128], BF16)
    c.gq = ext_in("gq", [128, 64], F32)
    c.gk = ext_in("gk", [128, 64], F32)
    c.rpbg = ext_in("rpbg", [128, 8, 896], F32)
    c.mask_i = ext_in("mask_i", [128, 896], F32)
    c.mask_a = ext_in("mask_a", [128, 896], F32)
    c.gao = ext_in("gao", [128, 512], F32)
    c.gate_b = ext_in("gate_b", [4, 4], F32)
    c.identf = ext_in("identf", [128, 128], F32)
    c.conv_w = ext_in("conv_w", [128, 8, 5], F32)
    c.conv_b = ext_in("conv_b", [128, 8], F32)
    c.sel = ext_in("sel", [4, 4, 128], F32)
    c.msk = ext_in("msk", [128, 2, 128], F32)
    c.gmn = ext_in("gmn", [128, 512], F32)
    c.w_out = ext_in("w_out", [D, D], F32)
    c.w_q = ext_in("w_q", [D, 2048], F32)
    c.skt = ext_in("skt", [128, 16, 128], F32)
    c.gn2 = ext_in("gn2", [128, D], F32)
    c.thr = ext_in("thr", [128, 16], F32)
    c.iot = ext_in("iot", [128, 16], F32)
    c.peer_uv = ext_in("peer_uv", [16384, 2 * D], F32)
    c.UVB = scratch("UVB", [16384, 2 * D], BF16)
    c.out = nc.dram_tensor("out", [S, D], F32, kind="ExternalOutput").ap()
    c.bOUT = bufs(NT, "OUT")
    c.dbg = dbg
    c.nexp = NT
    c.advn = 1
    c.gs = 4
    for d_ in dbg:
        if d_.startswith("gs"):
            c.gs = int(d_[2:])
    for d_ in dbg:
        if d_.startswith("adv"):
            c.advn = int(d_[3:])
    for d_ in dbg:
        if d_.startswith("exp"):
            c.nexp = int(d_[3:])
    if "EIDX" in dbg:
        c.EIDX = scratch("EIDX", [S, 128], I32)
        c.GTS = scratch("GTS", [S, 128], F32)
        c.X1 = scratch("X1", [S, D], F32)

    c.QT = scratch("QT", [4, 128, S], BF16)
    c.KT = scratch("KT", [4, 128, S], BF16)
    c.V = scratch("V", [S, 512], BF16)
    c.MV = scratch("MV", [S, 512], BF16)
    c.SIGO = scratch("SIGO", [S, 512], BF16)
    c.MQKT = scratch("MQKT", [1024, S], F32)
    c.GT = scratch("GT", [4, 4, S], F32)
    c.AOUT = scratch("AOUT", [S, 512], BF16)
    c.BCRD = scratch("BCRD", [2, 4, NT, 257], F32)
    c.bBCRD = bufs(2, "BCRD")
    c.HF = scratch("HF", [S, 512], F32)
    c.bHF = bufs(NT, "HF")
    c.HM = scratch("HM", [S, 512], BF16)
    c.bHM = bufs(NT, "HM")
    c.bAOUT = bufs(NT, "AOUT")
    c.bQKT = [bufs(NT, "QT"), bufs(NT, "KT")]
    c.bV, c.bMV, c.bSIGO, c.bMQKT, c.bGT = (bufs(NT, n) for n in ("V", "MV", "SIGO", "MQKT", "GT"))

    with ExitStack() as st:
        c.P = Prog(nc, st)
        if "a" in phases:
            phase_a(c)
        if "b" in phases:
            phase_b(c)
        if "c" in phases:
            phase_c(c)
        if "d" in phases:
            phase_t(c)
            phase_d(c)
        c.P.emit()
    return nc


def host_inputs(inputs, b):
    f32 = np.float32
    m = {}
    m["x"] = np.ascontiguousarray(inputs["x"][b], dtype=f32)
    m["w_in"] = np.ascontiguousarray(inputs["w_in"][0], dtype=f32)
    m["norm1_w"] = np.ascontiguousarray(inputs["norm1_w"][0].reshape(8, 128).T, dtype=f32)
    m["identb"] = np.eye(128, dtype=f32).astype(ml_dtypes.bfloat16)
    m["gq"] = np.ascontiguousarray(np.broadcast_to(inputs["q_norm_w"][0][None, :], (128, 64)), dtype=f32)
    m["gk"] = np.ascontiguousarray(np.broadcast_to(inputs["k_norm_w"][0][None, :], (128, 64)), dtype=f32)
    p = np.arange(128); kr = p // 64; kc = p % 64
    col = np.arange(128); rq = col // 64; cc = col % 64
    dt = np.arange(-3, 4)
    drow = 2 * dt[None, :, None] + kr[:, None, None] - rq[None, None, :]
    dcol = kc[:, None, None] - cc[None, None, :] + 0 * dt[None, :, None]
    rpb = inputs["attn_rpb"][0]
    g = rpb[:, np.clip(drow + 7, 0, 14), np.clip(dcol + 15, 0, 30)]
    m["rpbg"] = np.ascontiguousarray(g.transpose(1, 0, 2, 3).reshape(128, 8, 896), dtype=f32)
    cs = np.clip(cc - 8, 0, 48)
    colvalid = (kc[:, None, None] >= cs[None, None, :]) & (kc[:, None, None] < cs[None, None, :] + 16)
    colvalid = colvalid & (dt[None, :, None] > -100)
    m["mask_a"] = np.where(colvalid & (np.abs(drow) <= 7), 0.0, NEG).astype(f32).reshape(128, 896)
    m["mask_i"] = np.where(colvalid & (drow >= -4) & (drow <= 3), 0.0, NEG).astype(f32).reshape(128, 896)
    m["gate_b"] = np.ascontiguousarray(inputs["mlstm_gate_b"][0].T, dtype=f32)
    m["identf"] = np.eye(128, dtype=f32)
    m["conv_w"] = np.ascontiguousarray(inputs["mlstm_conv_w"][0].reshape(5, 8, 128).transpose(2, 1, 0), dtype=f32)
    m["conv_b"] = np.ascontiguousarray(inputs["mlstm_conv_b"][0].reshape(8, 128).T, dtype=f32)
    sel = np.zeros((4, 4, 128), f32)
    for hh in range(4):
        sel[hh, hh, :] = 1.0
    m["sel"] = sel
    ii = np.arange(128)
    msk = np.zeros((128, 2, 128), f32)
    msk[:, 0, :] = np.where(ii[:, None] <= ii[None, :], 0.0, NEG)
    msk[:, 1, :] = np.where(ii[:, None] >= ii[None, :], 0.0, NEG)
    m["msk"] = msk
    m["gmn"] = np.ascontiguousarray(np.broadcast_to(inputs["mlstm_norm_w"][0][None, :], (128, 512)), dtype=f32)
    m["w_out"] = np.ascontiguousarray(inputs["w_out"][0], dtype=f32)
    m["w_q"] = np.ascontiguousarray(inputs["peer_w_q"][0], dtype=f32)
    m["skt"] = np.ascontiguousarray(inputs["peer_sub_keys"][0].reshape(16, 128, 128).transpose(2, 0, 1), dtype=f32)
    m["gn2"] = np.ascontiguousarray(np.broadcast_to(inputs["norm2_w"][0][None, :], (128, D)), dtype=f32)
    thr = (np.arange(16, dtype=f32) + 1.0) * 16.0
    thr[15] = 1e9
    m["thr"] = np.ascontiguousarray(np.broadcast_to(thr[None, :], (128, 16)), dtype=f32)
    m["iot"] = np.ascontiguousarray(np.broadcast_to(np.arange(16, dtype=f32)[None, :], (128, 16)), dtype=f32)
    m["peer_uv"] = np.ascontiguousarray(np.concatenate([inputs["peer_u"][0], inputs["peer_v"][0]], axis=1), dtype=f32)
    m["gao"] = np.ascontiguousarray(np.broadcast_to(inputs["attn_out_norm_w"][0][None, :], (128, 512)), dtype=f32)
    return m


def kernel(**inputs):
    nc = build()
    in_maps = [host_inputs(inputs, b) for b in range(8)]
    res = run_bass_kernel_spmd(nc, in_maps, core_ids=list(range(8)))
    return np.stack([r["out"] for r in res.results], axis=0).astype(np.float32)
```

```python
import numpy as np
import ml_dtypes
import concourse.bass as bass
import concourse.mybir as mybir
from concourse.bass_utils import run_bass_kernel_spmd
from contextlib import ExitStack

F32 = mybir.dt.float32
BF16 = mybir.dt.bfloat16
I32 = mybir.dt.int32
U32 = mybir.dt.uint32
ALU = mybir.AluOpType
AF = mybir.ActivationFunctionType
AX = mybir.AxisListType

S = 4096
D = 1024
NT = 32
INW = 3600
EPS = 1e-6
NEG = -30000.0


class Buf:
    __slots__ = ("name", "w", "r")

    def __init__(self, name=""):
        self.name = name
        self.w = {}
        self.r = {}


class Prog:
    ENG = ("pe", "act", "dve", "pool", "sp")
    NRINGS = {"sp": 12, "act": 6, "pool": 48}

    def __init__(self, nc, stack):
        self.nc = nc
        self.sem = {e: stack.enter_context(nc.semaphore("s_" + e)) for e in self.ENG}
        self.ops = {e: [] for e in self.ENG}
        self.seen = {e: {} for e in self.ENG}
        self.ring = {}
        self.ring_i = {}
        self.ring_tok = {}
        for q in ("sp", "act", "pool"):
            self.ring[q] = [stack.enter_context(nc.semaphore("r_%s%d" % (q, i)))
                            for i in range(self.NRINGS[q])]
            self.ring_i[q] = 0
            self.ring_tok[q] = [None] * self.NRINGS[q]

    @staticmethod
    def _key(tok):
        return ("c", tok[1]) if tok[0] == "c" else ("d", id(tok[1]))

    @staticmethod
    def _val(tok):
        return tok[2]

    def _need(self, eng, tok, waits, same_ok=False):
        if tok[0] == "c" and tok[1] == eng and (eng == "pe" or same_ok):
            return
        k = self._key(tok)
        if self.seen[eng].get(k, -1) >= tok[2]:
            return
        self.seen[eng][k] = tok[2]
        waits.append(tok)

    def _deps(self, eng, reads, writes):
        waits = []
        for b in reads:
            for t in b.w.values():
                self._need(eng, t, waits)
        for b in writes:
            for t in b.w.values():
                self._need(eng, t, waits)
            for t in b.r.values():
                self._need(eng, t, waits)
        return waits

    def _commit(self, tok, reads, writes):
        k = self._key(tok)
        for b in reads:
            b.r[k] = tok
        for b in writes:
            b.w = {k: tok}
            b.r = {}

    def op(self, eng, fn, reads=(), writes=()):
        waits = self._deps(eng, reads, writes)
        tok = ("c", eng, len(self.ops[eng]))
        self.ops[eng].append([waits, fn, None])
        self._commit(tok, reads, writes)
        return tok

    def dma(self, q, fn, reads=(), writes=(), final=False):
        waits = self._deps(q, reads, writes)
        i = self.ring_i[q]
        nr = self.NRINGS[q]
        slot = i % nr
        prev = self.ring_tok[q][slot]
        if prev is not None:
            self._need(q, prev, waits)
        sem = self.ring[q][slot]
        val = 16 * (i // nr + 1)
        self.ring_i[q] = i + 1
        tok = ("d", sem, val, q)
        self.ring_tok[q][slot] = tok
        self.ops[q].append([waits, fn, (sem, 16)])
        self._commit(tok, reads, writes)
        return tok

    def _all_tokens(self):
        toks = []
        for e in ("pe", "act", "dve", "pool"):
            for idx in range(len(self.ops[e]) - 1, -1, -1):
                ent = self.ops[e][idx]
                if ent[1] is not None and ent[2] is None:
                    toks.append(("c", e, idx))
                    break
        for q in ("act", "pool", "sp"):
            for t in self.ring_tok[q]:
                if t is not None:
                    toks.append(t)
        return toks

    def barrier(self):
        toks = self._all_tokens()
        for e in self.ENG:
            waits = []
            for t in toks:
                k = self._key(t)
                if self.seen[e].get(k, -1) >= t[2]:
                    continue
                self.seen[e][k] = t[2]
                waits.append(t)
            if waits:
                self.ops[e].append([waits, None, None])

    def emit(self):
        nc = self.nc
        final_waits = []
        for t in self._all_tokens():
            self._need("sp", t, final_waits)
        signal = {e: set() for e in self.ENG}
        allw = [final_waits]
        for e in self.ENG:
            for ent in self.ops[e]:
                allw.append(ent[0])
        for ws in allw:
            for t in ws:
                if t[0] == "c":
                    signal[t[1]].add(t[2])
        semval = {e: {} for e in self.ENG}
        for e in self.ENG:
            n = 0
            for idx in sorted(signal[e]):
                n += 1
                semval[e][idx] = n

        def resolve(t):
            if t[0] == "c":
                return self.sem[t[1]], semval[t[1]][t[2]]
            return t[1], t[2]

        def run(e, handle, extra=None):
            for idx, (waits, fn, dinc) in enumerate(self.ops[e]):
                for t in waits:
                    sem, val = resolve(t)
                    handle.wait_ge(sem, val)
                if fn is not None:
                    ins = fn(handle)
                    if dinc is not None:
                        ins.then_inc(dinc[0], dinc[1])
                    elif idx in signal[e]:
                        ins.then_inc(self.sem[e], 1)
            if extra:
                for t in extra:
                    sem, val = resolve(t)
                    handle.wait_ge(sem, val)

        with nc.Block() as block:
            @block.tensor
            def _(h):
                run("pe", h)

            @block.scalar
            def _(h):
                run("act", h)

            @block.vector
            def _(h):
                run("dve", h)

            @block.gpsimd
            def _(h):
                run("pool", h)

            @block.sync
            def _(h):
                run("sp", h, final_waits)


class Ctx:
    pass


def bufs(n, name=""):
    return [Buf("%s%d" % (name, i)) for i in range(n)]


def phase_a(c):
    nc, P = c.nc, c.P
    with ExitStack() as st:
        sb = lambda n, s, d: st.enter_context(nc.sbuf_tensor(n, s, d))
        ps = lambda n, s, d: st.enter_context(nc.psum_tensor(n, s, d))
        Wbf = sb("a_Wbf", [128, 8, INW], BF16)
        stage = [sb("a_stage%d" % i, [128, INW], F32) for i in range(2)]
        w1 = sb("a_w1", [128, 8], F32)
        identb = sb("a_identb", [128, 128], BF16)
        gq = sb("a_gq", [128, 64], F32)
        gk = sb("a_gk", [128, 64], F32)
        xt = [sb("a_xt%d" % i, [128, D], F32) for i in range(2)]
        junk = sb("a_junk", [128, D], F32)
        ss = sb("a_ss", [128, 4], F32)
        xn = sb("a_xn", [128, D], BF16)
        xnT = sb("a_xnT", [128, 8, 128], BF16)
        sq = sb("a_sq", [128, 512], F32)
        tmp = sb("a_tmp", [128, 512], F32)
        s8 = sb("a_s8", [128, 24], F32)
        qn = sb("a_qn", [128, 512], BF16)
        qTs = sb("a_qTs", [128, 4, 128], BF16)
        ob = [sb("a_ob%d" % i, [128, 512], BF16) for i in range(2)]
        fm = [sb("a_fm%d" % i, [128, 4, 128], F32) for i in range(2)]
        gsb = sb("a_gsb", [4, 4, 128], F32)
        ps_t = ps("a_ps_t", [128, D], BF16)
        ps_g = [ps("a_ps_g%d" % i, [128, 512], F32) for i in range(2)]
        ps_q = ps("a_ps_q", [128, 4, 128], BF16)
        ps_f = [ps("a_ps_f%d" % i, [128, 4, 128], F32) for i in range(2)]
        ps_gt = ps("a_ps_gt", [4, 4, 128], F32)

        bW = bufs(8, "W")
        bst = bufs(2, "st")
        bc = Buf("consts")
        bxt = bufs(2, "xt")
        bjunk, bss, bxn, bxnT, bsq, btmp, bs8, bqn, bqTs = [Buf(n) for n in
            ("junk", "ss", "xn", "xnT", "sq", "tmp", "s8", "qn", "qTs")]
        bob = bufs(2, "ob")
        bfm = bufs(2, "fm")
        bgsb = Buf("gsb")
        bps_t, bps_q, bps_gt = Buf("ps_t"), Buf("ps_q"), Buf("ps_gt")
        bps_g = bufs(2, "ps_g")
        bps_f = bufs(2, "ps_f")

        P.dma("sp", lambda h: h.dma_start(out=w1[:], in_=c.norm1_w), writes=[bc])
        P.dma("sp", lambda h: h.dma_start(out=identb[:], in_=c.identb), writes=[bc])
        P.dma("sp", lambda h: h.dma_start(out=gq[:], in_=c.gq), writes=[bc])
        P.dma("sp", lambda h: h.dma_start(out=gk[:], in_=c.gk), writes=[bc])
        for kc in range(8):
            s_ = stage[kc % 2]
            P.dma("sp", lambda h, kc=kc, s_=s_: h.dma_start(out=s_[:], in_=c.w_in[kc * 128:(kc + 1) * 128, :]),
                  writes=[bst[kc % 2]])
            P.op("dve" if kc % 2 == 0 else "pool",
                 lambda h, kc=kc, s_=s_: h.tensor_scalar(out=Wbf[:, kc, :], in0=s_[:], scalar1=w1[:, kc:kc + 1],
                                                         scalar2=None, op0=ALU.mult),
                 reads=[bst[kc % 2], bc], writes=[bW[kc]])

        gi = [0]

        def mm_group_tok(cols, sub=None):
            k = gi[0] % 2
            gi[0] += 1
            c0, c1 = cols
            for kc in range(8):
                P.op("pe", lambda h, kc=kc, k=k: h.matmul(ps_g[k][:, 0:c1 - c0], lhsT=xnT[:, kc, :],
                                                       rhs=Wbf[:, kc, c0:c1], start=(kc == 0), stop=(kc == 7)),
                     reads=[bxnT, bW[kc]], writes=[bps_g[k]])
            return k

        oi = [0]
        fi = [0]
        xn2_ = [xn, sb("a_xn_b", [128, D], BF16)]
        xnT2 = [xnT, sb("a_xnT_b", [128, 8, 128], BF16)]
        ps_t2 = [ps_t, ps("a_ps_t_b", [128, D], BF16)]
        bxn2, bxnT2, bps_t2 = [bxn, Buf("xn_b")], [bxnT, Buf("xnT_b")], [bps_t, Buf("ps_t_b")]
        sq2 = [sq, sb("a_sq_b", [128, 512], F32)]
        tmp2 = [tmp, sb("a_tmp_b", [128, 512], F32)]
        s82 = [s8, sb("a_s8_b", [128, 24], F32)]
        qn2 = [qn, sb("a_qn_b", [128, 512], BF16)]
        qTs2 = [qTs, sb("a_qTs_b", [128, 4, 128], BF16)]
        bsq2, btmp2, bs82, bqn2, bqTs2 = [bsq, Buf("sq_b")], [btmp, Buf("tmp_b")], [bs8, Buf("s8_b")], [bqn, Buf("qn_b")], [bqTs, Buf("qTs_b")]

        def front(i):
            p = i % 2
            x_, bx_ = xt[p], bxt[p]
            P.dma("sp", lambda h: h.dma_start(out=x_[:], in_=c.x[i * 128:(i + 1) * 128, :]), writes=[bx_])
            P.op("act", lambda h: h.activation(out=junk[:], in_=x_[:], func=AF.Square, accum_out=ss[:, 0:1]),
                 reads=[bx_], writes=[bss])
            P.op("act", lambda h: h.activation(out=ss[:, 1:2], in_=ss[:, 0:1], func=AF.Sqrt, scale=1.0 / D, bias=EPS),
                 reads=[bss], writes=[bss])
            P.op("dve", lambda h: h.reciprocal(out=ss[:, 2:3], in_=ss[:, 1:2]), reads=[bss], writes=[bss])
            P.op("dve", lambda h: h.tensor_scalar(out=xn2_[p][:], in0=x_[:], scalar1=ss[:, 2:3], scalar2=None,
                                                  op0=ALU.mult), reads=[bx_, bss], writes=[bxn2[p]])
            for kc in range(8):
                P.op("pe", lambda h, kc=kc: h.transpose(out=ps_t2[p][:, kc * 128:(kc + 1) * 128],
                                                        in_=xn2_[p][:, kc * 128:(kc + 1) * 128], identity=identb[:]),
                     reads=[bxn2[p], bc], writes=[bps_t2[p]])
            P.op("act", lambda h: h.copy(out=xnT2[p][:].rearrange("p k t -> p (k t)"), in_=ps_t2[p][:]),
                 reads=[bps_t2[p]], writes=[bxnT2[p]])

        def body(i):
            p = i % 2
            xT, bxT = xnT2[p], bxnT2[p]

            def mm_tok(c0):
                k = gi[0] % 2
                gi[0] += 1
                for kc in range(8):
                    P.op("pe", lambda h, kc=kc, k=k: h.matmul(ps_g[k][:], lhsT=xT[:, kc, :], rhs=Wbf[:, kc, c0:c0 + 512],
                                                           start=(kc == 0), stop=(kc == 7)),
                         reads=[bxT, bW[kc]], writes=[bps_g[k]])
                return k

            def qk_chain(w, k, gain):
                P.op("act", lambda h: h.activation(out=sq2[w][:], in_=ps_g[k][:], func=AF.Square),
                     reads=[bps_g[k]], writes=[bsq2[w]])
                P.op("dve", lambda h: h.tensor_reduce(out=s82[w][:, 0:8], in_=sq2[w][:].rearrange("p (a b) -> p a b", b=64),
                                                      axis=AX.X, op=ALU.add), reads=[bsq2[w]], writes=[bs82[w]])
                P.op("act", lambda h: h.activation(out=s82[w][:, 8:16], in_=s82[w][:, 0:8], func=AF.Sqrt, scale=1.0 / 64,
                                                   bias=EPS), reads=[], writes=[bs82[w]])
                P.op("dve", lambda h: h.reciprocal(out=s82[w][:, 16:24], in_=s82[w][:, 8:16]), reads=[], writes=[bs82[w]])
                P.op("dve", lambda h: h.tensor_tensor(
                    out=tmp2[w][:].rearrange("p (a b) -> p a b", b=64),
                    in0=ps_g[k][:].rearrange("p (a b) -> p a b", b=64),
                    in1=s82[w][:, 16:24].unsqueeze(2).to_broadcast([128, 8, 64]), op=ALU.mult),
                    reads=[bps_g[k], bs82[w]], writes=[btmp2[w]])
                P.op("pool", lambda h: h.tensor_tensor(
                    out=qn2[w][:].rearrange("p (a b) -> p a b", b=64),
                    in0=tmp2[w][:].rearrange("p (a b) -> p a b", b=64),
                    in1=gain[:].unsqueeze(1).to_broadcast([128, 8, 64]), op=ALU.mult),
                    reads=[btmp2[w], bc], writes=[bqn2[w]])

            def qk_tr(w, dst):
                for hp in range(4):
                    P.op("pe", lambda h, hp=hp: h.transpose(out=ps_q[:, hp, :], in_=qn2[w][:, hp * 128:(hp + 1) * 128],
                                                            identity=identb[:]), reads=[bqn2[w], bc], writes=[bps_q])
                P.op("act", lambda h: h.copy(out=qTs2[w][:], in_=ps_q[:]), reads=[bps_q], writes=[bqTs2[w]])
                P.dma("sp", lambda h: h.dma_start(
                    out=dst[:, :, i * 128:(i + 1) * 128].rearrange("a p t -> p a t"), in_=qTs2[w][:]),
                    reads=[bqTs2[w]], writes=[c.bQKT[w][i]])

            def tok_out(c0, dst, bdst, fn):
                k = mm_tok(c0)
                o = oi[0] % 2
                oi[0] += 1
                P.op("act", lambda h: h.activation(out=ob[o][:], in_=ps_g[k][:], func=fn),
                     reads=[bps_g[k]], writes=[bob[o]])
                P.dma("sp", lambda h: h.dma_start(out=dst[i * 128:(i + 1) * 128, :], in_=ob[o][:]),
                      reads=[bob[o]], writes=[bdst[i]])

            def fm_half(half):
                f = fi[0] % 2
                fi[0] += 1
                for cc in range(4):
                    ch = half * 4 + cc
                    col = 1536 + ch * 128
                    for kc in range(8):
                        P.op("pe", lambda h, kc=kc, cc=cc, col=col: h.matmul(
                            ps_f[f][:, cc, :], lhsT=Wbf[:, kc, col:col + 128], rhs=xT[:, kc, :],
                            start=(kc == 0), stop=(kc == 7)), reads=[bxT, bW[kc]], writes=[bps_f[f]])
                P.op("dve", lambda h: h.tensor_copy(out=fm[f][:], in_=ps_f[f][:]), reads=[bps_f[f]], writes=[bfm[f]])
                P.dma("sp", lambda h: h.dma_start(
                    out=c.MQKT[half * 512:(half + 1) * 512, i * 128:(i + 1) * 128].rearrange("(a p) t -> p a t", p=128),
                    in_=fm[f][:]), reads=[bfm[f]], writes=[c.bMQKT[i]])

            kq = mm_tok(0)
            qk_chain(0, kq, gq)
            kk = mm_tok(512)
            qk_chain(1, kk, gk)
            tok_out(1024, c.V, c.bV, AF.Copy)
            qk_tr(0, c.QT)
            tok_out(2560, c.MV, c.bMV, AF.Copy)
            qk_tr(1, c.KT)
            tok_out(3072, c.SIGO, c.bSIGO, AF.Sigmoid)
            fm_half(0)
            fm_half(1)
            for g in range(4):
                col = 3584 + 4 * g
                for kc in range(8):
                    P.op("pe", lambda h, kc=kc, g=g, col=col: h.matmul(
                        ps_gt[:, g, :], lhsT=Wbf[:, kc, col:col + 4], rhs=xT[:, kc, :],
                        start=(kc == 0), stop=(kc == 7)), reads=[bxT, bW[kc]], writes=[bps_gt])
            P.op("dve", lambda h: h.tensor_copy(out=gsb[:], in_=ps_gt[:]), reads=[bps_gt], writes=[bgsb])
            P.dma("sp", lambda h: h.dma_start(out=c.GT[:, :, i * 128:(i + 1) * 128].rearrange("g a t -> a g t"),
                                              in_=gsb[:]), reads=[bgsb], writes=[c.bGT[i]])

        front(0)
        for i in range(NT):
            if i + 1 < NT:
                front(i + 1)
            body(i)
    P.barrier()


def phase_b(c):
    nc, P = c.nc, c.P
    with ExitStack() as st:
        sb = lambda n, s, d: st.enter_context(nc.sbuf_tensor(n, s, d))
        ps = lambda n, s, d: st.enter_context(nc.psum_tensor(n, s, d))
        QT = sb("b_QT", [128, 4, S], BF16)
        KT = sb("b_KT", [128, 4, S], BF16)
        V = sb("b_V", [128, NT, 8, 65], BF16)
        TBI = sb("b_TBI", [128, 8, 896], F32)
        TBA = sb("b_TBA", [128, 8, 896], F32)
        MI = sb("b_MI", [128, 896], F32)
        MA = sb("b_MA", [128, 896], F32)
        gao = sb("b_gao", [128, 512], F32)
        sT = [sb("b_sT%d" % i, [128, 640], F32) for i in range(3)]
        pT = [sb("b_pT%d" % i, [128, 640], BF16) for i in range(3)]
        ao = sb("b_ao", [128, 512], F32)
        junk = sb("b_junk", [128, 512], F32)
        rc = [sb("b_rc%d" % i, [128, 1], F32) for i in range(2)]
        ss = sb("b_ss", [128, 4], F32)
        aob = [sb("b_aob%d" % i, [128, 512], BF16) for i in range(2)]
        ps_s = [ps("b_ps_s%d" % i, [128, 1024], F32) for i in range(3)]
        ps_o = [ps("b_ps_o%d" % i, [128, 128], F32) for i in range(2)]

        bQT, bKT = bufs(4, "bQT"), bufs(4, "bKT")
        bVt = bufs(NT, "bV")
        bones, btb, bm, bgao = Buf("ones"), Buf("tb"), Buf("m"), Buf("gao")
        bsT, bpT, brc, baob = bufs(3, "sT"), bufs(3, "pT"), bufs(2, "rc"), bufs(2, "aob")
        bao, bjunk, bss = Buf("ao"), Buf("junk"), Buf("ss")
        bps_s, bps_o = bufs(3, "ps_s"), bufs(2, "ps_o")

        P.dma("sp", lambda h: h.dma_start(out=TBI[:], in_=c.rpbg), writes=[btb])
        P.dma("sp", lambda h: h.dma_start(out=MI[:], in_=c.mask_i), writes=[bm])
        P.dma("sp", lambda h: h.dma_start(out=MA[:], in_=c.mask_a), writes=[bm])
        P.dma("sp", lambda h: h.dma_start(out=gao[:], in_=c.gao), writes=[bgao])
        for hp in range(4):
            P.dma("sp", lambda h, hp=hp: h.dma_start(out=QT[:, hp, :], in_=c.QT[hp]),
                  reads=c.bQKT[0], writes=[bQT[hp]])
            P.dma("act", lambda h, hp=hp: h.dma_start(out=KT[:, hp, :], in_=c.KT[hp]),
                  reads=c.bQKT[1], writes=[bKT[hp]])
        P.op("pool", lambda h: h.memset(V[:, :, :, 64:65], 1.0), writes=[bones])
        for i in range(NT):
            P.dma("sp" if i % 2 == 0 else "act", lambda h, i=i: h.dma_start(
                out=V[:, i, :, 0:64], in_=c.V[i * 128:(i + 1) * 128, :].rearrange("p (a b) -> p a b", b=64)),
                reads=[c.bV[i]], writes=[bVt[i]])
        for hd in range(8):
            P.op("dve", lambda h, hd=hd: h.tensor_tensor(out=TBA[:, hd, :], in0=TBI[:, hd, :], in1=MA[:], op=ALU.add),
                 reads=[btb, bm], writes=[btb])
        for hd in range(8):
            P.op("dve", lambda h, hd=hd: h.tensor_tensor(out=TBI[:, hd, :], in0=TBI[:, hd, :], in1=MI[:], op=ALU.add),
                 reads=[btb, bm], writes=[btb])

        it = 0
        for j in range(NT):
            if 2 <= j <= 29:
                kts = list(range(j - 2, j + 3)); tb = TBI; s0 = 1
            elif j == 0:
                kts = [0, 1, 2, 3]; tb = TBA; s0 = 3
            elif j == 1:
                kts = [0, 1, 2, 3]; tb = TBA; s0 = 2
            elif j == 30:
                kts = [28, 29, 30, 31]; tb = TBA; s0 = 1
            else:
                kts = [28, 29, 30, 31]; tb = TBA; s0 = 0
            n = len(kts)
            def st_mm(hd, k):
                hp, hh = hd // 2, hd % 2
                p0, p1 = hh * 64, hh * 64 + 64
                for idx, kt in enumerate(kts):
                    P.op("pe", lambda h, k=k, idx=idx, kt=kt, hp=hp, p0=p0, p1=p1, j=j: h.matmul(
                        ps_s[k][:, idx * 128:(idx + 1) * 128], lhsT=KT[p0:p1, hp, kt * 128:(kt + 1) * 128],
                        rhs=QT[p0:p1, hp, j * 128:(j + 1) * 128], start=True, stop=True),
                        reads=[bKT[hp], bQT[hp]], writes=[bps_s[k]])

            def post(hd, k):
                P.op("dve", lambda h, k=k, n=n, tb=tb, s0=s0, hd=hd: h.scalar_tensor_tensor(
                    out=sT[k][:, 0:n * 128], in0=ps_s[k][:, 0:n * 128], scalar=0.125,
                    in1=tb[:, hd, s0 * 128:(s0 + n) * 128], op0=ALU.mult, op1=ALU.add),
                    reads=[bps_s[k], btb], writes=[bsT[k]])
                P.op("act", lambda h, k=k, n=n: h.activation(out=pT[k][:, 0:n * 128], in_=sT[k][:, 0:n * 128],
                                                             func=AF.Exp), reads=[bsT[k]], writes=[bpT[k]])

            def pv(hd, k, ko):
                for idx, kt in enumerate(kts):
                    P.op("pe", lambda h, k=k, ko=ko, idx=idx, kt=kt, hd=hd, n=n: h.matmul(
                        ps_o[ko][:, 0:65], lhsT=pT[k][:, idx * 128:(idx + 1) * 128], rhs=V[:, kt, hd, :],
                        start=(idx == 0), stop=(idx == n - 1)),
                        reads=[bpT[k], bVt[kt], bones], writes=[bps_o[ko]])
                P.op("dve", lambda h, ko=ko: h.reciprocal(out=rc[ko][:], in_=ps_o[ko][:, 64:65]),
                     reads=[bps_o[ko]], writes=[brc[ko]])
                P.op("dve", lambda h, ko=ko, hd=hd: h.tensor_scalar(
                    out=ao[:, hd * 64:(hd + 1) * 64], in0=ps_o[ko][:, 0:64], scalar1=rc[ko][:], scalar2=None,
                    op0=ALU.mult), reads=[bps_o[ko], brc[ko]], writes=[bao])

            st_mm(0, it % 3)
            st_mm(1, (it + 1) % 3)
            for hd in range(8):
                k = it % 3
                ko = it % 2
                it += 1
                post(hd, k)
                if hd + 2 < 8:
                    st_mm(hd + 2, (it + 1) % 3)
                pv(hd, k, ko)
            o = j % 2
            P.op("act", lambda h: h.activation(out=junk[:], in_=ao[:], func=AF.Square, accum_out=ss[:, 0:1]),
                 reads=[bao], writes=[bjunk, bss])
            P.op("act", lambda h: h.activation(out=ss[:, 1:2], in_=ss[:, 0:1], func=AF.Sqrt, scale=1.0 / 512, bias=EPS),
                 reads=[bss], writes=[bss])
            P.op("dve", lambda h: h.reciprocal(out=ss[:, 2:3], in_=ss[:, 1:2]), reads=[bss], writes=[bss])
            P.op("dve", lambda h, o=o: h.scalar_tensor_tensor(out=aob[o][:], in0=ao[:], scalar=ss[:, 2:3], in1=gao[:],
                                                          op0=ALU.mult, op1=ALU.mult),
                 reads=[bao, bss, bgao], writes=[baob[o]])
            P.dma("sp", lambda h, j=j, o=o: h.dma_start(out=c.AOUT[j * 128:(j + 1) * 128, :], in_=aob[o][:]),
                  reads=[baob[o]], writes=[c.bAOUT[j]])
    P.barrier()


def phase_c(c):
    nc, P = c.nc, c.P
    with ExitStack() as st0:
        sb0 = lambda n, s, d: st0.enter_context(nc.sbuf_tensor(n, s, d))
        COLS = sb0("c_COLS", [128, NT, 24], F32)
        bCOLS = Buf("COLS")
        with ExitStack() as st:
            sb = lambda n, s, d: st.enter_context(nc.sbuf_tensor(n, s, d))
            ps = lambda n, s, d: st.enter_context(nc.psum_tensor(n, s, d))
            G1, G2, CL, Aa, AA, ZER, T1 = [sb("c1_" + n, [4, S], F32) for n in ("G1", "G2", "CL", "Aa", "AA", "ZER", "T1")]
            bG1, bG2, bCL, bAa, bAA, bZER, bT1 = [Buf(n) for n in ("G1", "G2", "CL", "Aa", "AA", "ZER", "T1")]
            BCR = sb("c1_BCR", [4, NT, 257], F32)
            ROWS = sb("c1_ROWS", [24, S], F32)
            gb = sb("c1_gb", [4, 4], F32)
            ngb = sb("c1_ngb", [4, 4], F32)
            AE = sb("c1_AE", [4, NT], F32)
            APv = sb("c1_AP", [4, NT], F32)
            dd = sb("c1_dd", [4, NT], F32)
            identf = sb("c1_identf", [128, 128], F32)
            ps_c = ps("c1_ps_c", [128, NT, 32], F32)
            bBCR, bgb, bAE, bAPv, bdd, bid, bps_c = [Buf(n) for n in ("BCR", "gb", "AE", "AP", "dd", "id", "ps_c")]
            bROWS = bufs(6, "ROWS")
            P.dma("sp", lambda h: h.dma_start(out=gb[:], in_=c.gate_b), writes=[bgb])
            P.dma("sp", lambda h: h.dma_start(out=identf[:], in_=c.identf), writes=[bid])
            P.op("dve", lambda h: h.tensor_scalar(out=ngb[:], in0=gb[:], scalar1=-1.0, scalar2=None, op0=ALU.mult),
                 reads=[bgb], writes=[bgb])
            P.op("pool", lambda h: h.memset(ZER[:], 0.0), writes=[bZER])
            for d in range(2):
                rv = (lambda t: t[:, :]) if d == 0 else (lambda t: t[:, ::-1])
                gi_, gf_ = 2 * d, 2 * d + 1
                P.dma("sp", lambda h, gi_=gi_: h.dma_start(out=G1[:], in_=c.GT[gi_]), reads=c.bGT, writes=[bG1])
                P.dma("sp", lambda h, gf_=gf_: h.dma_start(out=G2[:], in_=c.GT[gf_]), reads=c.bGT, writes=[bG2])
                P.op("act", lambda h, gf_=gf_: h.activation(out=G2[:], in_=G2[:], func=AF.Exp, scale=-1.0,
                                                            bias=ngb[:, gf_:gf_ + 1]), reads=[bG2, bgb], writes=[bG2])
                P.op("act", lambda h: h.activation(out=G2[:], in_=G2[:], func=AF.Ln, bias=1.0), reads=[bG2], writes=[bG2])
                P.op("dve", lambda h, rv=rv: h.tensor_tensor_scan(out=rv(CL), data0=rv(G2), data1=ZER[:], initial=0.0,
                                                                  op0=ALU.add, op1=ALU.add),
                     reads=[bG2, bZER], writes=[bCL])
                P.op("dve", lambda h, gi_=gi_: h.scalar_tensor_tensor(out=Aa[:], in0=G1[:], scalar=gb[:, gi_:gi_ + 1],
                                                                      in1=CL[:], op0=ALU.add, op1=ALU.add),
                     reads=[bG1, bgb, bCL], writes=[bAa])
                P.op("dve", lambda h, rv=rv: h.tensor_tensor_scan(out=rv(AA), data0=rv(Aa), data1=ZER[:], initial=0.0,
                                                                  op0=ALU.max, op1=ALU.add),
                     reads=[bAa, bZER], writes=[bAA])
                P.op("dve", lambda h: h.tensor_tensor(out=T1[:], in0=CL[:], in1=AA[:], op=ALU.subtract),
                     reads=[bCL, bAA], writes=[bT1])
                P.op("act", lambda h: h.activation(out=T1[:], in_=T1[:], func=AF.Exp), reads=[bT1], writes=[bT1])
                P.dma("sp", lambda h, d=d: h.dma_start(out=ROWS[12 * d + 8:12 * d + 12, :], in_=T1[:]),
                      reads=[bT1], writes=[bROWS[3 * d + 2]])
                P.dma("sp", lambda h, d=d: h.dma_start(out=ROWS[12 * d:12 * d + 4, :], in_=Aa[:]),
                      reads=[bAa], writes=[bROWS[3 * d]])
                AAv = AA[:].rearrange("p (c t) -> p c t", t=128)
                epos = 127 if d == 0 else 0
                P.op("dve", lambda h, AAv=AAv, epos=epos: h.tensor_copy(out=AE[:], in_=AAv[:, :, epos]),
                     reads=[bAA], writes=[bAE])
                P.op("dve", lambda h: h.memset(APv[:], 0.0), writes=[bAPv])
                if d == 0:
                    P.op("dve", lambda h: h.tensor_copy(out=APv[:, 1:NT], in_=AE[:, 0:NT - 1]), reads=[bAE], writes=[bAPv])
                else:
                    P.op("dve", lambda h: h.tensor_copy(out=APv[:, 0:NT - 1], in_=AE[:, 1:NT]), reads=[bAE], writes=[bAPv])
                G1v = G1[:].rearrange("p (c t) -> p c t", t=128)
                G2v = G2[:].rearrange("p (c t) -> p c t", t=128)
                Aav = Aa[:].rearrange("p (c t) -> p c t", t=128)
                P.op("dve", lambda h, G1v=G1v, Aav=Aav: h.tensor_tensor(
                    out=G1v, in0=Aav, in1=AE[:].unsqueeze(2).to_broadcast([4, NT, 128]), op=ALU.subtract),
                    reads=[bAa, bAE], writes=[bG1])
                P.op("act", lambda h: h.activation(out=G1[:], in_=G1[:], func=AF.Exp), reads=[bG1], writes=[bG1])
                P.dma("sp", lambda h, d=d: h.dma_start(out=ROWS[12 * d + 4:12 * d + 8, :], in_=G1[:]),
                      reads=[bG1], writes=[bROWS[3 * d + 1]])
                P.op("dve", lambda h, AAv=AAv: h.tensor_scalar(out=BCR[:, :, 0:128], in0=AAv, scalar1=-1.0, scalar2=None,
                                                               op0=ALU.mult), reads=[bAA], writes=[bBCR])
                P.op("dve", lambda h, G2v=G2v, AAv=AAv: h.tensor_tensor(
                    out=G2v, in0=APv[:].unsqueeze(2).to_broadcast([4, NT, 128]), in1=AAv, op=ALU.subtract),
                    reads=[bAA, bAPv], writes=[bG2])
                P.op("act", lambda h, G2v=G2v: h.activation(out=BCR[:, :, 128:256], in_=G2v, func=AF.Exp),
                     reads=[bG2], writes=[bBCR])
                P.op("dve", lambda h: h.tensor_tensor(out=dd[:], in0=APv[:], in1=AE[:], op=ALU.subtract),
                     reads=[bAPv, bAE], writes=[bdd])
                P.op("act", lambda h: h.activation(out=BCR[:, :, 256], in_=dd[:], func=AF.Exp), reads=[bdd], writes=[bBCR])
                P.dma("sp", lambda h, d=d: h.dma_start(out=c.BCRD[d], in_=BCR[:]), reads=[bBCR], writes=[c.bBCRD[d]])
            for ch in range(NT):
                P.op("pe", lambda h, ch=ch: h.transpose(out=ps_c[:, ch, 0:24], in_=ROWS[0:24, ch * 128:(ch + 1) * 128],
                                                        identity=identf[0:24, 0:24]), reads=bROWS + [bid], writes=[bps_c])
            P.op("dve", lambda h: h.tensor_copy(out=COLS[:], in_=ps_c[:, :, 0:24]), reads=[bps_c], writes=[bCOLS])
        P.barrier()

        with ExitStack() as st:
            sb = lambda n, s, d: st.enter_context(nc.sbuf_tensor(n, s, d))
            ps = lambda n, s, d: st.enter_context(nc.psum_tensor(n, s, d))
            QKT = sb("c_QKT", [128, 8, S], BF16)
            bQKT = bufs(8, "cQKT")
            cw = sb("c_cw", [128, 8, 5], F32)
            cb = sb("c_cb", [128, 8], F32)
            bcw = Buf("cw")
            P.dma("sp", lambda h: h.dma_start(out=cw[:], in_=c.conv_w), writes=[bcw])
            P.dma("sp", lambda h: h.dma_start(out=cb[:], in_=c.conv_b), writes=[bcw])
            with ExitStack() as st2:
                sb2 = lambda n, s, d: st2.enter_context(nc.sbuf_tensor(n, s, d))
                xpad = [sb2("c2_xpad%d" % i, [128, S + 4], F32) for i in range(2)]
                acc = [sb2("c2_acc%d" % i, [128, S], F32) for i in range(2)]
                bxp, bacc = bufs(2, "xpad"), bufs(2, "acc")
                for i in range(2):
                    P.op("pool", lambda h, i=i: h.memset(xpad[i][:, 0:2], 0.0), writes=[bxp[i]])
                    P.op("pool", lambda h, i=i: h.memset(xpad[i][:, S + 2:S + 4], 0.0), writes=[bxp[i]])
                for cc in range(8):
                    k = cc % 2
                    P.dma("sp", lambda h, cc=cc, k=k: h.dma_start(out=xpad[k][:, 2:S + 2], in_=c.MQKT[cc * 128:(cc + 1) * 128, :]),
                          reads=c.bMQKT, writes=[bxp[k]])
                    P.op("dve", lambda h, cc=cc, k=k: h.tensor_scalar(out=acc[k][:], in0=xpad[k][:, 0:S], scalar1=cw[:, cc, 0:1],
                                                                      scalar2=cb[:, cc:cc + 1], op0=ALU.mult, op1=ALU.add),
                         reads=[bxp[k], bcw], writes=[bacc[k]])
                    for j in range(1, 5):
                        P.op("dve", lambda h, cc=cc, k=k, j=j: h.scalar_tensor_tensor(
                            out=acc[k][:], in0=xpad[k][:, j:j + S], scalar=cw[:, cc, j:j + 1], in1=acc[k][:],
                            op0=ALU.mult, op1=ALU.add), reads=[bxp[k], bcw, bacc[k]], writes=[bacc[k]])
                    if cc < 4:
                        P.op("act", lambda h, cc=cc, k=k: h.activation(out=QKT[:, cc, :], in_=acc[k][:], func=AF.Silu),
                             reads=[bacc[k]], writes=[bQKT[cc]])
                    else:
                        P.op("act", lambda h, cc=cc, k=k: h.activation(out=acc[k][:], in_=acc[k][:], func=AF.Silu),
                             reads=[bacc[k]], writes=[bacc[k]])
                        P.op("pool", lambda h, cc=cc, k=k: h.tensor_scalar(out=QKT[:, cc, :], in0=acc[k][:], scalar1=128.0 ** -0.5,
                                                                           scalar2=None, op0=ALU.mult),
                             reads=[bacc[k]], writes=[bQKT[cc]])
            P.barrier()

            MVs = sb("c_MVs", [128, NT, 4, 129], BF16)
            bMVs = bufs(NT, "cMV")
            bones = Buf("ones")
            SEL = sb("c_SEL", [4, 4, 128], F32)
            MSK = sb("c_MSK", [128, 2, 128], F32)
            identb = sb("c_identb", [128, 128], BF16)
            gmn = sb("c_gmn", [128, 512], F32)
            bconst = Buf("const")
            P.dma("sp", lambda h: h.dma_start(out=SEL[:], in_=c.sel), writes=[bconst])
            P.dma("sp", lambda h: h.dma_start(out=MSK[:], in_=c.msk), writes=[bconst])
            P.dma("sp", lambda h: h.dma_start(out=identb[:], in_=c.identb), writes=[bconst])
            P.dma("sp", lambda h: h.dma_start(out=gmn[:], in_=c.gmn), writes=[bconst])
            P.op("pool", lambda h: h.memset(MVs[:, :, :, 128:129], 1.0), writes=[bones])
            for i in range(NT):
                P.dma("sp" if i % 2 == 0 else "act", lambda h, i=i: h.dma_start(
                    out=MVs[:, i, :, 0:128], in_=c.MV[i * 128:(i + 1) * 128, :].rearrange("p (a b) -> p a b", b=128)),
                    reads=[c.bMV[i]], writes=[bMVs[i]])
            Cst = [sb("c_C%d" % i, [128, 129], F32) for i in range(4)]
            Cbf = [sb("c_Cbf%d" % i, [128, 129], BF16) for i in range(4)]
            bC, bCbf = bufs(4, "C"), bufs(4, "Cbf")
            bcr = [sb("c_bcr%d" % i, [4, 257], F32) for i in range(2)]
            bbcr = bufs(2, "bcr")
            Gt = [sb("c_G%d" % i, [128, 128], F32) for i in range(2)]
            Wt = [sb("c_W%d" % i, [128, 128], F32) for i in range(2)]
            PT = [sb("c_PT%d" % i, [128, 128], BF16) for i in range(2)]
            qs = [sb("c_qs%d" % i, [128, 128], BF16) for i in range(2)]
            kw = [sb("c_kw%d" % i, [128, 128], BF16) for i in range(2)]
            dec = [sb("c_dec%d" % i, [128, 1], F32) for i in range(2)]
            dn = [sb("c_dn%d" % i, [128, 2], F32) for i in range(2)]
            bG, bW, bPT, bqs, bkw, bdec, bdn = [bufs(2, n) for n in ("G", "W", "PT", "qs", "kw", "dec", "dn")]
            hbuf = [sb("c_hbuf%d" % i, [128, 512], F32) for i in range(2)]
            bhbuf = bufs(2, "hbuf")
            hf = sb("c_hf", [128, 512], F32)
            sg = sb("c_sg", [128, 512], BF16)
            sq = sb("c_sq", [128, 512], F32)
            s4 = sb("c_s4", [128, 12], F32)
            hmo = [sb("c_hmo%d" % i, [128, 512], BF16) for i in range(2)]
            bhf, bsg, bsq, bs4 = Buf("hf"), Buf("sg"), Buf("sq"), Buf("s4")
            bhmo = bufs(2, "hmo")
            ps_bc = [ps("c_ps_bc%d" % i, [128, 512], F32) for i in range(2)]
            ps_st = [ps("c_ps_st%d" % i, [128, 128], F32) for i in range(2)]
            ps_n = [ps("c_ps_n%d" % i, [128, 512], F32) for i in range(2)]
            ps_kt = ps("c_ps_kt", [128, 128], BF16)
            ps_dc = ps("c_ps_dc", [128, 512], F32)
            bps_bc, bps_st, bps_n = bufs(2, "ps_bc"), bufs(2, "ps_st"), bufs(2, "ps_n")
            bps_kt, bps_dc = Buf("ps_kt"), Buf("ps_dc")

            it = 0
            for d in range(2):
                for hd in range(4):
                    P.op("pool", lambda h, hd=hd: h.memset(Cst[hd][:], 0.0), writes=[bC[hd]])
                    P.op("pool", lambda h, hd=hd: h.memset(Cbf[hd][:], 0.0), writes=[bCbf[hd]])
                order = list(range(NT)) if d == 0 else list(range(NT - 1, -1, -1))
                for ci, ch in enumerate(order):
                    kb = ci % 2
                    P.dma("sp", lambda h, d=d, ch=ch, kb=kb: h.dma_start(out=bcr[kb][:], in_=c.BCRD[d, :, ch, :]),
                          reads=[c.bBCRD[d]], writes=[bbcr[kb]])
                    hb = hbuf[ci % 2]
                    bhb = bhbuf[ci % 2]
                    tsl = slice(ch * 128, (ch + 1) * 128)
                    def front(hd, k, ch=ch, tsl=tsl, kb=kb, d=d, hb=hb, bhb=bhb):
                        P.op("pe", lambda h: h.matmul(ps_bc[k][:, 0:257], lhsT=SEL[:, hd, :], rhs=bcr[kb][:], start=True, stop=True),
                             reads=[bconst, bbcr[kb]], writes=[bps_bc[k]])
                        P.op("pe", lambda h: h.matmul(ps_st[k][:], lhsT=QKT[:, 4 + hd, tsl], rhs=QKT[:, hd, tsl], start=True, stop=True),
                             reads=[bQKT[4 + hd], bQKT[hd]], writes=[bps_st[k]])
                        P.op("pe", lambda h: h.transpose(out=ps_kt[:], in_=QKT[:, 4 + hd, tsl], identity=identb[:]),
                             reads=[bQKT[4 + hd], bconst], writes=[bps_kt])

                    def mid(hd, k, ch=ch, tsl=tsl, kb=kb, d=d, hb=hb, bhb=bhb):
                        a_col = COLS[:, ch, 12 * d + hd:12 * d + hd + 1]
                        wk_col = COLS[:, ch, 12 * d + 4 + hd:12 * d + 5 + hd]
                        P.op("dve", lambda h: h.tensor_tensor(out=Gt[k][:], in0=ps_bc[k][:, 0:128], in1=MSK[:, d, :], op=ALU.add),
                             reads=[bps_bc[k], bconst], writes=[bG[k]])
                        P.op("act", lambda h: h.activation(out=kw[k][:], in_=ps_kt[:], func=AF.Copy, scale=wk_col),
                             reads=[bps_kt, bCOLS], writes=[bkw[k]])
                        P.op("act", lambda h: h.activation(out=Wt[k][:], in_=Gt[k][:], func=AF.Exp, bias=a_col),
                             reads=[bG[k], bCOLS], writes=[bW[k]])
                        P.op("dve", lambda h: h.tensor_tensor(out=qs[k][:], in0=ps_bc[k][:, 128:256], in1=QKT[:, hd, tsl], op=ALU.mult),
                             reads=[bps_bc[k], bQKT[hd]], writes=[bqs[k]])
                        P.op("act", lambda h: h.copy(out=dec[k][:], in_=ps_bc[k][:, 256:257]), reads=[bps_bc[k]], writes=[bdec[k]])
                        P.op("dve", lambda h: h.tensor_tensor(out=PT[k][:], in0=ps_st[k][:], in1=Wt[k][:], op=ALU.mult),
                             reads=[bps_st[k], bW[k]], writes=[bPT[k]])

                    def back(hd, k, ch=ch, tsl=tsl, kb=kb, d=d, hb=hb, bhb=bhb):
                        emt_col = COLS[:, ch, 12 * d + 8 + hd:12 * d + 9 + hd]
                        P.op("pe", lambda h: h.matmul(ps_dc[:, 0:129], lhsT=kw[k][:], rhs=MVs[:, ch, hd, :], start=True, stop=True),
                             reads=[bkw[k], bMVs[ch], bones], writes=[bps_dc])
                        P.op("pe", lambda h: h.matmul(ps_n[k][:, 0:129], lhsT=PT[k][:], rhs=MVs[:, ch, hd, :], start=True, stop=False),
                             reads=[bPT[k], bMVs[ch], bones], writes=[bps_n[k]])
                        P.op("pe", lambda h: h.matmul(ps_n[k][:, 0:129], lhsT=qs[k][:], rhs=Cbf[hd][:], start=False, stop=True),
                             reads=[bqs[k], bCbf[hd]], writes=[bps_n[k]])
                        P.op("dve", lambda h: h.scalar_tensor_tensor(out=Cst[hd][:], in0=Cst[hd][:], scalar=dec[k][:],
                                                                     in1=ps_dc[:, 0:129], op0=ALU.mult, op1=ALU.add),
                             reads=[bC[hd], bdec[k], bps_dc], writes=[bC[hd]])
                        P.op("act", lambda h: h.copy(out=Cbf[hd][:], in_=Cst[hd][:]), reads=[bC[hd]], writes=[bCbf[hd]])
                        P.op("act", lambda h: h.activation(out=dn[k][:, 1:2], in_=ps_n[k][:, 128:129], func=AF.Abs),
                             reads=[bps_n[k]], writes=[bdn[k]])
                        P.op("dve", lambda h: h.tensor_scalar(out=dn[k][:, 0:1], in0=dn[k][:, 1:2], scalar1=emt_col, scalar2=None, op0=ALU.max),
                             reads=[bdn[k], bCOLS], writes=[bdn[k]])
                        P.op("dve", lambda h: h.reciprocal(out=dn[k][:, 1:2], in_=dn[k][:, 0:1]), reads=[bdn[k]], writes=[bdn[k]])
                        P.op("dve", lambda h: h.tensor_scalar(out=hb[:, hd * 128:(hd + 1) * 128], in0=ps_n[k][:, 0:128],
                                                              scalar1=dn[k][:, 1:2], scalar2=None, op0=ALU.mult),
                             reads=[bps_n[k], bdn[k]], writes=[bhb])

                    front(0, it % 2)
                    for hd in range(4):
                        k = it % 2
                        it += 1
                        mid(hd, k)
                        if hd + 1 < 4:
                            front(hd + 1, it % 2)
                        back(hd, k)
                    if d == 0:
                        P.dma("sp", lambda h, ch=ch, hb=hb: h.dma_start(out=c.HF[ch * 128:(ch + 1) * 128, :], in_=hb[:]),
                              reads=[bhb], writes=[c.bHF[ch]])
                    else:
                        o = ci % 2
                        P.dma("sp", lambda h, ch=ch: h.dma_start(out=hf[:], in_=c.HF[ch * 128:(ch + 1) * 128, :]),
                              reads=[c.bHF[ch]], writes=[bhf])
                        P.dma("sp", lambda h, ch=ch: h.dma_start(out=sg[:], in_=c.SIGO[ch * 128:(ch + 1) * 128, :]),
                              reads=[c.bSIGO[ch]], writes=[bsg])
                        P.op("pool", lambda h, hb=hb: h.tensor_tensor(out=hf[:], in0=hf[:], in1=hb[:], op=ALU.add),
                             reads=[bhb, bhf], writes=[bhf])
                        P.op("act", lambda h: h.activation(out=sq[:], in_=hf[:], func=AF.Square), reads=[bhf], writes=[bsq])
                        P.op("dve", lambda h: h.tensor_reduce(out=s4[:, 0:4], in_=sq[:].rearrange("p (a b) -> p a b", b=128),
                                                              axis=AX.X, op=ALU.add), reads=[bsq], writes=[bs4])
                        P.op("act", lambda h: h.activation(out=s4[:, 4:8], in_=s4[:, 0:4], func=AF.Sqrt, scale=1.0 / 128, bias=EPS),
                             reads=[bs4], writes=[bs4])
                        P.op("dve", lambda h: h.reciprocal(out=s4[:, 8:12], in_=s4[:, 4:8]), reads=[bs4], writes=[bs4])
                        P.op("dve", lambda h: h.tensor_tensor(out=sq[:].rearrange("p (a b) -> p a b", b=128),
                                                              in0=hf[:].rearrange("p (a b) -> p a b", b=128),
                                                              in1=s4[:, 8:12].unsqueeze(2).to_broadcast([128, 4, 128]), op=ALU.mult),
                             reads=[bhf, bs4, bsq], writes=[bsq])
                        P.op("pool", lambda h: h.tensor_tensor(out=sq[:], in0=sq[:], in1=gmn[:], op=ALU.mult),
                             reads=[bsq, bconst], writes=[bsq])
                        P.op("pool", lambda h, o=o: h.tensor_tensor(out=hmo[o][:], in0=sq[:], in1=sg[:], op=ALU.mult),
                             reads=[bsq, bsg], writes=[bhmo[o]])
                        P.dma("sp", lambda h, ch=ch, o=o: h.dma_start(out=c.HM[ch * 128:(ch + 1) * 128, :], in_=hmo[o][:]),
                              reads=[bhmo[o]], writes=[c.bHM[ch]])
    P.barrier()


def phase_t(c):
    nc, P = c.nc, c.P
    JB = 4
    with ExitStack() as st:
        sb = lambda n, s, d: st.enter_context(nc.sbuf_tensor(n, s, d))
        tin = [sb("t_in%d" % i, [128, JB * 2 * D], F32) for i in range(2)]
        tout = [sb("t_out%d" % i, [128, JB * 2 * D], BF16) for i in range(2)]
        bin_, bout, bout2 = bufs(2, "tin"), bufs(2, "tout"), bufs(2, "tout2")
        src = c.peer_uv.rearrange("(p j) d -> p (j d)", p=128)
        dst = c.UVB.rearrange("(p j) d -> p (j d)", p=128)
        W = JB * 2 * D
        third = W // 4
        for stp in range(128 // JB):
            k = stp % 2
            P.dma("sp", lambda h, stp=stp, k=k: h.dma_start(out=tin[k][:], in_=src[:, stp * W:(stp + 1) * W]), writes=[bin_[k]])
            cut = (W * 5) // 8
            P.op("dve", lambda h, k=k, cut=cut: h.tensor_copy(out=tout[k][:, 0:cut], in_=tin[k][:, 0:cut]), reads=[bin_[k]], writes=[bout[k]])
            P.op("act", lambda h, k=k, cut=cut: h.copy(out=tout[k][:, cut:W], in_=tin[k][:, cut:W]), reads=[bin_[k]], writes=[bout2[k]])
            P.dma("sp", lambda h, stp=stp, k=k: h.dma_start(out=dst[:, stp * W:(stp + 1) * W], in_=tout[k][:]), reads=[bout[k], bout2[k]])
    P.barrier()


def phase_d(c):
    nc, P = c.nc, c.P
    with ExitStack() as st:
        sb = lambda n, s, d: st.enter_context(nc.sbuf_tensor(n, s, d))
        ps = lambda n, s, d: st.enter_context(nc.psum_tensor(n, s, d))
        Wo = sb("d_Wo", [128, 8, D], BF16)
        Wq = sb("d_Wq", [128, 8, 2048], BF16)
        SKT = sb("d_SKT", [128, 16, 128], BF16)
        gn2 = sb("d_gn2", [128, D], F32)
        identb = sb("d_identb", [128, 128], BF16)
        identf = sb("d_identf", [128, 128], F32)
        THR = sb("d_THR", [128, 16], F32)
        IOT = sb("d_IOT", [128, 16], F32)
        st_stage = ExitStack()
        stage = [st_stage.enter_context(nc.sbuf_tensor("d_stage%d" % i, [128, 2048], F32)) for i in range(2)]
        bconst = Buf("dconst")
        bst = bufs(2, "dst")
        bWo, bWq = bufs(8, "Wo"), bufs(8, "Wq")
        bSKT = Buf("SKT")
        for (t, src) in ((gn2, c.gn2), (identb, c.identb), (THR, c.thr), (IOT, c.iot), (identf, c.identf)):
            P.dma("sp", lambda h, t=t, src=src: h.dma_start(out=t[:], in_=src), writes=[bconst])
        n = 0
        for kc in range(8):
            k = n % 2; n += 1
            P.dma("sp", lambda h, kc=kc, k=k: h.dma_start(out=stage[k][:, 0:D], in_=c.w_out[kc * 128:(kc + 1) * 128, :]), writes=[bst[k]])
            P.op("dve", lambda h, kc=kc, k=k: h.tensor_copy(out=Wo[:, kc, :], in_=stage[k][:, 0:D]), reads=[bst[k]], writes=[bWo[kc]])
        for kc in range(8):
            k = n % 2; n += 1
            P.dma("sp", lambda h, kc=kc, k=k: h.dma_start(out=stage[k][:], in_=c.w_q[kc * 128:(kc + 1) * 128, :]), writes=[bst[k]])
            P.op("dve", lambda h, kc=kc, k=k: h.tensor_copy(out=Wq[:, kc, :], in_=stage[k][:]), reads=[bst[k]], writes=[bWq[kc]])
        k = n % 2; n += 1
        P.dma("sp", lambda h, k=k: h.dma_start(out=stage[k][:].rearrange("p (a b) -> p a b", b=128), in_=c.skt), writes=[bst[k]])
        P.op("dve", lambda h, k=k: h.tensor_copy(out=SKT[:].rearrange("p a b -> p (a b)"), in_=stage[k][:]), reads=[bst[k]], writes=[bSKT])
        P.barrier()
        st_stage.close()

        cat = sb("d_cat", [128, D], BF16)
        catT = sb("d_catT", [128, 8, 128], BF16)
        xt = sb("d_xt", [128, D], F32)
        junk = sb("d_junk", [128, D], F32)
        junk2 = sb("d_junk2", [128, D], F32)
        ss = sb("d_ss", [128, 4], F32)
        xn2b = sb("d_xn2b", [128, D], BF16)
        xn2T = sb("d_xn2T", [128, 8, 128], BF16)
        qT = sb("d_qT", [128, 16, 128], BF16)
        sc = sb("d_sc", [128, 16, 128], F32)
        m8 = sb("d_m8", [128, 16, 16], F32)
        i8 = sb("d_i8", [128, 16, 16], U32)
        i8f = sb("d_i8f", [128, 16, 16], F32)
        cand = sb("d_cand", [128, 8, 256], F32)
        t8 = sb("d_t8", [128, 8, 16], F32)
        j8 = sb("d_j8", [128, 8, 16], U32)
        jf = sb("d_jf", [128, 128], F32)
        T4 = sb("d_T4", [128, 128, 16], F32)
        af = sb("d_af", [128, 128], F32)
        bf_ = sb("d_bf", [128, 128], F32)
        E1 = sb("d_E1", [128, 128], F32)
        E2 = sb("d_E2", [128, 128], F32)
        g8 = sb("d_g8", [128, 16], F32)
        x1 = [sb("d_x1_%d" % i, [128, D], F32) for i in range(2)]
        xn2 = [sb("d_xn2_%d" % i, [128, D], F32) for i in range(2)]
        eidx = [sb("d_eidx%d" % i, [128, 128], I32) for i in range(2)]
        gts = [sb("d_gts%d" % i, [128, 8, 16], F32) for i in range(2)]
        adot = sb("d_adot", [128, 128], F32)
        ga = sb("d_ga", [128, 128], F32)
        NG = 16
        GS = c.gs
        NGRP = 128 // GS
        uv = [sb("d_uv%d" % i, [128, 2 * D], BF16) for i in range(NG)]
        dg = [sb("d_dg%d" % i, [128, 128], BF16) for i in range(4)]
        yacc = sb("d_y", [128, D], F32)
        ps_t = ps("d_ps_t", [128, D], BF16)
        ps_o = [ps("d_ps_o%d" % i, [128, 512], F32) for i in range(2)]
        ps_y = [ps("d_ps_y%d" % i, [128, 512], F32) for i in range(2)]
        ps_q = [ps("d_ps_q%d" % i, [128, 4, 128], F32) for i in range(2)]
        (bcat, bcatT, bxt, bss, bxn2b, bxn2T, bqT, bsc, bm8, bi8, bi8f, bcand, bt8, bj8,
         bjf, bT4, baf, bbf, bE1, bE2, bg8, by, bps_t) = [Buf(n_) for n_ in (
            "cat", "catT", "xt", "ss", "xn2b", "xn2T", "qT", "sc", "m8", "i8", "i8f", "cand",
            "t8", "j8", "jf", "T4", "af", "bf", "E1", "E2", "g8", "y", "ps_t")]
        bx1, bxn2, beidx, bgts = bufs(2, "x1"), bufs(2, "xn2"), bufs(2, "eidx"), bufs(2, "gts")
        badot, bga = bufs(NGRP, "adot"), bufs(NGRP, "ga")
        buv, bdg = bufs(NG, "uv"), bufs(4, "dg")
        bps_o, bps_q, bps_y = bufs(2, "ps_o"), bufs(2, "ps_q"), bufs(2, "ps_y")
        qi = [0]

        def routing(i):
            par = i % 2
            x1_, xn2_, eidx_, gts_ = x1[par], xn2[par], eidx[par], gts[par]
            bx1_, bxn2_, beidx_, bgts_ = bx1[par], bxn2[par], beidx[par], bgts[par]
            rows = slice(i * 128, (i + 1) * 128)
            P.dma("sp", lambda h: h.dma_start(out=cat[:, 0:512], in_=c.AOUT[rows, :]), reads=[c.bAOUT[i]], writes=[bcat])
            P.dma("sp", lambda h: h.dma_start(out=cat[:, 512:1024], in_=c.HM[rows, :]), reads=[c.bHM[i]], writes=[bcat])
            P.dma("sp", lambda h: h.dma_start(out=xt[:], in_=c.x[rows, :]), writes=[bxt])
            for kc in range(8):
                P.op("pe", lambda h, kc=kc: h.transpose(out=ps_t[:, kc * 128:(kc + 1) * 128], in_=cat[:, kc * 128:(kc + 1) * 128],
                                                        identity=identb[:]), reads=[bcat, bconst], writes=[bps_t])
            P.op("act", lambda h: h.copy(out=catT[:].rearrange("p k t -> p (k t)"), in_=ps_t[:]), reads=[bps_t], writes=[bcatT])
            yield
            for g in range(2):
                for kc in range(8):
                    P.op("pe", lambda h, g=g, kc=kc: h.matmul(ps_o[g][:], lhsT=catT[:, kc, :], rhs=Wo[:, kc, g * 512:(g + 1) * 512],
                                                             start=(kc == 0), stop=(kc == 7)), reads=[bcatT, bWo[kc]], writes=[bps_o[g]])
                P.op("dve", lambda h, g=g: h.tensor_tensor(out=x1_[:, g * 512:(g + 1) * 512], in0=ps_o[g][:], in1=xt[:, g * 512:(g + 1) * 512],
                                                          op=ALU.add), reads=[bps_o[g], bxt], writes=[bx1_])
                yield
            P.op("act", lambda h: h.activation(out=junk[:], in_=x1_[:], func=AF.Square, accum_out=ss[:, 0:1]), reads=[bx1_], writes=[bss])
            P.op("act", lambda h: h.activation(out=ss[:, 1:2], in_=ss[:, 0:1], func=AF.Sqrt, scale=1.0 / D, bias=EPS), reads=[bss], writes=[bss])
            P.op("dve", lambda h: h.reciprocal(out=ss[:, 2:3], in_=ss[:, 1:2]), reads=[bss], writes=[bss])
            P.op("dve", lambda h: h.scalar_tensor_tensor(out=xn2_[:], in0=x1_[:], scalar=ss[:, 2:3], in1=gn2[:], op0=ALU.mult, op1=ALU.mult),
                 reads=[bx1_, bss, bconst], writes=[bxn2_])
            P.op("act", lambda h: h.copy(out=xn2b[:], in_=xn2_[:]), reads=[bxn2_], writes=[bxn2b])
            yield
            for kc in range(8):
                P.op("pe", lambda h, kc=kc: h.transpose(out=ps_t[:, kc * 128:(kc + 1) * 128], in_=xn2b[:, kc * 128:(kc + 1) * 128],
                                                        identity=identb[:]), reads=[bxn2b, bconst], writes=[bps_t])
            P.op("act", lambda h: h.copy(out=xn2T[:].rearrange("p k t -> p (k t)"), in_=ps_t[:]), reads=[bps_t], writes=[bxn2T])
            yield
            for qg in range(4):
                k = qi[0] % 2
                qi[0] += 1
                for cc in range(4):
                    hp = qg * 4 + cc
                    for kc in range(8):
                        P.op("pe", lambda h, k=k, cc=cc, hp=hp, kc=kc: h.matmul(ps_q[k][:, cc, :], lhsT=Wq[:, kc, hp * 128:(hp + 1) * 128],
                                                                              rhs=xn2T[:, kc, :], start=(kc == 0), stop=(kc == 7)),
                             reads=[bWq[kc], bxn2T], writes=[bps_q[k]])
                P.op("act", lambda h, k=k, qg=qg: h.copy(out=qT[:, qg * 4:(qg + 1) * 4, :], in_=ps_q[k][:]), reads=[bps_q[k]], writes=[bqT])
                yield
            for qg in range(4):
                k = qi[0] % 2
                qi[0] += 1
                for cc in range(4):
                    hp = qg * 4 + cc
                    P.op("pe", lambda h, k=k, cc=cc, hp=hp: h.matmul(ps_q[k][:, cc, :], lhsT=qT[:, hp, :], rhs=SKT[:, hp, :],
                                                                   start=True, stop=True), reads=[bqT, bSKT], writes=[bps_q[k]])
                P.op("act", lambda h, k=k, qg=qg: h.copy(out=sc[:, qg * 4:(qg + 1) * 4, :], in_=ps_q[k][:]), reads=[bps_q[k]], writes=[bsc])
            yield
            for g in range(16):
                P.op("dve", lambda h, g=g: h.max(out=m8[:, g, 0:8], in_=sc[:, g, :]), reads=[bsc], writes=[bm8])
                P.op("dve", lambda h, g=g: h.max_index(out=i8[:, g, 0:8], in_max=m8[:, g, 0:8], in_values=sc[:, g, :]),
                     reads=[bsc, bm8], writes=[bi8])
                P.op("dve", lambda h, g=g: h.match_replace(out=sc[:, g, :], in_to_replace=m8[:, g, 0:8], in_values=sc[:, g, :],
                                                           imm_value=-1e30), reads=[bm8], writes=[bsc])
                P.op("dve", lambda h, g=g: h.max(out=m8[:, g, 8:16], in_=sc[:, g, :]), reads=[bsc], writes=[bm8])
                P.op("dve", lambda h, g=g: h.max_index(out=i8[:, g, 8:16], in_max=m8[:, g, 8:16], in_values=sc[:, g, :]),
                     reads=[bsc, bm8], writes=[bi8])
                yield
            m8v = m8[:].rearrange("p (a b) k -> p a b k", b=2)
            P.op("dve", lambda h: h.tensor_tensor(
                out=cand[:].rearrange("p a (x y) -> p a x y", y=16),
                in0=m8v[:, :, 0, :].unsqueeze(3).to_broadcast([128, 8, 16, 16]),
                in1=m8v[:, :, 1, :].unsqueeze(2).to_broadcast([128, 8, 16, 16]), op=ALU.add), reads=[bm8], writes=[bcand])
            yield
            for hd in range(8):
                P.op("dve", lambda h, hd=hd: h.max(out=t8[:, hd, 0:8], in_=cand[:, hd, :]), reads=[bcand], writes=[bt8])
                P.op("dve", lambda h, hd=hd: h.max_index(out=j8[:, hd, 0:8], in_max=t8[:, hd, 0:8], in_values=cand[:, hd, :]),
                     reads=[bcand, bt8], writes=[bj8])
                P.op("dve", lambda h, hd=hd: h.match_replace(out=cand[:, hd, :], in_to_replace=t8[:, hd, 0:8], in_values=cand[:, hd, :],
                                                             imm_value=-1e30), reads=[bt8], writes=[bcand])
                P.op("dve", lambda h, hd=hd: h.max(out=t8[:, hd, 8:16], in_=cand[:, hd, :]), reads=[bcand], writes=[bt8])
                P.op("dve", lambda h, hd=hd: h.max_index(out=j8[:, hd, 8:16], in_max=t8[:, hd, 8:16], in_values=cand[:, hd, :]),
                     reads=[bcand, bt8], writes=[bj8])
                yield
            P.op("dve", lambda h: h.tensor_copy(out=i8f[:], in_=i8[:]), reads=[bi8], writes=[bi8f])
            P.op("dve", lambda h: h.tensor_copy(out=jf[:], in_=j8[:].rearrange("p a k -> p (a k)")), reads=[bj8], writes=[bjf])
            P.op("dve", lambda h: h.tensor_tensor(out=T4[:], in0=jf[:].unsqueeze(2).to_broadcast([128, 128, 16]),
                                                  in1=THR[:].unsqueeze(1).to_broadcast([128, 128, 16]), op=ALU.is_ge),
                 reads=[bjf, bconst], writes=[bT4])
            yield
            P.op("dve", lambda h: h.tensor_reduce(out=af[:], in_=T4[:], axis=AX.X, op=ALU.add), reads=[bT4], writes=[baf])
            P.op("dve", lambda h: h.scalar_tensor_tensor(out=bf_[:], in0=af[:], scalar=-16.0, in1=jf[:], op0=ALU.mult, op1=ALU.add),
                 reads=[baf, bjf], writes=[bbf])
            yield
            i8v = i8f[:].rearrange("p (a b) k -> p a b k", b=2)
            for side, (idxt, Et, bE) in enumerate(((af, E1, bE1), (bf_, E2, bE2))):
                P.op("dve", lambda h, idxt=idxt: h.tensor_tensor(out=T4[:], in0=idxt[:].unsqueeze(2).to_broadcast([128, 128, 16]),
                                                                in1=IOT[:].unsqueeze(1).to_broadcast([128, 128, 16]), op=ALU.is_equal),
                     reads=[baf, bbf, bconst], writes=[bT4])
                yield
                P.op("dve", lambda h, side=side: h.tensor_tensor(
                    out=T4[:].rearrange("p (a k) x -> p a k x", k=16), in0=T4[:].rearrange("p (a k) x -> p a k x", k=16),
                    in1=i8v[:, :, side, :].unsqueeze(2).to_broadcast([128, 8, 16, 16]), op=ALU.mult),
                    reads=[bi8f], writes=[bT4])
                yield
                P.op("dve", lambda h, Et=Et: h.tensor_reduce(out=Et[:], in_=T4[:], axis=AX.X, op=ALU.add), reads=[bT4], writes=[bE])
                yield
            P.op("dve", lambda h: h.scalar_tensor_tensor(out=E1[:], in0=E1[:], scalar=128.0, in1=E2[:], op0=ALU.mult, op1=ALU.add),
                 reads=[bE2], writes=[bE1])
            P.op("dve", lambda h: h.tensor_copy(out=eidx_[:], in_=E1[:]), reads=[bE1], writes=[beidx_])
            P.op("dve", lambda h: h.tensor_tensor(out=gts_[:], in0=t8[:], in1=t8[:, :, 0:1].to_broadcast([128, 8, 16]), op=ALU.subtract),
                 reads=[bt8], writes=[bgts_])
            P.op("act", lambda h: h.activation(out=gts_[:], in_=gts_[:], func=AF.Exp), reads=[], writes=[bgts_])
            P.op("dve", lambda h: h.tensor_reduce(out=g8[:, 0:8], in_=gts_[:], axis=AX.X, op=ALU.add), reads=[bgts_], writes=[bg8])
            P.op("dve", lambda h: h.reciprocal(out=g8[:, 8:16], in_=g8[:, 0:8]), reads=[], writes=[bg8])
            P.op("dve", lambda h: h.tensor_tensor(out=gts_[:], in0=gts_[:], in1=g8[:, 8:16].unsqueeze(2).to_broadcast([128, 8, 16]),
                                                  op=ALU.mult), reads=[bg8], writes=[bgts_])
            if "EIDX" in c.dbg:
                P.dma("sp", lambda h: h.dma_start(out=c.EIDX[rows, :], in_=eidx_[:]), reads=[beidx_])
                P.dma("sp", lambda h: h.dma_start(out=c.GTS[rows, :], in_=gts_[:].rearrange("p a k -> p (a k)")), reads=[bgts_])
                P.dma("sp", lambda h: h.dma_start(out=c.X1[rows, :], in_=x1_[:]), reads=[bx1_])
            yield

        def experts(i, nxt):
            par = i % 2
            x1_, xn2_, eidx_, gts_ = x1[par], xn2[par], eidx[par], gts[par]
            bx1_, bxn2_, beidx_, bgts_ = bx1[par], bxn2[par], beidx[par], bgts[par]
            rows = slice(i * 128, (i + 1) * 128)
            gts_f = gts_[:].rearrange("p a k -> p (a k)")

            def stage_a(grp):
                for sl in range(grp * GS, (grp + 1) * GS):
                    k = sl % NG
                    P.dma("pool", lambda h, sl=sl, k=k: h.indirect_dma_start(
                        out=uv[k][:], out_offset=None, in_=c.UVB,
                        in_offset=bass.IndirectOffsetOnAxis(ap=eidx_[:, sl:sl + 1], axis=0)), reads=[beidx_], writes=[buv[k]])
                    P.op("dve", lambda h, sl=sl, k=k: h.scalar_tensor_tensor(out=junk2[:], in0=uv[k][:, 0:D], scalar=1.0, in1=xn2_[:],
                                                                            op0=ALU.mult, op1=ALU.mult, accum_out=adot[:, sl:sl + 1]),
                         reads=[buv[k], bxn2_], writes=[badot[grp]])

            def stage_b(grp):
                g0, g1 = grp * GS, (grp + 1) * GS
                P.op("act", lambda h: h.activation(out=ga[:, g0:g1], in_=adot[:, g0:g1], func=AF.Gelu),
                     reads=[badot[grp]], writes=[bga[grp]])
                for sl in range(g0, g1):
                    k = sl % NG
                    kd = sl % 4
                    P.op("act", lambda h, sl=sl: h.activation(out=ga[:, sl:sl + 1], in_=ga[:, sl:sl + 1], func=AF.Copy,
                                                              scale=gts_f[:, sl:sl + 1]), reads=[bgts_], writes=[bga[grp]])
                    P.op("act", lambda h, sl=sl, kd=kd: h.activation(out=dg[kd][:], in_=identf[:], func=AF.Copy, scale=ga[:, sl:sl + 1]),
                         reads=[bga[grp], bconst], writes=[bdg[kd]])
                    for g in range(2):
                        P.op("pe", lambda h, sl=sl, k=k, kd=kd, g=g: h.matmul(ps_y[g][:], lhsT=dg[kd][:],
                                                                             rhs=uv[k][:, D + g * 512:D + (g + 1) * 512],
                                                                             start=(sl == 0), stop=(sl == 127)),
                             reads=[bdg[kd], buv[k]], writes=[bps_y[g]])

            def adv(nsteps):
                if nxt is None:
                    return
                for _ in range(nsteps):
                    try:
                        next(nxt)
                    except StopIteration:
                        return

            stage_a(0)
            for grp in range(NGRP):
                if grp + 1 < NGRP:
                    stage_a(grp + 1)
                stage_b(grp)
                adv(c.advn)
            for g in range(2):
                P.op("dve", lambda h, g=g: h.tensor_tensor(out=yacc[:, g * 512:(g + 1) * 512], in0=ps_y[g][:], in1=x1_[:, g * 512:(g + 1) * 512],
                                                          op=ALU.add), reads=[bps_y[g], bx1_], writes=[by])
            P.dma("sp", lambda h: h.dma_start(out=c.out[rows, :], in_=yacc[:]), reads=[by], writes=[c.bOUT[i]])
            adv(1000)

        r0 = routing(0)
        for _ in r0:
            pass
        for i in range(NT):
            nxt = routing(i + 1) if i + 1 < NT else None
            if "noexp" in c.dbg or i >= c.nexp:
                par = i % 2
                P.dma("sp", lambda h, i=i, par=par: h.dma_start(out=c.out[i * 128:(i + 1) * 128, :], in_=x1[par][:]),
                      reads=[bx1[par]], writes=[c.bOUT[i]])
                if nxt is not None:
                    for _ in nxt:
                        pass
            else:
                if "noil" in c.dbg and nxt is not None:
                    for _ in nxt:
                        pass
                experts(i, nxt)
    P.barrier()


def build(dbg=(), phases="abcd"):
    nc = bass.Bass("TRN2", target_bir_lowering=False)
    c = Ctx()
    c.nc = nc
    ext_in = lambda n, s, d: nc.dram_tensor(n, s, d, kind="ExternalInput").ap()

    def scratch(n, s, d):
        kind = "ExternalOutput" if n in dbg else "Internal"
        return nc.dram_tensor(n, s, d, kind=kind).ap()

    c.x = ext_in("x", [S, D], F32)
    c.w_in = ext_in("w_in", [D, INW], F32)
    c.norm1_w = ext_in("norm1_w", [128, 8], F32)
    c.identb = ext_in("identb", [128, 128], BF16)
    c.gq = ext_in("gq", [128, 64], F32)
    c.gk = ext_in("gk", [128, 64], F32)
    c.rpbg = ext_in("rpbg", [128, 8, 896], F32)
    c.mask_i = ext_in("mask_i", [128, 896], F32)
    c.mask_a = ext_in("mask_a", [128, 896], F32)
    c.gao = ext_in("gao", [128, 512], F32)
    c.gate_b = ext_in("gate_b", [4, 4], F32)
    c.identf = ext_in("identf", [128, 128], F32)
    c.conv_w = ext_in("conv_w", [128, 8, 5], F32)
    c.conv_b = ext_in("conv_b", [128, 8], F32)
    c.sel = ext_in("sel", [4, 4, 128], F32)
    c.msk = ext_in("msk", [128, 2, 128], F32)
    c.gmn = ext_in("gmn", [128, 512], F32)
    c.w_out = ext_in("w_out", [D, D], F32)
    c.w_q = ext_in("w_q", [D, 2048], F32)
    c.skt = ext_in("skt", [128, 16, 128], F32)
    c.gn2 = ext_in("gn2", [128, D], F32)
    c.thr = ext_in("thr", [128, 16], F32)
    c.iot = ext_in("iot", [128, 16], F32)
    c.peer_uv = ext_in("peer_uv", [16384, 2 * D], F32)
    c.UVB = scratch("UVB", [16384, 2 * D], BF16)
    c.out = nc.dram_tensor("out", [S, D], F32, kind="ExternalOutput").ap()
    c.bOUT = bufs(NT, "OUT")
    c.dbg = dbg
    c.nexp = NT
    c.advn = 1
    c.gs = 4
    for d_ in dbg:
        if d_.startswith("gs"):
            c.gs = int(d_[2:])
    for d_ in dbg:
        if d_.startswith("adv"):
            c.advn = int(d_[3:])
    for d_ in dbg:
        if d_.startswith("exp"):
            c.nexp = int(d_[3:])
    if "EIDX" in dbg:
        c.EIDX = scratch("EIDX", [S, 128], I32)
        c.GTS = scratch("GTS", [S, 128], F32)
        c.X1 = scratch("X1", [S, D], F32)

    c.QT = scratch("QT", [4, 128, S], BF16)
    c.KT = scratch("KT", [4, 128, S], BF16)
    c.V = scratch("V", [S, 512], BF16)
    c.MV = scratch("MV", [S, 512], BF16)
    c.SIGO = scratch("SIGO", [S, 512], BF16)
    c.MQKT = scratch("MQKT", [1024, S], F32)
    c.GT = scratch("GT", [4, 4, S], F32)
    c.AOUT = scratch("AOUT", [S, 512], BF16)
    c.BCRD = scratch("BCRD", [2, 4, NT, 257], F32)
    c.bBCRD = bufs(2, "BCRD")
    c.HF = scratch("HF", [S, 512], F32)
    c.bHF = bufs(NT, "HF")
    c.HM = scratch("HM", [S, 512], BF16)
    c.bHM = bufs(NT, "HM")
    c.bAOUT = bufs(NT, "AOUT")
    c.bQKT = [bufs(NT, "QT"), bufs(NT, "KT")]
    c.bV, c.bMV, c.bSIGO, c.bMQKT, c.bGT = (bufs(NT, n) for n in ("V", "MV", "SIGO", "MQKT", "GT"))

    with ExitStack() as st:
        c.P = Prog(nc, st)
        if "a" in phases:
            phase_a(c)
        if "b" in phases:
            phase_b(c)
        if "c" in phases:
            phase_c(c)
        if "d" in phases:
            phase_t(c)
            phase_d(c)
        c.P.emit()
    return nc


def host_inputs(inputs, b):
    f32 = np.float32
    m = {}
    m["x"] = np.ascontiguousarray(inputs["x"][b], dtype=f32)
    m["w_in"] = np.ascontiguousarray(inputs["w_in"][0], dtype=f32)
    m["norm1_w"] = np.ascontiguousarray(inputs["norm1_w"][0].reshape(8, 128).T, dtype=f32)
    m["identb"] = np.eye(128, dtype=f32).astype(ml_dtypes.bfloat16)
    m["gq"] = np.ascontiguousarray(np.broadcast_to(inputs["q_norm_w"][0][None, :], (128, 64)), dtype=f32)
    m["gk"] = np.ascontiguousarray(np.broadcast_to(inputs["k_norm_w"][0][None, :], (128, 64)), dtype=f32)
    p = np.arange(128); kr = p // 64; kc = p % 64
    col = np.arange(128); rq = col // 64; cc = col % 64
    dt = np.arange(-3, 4)
    drow = 2 * dt[None, :, None] + kr[:, None, None] - rq[None, None, :]
    dcol = kc[:, None, None] - cc[None, None, :] + 0 * dt[None, :, None]
    rpb = inputs["attn_rpb"][0]
    g = rpb[:, np.clip(drow + 7, 0, 14), np.clip(dcol + 15, 0, 30)]
    m["rpbg"] = np.ascontiguousarray(g.transpose(1, 0, 2, 3).reshape(128, 8, 896), dtype=f32)
    cs = np.clip(cc - 8, 0, 48)
    colvalid = (kc[:, None, None] >= cs[None, None, :]) & (kc[:, None, None] < cs[None, None, :] + 16)
    colvalid = colvalid & (dt[None, :, None] > -100)
    m["mask_a"] = np.where(colvalid & (np.abs(drow) <= 7), 0.0, NEG).astype(f32).reshape(128, 896)
    m["mask_i"] = np.where(colvalid & (drow >= -4) & (drow <= 3), 0.0, NEG).astype(f32).reshape(128, 896)
    m["gate_b"] = np.ascontiguousarray(inputs["mlstm_gate_b"][0].T, dtype=f32)
    m["identf"] = np.eye(128, dtype=f32)
    m["conv_w"] = np.ascontiguousarray(inputs["mlstm_conv_w"][0].reshape(5, 8, 128).transpose(2, 1, 0), dtype=f32)
    m["conv_b"] = np.ascontiguousarray(inputs["mlstm_conv_b"][0].reshape(8, 128).T, dtype=f32)
    sel = np.zeros((4, 4, 128), f32)
    for hh in range(4):
        sel[hh, hh, :] = 1.0
    m["sel"] = sel
    ii = np.arange(128)
    msk = np.zeros((128, 2, 128), f32)
    msk[:, 0, :] = np.where(ii[:, None] <= ii[None, :], 0.0, NEG)
    msk[:, 1, :] = np.where(ii[:, None] >= ii[None, :], 0.0, NEG)
    m["msk"] = msk
    m["gmn"] = np.ascontiguousarray(np.broadcast_to(inputs["mlstm_norm_w"][0][None, :], (128, 512)), dtype=f32)
    m["w_out"] = np.ascontiguousarray(inputs["w_out"][0], dtype=f32)
    m["w_q"] = np.ascontiguousarray(inputs["peer_w_q"][0], dtype=f32)
    m["skt"] = np.ascontiguousarray(inputs["peer_sub_keys"][0].reshape(16, 128, 128).transpose(2, 0, 1), dtype=f32)
    m["gn2"] = np.ascontiguousarray(np.broadcast_to(inputs["norm2_w"][0][None, :], (128, D)), dtype=f32)
    thr = (np.arange(16, dtype=f32) + 1.0) * 16.0
    thr[15] = 1e9
    m["thr"] = np.ascontiguousarray(np.broadcast_to(thr[None, :], (128, 16)), dtype=f32)
    m["iot"] = np.ascontiguousarray(np.broadcast_to(np.arange(16, dtype=f32)[None, :], (128, 16)), dtype=f32)
    m["peer_uv"] = np.ascontiguousarray(np.concatenate([inputs["peer_u"][0], inputs["peer_v"][0]], axis=1), dtype=f32)
    m["gao"] = np.ascontiguousarray(np.broadcast_to(inputs["attn_out_norm_w"][0][None, :], (128, 512)), dtype=f32)
    return m


def kernel(**inputs):
    nc = build()
    in_maps = [host_inputs(inputs, b) for b in range(8)]
    res = run_bass_kernel_spmd(nc, in_maps, core_ids=list(range(8)))
    return np.stack([r["out"] for r in res.results], axis=0).astype(np.float32)
```
